# Optimizing a Trainium2 kernel written in Bass

```python
import jax, jax.numpy as jnp
from jax import lax
import numpy as np

D_MODEL = 2048
BATCH = 2
SEQ = 4096
DEPTH = 1

ATTN_HEAD_DIM = 64
ATTN_HEADS = D_MODEL // ATTN_HEAD_DIM
ATTN_KV_HEADS = ATTN_HEADS // 8
ATTN_GROUP = ATTN_HEADS // ATTN_KV_HEADS
WINDOW = 128
RET_HEADS = 8
RET_QK_DIM = D_MODEL // RET_HEADS
RET_V_DIM = D_MODEL // RET_HEADS
RET_CHUNK = 128
ROPE_BASE = 10000.0
N_GROUPS = 4
EXPERTS_PER_GROUP = 16
N_EXPERTS = N_GROUPS * EXPERTS_PER_GROUP
TOP_K = 2
EXPERT_DIM = D_MODEL // 2
MOE_BLOCK = 128
EPS = 1e-6

kernel_name = "hybrid_swa_retention_hmoe_block"


def rms_norm(x, gain):
    xf = x.astype(jnp.float32)
    y = xf * lax.rsqrt(jnp.mean(xf * xf, axis=-1, keepdims=True) + EPS)
    return (y * gain.astype(jnp.float32)).astype(x.dtype)


def modulate(h, shift, scale):
    return h * (1.0 + scale[:, None, :]) + shift[:, None, :]


def sliding_window_sink_attention(q, k, v, sinks):
    b, s, kvh, g, dh = q.shape
    nb = s // WINDOW
    qb = q.reshape(b, nb, WINDOW, kvh, g, dh)

    def band(t):
        tb = t.reshape(b, nb, WINDOW, kvh, dh)
        prev = jnp.concatenate([jnp.zeros_like(tb[:, :1]), tb[:, :-1]], axis=1)
        return jnp.concatenate([prev, tb], axis=2)

    kb, vb = band(k), band(v)
    scores = jnp.einsum('bnqkgd,bnskd->bnkgqs', qb, kb,
                        preferred_element_type=jnp.float32) * (dh ** -0.5)
    qi = jnp.arange(WINDOW)[:, None]
    sj = jnp.arange(2 * WINDOW)[None, :]
    in_band = (sj > qi) & (sj <= qi + WINDOW)
    has_prev = (jnp.arange(nb) > 0)[:, None, None] | (sj >= WINDOW)[None]
    valid = in_band[None] & has_prev
    scores = jnp.where(valid[None, :, None, None], scores, -jnp.inf)
    sink = sinks.astype(jnp.float32).reshape(kvh, g)[None, None, :, :, None, None]
    m = jnp.maximum(jnp.max(scores, axis=-1, keepdims=True), sink)
    p = jnp.exp(scores - m)
    denom = jnp.sum(p, axis=-1, keepdims=True) + jnp.exp(sink - m)
    probs = (p / denom).astype(v.dtype)
    out = jnp.einsum('bnkgqs,bnskd->bnqkgd', probs, vb)
    return out.reshape(b, s, kvh * g * dh)


def rotary(t, positions):
    half = t.shape[-1] // 2
    inv_freq = ROPE_BASE ** (-jnp.arange(half, dtype=jnp.float32) / half)
    ang = positions.astype(jnp.float32)[..., None] * inv_freq
    cos = jnp.cos(ang)[:, :, None, :]
    sin = jnp.sin(ang)[:, :, None, :]
    t1, t2 = t[..., :half], t[..., half:]
    return jnp.concatenate([t1 * cos - t2 * sin, t1 * sin + t2 * cos], axis=-1)


def chunkwise_retention(q, k, v):
    b, s, h, dk = q.shape
    dv = v.shape[-1]
    nc = s // RET_CHUNK
    log_gamma = jnp.log1p(-(2.0 ** (-5.0 - jnp.arange(h, dtype=jnp.float32))))
    idx = jnp.arange(RET_CHUNK, dtype=jnp.float32)
    diff = idx[:, None] - idx[None, :]
    decay_intra = jnp.where(diff >= 0,
                            jnp.exp(jnp.maximum(diff, 0.0) * log_gamma[:, None, None]), 0.0)
    decay_q = jnp.exp((idx + 1.0) * log_gamma[:, None])
    decay_k = jnp.exp((RET_CHUNK - 1.0 - idx) * log_gamma[:, None])
    decay_chunk = jnp.exp(RET_CHUNK * log_gamma)

    def to_chunks(t):
        return t.reshape(b, nc, RET_CHUNK, h, -1).transpose(1, 0, 3, 2, 4)

    def step(state, qkv):
        qc, kc, vc = qkv
        intra = jnp.einsum('bhcd,bhed->bhce', qc, kc) * decay_intra
        o = (jnp.einsum('bhce,bhef->bhcf', intra, vc)
             + jnp.einsum('bhcd,bhdf->bhcf', qc, state) * decay_q[None, :, :, None])
        state = (state * decay_chunk[None, :, None, None]
                 + jnp.einsum('bhcd,bhcf->bhdf', kc * decay_k[None, :, :, None], vc))
        return state, o

    state0 = jnp.zeros((b, h, dk, dv), jnp.float32)
    _, o = lax.scan(step, state0, (to_chunks(q), to_chunks(k), to_chunks(v)))
    return o.transpose(1, 0, 3, 2, 4).reshape(b, s, h, dv)


def token_mixers(h, positions, w_in, attn_sinks, ret_norm_gain, w_branch_attn, w_branch_ret, w_out):
    b, s, _ = h.shape
    widths = [ATTN_HEADS * ATTN_HEAD_DIM, ATTN_KV_HEADS * ATTN_HEAD_DIM, ATTN_KV_HEADS * ATTN_HEAD_DIM,
              RET_HEADS * RET_QK_DIM, RET_HEADS * RET_QK_DIM, RET_HEADS * RET_V_DIM, RET_HEADS * RET_V_DIM,
              D_MODEL, D_MODEL]
    splits = [int(v) for v in np.cumsum(widths)[:-1]]
    proj = h @ w_in
    q_a, k_a, v_a, q_r, k_r, v_r, g_r, gate_a, gate_r = jnp.split(proj, splits, axis=-1)

    attn = sliding_window_sink_attention(
        q_a.reshape(b, s, ATTN_KV_HEADS, ATTN_GROUP, ATTN_HEAD_DIM),
        k_a.reshape(b, s, ATTN_KV_HEADS, ATTN_HEAD_DIM),
        v_a.reshape(b, s, ATTN_KV_HEADS, ATTN_HEAD_DIM),
        attn_sinks)

    qr = rotary(q_r.reshape(b, s, RET_HEADS, RET_QK_DIM).astype(jnp.float32), positions)
    kr = rotary(k_r.reshape(b, s, RET_HEADS, RET_QK_DIM).astype(jnp.float32), positions) * (RET_QK_DIM ** -0.5)
    vr = v_r.reshape(b, s, RET_HEADS, RET_V_DIM).astype(jnp.float32)
    ret = chunkwise_retention(qr, kr, vr)
    ret = ret * lax.rsqrt(jnp.mean(ret * ret, axis=-1, keepdims=True) + EPS)
    ret = ret * ret_norm_gain.astype(jnp.float32).reshape(RET_HEADS, RET_V_DIM)
    ret = (jax.nn.silu(g_r.astype(jnp.float32)) * ret.reshape(b, s, -1)).astype(h.dtype)

    merged = (jax.nn.sigmoid(gate_a) * (attn @ w_branch_attn)
              + jax.nn.sigmoid(gate_r) * (ret @ w_branch_ret))
    return merged @ w_out


def hierarchical_moe(h, w_router_group, b_router_group, w_router_expert, b_router_expert,
                     w_expert_gate, w_expert_up, w_expert_down):
    b, s, d = h.shape
    t = b * s
    hf = h.reshape(t, d)
    g_logits = (hf @ w_router_group).astype(jnp.float32) + b_router_group.astype(jnp.float32)
    g_prob = jax.nn.softmax(g_logits, axis=-1)
    g_sel = jnp.argmax(g_logits, axis=-1).astype(jnp.int32)
    g_w = jnp.take_along_axis(g_prob, g_sel[:, None], axis=1)
    e_logits = ((hf @ w_router_expert).astype(jnp.float32)
                + b_router_expert.astype(jnp.float32)).reshape(t, N_GROUPS, EXPERTS_PER_GROUP)
    e_logits = jnp.take_along_axis(e_logits, g_sel[:, None, None], axis=1)[:, 0]
    top_v, top_i = lax.top_k(e_logits, TOP_K)
    weights = (jax.nn.softmax(top_v, axis=-1) * g_w).reshape(-1)
    expert_ids = (g_sel[:, None] * EXPERTS_PER_GROUP + top_i).reshape(-1).astype(jnp.int32)
    token_ids = jnp.repeat(jnp.arange(t, dtype=jnp.int32), TOP_K)

    n_assign = t * TOP_K
    n_pad = -(-(n_assign + N_EXPERTS * (MOE_BLOCK - 1)) // MOE_BLOCK) * MOE_BLOCK
    n_blocks = n_pad // MOE_BLOCK
    order = jnp.argsort(expert_ids)
    sorted_e = expert_ids[order]
    counts = jnp.bincount(expert_ids, length=N_EXPERTS)
    padded = (counts + MOE_BLOCK - 1) // MOE_BLOCK * MOE_BLOCK
    start = jnp.cumsum(counts) - counts
    pad_end = jnp.cumsum(padded)
    pad_start = pad_end - padded
    dest = pad_start[sorted_e] + jnp.arange(n_assign, dtype=jnp.int32) - start[sorted_e]
    row_token = jnp.full((n_pad,), t, jnp.int32).at[dest].set(token_ids[order])
    row_weight = jnp.zeros((n_pad,), jnp.float32).at[dest].set(weights[order])
    block_expert = jnp.minimum(
        jnp.searchsorted(pad_end, jnp.arange(n_blocks, dtype=jnp.int32) * MOE_BLOCK, side='right'),
        N_EXPERTS - 1).astype(jnp.int32)
    x_rows = jnp.concatenate([hf, jnp.zeros((1, d), hf.dtype)], axis=0)[row_token]
    x_rows = x_rows.reshape(n_blocks, MOE_BLOCK, d)

    def expert_block(args):
        xb, e = args
        return (jax.nn.silu(xb @ w_expert_gate[e]) * (xb @ w_expert_up[e])) @ w_expert_down[e]

    y = lax.map(expert_block, (x_rows, block_expert)).reshape(n_pad, d)
    out = jnp.zeros((t + 1, d), h.dtype).at[row_token].add(y * row_weight[:, None].astype(y.dtype))
    return out[:t].reshape(b, s, d)


def setup_inputs(seed: int = 0) -> dict:
    key = jax.random.key(seed)
    ks = jax.random.split(key, 24)
    f32 = jnp.float32
    d = D_MODEL
    p_in = (ATTN_HEADS + 2 * ATTN_KV_HEADS) * ATTN_HEAD_DIM + RET_HEADS * (2 * RET_QK_DIM + 2 * RET_V_DIM) + 2 * d
    attn_w = ATTN_HEADS * ATTN_HEAD_DIM
    ret_w = RET_HEADS * RET_V_DIM

    def nrm(k, shape, scale):
        return jax.random.normal(k, shape, f32) * scale

    offset = jax.random.randint(ks[2], (BATCH, 1), 0, 1024, dtype=jnp.int32)
    positions = (offset + jnp.arange(SEQ, dtype=jnp.int32)[None, :]).astype(jnp.int32)
    return {
        "x": nrm(ks[0], (BATCH, SEQ, d), 1.0),
        "c": nrm(ks[1], (BATCH, d), 1.0),
        "positions": positions,
        "norm1_gain": 1.0 + nrm(ks[3], (DEPTH, d), 0.05),
        "norm2_gain": 1.0 + nrm(ks[4], (DEPTH, d), 0.05),
        "final_norm_gain": 1.0 + nrm(ks[5], (d,), 0.05),
        "w_ada": nrm(ks[6], (DEPTH, d, 6 * d), 0.5 * d ** -0.5),
        "b_ada": nrm(ks[7], (DEPTH, 6 * d), 0.02),
        "w_in": nrm(ks[8], (DEPTH, d, p_in), d ** -0.5),
        "attn_sinks": nrm(ks[9], (DEPTH, ATTN_HEADS), 1.0),
        "ret_norm_gain": 1.0 + nrm(ks[10], (DEPTH, ret_w), 0.05),
        "w_branch_attn": nrm(ks[11], (DEPTH, attn_w, d), attn_w ** -0.5),
        "w_branch_ret": nrm(ks[12], (DEPTH, ret_w, d), ret_w ** -0.5),
        "w_out": nrm(ks[13], (DEPTH, d, d), d ** -0.5),
        "w_router_group": nrm(ks[14], (DEPTH, d, N_GROUPS), d ** -0.5),
        "b_router_group": nrm(ks[15], (DEPTH, N_GROUPS), 0.01),
        "w_router_expert": nrm(ks[16], (DEPTH, d, N_EXPERTS), d ** -0.5),
        "b_router_expert": nrm(ks[17], (DEPTH, N_EXPERTS), 0.01),
        "w_expert_gate": nrm(ks[18], (DEPTH, N_EXPERTS, d, EXPERT_DIM), d ** -0.5),
        "w_expert_up": nrm(ks[19], (DEPTH, N_EXPERTS, d, EXPERT_DIM), d ** -0.5),
        "w_expert_down": nrm(ks[20], (DEPTH, N_EXPERTS, EXPERT_DIM, d), EXPERT_DIM ** -0.5),
    }


def reference(x, c, positions, norm1_gain, norm2_gain, final_norm_gain, w_ada, b_ada, w_in,
              attn_sinks, ret_norm_gain, w_branch_attn, w_branch_ret, w_out,
              w_router_group, b_router_group, w_router_expert, b_router_expert,
              w_expert_gate, w_expert_up, w_expert_down):
    for layer in range(DEPTH):
        mod = jax.nn.silu(c) @ w_ada[layer] + b_ada[layer]
        shift1, scale1, gate1, shift2, scale2, gate2 = jnp.split(mod, 6, axis=-1)
        h = modulate(rms_norm(x, norm1_gain[layer]), shift1, scale1)
        mix = token_mixers(h, positions, w_in[layer], attn_sinks[layer], ret_norm_gain[layer],
                           w_branch_attn[layer], w_branch_ret[layer], w_out[layer])
        x = x + gate1[:, None, :] * mix
        h2 = modulate(rms_norm(x, norm2_gain[layer]), shift2, scale2)
        ffn = hierarchical_moe(h2, w_router_group[layer], b_router_group[layer],
                               w_router_expert[layer], b_router_expert[layer],
                               w_expert_gate[layer], w_expert_up[layer], w_expert_down[layer])
        x = x + gate2[:, None, :] * ffn
    return rms_norm(x, final_norm_gain)
```

```python
import numpy as np
import concourse.bass as bass
import concourse.mybir as mybir
from concourse.bass_utils import run_bass_kernel_spmd

F32 = mybir.dt.float32
BF16 = mybir.dt.bfloat16
I32 = mybir.dt.int32
U32 = mybir.dt.uint32
ALU = mybir.AluOpType
AF = mybir.ActivationFunctionType
AX = mybir.AxisListType


class Tr:
    def __init__(self):
        self.last_w = None
        self.readers = {}


class T:
    def __init__(self, h, name, tr=None):
        self.h = h
        self.name = name
        self.tr = tr or Tr()

    @property
    def last_w(self):
        return self.tr.last_w

    @last_w.setter
    def last_w(self, v):
        self.tr.last_w = v

    @property
    def readers(self):
        return self.tr.readers

    @readers.setter
    def readers(self, v):
        self.tr.readers = v

    def v(self, ap):
        return T(ap, self.name, self.tr)

    def __getitem__(self, k):
        return self.h[k]


class Sched:
    ENG = ('pe', 'dve', 'act', 'pool', 'sp')

    def __init__(self, nc, es):
        self.nc = nc
        self.es = es
        self.eng = {'pe': nc.tensor, 'dve': nc.vector, 'act': nc.scalar, 'pool': nc.gpsimd, 'sp': nc.sync}
        self.ops = {e: [] for e in self.ENG}
        self.cnt = {e: 0 for e in self.ENG}
        self.sem = {e: es.enter_context(nc.semaphore('s_' + e)) for e in self.ENG if e != 'sp'}
        self.seen = {e: {} for e in self.ENG}
        self.NP = 8
        self.dsem = {q: [es.enter_context(nc.semaphore('d_%s%d' % (q, i))) for i in range(self.NP)]
                     for q in ('sp', 'pool', 'act')}
        self.dn = {q: 0 for q in ('sp', 'pool', 'act')}
        self.ntile = 0
        self.out_tokens = []

    def sb(self, shape, dt, name=None, es=None):
        self.ntile += 1
        name = (name or 't') + '_%d' % self.ntile
        h = (es or self.es).enter_context(self.nc.sbuf_tensor(name, list(shape), dt))
        return T(h, name)

    def ps(self, shape, dt, name=None, es=None):
        self.ntile += 1
        name = (name or 'p') + '_%d' % self.ntile
        h = (es or self.es).enter_context(self.nc.psum_tensor(name, list(shape), dt))
        return T(h, name)

    def barrier(self):
        toks = [('eng', f, self.cnt[f]) for f in ('pe', 'dve', 'act', 'pool') if self.cnt[f] > 0]
        for q in ('sp', 'pool', 'act'):
            n = self.dn[q]
            for i in range(max(0, n - self.NP), n):
                toks.append(('dma', self.dsem[q][i % self.NP], 16 * (i // self.NP + 1)))
        for e in self.ENG:
            for tok in toks:
                self._wait(e, tok)

    def dram(self, name, shape, dt, kind='Internal'):
        h = self.nc.dram_tensor(name, list(shape), dt, kind=kind)
        return T(h, name)

    def _wait(self, e, tok):
        if tok is None:
            return
        kind, key, val = tok
        if kind == 'eng' and key == e and e == 'pe':
            return
        if kind == 'eng' and key == 'sp':
            return
        sk = (kind, key if kind == 'eng' else id(key))
        if self.seen[e].get(sk, 0) >= val:
            return
        self.seen[e][sk] = val
        sem = self.sem[key] if kind == 'eng' else key
        eng = self.eng[e]
        eng.wait_ge(sem, val)

    def _deps(self, e, reads, writes):
        for t in reads:
            self._wait(e, t.last_w)
        for t in writes:
            self._wait(e, t.last_w)
            for tok in list(t.readers.values()):
                self._wait(e, tok)

    def _update(self, tok, reads, writes):
        for t in reads:
            kind, key, val = tok
            rk = (kind, key if kind == 'eng' else (id(key)))
            t.readers[rk] = tok
        for t in writes:
            t.last_w = tok
            t.readers = {}

    def op(self, e, fn, reads=(), writes=()):
        assert e in ('pe', 'dve', 'act', 'pool')
        self._deps(e, reads, writes)
        self.cnt[e] += 1
        idx = self.cnt[e]
        eng = self.eng[e]
        sem = self.sem[e]
        fn(eng).then_inc(sem, 1)
        self._update(('eng', e, idx), reads, writes)

    def dma(self, q, fn, reads=(), writes=(), is_out=False):
        self._deps(q, reads, writes)
        n = self.dn[q]
        self.dn[q] += 1
        slot = n % self.NP
        sem = self.dsem[q][slot]
        if n >= self.NP:
            self._wait(q, ('dma', sem, 16 * (n // self.NP)))
        val = 16 * (n // self.NP + 1)
        eng = self.eng[q]
        fn(eng).then_inc(sem, 16)
        tok = ('dma', sem, val)
        self._update(tok, reads, writes)
        if is_out:
            self.out_tokens.append(tok)
        return tok

    def finish(self):
        for tok in self.out_tokens:
            self._wait('sp', tok)
        for q in ('sp', 'pool', 'act'):
            n = self.dn[q]
            for i in range(max(0, n - self.NP), n):
                self._wait('sp', ('dma', self.dsem[q][i % self.NP], 16 * (i // self.NP + 1)))


import math
import os
from contextlib import ExitStack

D = 2048
KC = 16
NOWN = 8
NPRE = 24
NBLK = 80
NOV = 16
OFF_QA, OFF_KA, OFF_VA, OFF_QR, OFF_KR, OFF_VR, OFF_GR, OFF_GA, OFF_GTR = 0, 2048, 2304, 2560, 4608, 6656, 8704, 10752, 12800
EPS = 1e-6
TWO_PI = 2.0 * math.pi
C1 = 6.28125
C2 = TWO_PI - C1
GAM = [1.0 - 2.0 ** (-5.0 - h) for h in range(8)]


def build(stop_after=None):
    nc = bass.Bass("TRN2", target_bir_lowering=False)

    def din(name, shape, dt=F32):
        return nc.dram_tensor(name, list(shape), dt, kind="ExternalInput")

    xo = din("xo", [1024, D]); xp = din("xp", [NPRE * 128, D])
    pos_o = din("pos_o", [128, NOWN], I32); pos_p = din("pos_p", [128, NPRE], I32)
    cT = din("cT", [128, KC]); pkd = din("pk", [128, NPRE, 8]); mask0d = din("mask0", [128, 256])
    w_ada = din("w_ada", [D, 6 * D]); b_ada = din("b_ada", [1, 6 * D]); w_in = din("w_in", [D, 14848])
    sinks = din("attn_sinks", [1, 32]); rgain = din("ret_norm_gain", [1, D])
    w_ba = din("w_branch_attn", [D, D]); w_br = din("w_branch_ret", [D, D]); w_o = din("w_out", [D, D])
    w_rt = din("w_router", [D, 68]); b_rt = din("b_router", [1, 68])
    if stop_after is None or stop_after == 'moe_full':
        w_eg = din("w_expert_gate", [64 * D, 1024]); w_eu = din("w_expert_up", [64 * D, 1024]); w_ed = din("w_expert_down", [64 * 1024, D])
    n1g = din("norm1_gain", [1, D]); n2g = din("norm2_gain", [1, D]); nfg = din("final_norm_gain", [1, D])
    identd = din("ident", [128, 128]); maskd = din("mask", [128, 256]); decTd = din("decT", [128, 8, 128])
    dqd = din("dq", [128, 8]); dkd = din("dk", [128, 8]); invfd = din("invf", [128, 128]); Lstd = din("Lst", [128, 128])
    tokidd = din("tokid", [128, NOWN], I32); blk128d = din("blk128", [128, NBLK]); kcoffd = din("kcoff", [128, 16]); pidxd = din("pidx", [128, 1]); e128d = din("e128", [128, 64])
    out = nc.dram_tensor("out", [1024, D], F32, kind="ExternalOutput")
    dbgo = {}

    def dout(name, shape, dt=F32):
        dbgo[name] = nc.dram_tensor(name, list(shape), dt, kind="ExternalOutput")
        return dbgo[name]

    modd = T(nc.dram_tensor("modd", [6, D], F32, kind="Internal"), "modd")
    xmid = T(nc.dram_tensor("xmid", [1024, D], F32, kind="Internal"), "xmid")
    H2 = T(nc.dram_tensor("H2", [1025, D], BF16, kind="Internal"), "H2")
    rinfo = T(nc.dram_tensor("rinfo", [NBLK * 128, 16], I32, kind="Internal"), "rinfo")
    Yd = T(nc.dram_tensor("Yd", [NBLK * 128, D], F32, kind="Internal"), "Yd")

    with ExitStack() as es:
        S = Sched(nc, es)
        qrr = [0]

        def ld(dst_T, dst_ap, src_ap, q='sp', reads=(), extra_w=()):
            return S.dma(q, lambda e: e.dma_start(out=dst_ap, in_=src_ap), reads=list(reads), writes=[dst_T] + list(extra_w))

        def st(dst_T, dst_ap, src_T, src_ap, q='sp', is_out=False):
            return S.dma(q, lambda e: e.dma_start(out=dst_ap, in_=src_ap), reads=[src_T], writes=[dst_T], is_out=is_out)

        idf = S.sb([128, 128], F32, 'idf'); ld(idf, idf[:], identd[:, :])
        idb = S.sb([128, 128], BF16, 'idb')
        S.op('dve', lambda e: e.tensor_copy(out=idb[:], in_=idf[:]), reads=[idf], writes=[idb])

        def transpose_bf(ps_T, ps_ap, src_T, src_ap):
            S.op('pe', lambda e: e.transpose(out=ps_ap, in_=src_ap, identity=idb[:]), reads=[src_T, idb], writes=[ps_T])

        def rms_rstd(x_T, x_ap, junk_T, ss, rstd, n=D):
            S.op('act', lambda e: e.activation(out=junk_T[:], in_=x_ap, func=AF.Square, accum_out=ss[:]), reads=[x_T], writes=[junk_T, ss])
            S.op('act', lambda e: e.activation(out=ss[:], in_=ss[:], func=AF.Sqrt, scale=1.0 / n, bias=EPS), reads=[ss], writes=[ss])
            S.op('dve', lambda e: e.reciprocal(out=rstd[:], in_=ss[:]), reads=[ss], writes=[rstd])

        with ExitStack() as ph:
            cs = S.sb([128, KC], F32, 'cs', ph); ld(cs, cs[:], cT[:, :])
            sc = S.sb([128, KC], F32, 'sc', ph)
            S.op('act', lambda e: e.activation(out=sc[:], in_=cs[:], func=AF.Silu), reads=[cs], writes=[sc])
            scb = S.sb([128, KC, 128], F32, 'scb', ph)
            S.op('dve', lambda e: e.tensor_copy(out=scb[:], in_=sc[:, :].unsqueeze(2).to_broadcast([128, KC, 128])), reads=[sc], writes=[scb])
            g1 = S.sb([128, D], F32, 'g1', ph); ld(g1, g1[:], n1g[0:1, :].partition_broadcast(128))
            g2 = S.sb([128, D], F32, 'g2', ph); ld(g2, g2[:], n2g[0:1, :].partition_broadcast(128))
            wbuf = [S.sb([128, KC, 512], F32, 'wada%d' % i, ph) for i in range(2)]
            bbuf = [S.sb([128, 512], F32, 'bada%d' % i, ph) for i in range(2)]
            rbuf = [S.sb([128, 512], F32, 'rada%d' % i, ph) for i in range(2)]
            pms = [S.ps([128, 512], F32, 'pada%d' % i, ph) for i in range(2)]
            it = 0
            for j in range(6):
                for nb in range(4):
                    c0 = j * D + nb * 512
                    wb, bb, rb, pm = wbuf[it % 2], bbuf[it % 2], rbuf[it % 2], pms[it % 2]
                    for kq in range(4):
                        ld(wb, wb[:, kq * 4:(kq + 1) * 4, :],
                           w_ada[kq * 512:(kq + 1) * 512, c0:c0 + 512].rearrange("(k p) n -> p k n", p=128))
                    ld(bb, bb[:], b_ada[0:1, c0:c0 + 512].partition_broadcast(128))
                    for k in range(KC):
                        S.op('pe', lambda e: e.matmul(pm[:], lhsT=scb[:, k, :], rhs=wb[:, k, :], start=(k == 0), stop=(k == KC - 1)),
                             reads=[scb, wb], writes=[pm])
                    S.op('dve', lambda e: e.tensor_tensor(out=rb[:], in0=pm[:], in1=bb[:], op=ALU.add), reads=[pm, bb], writes=[rb])
                    if j in (1, 4):
                        gg = g1 if j == 1 else g2
                        S.op('dve', lambda e: e.scalar_tensor_tensor(out=rb[:], in0=rb[:], scalar=1.0, in1=gg[:, nb * 512:(nb + 1) * 512],
                                                                     op0=ALU.add, op1=ALU.mult), reads=[rb, gg], writes=[rb])
                    st(modd, modd[j:j + 1, nb * 512:(nb + 1) * 512], rb, rb[0:1, :])
                    it += 1
            S.barrier()
        if stop_after == 'ada':
            o = dout("d_mod", [6, D])
            t = S.sb([6, D], F32, 'dbg'); ld(t, t[:], modd[:, :], reads=[modd])
            st(T(o, 'o'), o[:, :], t, t[:], is_out=True)
            S.finish()
            return nc, list(dbgo)
        mx = es.enter_context(ExitStack())
        state_f = S.sb([128, 8, 512], F32, 'state_f', mx)
        S.op('dve', lambda e: e.memset(state_f[:], 0.0), writes=[state_f])
        invf = S.sb([128, 128], F32, 'invf', mx); ld(invf, invf[:], invfd[:, :])
        rp_a = S.sb([128, 128], F32, 'rp_a', mx); rp_b = S.sb([128, 128], F32, 'rp_b', mx)
        rp_k = S.sb([128, 128], I32, 'rp_k', mx); rp_f = S.sb([128, 128], F32, 'rp_f', mx)
        ss = S.sb([128, 1], F32, 'ss', mx); rstd = S.sb([128, 1], F32, 'rstd', mx)

        def rope_tables(pos_T, pos_ap, cos_T, cos_ap, sin_T, sin_ap):
            S.op('dve', lambda e: e.tensor_scalar(out=rp_a[:], in0=invf[:], scalar1=pos_ap, scalar2=None, op0=ALU.mult),
                 reads=[invf, pos_T], writes=[rp_a])
            for which in (0, 1):
                dst_T, dst_ap = (sin_T, sin_ap) if which == 0 else (cos_T, cos_ap)
                if which == 1:
                    S.op('dve', lambda e: e.tensor_scalar(out=rp_a[:], in0=rp_a[:], scalar1=math.pi / 2, scalar2=None, op0=ALU.add),
                         reads=[rp_a], writes=[rp_a])
                S.op('dve', lambda e: e.tensor_scalar(out=rp_k[:], in0=rp_a[:], scalar1=1.0 / TWO_PI, scalar2=None, op0=ALU.mult),
                     reads=[rp_a], writes=[rp_k])
                S.op('dve', lambda e: e.tensor_copy(out=rp_f[:], in_=rp_k[:]), reads=[rp_k], writes=[rp_f])
                S.op('dve', lambda e: e.scalar_tensor_tensor(out=rp_b[:], in0=rp_f[:], scalar=-C1, in1=rp_a[:], op0=ALU.mult, op1=ALU.add),
                     reads=[rp_f, rp_a], writes=[rp_b])
                S.op('dve', lambda e: e.scalar_tensor_tensor(out=rp_b[:], in0=rp_f[:], scalar=-C2, in1=rp_b[:], op0=ALU.mult, op1=ALU.add),
                     reads=[rp_f, rp_b], writes=[rp_b])
                S.op('dve', lambda e: e.tensor_scalar(out=rp_b[:], in0=rp_b[:], scalar1=3.1415925, scalar2=-3.1415925, op0=ALU.min, op1=ALU.max),
                     reads=[rp_b], writes=[rp_b])
                S.op('act', lambda e: e.activation(out=dst_ap, in_=rp_b[:], func=AF.Sin), reads=[rp_b], writes=[dst_T])

        def layer_norm_mod(x_T, hb_T, Ab, shb):
            rms_rstd(x_T, x_T[:], hb_T, ss, rstd)
            S.op('dve', lambda e: e.scalar_tensor_tensor(out=x_T[:], in0=x_T[:], scalar=rstd[:, 0:1], in1=Ab[:], op0=ALU.mult, op1=ALU.mult),
                 reads=[x_T, rstd, Ab], writes=[x_T])
            S.op('dve', lambda e: e.tensor_tensor(out=hb_T[:], in0=x_T[:], in1=shb[:], op=ALU.add), reads=[x_T, shb], writes=[hb_T])

        def make_hT(hb_T, dst_T, dst_fn, pTs):
            for half in range(2):
                pT = pTs[half]
                pv_ = pT[:, :].bitcast(BF16)
                for kk in range(8):
                    k = half * 8 + kk
                    transpose_bf(pT, pv_[:, kk * 128:(kk + 1) * 128], hb_T, hb_T[:, k * 128:(k + 1) * 128])
                S.op('act', lambda e: e.copy(out=dst_fn(half), in_=pv_[:, 0:1024].rearrange("p (a b) -> p a b", a=8)),
                     reads=[pT], writes=[dst_T])

        def rotary(src_T, src_ap4, cos_T, cos_ap, sin_T, sin_ap, rot_T, rot_ap4, tA, tB, n):
            cb = cos_ap.unsqueeze(1).to_broadcast([128, n, 128]); sb_ = sin_ap.unsqueeze(1).to_broadcast([128, n, 128])
            t1 = src_ap4[:, :, 0, :]; t2 = src_ap4[:, :, 1, :]
            S.op('dve', lambda e: e.tensor_tensor(out=tA[:, 0:n, :], in0=t1, in1=cb, op=ALU.mult), reads=[src_T, cos_T], writes=[tA])
            S.op('dve', lambda e: e.tensor_tensor(out=tB[:, 0:n, :], in0=t2, in1=sb_, op=ALU.mult), reads=[src_T, sin_T], writes=[tB])
            S.op('dve', lambda e: e.tensor_tensor(out=rot_ap4[:, :, 0, :], in0=tA[:, 0:n, :], in1=tB[:, 0:n, :], op=ALU.subtract),
                 reads=[tA, tB], writes=[rot_T])
            S.op('dve', lambda e: e.tensor_tensor(out=tA[:, 0:n, :], in0=t1, in1=sb_, op=ALU.mult), reads=[src_T, sin_T], writes=[tA])
            S.op('dve', lambda e: e.tensor_tensor(out=tB[:, 0:n, :], in0=t2, in1=cb, op=ALU.mult), reads=[src_T, cos_T], writes=[tB])
            S.op('dve', lambda e: e.tensor_tensor(out=rot_ap4[:, :, 1, :], in0=tA[:, 0:n, :], in1=tB[:, 0:n, :], op=ALU.add),
                 reads=[tA, tB], writes=[rot_T])

        with ExitStack() as ph:
            Wk = [S.sb([128, 4, 2048], BF16, 'Wk%d' % i, ph) for i in range(4)]
            Wv = [S.sb([128, 4, 2048], BF16, 'Wv%d' % i, ph) for i in range(4)]
            for k in range(KC):
                S.dma('pool', lambda e: e.dma_start(out=Wk[k // 4][:, k % 4, :], in_=w_in[k * 128:(k + 1) * 128, OFF_KR:OFF_KR + 2048]), writes=[Wk[k // 4]])
                S.dma('pool', lambda e: e.dma_start(out=Wv[k // 4][:, k % 4, :], in_=w_in[k * 128:(k + 1) * 128, OFF_VR:OFF_VR + 2048]), writes=[Wv[k // 4]])
            A1b = S.sb([128, D], F32, 'A1b', ph); ld(A1b, A1b[:], modd[1:2, :].partition_broadcast(128), reads=[modd])
            sh1b = S.sb([128, D], F32, 'sh1b', ph); ld(sh1b, sh1b[:], modd[0:1, :].partition_broadcast(128), reads=[modd])
            posi = S.sb([128, NPRE], I32, 'posi', ph); ld(posi, posi[:], pos_p[:, :])
            posf = S.sb([128, NPRE], F32, 'posf', ph)
            S.op('dve', lambda e: e.tensor_copy(out=posf[:], in_=posi[:]), reads=[posi], writes=[posf])
            pkt = S.sb([128, NPRE, 8], F32, 'pkt', ph); ld(pkt, pkt[:], pkd[:, :, :])
            xc = [S.sb([128, D], F32, 'xc%d' % i, ph) for i in range(2)]
            hb = S.sb([128, D], BF16, 'hb', ph)
            hT = S.sb([128, KC, 128], BF16, 'hT', ph)
            cosT = S.sb([128, 128], F32, 'cosT', ph); sinT = S.sb([128, 128], F32, 'sinT', ph)
            rot = S.sb([128, 2, 2, 128], F32, 'rot', ph)
            tA = S.sb([128, 2, 128], F32, 'tA', ph); tB = S.sb([128, 2, 128], F32, 'tB', ph)
            ks = S.sb([128, 8, 256], BF16, 'ks', ph)
            vb = S.sb([128, D], BF16, 'vb', ph)
            pTs = [S.ps([128, 512], F32, 'pT%d' % i, ph) for i in range(2)]
            pkb = [S.ps([128, 512], F32, 'pkb%d' % i, ph) for i in range(2)]
            pvb = [S.ps([128, 512], F32, 'pvb%d' % i, ph) for i in range(2)]
            pst = [S.ps([128, 512], F32, 'pst%d' % i, ph) for i in range(2)]
            ld(xc[0], xc[0][:], xp[0:128, :])
            import os
            for j in range(NPRE if not os.environ.get('SKIP_PREFIX') else 0):
                xcur = xc[j % 2]
                if j + 1 < NPRE:
                    ld(xc[(j + 1) % 2], xc[(j + 1) % 2][:], xp[(j + 1) * 128:(j + 2) * 128, :])
                layer_norm_mod(xcur, hb, A1b, sh1b)
                make_hT(hb, hT, lambda half: hT[:, half * 8:(half + 1) * 8, :], pTs)
                rope_tables(posf, posf[:, j:j + 1], cosT, cosT[:], sinT, sinT[:])
                for nb in range(4):
                    pb = pkb[nb % 2]
                    for k in range(KC):
                        S.op('pe', lambda e: e.matmul(pb[:], lhsT=hT[:, k, :], rhs=Wk[k // 4][:, k % 4, nb * 512:(nb + 1) * 512],
                                                      start=(k == 0), stop=(k == KC - 1)), reads=[hT, Wk[k // 4]], writes=[pb])
                    rotary(pb, pb[:, :].rearrange("p (h t f) -> p h t f", h=2, t=2), cosT, cosT[:], sinT, sinT[:],
                           rot, rot[:], tA, tB, 2)
                    S.op('dve', lambda e: e.tensor_tensor(out=ks[:, 2 * nb:2 * nb + 2, :], in0=rot[:].rearrange("p h t f -> p h (t f)"),
                                                          in1=pkt[:, j, 2 * nb:2 * nb + 2].unsqueeze(2).to_broadcast([128, 2, 256]), op=ALU.mult),
                         reads=[rot, pkt], writes=[ks])
                for nb in range(4):
                    pb = pvb[nb % 2]
                    for k in range(KC):
                        S.op('pe', lambda e: e.matmul(pb[:], lhsT=hT[:, k, :], rhs=Wv[k // 4][:, k % 4, nb * 512:(nb + 1) * 512],
                                                      start=(k == 0), stop=(k == KC - 1)), reads=[hT, Wv[k // 4]], writes=[pb])
                    S.op('act', lambda e: e.copy(out=vb[:, nb * 512:(nb + 1) * 512], in_=pb[:]), reads=[pb], writes=[vb])
                for h in range(8):
                    pb = pst[h % 2]
                    for half in range(2):
                        S.op('pe', lambda e: e.matmul(pb[:, half * 256:(half + 1) * 256], lhsT=ks[:, h, half * 128:(half + 1) * 128],
                                                      rhs=vb[:, h * 256:(h + 1) * 256], start=True, stop=True), reads=[ks, vb], writes=[pb])
                    S.op('dve', lambda e: e.tensor_tensor(out=state_f[:, h, :], in0=state_f[:, h, :], in1=pb[:], op=ALU.add),
                         reads=[state_f, pb], writes=[state_f])
            S.barrier()
        if stop_after == 'prefix':
            o = dout("d_state", [128, 8, 512])
            st(T(o, 'o'), o[:, :, :], state_f, state_f[:], is_out=True)
            S.finish()
            return nc, list(dbgo)
        hT_all = S.sb([128, KC, 1152], BF16, 'hT_all', mx)
        retT = S.sb([128, KC, 1024], BF16, 'retT', mx)
        attnT = S.sb([128, KC, 1024], BF16, 'attnT', mx)
        cos_o = S.sb([128, NOWN, 128], F32, 'cos_o', mx); sin_o = S.sb([128, NOWN, 128], F32, 'sin_o', mx)
        with ExitStack() as ph:
            A1b = S.sb([128, D], F32, 'A1b', ph); ld(A1b, A1b[:], modd[1:2, :].partition_broadcast(128), reads=[modd])
            sh1b = S.sb([128, D], F32, 'sh1b', ph); ld(sh1b, sh1b[:], modd[0:1, :].partition_broadcast(128), reads=[modd])
            posi = S.sb([128, NOWN], I32, 'posio', ph); ld(posi, posi[:], pos_o[:, :])
            posf = S.sb([128, NOWN], F32, 'posfo', ph)
            S.op('dve', lambda e: e.tensor_copy(out=posf[:], in_=posi[:]), reads=[posi], writes=[posf])
            xc = [S.sb([128, D], F32, 'xc%d' % i, ph) for i in range(2)]
            hb = [S.sb([128, D], BF16, 'hb%d' % i, ph) for i in range(2)]
            pTs = [S.ps([128, 512], F32, 'pT%d' % i, ph) for i in range(2)]
            for ci in range(9):
                src = xp[(NPRE - 1) * 128:NPRE * 128, :] if ci == 0 else xo[(ci - 1) * 128:ci * 128, :]
                xcur = xc[ci % 2]; hcur = hb[ci % 2]
                ld(xcur, xcur[:], src)
                layer_norm_mod(xcur, hcur, A1b, sh1b)
                make_hT(hcur, hT_all, lambda half: hT_all[:, half * 8:(half + 1) * 8, ci * 128:(ci + 1) * 128], pTs)
            for n in range(NOWN):
                rope_tables(posf, posf[:, n:n + 1], cos_o, cos_o[:, n, :], sin_o, sin_o[:, n, :])
            S.barrier()

        with ExitStack() as ph:
            Wh = [S.sb([128, 4, 1024], BF16, 'Wh%d' % i, ph) for i in range(4)]
            decT = S.sb([128, 8, 128], F32, 'decT', ph); ld(decT, decT[:], decTd[:, :, :])
            dq = S.sb([128, 8], F32, 'dq', ph); ld(dq, dq[:], dqd[:, :])
            dk = S.sb([128, 8], F32, 'dk', ph); ld(dk, dk[:], dkd[:, :])
            rgb = S.sb([128, D], F32, 'rgb', ph); ld(rgb, rgb[:], rgain[0:1, :].partition_broadcast(128))
            state_b = S.sb([128, 8, 512], BF16, 'state_b', ph)
            S.op('act', lambda e: e.copy(out=state_b[:], in_=state_f[:]), reads=[state_f], writes=[state_b])
            rotq = S.sb([128, 2, 2, 128], F32, 'rotq', ph)
            tA = S.sb([128, 2, 128], F32, 'tA', ph); tB = S.sb([128, 2, 128], F32, 'tB', ph)
            qkb = S.sb([128, 3, 256], BF16, 'qkb', ph)
            kd = S.sb([128, 256], BF16, 'kd', ph)
            vbh = S.sb([128, 256], BF16, 'vbh', ph)
            gs = S.sb([128, 256], F32, 'gs', ph)
            qkT = S.sb([128, 6, 128], BF16, 'qkT', ph)
            iT = S.sb([128, 128], BF16, 'iT', ph)
            junk = S.sb([128, 256], F32, 'junk', ph)
            rtmp = S.sb([128, 256], F32, 'rtmp', ph)
            reth = S.sb([128, 256], BF16, 'reth', ph)
            ss2 = S.sb([128, 1], F32, 'ss2', ph); r2 = S.sb([128, 1], F32, 'r2', ph)
            pp = [S.ps([128, 512], F32, 'pp%d' % i, ph) for i in range(4)]
            pTq = S.ps([128, 512], F32, 'pTq', ph); pTqv = pTq[:, :].bitcast(BF16)
            pi_ = S.ps([128, 512], F32, 'pi', ph); po_ = S.ps([128, 512], F32, 'po', ph); pds = S.ps([128, 512], F32, 'pds', ph)
            it = 0
            for h in range(8 if not os.environ.get('SKIP_RET') else 0):
                offs = [OFF_QR + h * 256, OFF_KR + h * 256, OFF_VR + h * 256, OFF_GR + h * 256]
                for sl in range(4):
                    for wi, off in enumerate(offs):
                        S.dma('pool', lambda e: e.dma_start(out=Wh[sl][:, :, wi * 256:(wi + 1) * 256],
                                                            in_=w_in[sl * 512:(sl + 1) * 512, off:off + 256].rearrange("(k p) n -> p k n", p=128)),
                              writes=[Wh[sl]])
                for n in range(NOWN):
                    tok = slice((n + 1) * 128, (n + 2) * 128)
                    pb0, pb1 = pp[2 * (it % 2)], pp[2 * (it % 2) + 1]
                    for blk, pb in ((0, pb0), (1, pb1)):
                        for k in range(KC):
                            S.op('pe', lambda e: e.matmul(pb[:], lhsT=hT_all[:, k, tok], rhs=Wh[k // 4][:, k % 4, blk * 512:(blk + 1) * 512],
                                                          start=(k == 0), stop=(k == KC - 1)), reads=[hT_all, Wh[k // 4]], writes=[pb])
                    rotary(pb0, pb0[:, :].rearrange("p (h t f) -> p h t f", h=2, t=2), cos_o, cos_o[:, n, :], sin_o, sin_o[:, n, :],
                           rotq, rotq[:], tA, tB, 2)
                    rq = rotq[:, 0, :, :].rearrange("p t f -> p (t f)"); rk = rotq[:, 1, :, :].rearrange("p t f -> p (t f)")
                    S.op('act', lambda e: e.copy(out=qkb[:, 0, :], in_=rq), reads=[rotq], writes=[qkb])
                    S.op('dve', lambda e: e.tensor_scalar(out=qkb[:, 1, :], in0=rq, scalar1=dq[:, h:h + 1], scalar2=None, op0=ALU.mult),
                         reads=[rotq, dq], writes=[qkb])
                    S.op('act', lambda e: e.mul(out=qkb[:, 2, :], in_=rk, mul=1.0 / 16.0), reads=[rotq], writes=[qkb])
                    S.op('dve', lambda e: e.tensor_scalar(out=kd[:], in0=rk, scalar1=dk[:, h:h + 1], scalar2=None, op0=ALU.mult),
                         reads=[rotq, dk], writes=[kd])
                    S.op('act', lambda e: e.copy(out=vbh[:], in_=pb1[:, 0:256]), reads=[pb1], writes=[vbh])
                    S.op('act', lambda e: e.activation(out=gs[:], in_=pb1[:, 256:512], func=AF.Silu), reads=[pb1], writes=[gs])
                    for a in range(3):
                        for half in range(2):
                            transpose_bf(pTq, pTqv[:, (a * 2 + half) * 128:(a * 2 + half + 1) * 128], qkb, qkb[:, a, half * 128:(half + 1) * 128])
                    S.op('act', lambda e: e.copy(out=qkT[:], in_=pTqv[:, 0:768].rearrange("p (a b) -> p a b", a=6)), reads=[pTq], writes=[qkT])
                    for half in range(2):
                        S.op('pe', lambda e: e.matmul(pi_[:, 0:128], lhsT=qkT[:, 4 + half, :], rhs=qkT[:, half, :], start=(half == 0), stop=(half == 1)),
                             reads=[qkT], writes=[pi_])
                    S.op('dve', lambda e: e.tensor_tensor(out=iT[:], in0=pi_[:, 0:128], in1=decT[:, h, :], op=ALU.mult), reads=[pi_, decT], writes=[iT])
                    S.op('pe', lambda e: e.matmul(po_[:, 0:256], lhsT=iT[:], rhs=vbh[:], start=True, stop=False), reads=[iT, vbh], writes=[po_])
                    for half in range(2):
                        S.op('pe', lambda e: e.matmul(po_[:, 0:256], lhsT=qkT[:, 2 + half, :], rhs=state_b[:, h, half * 256:(half + 1) * 256],
                                                      start=False, stop=(half == 1)), reads=[qkT, state_b], writes=[po_])
                    rms_rstd(po_, po_[:, 0:256], junk, ss2, r2, n=256)
                    S.op('dve', lambda e: e.scalar_tensor_tensor(out=rtmp[:], in0=po_[:, 0:256], scalar=r2[:, 0:1], in1=rgb[:, h * 256:(h + 1) * 256],
                                                                 op0=ALU.mult, op1=ALU.mult), reads=[po_, r2, rgb], writes=[rtmp])
                    S.op('dve', lambda e: e.tensor_tensor(out=reth[:], in0=rtmp[:], in1=gs[:], op=ALU.mult), reads=[rtmp, gs], writes=[reth])
                    for half in range(2):
                        transpose_bf(pTq, pTqv[:, 768 + half * 128:768 + (half + 1) * 128], reth, reth[:, half * 128:(half + 1) * 128])
                    S.op('act', lambda e: e.copy(out=retT[:, 2 * h:2 * h + 2, n * 128:(n + 1) * 128],
                                                 in_=pTqv[:, 768:1024].rearrange("p (a b) -> p a b", a=2)), reads=[pTq], writes=[retT])
                    for half in range(2):
                        S.op('pe', lambda e: e.matmul(pds[:, half * 256:(half + 1) * 256], lhsT=kd[:, half * 128:(half + 1) * 128], rhs=vbh[:],
                                                      start=True, stop=True), reads=[kd, vbh], writes=[pds])
                    S.op('dve', lambda e: e.scalar_tensor_tensor(out=state_f[:, h, :], in0=state_f[:, h, :], scalar=float(GAM[h] ** 128), in1=pds[:],
                                                                 op0=ALU.mult, op1=ALU.add), reads=[state_f, pds], writes=[state_f])
                    S.op('act', lambda e: e.copy(out=state_b[:, h, :], in_=state_f[:, h, :]), reads=[state_f], writes=[state_b])
                    it += 1
            S.barrier()
        if stop_after == 'ret':
            o = dout("d_retT", [128, KC, 1024], BF16)
            st(T(o, 'o'), o[:, :, :], retT, retT[:], is_out=True)
            S.finish()
            return nc, list(dbgo)
        with ExitStack() as ph:
            Wg = [S.sb([128, 4, 640], BF16, 'Wg%d' % i, ph) for i in range(4)]
            maskt = S.sb([128, 256], F32, 'maskt', ph); ld(maskt, maskt[:], maskd[:, :])
            mask0t = S.sb([128, 256], F32, 'mask0t', ph); ld(mask0t, mask0t[:], mask0d[:, :])
            sinkb = S.sb([128, 32], F32, 'sinkb', ph); ld(sinkb, sinkb[:], sinks[0:1, :].partition_broadcast(128))
            kT_all = S.sb([128, 2, 1152], BF16, 'kT_all', ph)
            v_all = S.sb([128, 9, 64], BF16, 'v_all', ph)
            qT = S.sb([128, 4, 1024], BF16, 'qT', ph)
            qb = S.sb([128, 512], BF16, 'qb', ph); k2 = S.sb([128, 2, 128], BF16, 'k2', ph)
            S.op('dve', lambda e: e.memset(k2[:], 0.0), writes=[k2])
            S_sb = S.sb([128, 8, 256], F32, 'S_sb', ph)
            P_bf = S.sb([128, 8, 256], BF16, 'P_bf', ph)
            PT = S.sb([128, 8, 2, 128], BF16, 'PT', ph)
            mx8 = S.sb([128, 8], F32, 'mx8', ph); negm = S.sb([128, 8], F32, 'negm', ph); rs = S.sb([128, 8], F32, 'rs', ph)
            es8 = S.sb([128, 8], F32, 'es8', ph); rden = S.sb([128, 8], F32, 'rden', ph)
            at_tok = S.sb([128, 8, 64], BF16, 'at_tok', ph)
            pq0 = S.ps([128, 512], F32, 'pq0', ph); pq1 = S.ps([128, 512], F32, 'pq1', ph)
            pTa = S.ps([128, 512], F32, 'pTa', ph); pTav = pTa[:, :].bitcast(BF16)
            psc = [S.ps([128, 512], F32, 'psc%d' % i, ph) for i in range(2)]
            pPT = S.ps([128, 512], F32, 'pPT', ph); pPTv = pPT[:, :].bitcast(BF16)
            ppv = S.ps([128, 512], F32, 'ppv', ph)
            for g in range(4):
                offs = [(OFF_QA + g * 512, 512, 0), (OFF_KA + g * 64, 64, 512), (OFF_VA + g * 64, 64, 576)]
                for sl in range(4):
                    for off, wd, dst in offs:
                        S.dma('pool', lambda e: e.dma_start(out=Wg[sl][:, :, dst:dst + wd],
                                                            in_=w_in[sl * 512:(sl + 1) * 512, off:off + wd].rearrange("(k p) n -> p k n", p=128)),
                              writes=[Wg[sl]])
                for ci in range(9):
                    tok = slice(ci * 128, (ci + 1) * 128)
                    if ci >= 1:
                        for k in range(KC):
                            S.op('pe', lambda e: e.matmul(pq0[:], lhsT=hT_all[:, k, tok], rhs=Wg[k // 4][:, k % 4, 0:512], start=(k == 0), stop=(k == KC - 1)),
                                 reads=[hT_all, Wg[k // 4]], writes=[pq0])
                        S.op('act', lambda e: e.mul(out=qb[:], in_=pq0[:], mul=0.125), reads=[pq0], writes=[qb])
                    for k in range(KC):
                        S.op('pe', lambda e: e.matmul(pq1[:, 0:128], lhsT=hT_all[:, k, tok], rhs=Wg[k // 4][:, k % 4, 512:640], start=(k == 0), stop=(k == KC - 1)),
                             reads=[hT_all, Wg[k // 4]], writes=[pq1])
                    S.op('dve', lambda e: e.tensor_copy(out=k2[:, 0, 0:64], in_=pq1[:, 0:64]), reads=[pq1], writes=[k2])
                    S.op('dve', lambda e: e.tensor_copy(out=k2[:, 1, 64:128], in_=pq1[:, 0:64]), reads=[pq1], writes=[k2])
                    S.op('dve', lambda e: e.tensor_copy(out=v_all[:, ci, :], in_=pq1[:, 64:128]), reads=[pq1], writes=[v_all])
                    if ci >= 1:
                        for i in range(4):
                            transpose_bf(pTa, pTav[:, i * 128:(i + 1) * 128], qb, qb[:, i * 128:(i + 1) * 128])
                    transpose_bf(pTa, pTav[:, 512:640], k2, k2[:, 0, :])
                    transpose_bf(pTa, pTav[:, 640:768], k2, k2[:, 1, :])
                    if ci >= 1:
                        S.op('act', lambda e: e.copy(out=qT[:, :, (ci - 1) * 128:ci * 128], in_=pTav[:, 0:512].rearrange("p (a b) -> p a b", a=4)),
                             reads=[pTa], writes=[qT])
                    S.op('act', lambda e: e.copy(out=kT_all[:, :, tok], in_=pTav[:, 512:768].rearrange("p (a b) -> p a b", a=2)), reads=[pTa], writes=[kT_all])
                import os
                ATS = int(os.environ.get('ATS', '9'))
                for n in range(NOWN if ATS >= 2 else 0):
                    mk = mask0t if n == 0 else maskt
                    for hh in range(2):
                        for j4 in range(4):
                            j = hh * 4 + j4; i = j // 2; r0 = (j % 2) * 64
                            pb = psc[j4 // 2]
                            S.op('pe', lambda e: e.matmul(pb[:, (j4 % 2) * 256:(j4 % 2 + 1) * 256], lhsT=qT[:, i, n * 128:(n + 1) * 128],
                                                          rhs=kT_all[:, j % 2, n * 128:n * 128 + 256], start=True, stop=True),
                                 reads=[qT, kT_all], writes=[pb])
                        for bnk in range(2):
                            S.op('dve', lambda e: e.tensor_tensor(out=S_sb[:, hh * 4 + bnk * 2:hh * 4 + bnk * 2 + 2, :],
                                                                  in0=psc[bnk][:, :].rearrange("p (a b) -> p a b", a=2),
                                                                  in1=mk[:, :].unsqueeze(1).to_broadcast([128, 2, 256]), op=ALU.add),
                                 reads=[psc[bnk], mk], writes=[S_sb])
                    ATQ = int(os.environ.get('ATQ', '9'))
                    if ATQ < 2:
                        continue
                    S.op('dve', lambda e: e.tensor_reduce(out=mx8[:], in_=S_sb[:], axis=AX.X, op=ALU.max), reads=[S_sb], writes=[mx8])
                    S.op('dve', lambda e: e.tensor_tensor(out=mx8[:], in0=mx8[:], in1=sinkb[:, g * 8:(g + 1) * 8], op=ALU.max), reads=[mx8, sinkb], writes=[mx8])
                    S.op('dve', lambda e: e.tensor_scalar(out=negm[:], in0=mx8[:], scalar1=-1.0, scalar2=None, op0=ALU.mult), reads=[mx8], writes=[negm])
                    if ATQ < 3:
                        continue
                    for j in range(8):
                        S.op('act', lambda e: e.activation(out=P_bf[:, j, :], in_=S_sb[:, j, :], func=AF.Exp, bias=negm[:, j:j + 1], scale=1.0,
                                                           accum_out=rs[:, j:j + 1]), reads=[S_sb, negm], writes=[P_bf, rs])
                    if ATQ < 4:
                        continue
                    S.op('dve', lambda e: e.tensor_tensor(out=es8[:], in0=sinkb[:, g * 8:(g + 1) * 8], in1=mx8[:], op=ALU.subtract), reads=[sinkb, mx8], writes=[es8])
                    S.op('act', lambda e: e.activation(out=es8[:], in_=es8[:], func=AF.Exp), reads=[es8], writes=[es8])
                    S.op('dve', lambda e: e.tensor_tensor(out=es8[:], in0=es8[:], in1=rs[:], op=ALU.add), reads=[es8, rs], writes=[es8])
                    S.op('dve', lambda e: e.reciprocal(out=rden[:], in_=es8[:]), reads=[es8], writes=[rden])
                    if ATS < 3:
                        continue
                    for hh in range(2):
                        for j4 in range(4):
                            for t in range(2):
                                transpose_bf(pPT, pPTv[:, (j4 * 2 + t) * 128:(j4 * 2 + t + 1) * 128], P_bf, P_bf[:, hh * 4 + j4, t * 128:(t + 1) * 128])
                        S.op('act', lambda e: e.copy(out=PT[:, hh * 4:(hh + 1) * 4, :, :], in_=pPTv[:, 0:1024].rearrange("p (a t b) -> p a t b", a=4, t=2)),
                             reads=[pPT], writes=[PT])
                    for j in range(8):
                        for t in range(2):
                            S.op('pe', lambda e: e.matmul(ppv[:, j * 64:(j + 1) * 64], lhsT=PT[:, j, t, :], rhs=v_all[:, n + t, :], start=(t == 0), stop=(t == 1)),
                                 reads=[PT, v_all], writes=[ppv])
                    S.op('dve', lambda e: e.tensor_tensor(out=at_tok[:], in0=ppv[:, :].rearrange("p (a b) -> p a b", a=8),
                                                          in1=rden[:, :].unsqueeze(2).to_broadcast([128, 8, 64]), op=ALU.mult), reads=[ppv, rden], writes=[at_tok])
                    for i in range(4):
                        transpose_bf(pTa, pTav[:, i * 128:(i + 1) * 128], at_tok, at_tok[:].rearrange("p a b -> p (a b)")[:, i * 128:(i + 1) * 128])
                    S.op('act', lambda e: e.copy(out=attnT[:, g * 4:(g + 1) * 4, n * 128:(n + 1) * 128], in_=pTav[:, 0:512].rearrange("p (a b) -> p a b", a=4)),
                         reads=[pTa], writes=[attnT])
            S.barrier()
        if stop_after == 'attn':
            o = dout("d_attnT", [128, KC, 1024], BF16)
            st(T(o, 'o'), o[:, :, :], attnT, attnT[:], is_out=True)
            S.finish()
            return nc, list(dbgo)
        with ExitStack() as ph:
            mT = S.sb([128, KC, 512], BF16, 'mT', ph)
            Wm = [S.sb([128, KC, 128], BF16, 'Wm%d' % i, ph) for i in range(4)]
            Wo = [S.sb([128, 4, 512], BF16, 'Wo%d' % i, ph) for i in range(4)]
            g1b = S.sb([128, D], F32, 'g1b', ph); ld(g1b, g1b[:], modd[2:3, :].partition_broadcast(128), reads=[modd])
            sga = S.sb([128, 512], F32, 'sga', ph); sgr = S.sb([128, 512], F32, 'sgr', ph)
            rr = [S.sb([128, 512], F32, 'rr%d' % i, ph) for i in range(2)]
            xs = [S.sb([128, 512], F32, 'xs%d' % i, ph) for i in range(2)]
            pA = S.ps([128, 512], F32, 'pA', ph); pR = S.ps([128, 512], F32, 'pR', ph)
            pGa = S.ps([128, 512], F32, 'pGa', ph); pGr = S.ps([128, 512], F32, 'pGr', ph)
            po2 = [S.ps([128, 512], F32, 'po2%d' % i, ph) for i in range(2)]
            srcs = [(w_ba, 0), (w_br, 0), (w_in, OFF_GA), (w_in, OFF_GTR)]
            for th in range(2):
                tsl = slice(th * 512, (th + 1) * 512)
                hsl = slice(128 + th * 512, 128 + (th + 1) * 512)
                for cg in range(16):
                    for wi, (wsrc, off) in enumerate(srcs):
                        for kq in range(4):
                            S.dma('pool', lambda e: e.dma_start(out=Wm[wi][:, kq * 4:(kq + 1) * 4, :],
                                                                in_=wsrc[kq * 512:(kq + 1) * 512, off + cg * 128:off + (cg + 1) * 128].rearrange("(k p) n -> p k n", p=128)),
                                  writes=[Wm[wi]])
                    for jj in range(1):
                        csl = slice(0, 128)
                        for pb, wt, act_T, asl in ((pA, Wm[0], attnT, tsl), (pR, Wm[1], retT, tsl), (pGa, Wm[2], hT_all, hsl), (pGr, Wm[3], hT_all, hsl)):
                            for k in range(KC):
                                S.op('pe', lambda e: e.matmul(pb[:], lhsT=wt[:, k, csl], rhs=act_T[:, k, asl], start=(k == 0), stop=(k == KC - 1)),
                                     reads=[wt, act_T], writes=[pb])
                        S.op('act', lambda e: e.activation(out=sga[:], in_=pGa[:], func=AF.Sigmoid), reads=[pGa], writes=[sga])
                        S.op('act', lambda e: e.activation(out=sgr[:], in_=pGr[:], func=AF.Sigmoid), reads=[pGr], writes=[sgr])
                        S.op('dve', lambda e: e.tensor_tensor(out=sga[:], in0=sga[:], in1=pA[:], op=ALU.mult), reads=[sga, pA], writes=[sga])
                        S.op('dve', lambda e: e.tensor_tensor(out=sgr[:], in0=sgr[:], in1=pR[:], op=ALU.mult), reads=[sgr, pR], writes=[sgr])
                        S.op('dve', lambda e: e.tensor_tensor(out=mT[:, cg, :], in0=sga[:], in1=sgr[:], op=ALU.add), reads=[sga, sgr], writes=[mT])
                it = 0
                for nb in range(4):
                    for kq in range(4):
                        S.dma('pool', lambda e: e.dma_start(out=Wo[kq][:], in_=w_o[kq * 512:(kq + 1) * 512, nb * 512:(nb + 1) * 512].rearrange("(k p) n -> p k n", p=128)),
                              writes=[Wo[kq]])
                    for c4 in range(4):
                        n = th * 4 + c4
                        pb = po2[it % 2]; r_ = rr[it % 2]; x_ = xs[it % 2]
                        ld(x_, x_[:], xo[n * 128:(n + 1) * 128, nb * 512:(nb + 1) * 512])
                        for k in range(KC):
                            S.op('pe', lambda e: e.matmul(pb[:], lhsT=mT[:, k, c4 * 128:(c4 + 1) * 128], rhs=Wo[k // 4][:, k % 4, :], start=(k == 0), stop=(k == KC - 1)),
                                 reads=[mT, Wo[k // 4]], writes=[pb])
                        S.op('dve', lambda e: e.tensor_tensor(out=r_[:], in0=pb[:], in1=g1b[:, nb * 512:(nb + 1) * 512], op=ALU.mult), reads=[pb, g1b], writes=[r_])
                        S.op('dve', lambda e: e.tensor_tensor(out=r_[:], in0=r_[:], in1=x_[:], op=ALU.add), reads=[r_, x_], writes=[r_])
                        st(xmid, xmid[n * 128:(n + 1) * 128, nb * 512:(nb + 1) * 512], r_, r_[:])
                        it += 1
            S.barrier()
        mx.close()
        S.barrier()
        if stop_after == 'mix':
            o = dout("d_xmid", [1024, D])
            t = S.sb([128, NOWN, D], F32, 'dbgx')
            ld(t, t[:], xmid[:, :].rearrange("(n p) d -> p n d", p=128), reads=[xmid])
            st(T(o, 'o'), o[:, :].rearrange("(n p) d -> p n d", p=128), t, t[:], is_out=True)
            S.finish()
            return nc, list(dbgo)
        moe = es.enter_context(ExitStack())
        slot_i = S.sb([128, 2, NOWN], I32, 'slot_i', moe)
        idxg_i = S.sb([128, NOV, 16], I32, 'idxg_i', moe)
        idxd_i = S.sb([128, NOV, 8], I32, 'idxd_i', moe)
        ss = S.sb([128, 1], F32, 'ss_m', moe); rstd = S.sb([128, 1], F32, 'rstd_m', moe)
        with ExitStack() as ph:
            A2b = S.sb([128, D], F32, 'A2b', ph); ld(A2b, A2b[:], modd[4:5, :].partition_broadcast(128), reads=[modd])
            sh2b = S.sb([128, D], F32, 'sh2b', ph); ld(sh2b, sh2b[:], modd[3:4, :].partition_broadcast(128), reads=[modd])
            Wr_sb = S.sb([128, KC, 68], F32, 'Wr_sb', ph); ld(Wr_sb, Wr_sb[:], w_rt[:, :].rearrange("(k p) n -> p k n", p=128))
            brb = S.sb([128, 68], F32, 'brb', ph); ld(brb, brb[:], b_rt[0:1, :].partition_broadcast(128))
            zt = S.sb([1, D], BF16, 'zt', ph)
            S.op('dve', lambda e: e.memset(zt[:], 0.0), writes=[zt])
            st(H2, H2[1024:1025, :], zt, zt[:])
            xm = [S.sb([128, D], F32, 'xm%d' % i, ph) for i in range(2)]
            h2b = S.sb([128, D], BF16, 'h2b', ph)
            h2T = S.sb([128, KC, 128], F32, 'h2T', ph)
            lg = S.sb([128, 68], F32, 'lg', ph)
            sm = {k: S.sb([128, 1], F32, 'sm_' + k, ph) for k in ('gmax', 'negg', 'gsum', 'gw', 'm1', 'm2', 'd', 'p1')}
            ohg = S.sb([128, 4], F32, 'ohg', ph); gej = S.sb([128, 4], F32, 'gej', ph)
            t416 = S.sb([128, 4, 16], F32, 't416', ph)
            el = S.sb([128, 16], F32, 'el', ph); el2 = S.sb([128, 16], F32, 'el2', ph)
            oh1 = S.sb([128, 16], F32, 'oh1', ph); oh2 = S.sb([128, 16], F32, 'oh2', ph)
            OH = [S.sb([128, NOWN, 64], F32, 'OH%d' % i, ph) for i in range(2)]
            wts = S.sb([128, 2, NOWN], F32, 'wts', ph)
            Cb = S.sb([128, NOWN, 64], BF16, 'Cb', ph)
            pTf = [S.ps([128, 512], F32, 'pTf%d' % i, ph) for i in range(2)]
            plg = S.ps([128, 512], F32, 'plg', ph)
            pPC = S.ps([128, 512], F32, 'pPC', ph); pcnt = S.ps([128, 512], F32, 'pcnt', ph)
            for n in range(NOWN):
                x_ = xm[n % 2]
                ld(x_, x_[:], xmid[n * 128:(n + 1) * 128, :], reads=[xmid])
                rms_rstd(x_, x_[:], h2b, ss, rstd)
                S.op('dve', lambda e: e.scalar_tensor_tensor(out=x_[:], in0=x_[:], scalar=rstd[:, 0:1], in1=A2b[:], op0=ALU.mult, op1=ALU.mult),
                     reads=[x_, rstd, A2b], writes=[x_])
                S.op('dve', lambda e: e.tensor_tensor(out=x_[:], in0=x_[:], in1=sh2b[:], op=ALU.add), reads=[x_, sh2b], writes=[x_])
                S.op('act', lambda e: e.copy(out=h2b[:], in_=x_[:]), reads=[x_], writes=[h2b])
                st(H2, H2[n * 128:(n + 1) * 128, :], h2b, h2b[:])
                for grp in range(4):
                    pb = pTf[grp % 2]
                    for kk in range(4):
                        k = grp * 4 + kk
                        S.op('pe', lambda e: e.matmul(pb[:, kk * 128:(kk + 1) * 128], lhsT=x_[:, k * 128:(k + 1) * 128], rhs=idf[:], start=True, stop=True),
                             reads=[x_, idf], writes=[pb])
                    S.op('act', lambda e: e.copy(out=h2T[:, grp * 4:(grp + 1) * 4, :], in_=pb[:, :].rearrange("p (a b) -> p a b", a=4)), reads=[pb], writes=[h2T])
                for k in range(KC):
                    S.op('pe', lambda e: e.matmul(plg[:, 0:68], lhsT=h2T[:, k, :], rhs=Wr_sb[:, k, :], start=(k == 0), stop=(k == KC - 1)),
                         reads=[h2T, Wr_sb], writes=[plg])
                S.op('dve', lambda e: e.tensor_tensor(out=lg[:], in0=plg[:, 0:68], in1=brb[:], op=ALU.add), reads=[plg, brb], writes=[lg])
                S.op('dve', lambda e: e.tensor_reduce(out=sm['gmax'][:], in_=lg[:, 0:4], axis=AX.X, op=ALU.max), reads=[lg], writes=[sm['gmax']])
                S.op('dve', lambda e: e.tensor_scalar(out=ohg[:], in0=lg[:, 0:4], scalar1=sm['gmax'][:, 0:1], scalar2=None, op0=ALU.is_ge), reads=[lg, sm['gmax']], writes=[ohg])
                S.op('dve', lambda e: e.tensor_scalar(out=sm['negg'][:], in0=sm['gmax'][:], scalar1=-1.0, scalar2=None, op0=ALU.mult), reads=[sm['gmax']], writes=[sm['negg']])
                S.op('act', lambda e: e.activation(out=gej[:], in_=lg[:, 0:4], func=AF.Exp, bias=sm['negg'][:, 0:1], scale=1.0, accum_out=sm['gsum'][:]),
                     reads=[lg, sm['negg']], writes=[gej, sm['gsum']])
                S.op('dve', lambda e: e.reciprocal(out=sm['gw'][:], in_=sm['gsum'][:]), reads=[sm['gsum']], writes=[sm['gw']])
                S.op('dve', lambda e: e.tensor_tensor(out=t416[:], in0=lg[:, 4:68].rearrange("p (g e) -> p g e", g=4),
                                                      in1=ohg[:, :].unsqueeze(2).to_broadcast([128, 4, 16]), op=ALU.mult), reads=[lg, ohg], writes=[t416])
                S.op('dve', lambda e: e.tensor_reduce(out=el[:], in_=t416[:].rearrange("p g e -> p e g"), axis=AX.X, op=ALU.add), reads=[t416], writes=[el])
                S.op('dve', lambda e: e.tensor_reduce(out=sm['m1'][:], in_=el[:], axis=AX.X, op=ALU.max), reads=[el], writes=[sm['m1']])
                S.op('dve', lambda e: e.tensor_scalar(out=oh1[:], in0=el[:], scalar1=sm['m1'][:, 0:1], scalar2=None, op0=ALU.is_ge), reads=[el, sm['m1']], writes=[oh1])
                S.op('dve', lambda e: e.scalar_tensor_tensor(out=el2[:], in0=oh1[:], scalar=-1e30, in1=el[:], op0=ALU.mult, op1=ALU.add), reads=[oh1, el], writes=[el2])
                S.op('dve', lambda e: e.tensor_reduce(out=sm['m2'][:], in_=el2[:], axis=AX.X, op=ALU.max), reads=[el2], writes=[sm['m2']])
                S.op('dve', lambda e: e.tensor_scalar(out=oh2[:], in0=el2[:], scalar1=sm['m2'][:, 0:1], scalar2=None, op0=ALU.is_ge), reads=[el2, sm['m2']], writes=[oh2])
                S.op('dve', lambda e: e.tensor_tensor(out=sm['d'][:], in0=sm['m2'][:], in1=sm['m1'][:], op=ALU.subtract), reads=[sm['m1'], sm['m2']], writes=[sm['d']])
                S.op('act', lambda e: e.activation(out=sm['d'][:], in_=sm['d'][:], func=AF.Exp), reads=[sm['d']], writes=[sm['d']])
                S.op('dve', lambda e: e.tensor_scalar(out=sm['d'][:], in0=sm['d'][:], scalar1=1.0, scalar2=None, op0=ALU.add), reads=[sm['d']], writes=[sm['d']])
                S.op('dve', lambda e: e.reciprocal(out=sm['p1'][:], in_=sm['d'][:]), reads=[sm['d']], writes=[sm['p1']])
                S.op('dve', lambda e: e.tensor_tensor(out=wts[:, 0, n:n + 1], in0=sm['p1'][:], in1=sm['gw'][:], op=ALU.mult), reads=[sm['p1'], sm['gw']], writes=[wts])
                S.op('dve', lambda e: e.tensor_tensor(out=wts[:, 1, n:n + 1], in0=sm['gw'][:], in1=wts[:, 0, n:n + 1], op=ALU.subtract), reads=[sm['gw'], wts], writes=[wts])
                for kk, oh in ((0, oh1), (1, oh2)):
                    S.op('dve', lambda e: e.tensor_tensor(out=OH[kk][:, n, :].rearrange("p (g e) -> p g e", g=4),
                                                          in0=ohg[:, :].unsqueeze(2).to_broadcast([128, 4, 16]),
                                                          in1=oh[:, :].unsqueeze(1).to_broadcast([128, 4, 16]), op=ALU.mult), reads=[ohg, oh], writes=[OH[kk]])
                S.op('dve', lambda e: e.tensor_tensor(out=Cb[:, n, :], in0=OH[0][:, n, :], in1=OH[1][:, n, :], op=ALU.add), reads=[OH[0], OH[1]], writes=[Cb])
            ones_bf = S.sb([128, 128], BF16, 'ones_bf', ph)
            S.op('dve', lambda e: e.memset(ones_bf[:], 1.0), writes=[ones_bf])
            Lf = S.sb([128, 128], F32, 'Lf', ph); ld(Lf, Lf[:], Lstd[:, :])
            Lb = S.sb([128, 128], BF16, 'Lb', ph)
            S.op('dve', lambda e: e.tensor_copy(out=Lb[:], in_=Lf[:]), reads=[Lf], writes=[Lb])
            for n in range(NOWN):
                for m in range(n + 1):
                    S.op('pe', lambda e: e.matmul(pPC[:, n * 64:(n + 1) * 64], lhsT=(Lb[:] if m == n else ones_bf[:]), rhs=Cb[:, m, :], start=(m == 0), stop=(m == n)),
                         reads=[Lb, ones_bf, Cb], writes=[pPC])
            for m in range(NOWN):
                S.op('pe', lambda e: e.matmul(pcnt[:, 0:64], lhsT=ones_bf[:], rhs=Cb[:, m, :], start=(m == 0), stop=(m == NOWN - 1)), reads=[ones_bf, Cb], writes=[pcnt])
            CAP = int(os.environ.get('OVCAP', 128))
            e128 = S.sb([128, 64], F32, 'e128', ph); ld(e128, e128[:], e128d[:, :])
            ocf = S.sb([128, 64], F32, 'ocf', ph); cnti = S.sb([128, 64], I32, 'cnti', ph); padf = S.sb([128, 64], F32, 'padf', ph)
            S.op('dve', lambda e: e.tensor_scalar(out=ocf[:], in0=pcnt[:, 0:64], scalar1=-float(CAP), scalar2=0.0, op0=ALU.add, op1=ALU.max), reads=[pcnt], writes=[ocf])
            S.op('dve', lambda e: e.tensor_scalar(out=ocf[:], in0=ocf[:], scalar1=127.0, scalar2=None, op0=ALU.add), reads=[ocf], writes=[ocf])
            S.op('dve', lambda e: e.tensor_copy(out=cnti[:], in_=ocf[:]), reads=[ocf], writes=[cnti])
            S.op('dve', lambda e: e.tensor_scalar(out=cnti[:], in0=cnti[:], scalar1=7, scalar2=7, op0=ALU.arith_shift_right, op1=ALU.logical_shift_left), reads=[cnti], writes=[cnti])
            S.op('dve', lambda e: e.tensor_copy(out=padf[:], in_=cnti[:]), reads=[cnti], writes=[padf])
            cs = [S.sb([128, 64], F32, 'cs%d' % i, ph) for i in range(2)]
            S.op('dve', lambda e: e.tensor_copy(out=cs[0][:], in_=padf[:]), reads=[padf], writes=[cs[0]])
            cur = 0
            for s_ in (1, 2, 4, 8, 16, 32):
                a, b_ = cs[cur], cs[1 - cur]
                S.op('dve', lambda e: e.tensor_copy(out=b_[:, 0:s_], in_=a[:, 0:s_]), reads=[a], writes=[b_])
                S.op('dve', lambda e: e.tensor_tensor(out=b_[:, s_:64], in0=a[:, s_:64], in1=a[:, 0:64 - s_], op=ALU.add), reads=[a], writes=[b_])
                cur = 1 - cur
            pend = cs[cur]; ob = cs[1 - cur]
            S.op('dve', lambda e: e.tensor_tensor(out=ob[:], in0=pend[:], in1=padf[:], op=ALU.subtract), reads=[pend, padf], writes=[ob])
            S.op('dve', lambda e: e.tensor_scalar(out=ob[:], in0=ob[:], scalar1=float(64 * 128 - CAP), scalar2=None, op0=ALU.add), reads=[ob], writes=[ob])
            slot_f = S.sb([128, 2, NOWN], F32, 'slot_f', ph)
            tmpb = S.sb([128, NOWN, 64], F32, 'tmpb', ph)
            rk = S.sb([128, NOWN], F32, 'rk', ph); eb = S.sb([128, NOWN], F32, 'eb', ph); obk = S.sb([128, NOWN], F32, 'obk', ph); isov = S.sb([128, NOWN], F32, 'isov', ph)
            for kk in range(2):
                S.op('dve', lambda e: e.tensor_tensor(out=tmpb[:], in0=OH[kk][:], in1=pPC[:, :].rearrange("p (n e) -> p n e", n=NOWN), op=ALU.mult), reads=[OH[kk], pPC], writes=[tmpb])
                S.op('dve', lambda e: e.tensor_reduce(out=rk[:], in_=tmpb[:], axis=AX.X, op=ALU.add), reads=[tmpb], writes=[rk])
                S.op('dve', lambda e: e.tensor_tensor(out=tmpb[:], in0=OH[kk][:], in1=e128[:, :].unsqueeze(1).to_broadcast([128, NOWN, 64]), op=ALU.mult), reads=[OH[kk], e128], writes=[tmpb])
                S.op('dve', lambda e: e.tensor_reduce(out=eb[:], in_=tmpb[:], axis=AX.X, op=ALU.add), reads=[tmpb], writes=[eb])
                S.op('dve', lambda e: e.tensor_tensor(out=tmpb[:], in0=OH[kk][:], in1=ob[:, :].unsqueeze(1).to_broadcast([128, NOWN, 64]), op=ALU.mult), reads=[OH[kk], ob], writes=[tmpb])
                S.op('dve', lambda e: e.tensor_reduce(out=obk[:], in_=tmpb[:], axis=AX.X, op=ALU.add), reads=[tmpb], writes=[obk])
                S.op('dve', lambda e: e.tensor_scalar(out=isov[:], in0=rk[:], scalar1=float(CAP), scalar2=None, op0=ALU.is_ge), reads=[rk], writes=[isov])
                S.op('dve', lambda e: e.tensor_tensor(out=obk[:], in0=obk[:], in1=eb[:], op=ALU.subtract), reads=[obk, eb], writes=[obk])
                S.op('dve', lambda e: e.tensor_tensor(out=obk[:], in0=obk[:], in1=isov[:], op=ALU.mult), reads=[obk, isov], writes=[obk])
                S.op('dve', lambda e: e.tensor_tensor(out=rk[:], in0=rk[:], in1=eb[:], op=ALU.add), reads=[rk, eb], writes=[rk])
                S.op('dve', lambda e: e.tensor_tensor(out=slot_f[:, kk, :], in0=rk[:], in1=obk[:], op=ALU.add), reads=[rk, obk], writes=[slot_f])
            S.op('dve', lambda e: e.tensor_copy(out=slot_i[:], in_=slot_f[:]), reads=[slot_f], writes=[slot_i])
            blk128 = S.sb([128, NOV], F32, 'blk128', ph); ld(blk128, blk128[:], blk128d[:, 0:NOV])
            kcoff = S.sb([128, 16], F32, 'kcoff', ph); ld(kcoff, kcoff[:], kcoffd[:, :])
            pidx = S.sb([128, 1], F32, 'pidx', ph); ld(pidx, pidx[:], pidxd[:, :])
            cmp = S.sb([128, NOV, 64], BF16, 'cmp', ph)
            S.op('dve', lambda e: e.tensor_tensor(out=cmp[:], in0=pend[:, :].unsqueeze(1).to_broadcast([128, NOV, 64]),
                                                  in1=blk128[:, :].unsqueeze(2).to_broadcast([128, NOV, 64]), op=ALU.is_le), reads=[pend, blk128], writes=[cmp])
            bef = S.sb([128, NOV], F32, 'bef', ph); gb = S.sb([128, NOV], F32, 'gb', ph)
            S.op('dve', lambda e: e.tensor_reduce(out=bef[:], in_=cmp[:], axis=AX.X, op=ALU.add), reads=[cmp], writes=[bef])
            S.op('dve', lambda e: e.tensor_scalar(out=gb[:], in0=bef[:], scalar1=64.0, scalar2=None, op0=ALU.is_lt), reads=[bef], writes=[gb])
            S.op('dve', lambda e: e.tensor_tensor(out=bef[:], in0=bef[:], in1=gb[:], op=ALU.mult), reads=[bef, gb], writes=[bef])
            idxf = S.sb([128, NOV, 16], F32, 'idxf', ph)
            for mult, nk, dst in ((2048.0, 16, idxg_i), (1024.0, 8, idxd_i)):
                S.op('dve', lambda e: e.tensor_scalar(out=gb[:], in0=bef[:], scalar1=mult, scalar2=pidx[:, 0:1], op0=ALU.mult, op1=ALU.add), reads=[bef, pidx], writes=[gb])
                S.op('dve', lambda e: e.tensor_tensor(out=idxf[:, :, 0:nk], in0=gb[:, :].unsqueeze(2).to_broadcast([128, NOV, nk]),
                                                      in1=kcoff[:, 0:nk].unsqueeze(1).to_broadcast([128, NOV, nk]), op=ALU.add), reads=[gb, kcoff], writes=[idxf])
                S.op('dve', lambda e: e.tensor_copy(out=dst[:], in_=idxf[:, :, 0:nk]), reads=[idxf], writes=[dst])
            ri0 = S.sb([128, NBLK, 16], I32, 'ri0', ph)
            S.op('dve', lambda e: e.memset(ri0[:], 0), writes=[ri0])
            S.op('dve', lambda e: e.memset(ri0[:, :, 0:1], 1024), writes=[ri0])
            st(rinfo, rinfo[:, :].rearrange("(b p) c -> p b c", p=128), ri0, ri0[:])
            tokid = S.sb([128, NOWN], I32, 'tokid', ph); ld(tokid, tokid[:], tokidd[:, :])
            ris = [S.sb([128, 16], I32, 'ri%d' % i, ph) for i in range(4)]
            for r_ in ris:
                S.op('dve', lambda e: e.memset(r_[:], 0), writes=[r_])
            it = 0
            for n in range(NOWN):
                for kk in range(2):
                    r_ = ris[it % 4]
                    S.op('dve', lambda e: e.tensor_copy(out=r_[:, 0:1], in_=tokid[:, n:n + 1]), reads=[tokid], writes=[r_])
                    S.op('dve', lambda e: e.tensor_copy(out=r_[:, 1:2].bitcast(F32), in_=wts[:, kk, n:n + 1]), reads=[wts], writes=[r_])
                    S.dma('pool', lambda e: e.indirect_dma_start(out=rinfo[:, :], out_offset=bass.IndirectOffsetOnAxis(ap=slot_i[:, kk, n:n + 1], axis=0),
                                                                 in_=r_[:], in_offset=None), reads=[r_, slot_i], writes=[rinfo])
                    it += 1
            S.barrier()
        if stop_after == 'moe_route':
            o1 = dout("d_slot", [128, 2, NOWN], I32); st(T(o1, 'o'), o1[:, :, :], slot_i, slot_i[:], is_out=True)
            o2 = dout("d_idxg", [128, NOV, 16], I32); st(T(o2, 'o'), o2[:, :, :], idxg_i, idxg_i[:], is_out=True)
            o3 = dout("d_rinfo", [NBLK * 128, 16], I32)
            t = S.sb([128, NBLK, 16], I32, 'dbgr')
            ld(t, t[:], rinfo[:, :].rearrange("(b p) c -> p b c", p=128), reads=[rinfo])
            st(T(o3, 'o'), o3[:, :].rearrange("(b p) c -> p b c", p=128), t, t[:], is_out=True)
            S.finish()
            return nc, list(dbgo)
        with ExitStack() as ph:
            Wg_t = [S.sb([128, 1024], BF16, 'Wg_t%d' % i, ph) for i in range(16)]
            Wu_t = [S.sb([128, 1024], BF16, 'Wu_t%d' % i, ph) for i in range(16)]
            Wd_t = [S.sb([128, D], BF16, 'Wd_t%d' % i, ph) for i in range(8)]
            rts = [S.sb([128, 16], I32, 'rt%d' % i, ph) for i in range(2)]
            xgs = [S.sb([128, D], BF16, 'xg%d' % i, ph) for i in range(2)]
            xgT = S.sb([128, KC, 128], BF16, 'xgT', ph)
            sg = S.sb([128, 1024], F32, 'sg', ph)
            actb = S.sb([128, 1024], BF16, 'actb', ph)
            actT = S.sb([128, 8, 128], BF16, 'actT', ph)
            ybs = [S.sb([128, D], F32, 'yb%d' % i, ph) for i in range(2)]
            pT1 = S.ps([128, 512], F32, 'pT1', ph); pT1v = pT1[:, :].bitcast(BF16)
            pT2 = S.ps([128, 512], F32, 'pT2', ph); pT2v = pT2[:, :].bitcast(BF16)
            pgu = [S.ps([128, 512], F32, 'pgu%d' % i, ph) for i in range(4)]
            pdn = [S.ps([128, 512], F32, 'pdn%d' % i, ph) for i in range(2)]
            NB_RUN = int(os.environ.get('NB_RUN', NBLK))
            for b in range(NB_RUN):
                rt = rts[b % 2]; xg = xgs[b % 2]; yb = ybs[b % 2]
                ld(rt, rt[:], rinfo[b * 128:(b + 1) * 128, :], reads=[rinfo])
                S.dma('pool', lambda e: e.indirect_dma_start(out=xg[:], out_offset=None, in_=H2[:, :],
                                                             in_offset=bass.IndirectOffsetOnAxis(ap=rt[:, 0:1], axis=0)), reads=[rt, H2], writes=[xg])
                if b < 64:
                    for kc in range(16):
                        r0 = b * 2048 + kc * 128
                        S.dma('pool', lambda e: e.dma_start(out=Wg_t[kc][:], in_=w_eg[r0:r0 + 128, :]), writes=[Wg_t[kc]])
                        S.dma('pool', lambda e: e.dma_start(out=Wu_t[kc][:], in_=w_eu[r0:r0 + 128, :]), writes=[Wu_t[kc]])
                    for kc in range(8):
                        r0 = b * 1024 + kc * 128
                        S.dma('pool', lambda e: e.dma_start(out=Wd_t[kc][:], in_=w_ed[r0:r0 + 128, :]), writes=[Wd_t[kc]])
                else:
                    ob_ = b - 64
                    for kc in range(16):
                        S.dma('pool', lambda e: e.indirect_dma_start(out=Wg_t[kc][:], out_offset=None, in_=w_eg[:, :],
                                                                     in_offset=bass.IndirectOffsetOnAxis(ap=idxg_i[:, ob_, kc:kc + 1], axis=0)), reads=[idxg_i], writes=[Wg_t[kc]])
                        S.dma('pool', lambda e: e.indirect_dma_start(out=Wu_t[kc][:], out_offset=None, in_=w_eu[:, :],
                                                                     in_offset=bass.IndirectOffsetOnAxis(ap=idxg_i[:, ob_, kc:kc + 1], axis=0)), reads=[idxg_i], writes=[Wu_t[kc]])
                    for kc in range(8):
                        S.dma('pool', lambda e: e.indirect_dma_start(out=Wd_t[kc][:], out_offset=None, in_=w_ed[:, :],
                                                                     in_offset=bass.IndirectOffsetOnAxis(ap=idxd_i[:, ob_, kc:kc + 1], axis=0)), reads=[idxd_i], writes=[Wd_t[kc]])
                for half in range(2):
                    for kk in range(8):
                        k = half * 8 + kk
                        transpose_bf(pT1, pT1v[:, kk * 128:(kk + 1) * 128], xg, xg[:, k * 128:(k + 1) * 128])
                    S.op('act', lambda e: e.copy(out=xgT[:, half * 8:(half + 1) * 8, :], in_=pT1v[:, 0:1024].rearrange("p (a b) -> p a b", a=8)), reads=[pT1], writes=[xgT])
                for kc in range(16):
                    for wi, wt in ((0, Wg_t[kc]), (1, Wu_t[kc])):
                        for nb in range(2):
                            pb = pgu[wi * 2 + nb]
                            S.op('pe', lambda e: e.matmul(pb[:], lhsT=xgT[:, kc, :], rhs=wt[:, nb * 512:(nb + 1) * 512], start=(kc == 0), stop=(kc == 15)),
                                 reads=[xgT, wt], writes=[pb])
                for nb in range(2):
                    S.op('act', lambda e: e.activation(out=sg[:, nb * 512:(nb + 1) * 512], in_=pgu[nb][:], func=AF.Silu), reads=[pgu[nb]], writes=[sg])
                    S.op('dve', lambda e: e.tensor_tensor(out=actb[:, nb * 512:(nb + 1) * 512], in0=sg[:, nb * 512:(nb + 1) * 512], in1=pgu[2 + nb][:], op=ALU.mult),
                         reads=[sg, pgu[2 + nb]], writes=[actb])
                for kk in range(8):
                    transpose_bf(pT2, pT2v[:, kk * 128:(kk + 1) * 128], actb, actb[:, kk * 128:(kk + 1) * 128])
                S.op('act', lambda e: e.copy(out=actT[:], in_=pT2v[:, 0:1024].rearrange("p (a b) -> p a b", a=8)), reads=[pT2], writes=[actT])
                for hf in range(2):
                    for kc in range(8):
                        for i in range(2):
                            c0 = hf * 1024 + i * 512
                            S.op('pe', lambda e: e.matmul(pdn[i][:], lhsT=actT[:, kc, :], rhs=Wd_t[kc][:, c0:c0 + 512], start=(kc == 0), stop=(kc == 7)),
                                 reads=[actT, Wd_t[kc]], writes=[pdn[i]])
                    for i in range(2):
                        c0 = hf * 1024 + i * 512
                        S.op('dve', lambda e: e.tensor_scalar(out=yb[:, c0:c0 + 512], in0=pdn[i][:], scalar1=rt[:, 1:2].bitcast(F32), scalar2=None, op0=ALU.mult),
                             reads=[pdn[i], rt], writes=[yb])
                st(Yd, Yd[b * 128:(b + 1) * 128, :], yb, yb[:])
            S.barrier()
        with ExitStack() as ph:
            g2b = S.sb([128, D], F32, 'g2b', ph); ld(g2b, g2b[:], modd[5:6, :].partition_broadcast(128), reads=[modd])
            gfb = S.sb([128, D], F32, 'gfb', ph); ld(gfb, gfb[:], nfg[0:1, :].partition_broadcast(128))
            xm = [S.sb([128, D], F32, 'xm%d' % i, ph) for i in range(2)]
            y1 = [S.sb([128, D], F32, 'y1%d' % i, ph) for i in range(2)]
            y2 = [S.sb([128, D], F32, 'y2%d' % i, ph) for i in range(2)]
            junkb = S.sb([128, D], BF16, 'junkb', ph)
            for n in range(NOWN):
                x_, a_, b_ = xm[n % 2], y1[n % 2], y2[n % 2]
                ld(x_, x_[:], xmid[n * 128:(n + 1) * 128, :], reads=[xmid])
                for kk, dst in ((0, a_), (1, b_)):
                    S.dma('pool', lambda e: e.indirect_dma_start(out=dst[:], out_offset=None, in_=Yd[:, :],
                                                                 in_offset=bass.IndirectOffsetOnAxis(ap=slot_i[:, kk, n:n + 1], axis=0)), reads=[slot_i, Yd], writes=[dst])
                S.op('dve', lambda e: e.tensor_tensor(out=a_[:], in0=a_[:], in1=b_[:], op=ALU.add), reads=[a_, b_], writes=[a_])
                S.op('dve', lambda e: e.tensor_tensor(out=a_[:], in0=a_[:], in1=g2b[:], op=ALU.mult), reads=[a_, g2b], writes=[a_])
                S.op('dve', lambda e: e.tensor_tensor(out=a_[:], in0=a_[:], in1=x_[:], op=ALU.add), reads=[a_, x_], writes=[a_])
                rms_rstd(a_, a_[:], junkb, ss, rstd)
                S.op('dve', lambda e: e.scalar_tensor_tensor(out=a_[:], in0=a_[:], scalar=rstd[:, 0:1], in1=gfb[:], op0=ALU.mult, op1=ALU.mult),
                     reads=[a_, rstd, gfb], writes=[a_])
                st(T(out, 'out'), out[n * 128:(n + 1) * 128, :], a_, a_[:], is_out=True)
        S.finish()
    return nc, list(dbgo)


def _consts():
    c = {}
    c["ident"] = np.eye(128, dtype=np.float32)
    i = np.arange(128)[:, None]; j = np.arange(256)[None, :]
    valid = (j > i) & (j <= i + 128)
    c["mask"] = np.where(valid, 0.0, -1e30).astype(np.float32)
    gam = np.array(GAM, dtype=np.float64)
    lg = np.log(gam)
    e = np.arange(128)[:, None, None]; cc = np.arange(128)[None, None, :]
    diff = cc - e
    c["decT"] = np.where(diff >= 0, np.exp(np.maximum(diff, 0) * lg[None, :, None]), 0.0).astype(np.float32)
    idx = np.arange(128)[:, None].astype(np.float64)
    c["dq"] = np.exp((idx + 1.0) * lg[None, :]).astype(np.float32)
    c["dk"] = (np.exp((127.0 - idx) * lg[None, :]) / 16.0).astype(np.float32)
    invf = (10000.0 ** (-(np.arange(128, dtype=np.float32) / np.float32(128)))).astype(np.float32)
    c["invf"] = np.broadcast_to(invf[None, :], (128, 128)).copy()
    c["Lst"] = (np.arange(128)[:, None] < np.arange(128)[None, :]).astype(np.float32)
    c["tokid"] = (np.arange(NOWN)[None, :] * 128 + np.arange(128)[:, None]).astype(np.int32)
    c["blk128"] = np.broadcast_to((np.arange(NBLK, dtype=np.float32) * 128.0)[None, :], (128, NBLK)).copy()
    c["kcoff"] = np.broadcast_to((np.arange(16, dtype=np.float32) * 128.0)[None, :], (128, 16)).copy()
    c["pidx"] = np.arange(128, dtype=np.float32)[:, None].copy()
    c["e128"] = np.broadcast_to((np.arange(64, dtype=np.float32) * 128.0)[None, :], (128, 64)).copy()
    return c


def prep_inputs(x, c, positions, norm1_gain, norm2_gain, final_norm_gain, w_ada, b_ada, w_in,
                attn_sinks, ret_norm_gain, w_branch_attn, w_branch_ret, w_out,
                w_router_group, b_router_group, w_router_expert, b_router_expert,
                w_expert_gate, w_expert_up, w_expert_down):
    f = lambda a: np.ascontiguousarray(np.asarray(a))
    x = f(x); c = f(c); positions = f(positions)
    shared = dict(
        w_ada=f(w_ada)[0], b_ada=f(b_ada), w_in=f(w_in)[0], attn_sinks=f(attn_sinks), ret_norm_gain=f(ret_norm_gain),
        w_branch_attn=f(w_branch_attn)[0], w_branch_ret=f(w_branch_ret)[0], w_out=f(w_out)[0],
        w_router=np.concatenate([f(w_router_group)[0], f(w_router_expert)[0]], axis=1),
        b_router=np.concatenate([f(b_router_group), f(b_router_expert)], axis=1),
        w_expert_gate=f(w_expert_gate).reshape(64 * D, 1024), w_expert_up=f(w_expert_up).reshape(64 * D, 1024),
        w_expert_down=f(w_expert_down).reshape(64 * 1024, D),
        norm1_gain=f(norm1_gain), norm2_gain=f(norm2_gain), final_norm_gain=f(final_norm_gain).reshape(1, D),
    )
    shared.update(_consts())
    gam = np.array(GAM, dtype=np.float64); lg = np.log(gam)
    maps = []
    for core in range(8):
        b, q = core // 4, core % 4
        m = dict(shared)
        m["xo"] = x[b, q * 1024:(q + 1) * 1024]
        npre = q * 8
        xp = np.zeros((NPRE * 128, D), np.float32)
        pp = np.zeros((NPRE * 128,), np.int32)
        if npre:
            xp[(NPRE - npre) * 128:] = x[b, :q * 1024]
            pp[(NPRE - npre) * 128:] = positions[b, :q * 1024]
        m["xp"] = xp
        m["pos_o"] = np.ascontiguousarray(positions[b, q * 1024:(q + 1) * 1024].reshape(NOWN, 128).T)
        m["pos_p"] = np.ascontiguousarray(pp.reshape(NPRE, 128).T)
        m["cT"] = np.ascontiguousarray(c[b].reshape(KC, 128).T)
        valid = (np.arange(NPRE) >= NPRE - npre).astype(np.float64)
        idx = np.arange(128)[:, None, None].astype(np.float64)
        jj = np.arange(NPRE)[None, :, None].astype(np.float64)
        pk = np.exp((127.0 - idx) * lg[None, None, :] + 128.0 * (NPRE - 1 - jj) * lg[None, None, :]) / 16.0 * valid[None, :, None]
        m["pk"] = pk.astype(np.float32)
        mk = shared["mask"].copy()
        if q == 0:
            mk[:, :128] = -1e30
        m["mask0"] = mk
        maps.append(m)
    return maps


def kernel(**inputs):
    maps = prep_inputs(**inputs)
    nc, _ = build()
    res = run_bass_kernel_spmd(nc, maps, core_ids=list(range(8)))
    outp = np.zeros((2, 4096, D), np.float32)
    for core in range(8):
        b, q = core // 4, core % 4
        outp[b, q * 1024:(q + 1) * 1024] = res.results[core]["out"]
    return outp
```

```python
import numpy as np
import concourse.bass as bass
import concourse.mybir as mybir
from concourse.bass_utils import run_bass_kernel_spmd

F32 = mybir.dt.float32
BF16 = mybir.dt.bfloat16
I32 = mybir.dt.int32
U32 = mybir.dt.uint32
ALU = mybir.AluOpType
AF = mybir.ActivationFunctionType
AX = mybir.AxisListType


class Tr:
    def __init__(self):
        self.last_w = None
        self.readers = {}


class T:
    def __init__(self, h, name, tr=None):
        self.h = h
        self.name = name
        self.tr = tr or Tr()

    @property
    def last_w(self):
        return self.tr.last_w

    @last_w.setter
    def last_w(self, v):
        self.tr.last_w = v

    @property
    def readers(self):
        return self.tr.readers

    @readers.setter
    def readers(self, v):
        self.tr.readers = v

    def v(self, ap):
        return T(ap, self.name, self.tr)

    def __getitem__(self, k):
        return self.h[k]


class Sched:
    ENG = ('pe', 'dve', 'act', 'pool', 'sp')

    def __init__(self, nc, es):
        self.nc = nc
        self.es = es
        self.eng = {'pe': nc.tensor, 'dve': nc.vector, 'act': nc.scalar, 'pool': nc.gpsimd, 'sp': nc.sync}
        self.ops = {e: [] for e in self.ENG}
        self.cnt = {e: 0 for e in self.ENG}
        self.sem = {e: es.enter_context(nc.semaphore('s_' + e)) for e in self.ENG if e != 'sp'}
        self.seen = {e: {} for e in self.ENG}
        self.NP = 8
        self.dsem = {q: [es.enter_context(nc.semaphore('d_%s%d' % (q, i))) for i in range(self.NP)]
                     for q in ('sp', 'pool', 'act')}
        self.dn = {q: 0 for q in ('sp', 'pool', 'act')}
        self.ntile = 0
        self.out_tokens = []

    def sb(self, shape, dt, name=None, es=None):
        self.ntile += 1
        name = (name or 't') + '_%d' % self.ntile
        h = (es or self.es).enter_context(self.nc.sbuf_tensor(name, list(shape), dt))
        return T(h, name)

    def ps(self, shape, dt, name=None, es=None):
        self.ntile += 1
        name = (name or 'p') + '_%d' % self.ntile
        h = (es or self.es).enter_context(self.nc.psum_tensor(name, list(shape), dt))
        return T(h, name)

    def barrier(self):
        toks = [('eng', f, self.cnt[f]) for f in ('pe', 'dve', 'act', 'pool') if self.cnt[f] > 0]
        for q in ('sp', 'pool', 'act'):
            n = self.dn[q]
            for i in range(max(0, n - self.NP), n):
                toks.append(('dma', self.dsem[q][i % self.NP], 16 * (i // self.NP + 1)))
        for e in self.ENG:
            for tok in toks:
                self._wait(e, tok)

    def dram(self, name, shape, dt, kind='Internal'):
        h = self.nc.dram_tensor(name, list(shape), dt, kind=kind)
        return T(h, name)

    def _wait(self, e, tok):
        if tok is None:
            return
        kind, key, val = tok
        if kind == 'eng' and key == e and e == 'pe':
            return
        if kind == 'eng' and key == 'sp':
            return
        sk = (kind, key if kind == 'eng' else id(key))
        if self.seen[e].get(sk, 0) >= val:
            return
        self.seen[e][sk] = val
        sem = self.sem[key] if kind == 'eng' else key
        eng = self.eng[e]
        eng.wait_ge(sem, val)

    def _deps(self, e, reads, writes):
        for t in reads:
            self._wait(e, t.last_w)
        for t in writes:
            self._wait(e, t.last_w)
            for tok in list(t.readers.values()):
                self._wait(e, tok)

    def _update(self, tok, reads, writes):
        for t in reads:
            kind, key, val = tok
            rk = (kind, key if kind == 'eng' else (id(key)))
            t.readers[rk] = tok
        for t in writes:
            t.last_w = tok
            t.readers = {}

    def op(self, e, fn, reads=(), writes=()):
        assert e in ('pe', 'dve', 'act', 'pool')
        self._deps(e, reads, writes)
        self.cnt[e] += 1
        idx = self.cnt[e]
        eng = self.eng[e]
        sem = self.sem[e]
        fn(eng).then_inc(sem, 1)
        self._update(('eng', e, idx), reads, writes)

    def dma(self, q, fn, reads=(), writes=(), is_out=False):
        self._deps(q, reads, writes)
        n = self.dn[q]
        self.dn[q] += 1
        slot = n % self.NP
        sem = self.dsem[q][slot]
        if n >= self.NP:
            self._wait(q, ('dma', sem, 16 * (n // self.NP)))
        val = 16 * (n // self.NP + 1)
        eng = self.eng[q]
        fn(eng).then_inc(sem, 16)
        tok = ('dma', sem, val)
        self._update(tok, reads, writes)
        if is_out:
            self.out_tokens.append(tok)
        return tok

    def finish(self):
        for tok in self.out_tokens:
            self._wait('sp', tok)
        for q in ('sp', 'pool', 'act'):
            n = self.dn[q]
            for i in range(max(0, n - self.NP), n):
                self._wait('sp', ('dma', self.dsem[q][i % self.NP], 16 * (i // self.NP + 1)))


import math
import os
from contextlib import ExitStack

D = 2048
KC = 16
NOWN = 8
NPRE = 24
NBLK = 80
NOV = 16
OFF_QA, OFF_KA, OFF_VA, OFF_QR, OFF_KR, OFF_VR, OFF_GR, OFF_GA, OFF_GTR = 0, 2048, 2304, 2560, 4608, 6656, 8704, 10752, 12800
EPS = 1e-6
TWO_PI = 2.0 * math.pi
C1 = 6.28125
C2 = TWO_PI - C1
GAM = [1.0 - 2.0 ** (-5.0 - h) for h in range(8)]


def build(stop_after=None):
    nc = bass.Bass("TRN2", target_bir_lowering=False)

    def din(name, shape, dt=F32):
        return nc.dram_tensor(name, list(shape), dt, kind="ExternalInput")

    xo = din("xo", [1024, D]); xp = din("xp", [NPRE * 128, D])
    pos_o = din("pos_o", [128, NOWN], I32); pos_p = din("pos_p", [128, NPRE], I32)
    cT = din("cT", [128, KC]); pkd = din("pk", [128, NPRE, 8]); mask0d = din("mask0", [128, 256])
    w_ada = din("w_ada", [D, 6 * D]); b_ada = din("b_ada", [1, 6 * D]); w_in = din("w_in", [D, 14848])
    sinks = din("attn_sinks", [1, 32]); rgain = din("ret_norm_gain", [1, D])
    wm_t = din("wm_t", [4 * 16 * 128, D]); w_o = din("w_out", [D, D])
    w_rt = din("w_router", [D, 68]); b_rt = din("b_router", [1, 68])
    if stop_after is None or stop_after == 'moe_full':
        w_eg = din("w_expert_gate", [64 * D, 1024]); w_eu = din("w_expert_up", [64 * D, 1024]); w_ed = din("w_expert_down", [64 * 1024, D])
    n1g = din("norm1_gain", [1, D]); n2g = din("norm2_gain", [1, D]); nfg = din("final_norm_gain", [1, D])
    identd = din("ident", [128, 128]); maskd = din("mask", [128, 256]); decTd = din("decT", [128, 8, 128])
    dqd = din("dq", [128, 8]); dkd = din("dk", [128, 8]); invfd = din("invf", [128, 128]); Lstd = din("Lst", [128, 128])
    tokidd = din("tokid", [128, NOWN], I32); blk128d = din("blk128", [128, NBLK]); kcoffd = din("kcoff", [128, 16]); pidxd = din("pidx", [128, 1]); e128d = din("e128", [128, 64])
    out = nc.dram_tensor("out", [1024, D], F32, kind="ExternalOutput")
    dbgo = {}

    def dout(name, shape, dt=F32):
        dbgo[name] = nc.dram_tensor(name, list(shape), dt, kind="ExternalOutput")
        return dbgo[name]

    modd = T(nc.dram_tensor("modd", [6, D], F32, kind="Internal"), "modd")
    xmid = T(nc.dram_tensor("xmid", [1024, D], F32, kind="Internal"), "xmid")
    H2 = T(nc.dram_tensor("H2", [1025, D], BF16, kind="Internal"), "H2")
    rinfo = T(nc.dram_tensor("rinfo", [NBLK * 128, 16], I32, kind="Internal"), "rinfo")
    Yd = T(nc.dram_tensor("Yd", [NBLK * 128, D], F32, kind="Internal"), "Yd")

    with ExitStack() as es:
        S = Sched(nc, es)
        qrr = [0]

        def ld(dst_T, dst_ap, src_ap, q='sp', reads=(), extra_w=()):
            return S.dma(q, lambda e: e.dma_start(out=dst_ap, in_=src_ap), reads=list(reads), writes=[dst_T] + list(extra_w))

        def st(dst_T, dst_ap, src_T, src_ap, q='sp', is_out=False):
            return S.dma(q, lambda e: e.dma_start(out=dst_ap, in_=src_ap), reads=[src_T], writes=[dst_T], is_out=is_out)

        idf = S.sb([128, 128], F32, 'idf'); ld(idf, idf[:], identd[:, :])
        idb = S.sb([128, 128], BF16, 'idb')
        S.op('dve', lambda e: e.tensor_copy(out=idb[:], in_=idf[:]), reads=[idf], writes=[idb])

        def transpose_bf(ps_T, ps_ap, src_T, src_ap):
            S.op('pe', lambda e: e.transpose(out=ps_ap, in_=src_ap, identity=idb[:]), reads=[src_T, idb], writes=[ps_T])

        def rms_rstd(x_T, x_ap, junk_T, ss, rstd, n=D):
            S.op('act', lambda e: e.activation(out=junk_T[:], in_=x_ap, func=AF.Square, accum_out=ss[:]), reads=[x_T], writes=[junk_T, ss])
            S.op('act', lambda e: e.activation(out=ss[:], in_=ss[:], func=AF.Sqrt, scale=1.0 / n, bias=EPS), reads=[ss], writes=[ss])
            S.op('dve', lambda e: e.reciprocal(out=rstd[:], in_=ss[:]), reads=[ss], writes=[rstd])

        with ExitStack() as ph:
            cs = S.sb([128, KC], F32, 'cs', ph); ld(cs, cs[:], cT[:, :])
            sc = S.sb([128, KC], F32, 'sc', ph)
            S.op('act', lambda e: e.activation(out=sc[:], in_=cs[:], func=AF.Silu), reads=[cs], writes=[sc])
            scb = S.sb([128, KC, 128], BF16, 'scb', ph)
            S.op('dve', lambda e: e.tensor_copy(out=scb[:], in_=sc[:, :].unsqueeze(2).to_broadcast([128, KC, 128])), reads=[sc], writes=[scb])
            g1 = S.sb([128, D], F32, 'g1', ph); ld(g1, g1[:], n1g[0:1, :].partition_broadcast(128))
            g2 = S.sb([128, D], F32, 'g2', ph); ld(g2, g2[:], n2g[0:1, :].partition_broadcast(128))
            wbuf = [S.sb([128, KC, 512], BF16, 'wada%d' % i, ph) for i in range(3)]
            bbuf = [S.sb([128, 512], F32, 'bada%d' % i, ph) for i in range(2)]
            rbuf = [S.sb([128, 512], F32, 'rada%d' % i, ph) for i in range(2)]
            pms = [S.ps([128, 512], F32, 'pada%d' % i, ph) for i in range(2)]
            it = 0
            for j in range(6):
                for nb in range(4):
                    c0 = j * D + nb * 512
                    wb, bb, rb, pm = wbuf[it % 3], bbuf[it % 2], rbuf[it % 2], pms[it % 2]
                    for kq in range(4):
                        S.dma('pool', lambda e: e.dma_start(out=wb[:, kq * 4:(kq + 1) * 4, :],
                                                            in_=w_ada[kq * 512:(kq + 1) * 512, c0:c0 + 512].rearrange("(k p) n -> p k n", p=128)), writes=[wb])
                    ld(bb, bb[:], b_ada[0:1, c0:c0 + 512].partition_broadcast(128))
                    for k in range(KC):
                        S.op('pe', lambda e: e.matmul(pm[:], lhsT=scb[:, k, :], rhs=wb[:, k, :], start=(k == 0), stop=(k == KC - 1)),
                             reads=[scb, wb], writes=[pm])
                    S.op('dve', lambda e: e.tensor_tensor(out=rb[:], in0=pm[:], in1=bb[:], op=ALU.add), reads=[pm, bb], writes=[rb])
                    if j in (1, 4):
                        gg = g1 if j == 1 else g2
                        S.op('dve', lambda e: e.scalar_tensor_tensor(out=rb[:], in0=rb[:], scalar=1.0, in1=gg[:, nb * 512:(nb + 1) * 512],
                                                                     op0=ALU.add, op1=ALU.mult), reads=[rb, gg], writes=[rb])
                    st(modd, modd[j:j + 1, nb * 512:(nb + 1) * 512], rb, rb[0:1, :])
                    it += 1
            S.barrier()
        if stop_after == 'ada':
            o = dout("d_mod", [6, D])
            t = S.sb([6, D], F32, 'dbg'); ld(t, t[:], modd[:, :], reads=[modd])
            st(T(o, 'o'), o[:, :], t, t[:], is_out=True)
            S.finish()
            return nc, list(dbgo)
        mx = es.enter_context(ExitStack())
        state_f = S.sb([128, 8, 512], F32, 'state_f', mx)
        S.op('dve', lambda e: e.memset(state_f[:], 0.0), writes=[state_f])
        invf = S.sb([128, 128], F32, 'invf', mx); ld(invf, invf[:], invfd[:, :])
        rp_a = S.sb([128, 128], F32, 'rp_a', mx); rp_b = S.sb([128, 128], F32, 'rp_b', mx)
        rp_k = S.sb([128, 128], I32, 'rp_k', mx); rp_f = S.sb([128, 128], F32, 'rp_f', mx)
        ss = S.sb([128, 1], F32, 'ss', mx); rstd = S.sb([128, 1], F32, 'rstd', mx)

        def rope_tables(pos_T, pos_ap, cos_T, cos_ap, sin_T, sin_ap):
            S.op('dve', lambda e: e.tensor_scalar(out=rp_a[:], in0=invf[:], scalar1=pos_ap, scalar2=None, op0=ALU.mult),
                 reads=[invf, pos_T], writes=[rp_a])
            for which in (0, 1):
                dst_T, dst_ap = (sin_T, sin_ap) if which == 0 else (cos_T, cos_ap)
                if which == 1:
                    S.op('dve', lambda e: e.tensor_scalar(out=rp_a[:], in0=rp_a[:], scalar1=math.pi / 2, scalar2=None, op0=ALU.add),
                         reads=[rp_a], writes=[rp_a])
                S.op('dve', lambda e: e.tensor_scalar(out=rp_k[:], in0=rp_a[:], scalar1=1.0 / TWO_PI, scalar2=None, op0=ALU.mult),
                     reads=[rp_a], writes=[rp_k])
                S.op('dve', lambda e: e.tensor_copy(out=rp_f[:], in_=rp_k[:]), reads=[rp_k], writes=[rp_f])
                S.op('dve', lambda e: e.scalar_tensor_tensor(out=rp_b[:], in0=rp_f[:], scalar=-C1, in1=rp_a[:], op0=ALU.mult, op1=ALU.add),
                     reads=[rp_f, rp_a], writes=[rp_b])
                S.op('dve', lambda e: e.scalar_tensor_tensor(out=rp_b[:], in0=rp_f[:], scalar=-C2, in1=rp_b[:], op0=ALU.mult, op1=ALU.add),
                     reads=[rp_f, rp_b], writes=[rp_b])
                S.op('dve', lambda e: e.tensor_scalar(out=rp_b[:], in0=rp_b[:], scalar1=3.1415925, scalar2=-3.1415925, op0=ALU.min, op1=ALU.max),
                     reads=[rp_b], writes=[rp_b])
                S.op('act', lambda e: e.activation(out=dst_ap, in_=rp_b[:], func=AF.Sin), reads=[rp_b], writes=[dst_T])

        def layer_norm_mod(x_T, hb_T, Ab, shb):
            rms_rstd(x_T, x_T[:], hb_T, ss, rstd)
            S.op('dve', lambda e: e.scalar_tensor_tensor(out=x_T[:], in0=x_T[:], scalar=rstd[:, 0:1], in1=Ab[:], op0=ALU.mult, op1=ALU.mult),
                 reads=[x_T, rstd, Ab], writes=[x_T])
            S.op('dve', lambda e: e.tensor_tensor(out=hb_T[:], in0=x_T[:], in1=shb[:], op=ALU.add), reads=[x_T, shb], writes=[hb_T])

        def make_hT(hb_T, dst_T, dst_fn, pTs):
            for half in range(2):
                pT = pTs[half]
                pv_ = pT[:, :].bitcast(BF16)
                for kk in range(8):
                    k = half * 8 + kk
                    transpose_bf(pT, pv_[:, kk * 128:(kk + 1) * 128], hb_T, hb_T[:, k * 128:(k + 1) * 128])
                S.op('act', lambda e: e.copy(out=dst_fn(half), in_=pv_[:, 0:1024].rearrange("p (a b) -> p a b", a=8)),
                     reads=[pT], writes=[dst_T])

        def rotary(src_T, src_ap4, cos_T, cos_ap, sin_T, sin_ap, rot_T, rot_ap4, tA, tB, n):
            cb = cos_ap.unsqueeze(1).to_broadcast([128, n, 128]); sb_ = sin_ap.unsqueeze(1).to_broadcast([128, n, 128])
            t1 = src_ap4[:, :, 0, :]; t2 = src_ap4[:, :, 1, :]
            S.op('dve', lambda e: e.tensor_tensor(out=tA[:, 0:n, :], in0=t1, in1=cb, op=ALU.mult), reads=[src_T, cos_T], writes=[tA])
            S.op('dve', lambda e: e.tensor_tensor(out=tB[:, 0:n, :], in0=t2, in1=sb_, op=ALU.mult), reads=[src_T, sin_T], writes=[tB])
            S.op('dve', lambda e: e.tensor_tensor(out=rot_ap4[:, :, 0, :], in0=tA[:, 0:n, :], in1=tB[:, 0:n, :], op=ALU.subtract),
                 reads=[tA, tB], writes=[rot_T])
            S.op('dve', lambda e: e.tensor_tensor(out=tA[:, 0:n, :], in0=t1, in1=sb_, op=ALU.mult), reads=[src_T, sin_T], writes=[tA])
            S.op('dve', lambda e: e.tensor_tensor(out=tB[:, 0:n, :], in0=t2, in1=cb, op=ALU.mult), reads=[src_T, cos_T], writes=[tB])
            S.op('dve', lambda e: e.tensor_tensor(out=rot_ap4[:, :, 1, :], in0=tA[:, 0:n, :], in1=tB[:, 0:n, :], op=ALU.add),
                 reads=[tA, tB], writes=[rot_T])

        with ExitStack() as ph:
            Wk = [S.sb([128, 4, 2048], BF16, 'Wk%d' % i, ph) for i in range(4)]
            Wv = [S.sb([128, 4, 2048], BF16, 'Wv%d' % i, ph) for i in range(4)]
            for k in range(KC):
                S.dma('pool', lambda e: e.dma_start(out=Wk[k // 4][:, k % 4, :], in_=w_in[k * 128:(k + 1) * 128, OFF_KR:OFF_KR + 2048]), writes=[Wk[k // 4]])
                S.dma('pool', lambda e: e.dma_start(out=Wv[k // 4][:, k % 4, :], in_=w_in[k * 128:(k + 1) * 128, OFF_VR:OFF_VR + 2048]), writes=[Wv[k // 4]])
            A1b = S.sb([128, D], F32, 'A1b', ph); ld(A1b, A1b[:], modd[1:2, :].partition_broadcast(128), reads=[modd])
            sh1b = S.sb([128, D], F32, 'sh1b', ph); ld(sh1b, sh1b[:], modd[0:1, :].partition_broadcast(128), reads=[modd])
            posi = S.sb([128, NPRE], I32, 'posi', ph); ld(posi, posi[:], pos_p[:, :])
            posf = S.sb([128, NPRE], F32, 'posf', ph)
            S.op('dve', lambda e: e.tensor_copy(out=posf[:], in_=posi[:]), reads=[posi], writes=[posf])
            pkt = S.sb([128, NPRE, 8], F32, 'pkt', ph); ld(pkt, pkt[:], pkd[:, :, :])
            xc = [S.sb([128, D], F32, 'xc%d' % i, ph) for i in range(2)]
            hb = S.sb([128, D], BF16, 'hb', ph)
            hT = S.sb([128, KC, 128], BF16, 'hT', ph)
            cosT = S.sb([128, 128], F32, 'cosT', ph); sinT = S.sb([128, 128], F32, 'sinT', ph)
            rot = S.sb([128, 2, 2, 128], F32, 'rot', ph)
            tA = S.sb([128, 2, 128], F32, 'tA', ph); tB = S.sb([128, 2, 128], F32, 'tB', ph)
            ks = S.sb([128, 8, 256], BF16, 'ks', ph)
            vb = S.sb([128, D], BF16, 'vb', ph)
            pTs = [S.ps([128, 512], F32, 'pT%d' % i, ph) for i in range(2)]
            pkb = [S.ps([128, 512], F32, 'pkb%d' % i, ph) for i in range(2)]
            pvb = [S.ps([128, 512], F32, 'pvb%d' % i, ph) for i in range(2)]
            pst = [S.ps([128, 512], F32, 'pst%d' % i, ph) for i in range(2)]
            ld(xc[0], xc[0][:], xp[0:128, :])
            import os
            for j in range(NPRE if not os.environ.get('SKIP_PREFIX') else 0):
                xcur = xc[j % 2]
                if j + 1 < NPRE:
                    ld(xc[(j + 1) % 2], xc[(j + 1) % 2][:], xp[(j + 1) * 128:(j + 2) * 128, :])
                layer_norm_mod(xcur, hb, A1b, sh1b)
                make_hT(hb, hT, lambda half: hT[:, half * 8:(half + 1) * 8, :], pTs)
                rope_tables(posf, posf[:, j:j + 1], cosT, cosT[:], sinT, sinT[:])
                for nb in range(4):
                    pb = pkb[nb % 2]
                    for k in range(KC):
                        S.op('pe', lambda e: e.matmul(pb[:], lhsT=hT[:, k, :], rhs=Wk[k // 4][:, k % 4, nb * 512:(nb + 1) * 512],
                                                      start=(k == 0), stop=(k == KC - 1)), reads=[hT, Wk[k // 4]], writes=[pb])
                    rotary(pb, pb[:, :].rearrange("p (h t f) -> p h t f", h=2, t=2), cosT, cosT[:], sinT, sinT[:],
                           rot, rot[:], tA, tB, 2)
                    S.op('dve', lambda e: e.tensor_tensor(out=ks[:, 2 * nb:2 * nb + 2, :], in0=rot[:].rearrange("p h t f -> p h (t f)"),
                                                          in1=pkt[:, j, 2 * nb:2 * nb + 2].unsqueeze(2).to_broadcast([128, 2, 256]), op=ALU.mult),
                         reads=[rot, pkt], writes=[ks])
                for nb in range(4):
                    pb = pvb[nb % 2]
                    for k in range(KC):
                        S.op('pe', lambda e: e.matmul(pb[:], lhsT=hT[:, k, :], rhs=Wv[k // 4][:, k % 4, nb * 512:(nb + 1) * 512],
                                                      start=(k == 0), stop=(k == KC - 1)), reads=[hT, Wv[k // 4]], writes=[pb])
                    S.op('act', lambda e: e.copy(out=vb[:, nb * 512:(nb + 1) * 512], in_=pb[:]), reads=[pb], writes=[vb])
                for h in range(8):
                    pb = pst[h % 2]
                    for half in range(2):
                        S.op('pe', lambda e: e.matmul(pb[:, half * 256:(half + 1) * 256], lhsT=ks[:, h, half * 128:(half + 1) * 128],
                                                      rhs=vb[:, h * 256:(h + 1) * 256], start=True, stop=True), reads=[ks, vb], writes=[pb])
                    S.op('dve', lambda e: e.tensor_tensor(out=state_f[:, h, :], in0=state_f[:, h, :], in1=pb[:], op=ALU.add),
                         reads=[state_f, pb], writes=[state_f])
            S.barrier()
        if stop_after == 'prefix':
            o = dout("d_state", [128, 8, 512])
            st(T(o, 'o'), o[:, :, :], state_f, state_f[:], is_out=True)
            S.finish()
            return nc, list(dbgo)
        hT_all = S.sb([128, KC, 1152], BF16, 'hT_all', mx)
        retT = S.sb([128, KC, 1024], BF16, 'retT', mx)
        attnT = S.sb([128, KC, 1024], BF16, 'attnT', mx)
        cos_o = S.sb([128, NOWN, 128], F32, 'cos_o', mx); sin_o = S.sb([128, NOWN, 128], F32, 'sin_o', mx)
        with ExitStack() as ph:
            A1b = S.sb([128, D], F32, 'A1b', ph); ld(A1b, A1b[:], modd[1:2, :].partition_broadcast(128), reads=[modd])
            sh1b = S.sb([128, D], F32, 'sh1b', ph); ld(sh1b, sh1b[:], modd[0:1, :].partition_broadcast(128), reads=[modd])
            posi = S.sb([128, NOWN], I32, 'posio', ph); ld(posi, posi[:], pos_o[:, :])
            posf = S.sb([128, NOWN], F32, 'posfo', ph)
            S.op('dve', lambda e: e.tensor_copy(out=posf[:], in_=posi[:]), reads=[posi], writes=[posf])
            xc = [S.sb([128, D], F32, 'xc%d' % i, ph) for i in range(2)]
            hb = [S.sb([128, D], BF16, 'hb%d' % i, ph) for i in range(2)]
            pTs = [S.ps([128, 512], F32, 'pT%d' % i, ph) for i in range(2)]
            for ci in range(9):
                src = xp[(NPRE - 1) * 128:NPRE * 128, :] if ci == 0 else xo[(ci - 1) * 128:ci * 128, :]
                xcur = xc[ci % 2]; hcur = hb[ci % 2]
                ld(xcur, xcur[:], src)
                layer_norm_mod(xcur, hcur, A1b, sh1b)
                make_hT(hcur, hT_all, lambda half: hT_all[:, half * 8:(half + 1) * 8, ci * 128:(ci + 1) * 128], pTs)
            for n in range(NOWN):
                rope_tables(posf, posf[:, n:n + 1], cos_o, cos_o[:, n, :], sin_o, sin_o[:, n, :])
            S.barrier()

        with ExitStack() as ph:
            Wh = [S.sb([128, 4, 1024], BF16, 'Wh%d' % i, ph) for i in range(4)]
            decT = S.sb([128, 8, 128], F32, 'decT', ph); ld(decT, decT[:], decTd[:, :, :])
            dq = S.sb([128, 8], F32, 'dq', ph); ld(dq, dq[:], dqd[:, :])
            dk = S.sb([128, 8], F32, 'dk', ph); ld(dk, dk[:], dkd[:, :])
            rgb = S.sb([128, D], F32, 'rgb', ph); ld(rgb, rgb[:], rgain[0:1, :].partition_broadcast(128))
            state_b = S.sb([128, 8, 512], BF16, 'state_b', ph)
            S.op('act', lambda e: e.copy(out=state_b[:], in_=state_f[:]), reads=[state_f], writes=[state_b])
            rotq = S.sb([128, 2, 2, 128], F32, 'rotq', ph)
            tA = S.sb([128, 2, 128], F32, 'tA', ph); tB = S.sb([128, 2, 128], F32, 'tB', ph)
            qkb = S.sb([128, 3, 256], BF16, 'qkb', ph)
            kd = S.sb([128, 256], BF16, 'kd', ph)
            vbh = S.sb([128, 256], BF16, 'vbh', ph)
            gs = S.sb([128, 256], F32, 'gs', ph)
            qkT = S.sb([128, 6, 128], BF16, 'qkT', ph)
            iT = S.sb([128, 128], BF16, 'iT', ph)
            junk = S.sb([128, 256], F32, 'junk', ph)
            rtmp = S.sb([128, 256], F32, 'rtmp', ph)
            reth = S.sb([128, 256], BF16, 'reth', ph)
            ss2 = S.sb([128, 1], F32, 'ss2', ph); r2 = S.sb([128, 1], F32, 'r2', ph)
            pp = [S.ps([128, 512], F32, 'pp%d' % i, ph) for i in range(4)]
            pTq = S.ps([128, 512], F32, 'pTq', ph); pTqv = pTq[:, :].bitcast(BF16)
            pi_ = S.ps([128, 512], F32, 'pi', ph); po_ = S.ps([128, 512], F32, 'po', ph); pds = S.ps([128, 512], F32, 'pds', ph)
            it = 0
            for h in range(8 if not os.environ.get('SKIP_RET') else 0):
                offs = [OFF_QR + h * 256, OFF_KR + h * 256, OFF_VR + h * 256, OFF_GR + h * 256]
                for sl in range(4):
                    for wi, off in enumerate(offs):
                        S.dma('pool', lambda e: e.dma_start(out=Wh[sl][:, :, wi * 256:(wi + 1) * 256],
                                                            in_=w_in[sl * 512:(sl + 1) * 512, off:off + 256].rearrange("(k p) n -> p k n", p=128)),
                              writes=[Wh[sl]])
                for n in range(NOWN):
                    tok = slice((n + 1) * 128, (n + 2) * 128)
                    pb0, pb1 = pp[2 * (it % 2)], pp[2 * (it % 2) + 1]
                    for blk, pb in ((0, pb0), (1, pb1)):
                        for k in range(KC):
                            S.op('pe', lambda e: e.matmul(pb[:], lhsT=hT_all[:, k, tok], rhs=Wh[k // 4][:, k % 4, blk * 512:(blk + 1) * 512],
                                                          start=(k == 0), stop=(k == KC - 1)), reads=[hT_all, Wh[k // 4]], writes=[pb])
                    rotary(pb0, pb0[:, :].rearrange("p (h t f) -> p h t f", h=2, t=2), cos_o, cos_o[:, n, :], sin_o, sin_o[:, n, :],
                           rotq, rotq[:], tA, tB, 2)
                    rq = rotq[:, 0, :, :].rearrange("p t f -> p (t f)"); rk = rotq[:, 1, :, :].rearrange("p t f -> p (t f)")
                    S.op('act', lambda e: e.copy(out=qkb[:, 0, :], in_=rq), reads=[rotq], writes=[qkb])
                    S.op('dve', lambda e: e.tensor_scalar(out=qkb[:, 1, :], in0=rq, scalar1=dq[:, h:h + 1], scalar2=None, op0=ALU.mult),
                         reads=[rotq, dq], writes=[qkb])
                    S.op('act', lambda e: e.mul(out=qkb[:, 2, :], in_=rk, mul=1.0 / 16.0), reads=[rotq], writes=[qkb])
                    S.op('dve', lambda e: e.tensor_scalar(out=kd[:], in0=rk, scalar1=dk[:, h:h + 1], scalar2=None, op0=ALU.mult),
                         reads=[rotq, dk], writes=[kd])
                    S.op('act', lambda e: e.copy(out=vbh[:], in_=pb1[:, 0:256]), reads=[pb1], writes=[vbh])
                    S.op('act', lambda e: e.activation(out=gs[:], in_=pb1[:, 256:512], func=AF.Silu), reads=[pb1], writes=[gs])
                    for a in range(3):
                        for half in range(2):
                            transpose_bf(pTq, pTqv[:, (a * 2 + half) * 128:(a * 2 + half + 1) * 128], qkb, qkb[:, a, half * 128:(half + 1) * 128])
                    S.op('act', lambda e: e.copy(out=qkT[:], in_=pTqv[:, 0:768].rearrange("p (a b) -> p a b", a=6)), reads=[pTq], writes=[qkT])
                    for half in range(2):
                        S.op('pe', lambda e: e.matmul(pi_[:, 0:128], lhsT=qkT[:, 4 + half, :], rhs=qkT[:, half, :], start=(half == 0), stop=(half == 1)),
                             reads=[qkT], writes=[pi_])
                    S.op('dve', lambda e: e.tensor_tensor(out=iT[:], in0=pi_[:, 0:128], in1=decT[:, h, :], op=ALU.mult), reads=[pi_, decT], writes=[iT])
                    S.op('pe', lambda e: e.matmul(po_[:, 0:256], lhsT=iT[:], rhs=vbh[:], start=True, stop=False), reads=[iT, vbh], writes=[po_])
                    for half in range(2):
                        S.op('pe', lambda e: e.matmul(po_[:, 0:256], lhsT=qkT[:, 2 + half, :], rhs=state_b[:, h, half * 256:(half + 1) * 256],
                                                      start=False, stop=(half == 1)), reads=[qkT, state_b], writes=[po_])
                    rms_rstd(po_, po_[:, 0:256], junk, ss2, r2, n=256)
                    S.op('dve', lambda e: e.scalar_tensor_tensor(out=rtmp[:], in0=po_[:, 0:256], scalar=r2[:, 0:1], in1=rgb[:, h * 256:(h + 1) * 256],
                                                                 op0=ALU.mult, op1=ALU.mult), reads=[po_, r2, rgb], writes=[rtmp])
                    S.op('dve', lambda e: e.tensor_tensor(out=reth[:], in0=rtmp[:], in1=gs[:], op=ALU.mult), reads=[rtmp, gs], writes=[reth])
                    for half in range(2):
                        transpose_bf(pTq, pTqv[:, 768 + half * 128:768 + (half + 1) * 128], reth, reth[:, half * 128:(half + 1) * 128])
                    S.op('act', lambda e: e.copy(out=retT[:, 2 * h:2 * h + 2, n * 128:(n + 1) * 128],
                                                 in_=pTqv[:, 768:1024].rearrange("p (a b) -> p a b", a=2)), reads=[pTq], writes=[retT])
                    for half in range(2):
                        S.op('pe', lambda e: e.matmul(pds[:, half * 256:(half + 1) * 256], lhsT=kd[:, half * 128:(half + 1) * 128], rhs=vbh[:],
                                                      start=True, stop=True), reads=[kd, vbh], writes=[pds])
                    S.op('dve', lambda e: e.scalar_tensor_tensor(out=state_f[:, h, :], in0=state_f[:, h, :], scalar=float(GAM[h] ** 128), in1=pds[:],
                                                                 op0=ALU.mult, op1=ALU.add), reads=[state_f, pds], writes=[state_f])
                    S.op('act', lambda e: e.copy(out=state_b[:, h, :], in_=state_f[:, h, :]), reads=[state_f], writes=[state_b])
                    it += 1
            S.barrier()
        if stop_after == 'ret':
            o = dout("d_retT", [128, KC, 1024], BF16)
            st(T(o, 'o'), o[:, :, :], retT, retT[:], is_out=True)
            S.finish()
            return nc, list(dbgo)
        with ExitStack() as ph:
            Wg = [S.sb([128, 4, 640], BF16, 'Wg%d' % i, ph) for i in range(4)]
            maskt = S.sb([128, 256], F32, 'maskt', ph); ld(maskt, maskt[:], maskd[:, :])
            mask0t = S.sb([128, 256], F32, 'mask0t', ph); ld(mask0t, mask0t[:], mask0d[:, :])
            sinkb = S.sb([128, 32], F32, 'sinkb', ph); ld(sinkb, sinkb[:], sinks[0:1, :].partition_broadcast(128))
            kT_all = S.sb([128, 2, 1152], BF16, 'kT_all', ph)
            v_all = S.sb([128, 9, 64], BF16, 'v_all', ph)
            qT = S.sb([128, 4, 1024], BF16, 'qT', ph)
            qb = S.sb([128, 512], BF16, 'qb', ph); k2 = S.sb([128, 2, 128], BF16, 'k2', ph)
            S.op('dve', lambda e: e.memset(k2[:], 0.0), writes=[k2])
            S_sb = S.sb([128, 8, 256], F32, 'S_sb', ph)
            P_bf = S.sb([128, 8, 256], BF16, 'P_bf', ph)
            PT = S.sb([128, 8, 2, 128], BF16, 'PT', ph)
            mx8 = S.sb([128, 8], F32, 'mx8', ph); negm = S.sb([128, 8], F32, 'negm', ph); rs = S.sb([128, 8], F32, 'rs', ph)
            es8 = S.sb([128, 8], F32, 'es8', ph); rden = S.sb([128, 8], F32, 'rden', ph)
            at_tok = S.sb([128, 8, 64], BF16, 'at_tok', ph)
            pq0 = S.ps([128, 512], F32, 'pq0', ph); pq1 = S.ps([128, 512], F32, 'pq1', ph)
            pTa = S.ps([128, 512], F32, 'pTa', ph); pTav = pTa[:, :].bitcast(BF16)
            psc = [S.ps([128, 512], F32, 'psc%d' % i, ph) for i in range(2)]
            pPT = S.ps([128, 512], F32, 'pPT', ph); pPTv = pPT[:, :].bitcast(BF16)
            ppv = S.ps([128, 512], F32, 'ppv', ph)
            for g in range(4):
                offs = [(OFF_QA + g * 512, 512, 0), (OFF_KA + g * 64, 64, 512), (OFF_VA + g * 64, 64, 576)]
                for sl in range(4):
                    for off, wd, dst in offs:
                        S.dma('pool', lambda e: e.dma_start(out=Wg[sl][:, :, dst:dst + wd],
                                                            in_=w_in[sl * 512:(sl + 1) * 512, off:off + wd].rearrange("(k p) n -> p k n", p=128)),
                              writes=[Wg[sl]])
                for ci in range(9):
                    tok = slice(ci * 128, (ci + 1) * 128)
                    if ci >= 1:
                        for k in range(KC):
                            S.op('pe', lambda e: e.matmul(pq0[:], lhsT=hT_all[:, k, tok], rhs=Wg[k // 4][:, k % 4, 0:512], start=(k == 0), stop=(k == KC - 1)),
                                 reads=[hT_all, Wg[k // 4]], writes=[pq0])
                        S.op('act', lambda e: e.mul(out=qb[:], in_=pq0[:], mul=0.125), reads=[pq0], writes=[qb])
                    for k in range(KC):
                        S.op('pe', lambda e: e.matmul(pq1[:, 0:128], lhsT=hT_all[:, k, tok], rhs=Wg[k // 4][:, k % 4, 512:640], start=(k == 0), stop=(k == KC - 1)),
                             reads=[hT_all, Wg[k // 4]], writes=[pq1])
                    S.op('dve', lambda e: e.tensor_copy(out=k2[:, 0, 0:64], in_=pq1[:, 0:64]), reads=[pq1], writes=[k2])
                    S.op('dve', lambda e: e.tensor_copy(out=k2[:, 1, 64:128], in_=pq1[:, 0:64]), reads=[pq1], writes=[k2])
                    S.op('dve', lambda e: e.tensor_copy(out=v_all[:, ci, :], in_=pq1[:, 64:128]), reads=[pq1], writes=[v_all])
                    if ci >= 1:
                        for i in range(4):
                            transpose_bf(pTa, pTav[:, i * 128:(i + 1) * 128], qb, qb[:, i * 128:(i + 1) * 128])
                    transpose_bf(pTa, pTav[:, 512:640], k2, k2[:, 0, :])
                    transpose_bf(pTa, pTav[:, 640:768], k2, k2[:, 1, :])
                    if ci >= 1:
                        S.op('act', lambda e: e.copy(out=qT[:, :, (ci - 1) * 128:ci * 128], in_=pTav[:, 0:512].rearrange("p (a b) -> p a b", a=4)),
                             reads=[pTa], writes=[qT])
                    S.op('act', lambda e: e.copy(out=kT_all[:, :, tok], in_=pTav[:, 512:768].rearrange("p (a b) -> p a b", a=2)), reads=[pTa], writes=[kT_all])
                import os
                ATS = int(os.environ.get('ATS', '9'))
                for n in range(NOWN if ATS >= 2 else 0):
                    mk = mask0t if n == 0 else maskt
                    for hh in range(2):
                        for j4 in range(4):
                            j = hh * 4 + j4; i = j // 2; r0 = (j % 2) * 64
                            pb = psc[j4 // 2]
                            S.op('pe', lambda e: e.matmul(pb[:, (j4 % 2) * 256:(j4 % 2 + 1) * 256], lhsT=qT[:, i, n * 128:(n + 1) * 128],
                                                          rhs=kT_all[:, j % 2, n * 128:n * 128 + 256], start=True, stop=True),
                                 reads=[qT, kT_all], writes=[pb])
                        for bnk in range(2):
                            S.op('dve', lambda e: e.tensor_tensor(out=S_sb[:, hh * 4 + bnk * 2:hh * 4 + bnk * 2 + 2, :],
                                                                  in0=psc[bnk][:, :].rearrange("p (a b) -> p a b", a=2),
                                                                  in1=mk[:, :].unsqueeze(1).to_broadcast([128, 2, 256]), op=ALU.add),
                                 reads=[psc[bnk], mk], writes=[S_sb])
                    ATQ = int(os.environ.get('ATQ', '9'))
                    if ATQ < 2:
                        continue
                    S.op('dve', lambda e: e.tensor_reduce(out=mx8[:], in_=S_sb[:], axis=AX.X, op=ALU.max), reads=[S_sb], writes=[mx8])
                    S.op('dve', lambda e: e.tensor_tensor(out=mx8[:], in0=mx8[:], in1=sinkb[:, g * 8:(g + 1) * 8], op=ALU.max), reads=[mx8, sinkb], writes=[mx8])
                    S.op('dve', lambda e: e.tensor_scalar(out=negm[:], in0=mx8[:], scalar1=-1.0, scalar2=None, op0=ALU.mult), reads=[mx8], writes=[negm])
                    if ATQ < 3:
                        continue
                    for j in range(8):
                        S.op('act', lambda e: e.activation(out=P_bf[:, j, :], in_=S_sb[:, j, :], func=AF.Exp, bias=negm[:, j:j + 1], scale=1.0,
                                                           accum_out=rs[:, j:j + 1]), reads=[S_sb, negm], writes=[P_bf, rs])
                    if ATQ < 4:
                        continue
                    S.op('dve', lambda e: e.tensor_tensor(out=es8[:], in0=sinkb[:, g * 8:(g + 1) * 8], in1=mx8[:], op=ALU.subtract), reads=[sinkb, mx8], writes=[es8])
                    S.op('act', lambda e: e.activation(out=es8[:], in_=es8[:], func=AF.Exp), reads=[es8], writes=[es8])
                    S.op('dve', lambda e: e.tensor_tensor(out=es8[:], in0=es8[:], in1=rs[:], op=ALU.add), reads=[es8, rs], writes=[es8])
                    S.op('dve', lambda e: e.reciprocal(out=rden[:], in_=es8[:]), reads=[es8], writes=[rden])
                    if ATS < 3:
                        continue
                    for hh in range(2):
                        for j4 in range(4):
                            for t in range(2):
                                transpose_bf(pPT, pPTv[:, (j4 * 2 + t) * 128:(j4 * 2 + t + 1) * 128], P_bf, P_bf[:, hh * 4 + j4, t * 128:(t + 1) * 128])
                        S.op('act', lambda e: e.copy(out=PT[:, hh * 4:(hh + 1) * 4, :, :], in_=pPTv[:, 0:1024].rearrange("p (a t b) -> p a t b", a=4, t=2)),
                             reads=[pPT], writes=[PT])
                    for j in range(8):
                        for t in range(2):
                            S.op('pe', lambda e: e.matmul(ppv[:, j * 64:(j + 1) * 64], lhsT=PT[:, j, t, :], rhs=v_all[:, n + t, :], start=(t == 0), stop=(t == 1)),
                                 reads=[PT, v_all], writes=[ppv])
                    S.op('dve', lambda e: e.tensor_tensor(out=at_tok[:], in0=ppv[:, :].rearrange("p (a b) -> p a b", a=8),
                                                          in1=rden[:, :].unsqueeze(2).to_broadcast([128, 8, 64]), op=ALU.mult), reads=[ppv, rden], writes=[at_tok])
                    for i in range(4):
                        transpose_bf(pTa, pTav[:, i * 128:(i + 1) * 128], at_tok, at_tok[:].rearrange("p a b -> p (a b)")[:, i * 128:(i + 1) * 128])
                    S.op('act', lambda e: e.copy(out=attnT[:, g * 4:(g + 1) * 4, n * 128:(n + 1) * 128], in_=pTav[:, 0:512].rearrange("p (a b) -> p a b", a=4)),
                         reads=[pTa], writes=[attnT])
            S.barrier()
        if stop_after == 'attn':
            o = dout("d_attnT", [128, KC, 1024], BF16)
            st(T(o, 'o'), o[:, :, :], attnT, attnT[:], is_out=True)
            S.finish()
            return nc, list(dbgo)
        with ExitStack() as ph:
            mT = S.sb([128, KC, 512], BF16, 'mT', ph)
            Wm = [S.sb([128, KC, 128], BF16, 'Wm%d' % i, ph) for i in range(4)]
            Wo = [S.sb([128, 4, 512], BF16, 'Wo%d' % i, ph) for i in range(4)]
            g1b = S.sb([128, D], F32, 'g1b', ph); ld(g1b, g1b[:], modd[2:3, :].partition_broadcast(128), reads=[modd])
            sga = S.sb([128, 512], F32, 'sga', ph); sgr = S.sb([128, 512], F32, 'sgr', ph)
            rr = [S.sb([128, 512], F32, 'rr%d' % i, ph) for i in range(2)]
            xs = [S.sb([128, 512], F32, 'xs%d' % i, ph) for i in range(2)]
            pA = S.ps([128, 512], F32, 'pA', ph); pR = S.ps([128, 512], F32, 'pR', ph)
            pGa = S.ps([128, 512], F32, 'pGa', ph); pGr = S.ps([128, 512], F32, 'pGr', ph)
            po2 = [S.ps([128, 512], F32, 'po2%d' % i, ph) for i in range(2)]
            for th in range(2):
                tsl = slice(th * 512, (th + 1) * 512)
                hsl = slice(128 + th * 512, 128 + (th + 1) * 512)
                for cg in range(16):
                    for wi in range(4):
                        r0 = (wi * 16 + cg) * 128
                        S.dma('pool', lambda e: e.dma_start(out=Wm[wi][:].rearrange("p k n -> p (k n)"), in_=wm_t[r0:r0 + 128, :]), writes=[Wm[wi]])
                    for jj in range(1):
                        csl = slice(0, 128)
                        for pb, wt, act_T, asl in ((pA, Wm[0], attnT, tsl), (pR, Wm[1], retT, tsl), (pGa, Wm[2], hT_all, hsl), (pGr, Wm[3], hT_all, hsl)):
                            for k in range(KC):
                                S.op('pe', lambda e: e.matmul(pb[:], lhsT=wt[:, k, csl], rhs=act_T[:, k, asl], start=(k == 0), stop=(k == KC - 1)),
                                     reads=[wt, act_T], writes=[pb])
                        S.op('act', lambda e: e.activation(out=sga[:], in_=pGa[:], func=AF.Sigmoid), reads=[pGa], writes=[sga])
                        S.op('act', lambda e: e.activation(out=sgr[:], in_=pGr[:], func=AF.Sigmoid), reads=[pGr], writes=[sgr])
                        S.op('dve', lambda e: e.tensor_tensor(out=sga[:], in0=sga[:], in1=pA[:], op=ALU.mult), reads=[sga, pA], writes=[sga])
                        S.op('dve', lambda e: e.tensor_tensor(out=sgr[:], in0=sgr[:], in1=pR[:], op=ALU.mult), reads=[sgr, pR], writes=[sgr])
                        S.op('dve', lambda e: e.tensor_tensor(out=mT[:, cg, :], in0=sga[:], in1=sgr[:], op=ALU.add), reads=[sga, sgr], writes=[mT])
                it = 0
                for nb in range(4):
                    for kq in range(4):
                        S.dma('pool', lambda e: e.dma_start(out=Wo[kq][:], in_=w_o[kq * 512:(kq + 1) * 512, nb * 512:(nb + 1) * 512].rearrange("(k p) n -> p k n", p=128)),
                              writes=[Wo[kq]])
                    for c4 in range(4):
                        n = th * 4 + c4
                        pb = po2[it % 2]; r_ = rr[it % 2]; x_ = xs[it % 2]
                        ld(x_, x_[:], xo[n * 128:(n + 1) * 128, nb * 512:(nb + 1) * 512])
                        for k in range(KC):
                            S.op('pe', lambda e: e.matmul(pb[:], lhsT=mT[:, k, c4 * 128:(c4 + 1) * 128], rhs=Wo[k // 4][:, k % 4, :], start=(k == 0), stop=(k == KC - 1)),
                                 reads=[mT, Wo[k // 4]], writes=[pb])
                        S.op('dve', lambda e: e.tensor_tensor(out=r_[:], in0=pb[:], in1=g1b[:, nb * 512:(nb + 1) * 512], op=ALU.mult), reads=[pb, g1b], writes=[r_])
                        S.op('dve', lambda e: e.tensor_tensor(out=r_[:], in0=r_[:], in1=x_[:], op=ALU.add), reads=[r_, x_], writes=[r_])
                        st(xmid, xmid[n * 128:(n + 1) * 128, nb * 512:(nb + 1) * 512], r_, r_[:])
                        it += 1
            S.barrier()
        mx.close()
        S.barrier()
        if stop_after == 'mix':
            o = dout("d_xmid", [1024, D])
            t = S.sb([128, NOWN, D], F32, 'dbgx')
            ld(t, t[:], xmid[:, :].rearrange("(n p) d -> p n d", p=128), reads=[xmid])
            st(T(o, 'o'), o[:, :].rearrange("(n p) d -> p n d", p=128), t, t[:], is_out=True)
            S.finish()
            return nc, list(dbgo)
        moe = es.enter_context(ExitStack())
        slot_i = S.sb([128, 2, NOWN], I32, 'slot_i', moe)
        idxg_i = S.sb([128, NOV, 16], I32, 'idxg_i', moe)
        idxd_i = S.sb([128, NOV, 8], I32, 'idxd_i', moe)
        ss = S.sb([128, 1], F32, 'ss_m', moe); rstd = S.sb([128, 1], F32, 'rstd_m', moe)
        with ExitStack() as ph:
            A2b = S.sb([128, D], F32, 'A2b', ph); ld(A2b, A2b[:], modd[4:5, :].partition_broadcast(128), reads=[modd])
            sh2b = S.sb([128, D], F32, 'sh2b', ph); ld(sh2b, sh2b[:], modd[3:4, :].partition_broadcast(128), reads=[modd])
            Wr_sb = S.sb([128, KC, 68], F32, 'Wr_sb', ph); ld(Wr_sb, Wr_sb[:], w_rt[:, :].rearrange("(k p) n -> p k n", p=128))
            brb = S.sb([128, 68], F32, 'brb', ph); ld(brb, brb[:], b_rt[0:1, :].partition_broadcast(128))
            zt = S.sb([1, D], BF16, 'zt', ph)
            S.op('dve', lambda e: e.memset(zt[:], 0.0), writes=[zt])
            st(H2, H2[1024:1025, :], zt, zt[:])
            xm = [S.sb([128, D], F32, 'xm%d' % i, ph) for i in range(2)]
            h2b = S.sb([128, D], BF16, 'h2b', ph)
            h2T = S.sb([128, KC, 128], F32, 'h2T', ph)
            lg = S.sb([128, 68], F32, 'lg', ph)
            sm = {k: S.sb([128, 1], F32, 'sm_' + k, ph) for k in ('gmax', 'negg', 'gsum', 'gw', 'm1', 'm2', 'd', 'p1')}
            ohg = S.sb([128, 4], F32, 'ohg', ph); gej = S.sb([128, 4], F32, 'gej', ph)
            t416 = S.sb([128, 4, 16], F32, 't416', ph)
            el = S.sb([128, 16], F32, 'el', ph); el2 = S.sb([128, 16], F32, 'el2', ph)
            oh1 = S.sb([128, 16], F32, 'oh1', ph); oh2 = S.sb([128, 16], F32, 'oh2', ph)
            OH = [S.sb([128, NOWN, 64], F32, 'OH%d' % i, ph) for i in range(2)]
            wts = S.sb([128, 2, NOWN], F32, 'wts', ph)
            Cb = S.sb([128, NOWN, 64], BF16, 'Cb', ph)
            pTf = [S.ps([128, 512], F32, 'pTf%d' % i, ph) for i in range(2)]
            plg = S.ps([128, 512], F32, 'plg', ph)
            pPC = S.ps([128, 512], F32, 'pPC', ph); pcnt = S.ps([128, 512], F32, 'pcnt', ph)
            for n in range(NOWN):
                x_ = xm[n % 2]
                ld(x_, x_[:], xmid[n * 128:(n + 1) * 128, :], reads=[xmid])
                rms_rstd(x_, x_[:], h2b, ss, rstd)
                S.op('dve', lambda e: e.scalar_tensor_tensor(out=x_[:], in0=x_[:], scalar=rstd[:, 0:1], in1=A2b[:], op0=ALU.mult, op1=ALU.mult),
                     reads=[x_, rstd, A2b], writes=[x_])
                S.op('dve', lambda e: e.tensor_tensor(out=x_[:], in0=x_[:], in1=sh2b[:], op=ALU.add), reads=[x_, sh2b], writes=[x_])
                S.op('act', lambda e: e.copy(out=h2b[:], in_=x_[:]), reads=[x_], writes=[h2b])
                st(H2, H2[n * 128:(n + 1) * 128, :], h2b, h2b[:])
                for grp in range(4):
                    pb = pTf[grp % 2]
                    for kk in range(4):
                        k = grp * 4 + kk
                        S.op('pe', lambda e: e.matmul(pb[:, kk * 128:(kk + 1) * 128], lhsT=x_[:, k * 128:(k + 1) * 128], rhs=idf[:], start=True, stop=True),
                             reads=[x_, idf], writes=[pb])
                    S.op('act', lambda e: e.copy(out=h2T[:, grp * 4:(grp + 1) * 4, :], in_=pb[:, :].rearrange("p (a b) -> p a b", a=4)), reads=[pb], writes=[h2T])
                for k in range(KC):
                    S.op('pe', lambda e: e.matmul(plg[:, 0:68], lhsT=h2T[:, k, :], rhs=Wr_sb[:, k, :], start=(k == 0), stop=(k == KC - 1)),
                         reads=[h2T, Wr_sb], writes=[plg])
                S.op('dve', lambda e: e.tensor_tensor(out=lg[:], in0=plg[:, 0:68], in1=brb[:], op=ALU.add), reads=[plg, brb], writes=[lg])
                S.op('dve', lambda e: e.tensor_reduce(out=sm['gmax'][:], in_=lg[:, 0:4], axis=AX.X, op=ALU.max), reads=[lg], writes=[sm['gmax']])
                S.op('dve', lambda e: e.tensor_scalar(out=ohg[:], in0=lg[:, 0:4], scalar1=sm['gmax'][:, 0:1], scalar2=None, op0=ALU.is_ge), reads=[lg, sm['gmax']], writes=[ohg])
                S.op('dve', lambda e: e.tensor_scalar(out=sm['negg'][:], in0=sm['gmax'][:], scalar1=-1.0, scalar2=None, op0=ALU.mult), reads=[sm['gmax']], writes=[sm['negg']])
                S.op('act', lambda e: e.activation(out=gej[:], in_=lg[:, 0:4], func=AF.Exp, bias=sm['negg'][:, 0:1], scale=1.0, accum_out=sm['gsum'][:]),
                     reads=[lg, sm['negg']], writes=[gej, sm['gsum']])
                S.op('dve', lambda e: e.reciprocal(out=sm['gw'][:], in_=sm['gsum'][:]), reads=[sm['gsum']], writes=[sm['gw']])
                S.op('dve', lambda e: e.tensor_tensor(out=t416[:], in0=lg[:, 4:68].rearrange("p (g e) -> p g e", g=4),
                                                      in1=ohg[:, :].unsqueeze(2).to_broadcast([128, 4, 16]), op=ALU.mult), reads=[lg, ohg], writes=[t416])
                S.op('dve', lambda e: e.tensor_reduce(out=el[:], in_=t416[:].rearrange("p g e -> p e g"), axis=AX.X, op=ALU.add), reads=[t416], writes=[el])
                S.op('dve', lambda e: e.tensor_reduce(out=sm['m1'][:], in_=el[:], axis=AX.X, op=ALU.max), reads=[el], writes=[sm['m1']])
                S.op('dve', lambda e: e.tensor_scalar(out=oh1[:], in0=el[:], scalar1=sm['m1'][:, 0:1], scalar2=None, op0=ALU.is_ge), reads=[el, sm['m1']], writes=[oh1])
                S.op('dve', lambda e: e.scalar_tensor_tensor(out=el2[:], in0=oh1[:], scalar=-1e30, in1=el[:], op0=ALU.mult, op1=ALU.add), reads=[oh1, el], writes=[el2])
                S.op('dve', lambda e: e.tensor_reduce(out=sm['m2'][:], in_=el2[:], axis=AX.X, op=ALU.max), reads=[el2], writes=[sm['m2']])
                S.op('dve', lambda e: e.tensor_scalar(out=oh2[:], in0=el2[:], scalar1=sm['m2'][:, 0:1], scalar2=None, op0=ALU.is_ge), reads=[el2, sm['m2']], writes=[oh2])
                S.op('dve', lambda e: e.tensor_tensor(out=sm['d'][:], in0=sm['m2'][:], in1=sm['m1'][:], op=ALU.subtract), reads=[sm['m1'], sm['m2']], writes=[sm['d']])
                S.op('act', lambda e: e.activation(out=sm['d'][:], in_=sm['d'][:], func=AF.Exp), reads=[sm['d']], writes=[sm['d']])
                S.op('dve', lambda e: e.tensor_scalar(out=sm['d'][:], in0=sm['d'][:], scalar1=1.0, scalar2=None, op0=ALU.add), reads=[sm['d']], writes=[sm['d']])
                S.op('dve', lambda e: e.reciprocal(out=sm['p1'][:], in_=sm['d'][:]), reads=[sm['d']], writes=[sm['p1']])
                S.op('dve', lambda e: e.tensor_tensor(out=wts[:, 0, n:n + 1], in0=sm['p1'][:], in1=sm['gw'][:], op=ALU.mult), reads=[sm['p1'], sm['gw']], writes=[wts])
                S.op('dve', lambda e: e.tensor_tensor(out=wts[:, 1, n:n + 1], in0=sm['gw'][:], in1=wts[:, 0, n:n + 1], op=ALU.subtract), reads=[sm['gw'], wts], writes=[wts])
                for kk, oh in ((0, oh1), (1, oh2)):
                    S.op('dve', lambda e: e.tensor_tensor(out=OH[kk][:, n, :].rearrange("p (g e) -> p g e", g=4),
                                                          in0=ohg[:, :].unsqueeze(2).to_broadcast([128, 4, 16]),
                                                          in1=oh[:, :].unsqueeze(1).to_broadcast([128, 4, 16]), op=ALU.mult), reads=[ohg, oh], writes=[OH[kk]])
                S.op('dve', lambda e: e.tensor_tensor(out=Cb[:, n, :], in0=OH[0][:, n, :], in1=OH[1][:, n, :], op=ALU.add), reads=[OH[0], OH[1]], writes=[Cb])
            ones_bf = S.sb([128, 128], BF16, 'ones_bf', ph)
            S.op('dve', lambda e: e.memset(ones_bf[:], 1.0), writes=[ones_bf])
            Lf = S.sb([128, 128], F32, 'Lf', ph); ld(Lf, Lf[:], Lstd[:, :])
            Lb = S.sb([128, 128], BF16, 'Lb', ph)
            S.op('dve', lambda e: e.tensor_copy(out=Lb[:], in_=Lf[:]), reads=[Lf], writes=[Lb])
            for n in range(NOWN):
                for m in range(n + 1):
                    S.op('pe', lambda e: e.matmul(pPC[:, n * 64:(n + 1) * 64], lhsT=(Lb[:] if m == n else ones_bf[:]), rhs=Cb[:, m, :], start=(m == 0), stop=(m == n)),
                         reads=[Lb, ones_bf, Cb], writes=[pPC])
            for m in range(NOWN):
                S.op('pe', lambda e: e.matmul(pcnt[:, 0:64], lhsT=ones_bf[:], rhs=Cb[:, m, :], start=(m == 0), stop=(m == NOWN - 1)), reads=[ones_bf, Cb], writes=[pcnt])
            CAP = int(os.environ.get('OVCAP', 128))
            e128 = S.sb([128, 64], F32, 'e128', ph); ld(e128, e128[:], e128d[:, :])
            ocf = S.sb([128, 64], F32, 'ocf', ph); cnti = S.sb([128, 64], I32, 'cnti', ph); padf = S.sb([128, 64], F32, 'padf', ph)
            S.op('dve', lambda e: e.tensor_scalar(out=ocf[:], in0=pcnt[:, 0:64], scalar1=-float(CAP), scalar2=0.0, op0=ALU.add, op1=ALU.max), reads=[pcnt], writes=[ocf])
            S.op('dve', lambda e: e.tensor_scalar(out=ocf[:], in0=ocf[:], scalar1=127.0, scalar2=None, op0=ALU.add), reads=[ocf], writes=[ocf])
            S.op('dve', lambda e: e.tensor_copy(out=cnti[:], in_=ocf[:]), reads=[ocf], writes=[cnti])
            S.op('dve', lambda e: e.tensor_scalar(out=cnti[:], in0=cnti[:], scalar1=7, scalar2=7, op0=ALU.arith_shift_right, op1=ALU.logical_shift_left), reads=[cnti], writes=[cnti])
            S.op('dve', lambda e: e.tensor_copy(out=padf[:], in_=cnti[:]), reads=[cnti], writes=[padf])
            cs = [S.sb([128, 64], F32, 'cs%d' % i, ph) for i in range(2)]
            S.op('dve', lambda e: e.tensor_copy(out=cs[0][:], in_=padf[:]), reads=[padf], writes=[cs[0]])
            cur = 0
            for s_ in (1, 2, 4, 8, 16, 32):
                a, b_ = cs[cur], cs[1 - cur]
                S.op('dve', lambda e: e.tensor_copy(out=b_[:, 0:s_], in_=a[:, 0:s_]), reads=[a], writes=[b_])
                S.op('dve', lambda e: e.tensor_tensor(out=b_[:, s_:64], in0=a[:, s_:64], in1=a[:, 0:64 - s_], op=ALU.add), reads=[a], writes=[b_])
                cur = 1 - cur
            pend = cs[cur]; ob = cs[1 - cur]
            S.op('dve', lambda e: e.tensor_tensor(out=ob[:], in0=pend[:], in1=padf[:], op=ALU.subtract), reads=[pend, padf], writes=[ob])
            S.op('dve', lambda e: e.tensor_scalar(out=ob[:], in0=ob[:], scalar1=float(64 * 128 - CAP), scalar2=None, op0=ALU.add), reads=[ob], writes=[ob])
            slot_f = S.sb([128, 2, NOWN], F32, 'slot_f', ph)
            tmpb = S.sb([128, NOWN, 64], F32, 'tmpb', ph)
            rk = S.sb([128, NOWN], F32, 'rk', ph); eb = S.sb([128, NOWN], F32, 'eb', ph); obk = S.sb([128, NOWN], F32, 'obk', ph); isov = S.sb([128, NOWN], F32, 'isov', ph)
            for kk in range(2):
                S.op('dve', lambda e: e.tensor_tensor(out=tmpb[:], in0=OH[kk][:], in1=pPC[:, :].rearrange("p (n e) -> p n e", n=NOWN), op=ALU.mult), reads=[OH[kk], pPC], writes=[tmpb])
                S.op('dve', lambda e: e.tensor_reduce(out=rk[:], in_=tmpb[:], axis=AX.X, op=ALU.add), reads=[tmpb], writes=[rk])
                S.op('dve', lambda e: e.tensor_tensor(out=tmpb[:], in0=OH[kk][:], in1=e128[:, :].unsqueeze(1).to_broadcast([128, NOWN, 64]), op=ALU.mult), reads=[OH[kk], e128], writes=[tmpb])
                S.op('dve', lambda e: e.tensor_reduce(out=eb[:], in_=tmpb[:], axis=AX.X, op=ALU.add), reads=[tmpb], writes=[eb])
                S.op('dve', lambda e: e.tensor_tensor(out=tmpb[:], in0=OH[kk][:], in1=ob[:, :].unsqueeze(1).to_broadcast([128, NOWN, 64]), op=ALU.mult), reads=[OH[kk], ob], writes=[tmpb])
                S.op('dve', lambda e: e.tensor_reduce(out=obk[:], in_=tmpb[:], axis=AX.X, op=ALU.add), reads=[tmpb], writes=[obk])
                S.op('dve', lambda e: e.tensor_scalar(out=isov[:], in0=rk[:], scalar1=float(CAP), scalar2=None, op0=ALU.is_ge), reads=[rk], writes=[isov])
                S.op('dve', lambda e: e.tensor_tensor(out=obk[:], in0=obk[:], in1=eb[:], op=ALU.subtract), reads=[obk, eb], writes=[obk])
                S.op('dve', lambda e: e.tensor_tensor(out=obk[:], in0=obk[:], in1=isov[:], op=ALU.mult), reads=[obk, isov], writes=[obk])
                S.op('dve', lambda e: e.tensor_tensor(out=rk[:], in0=rk[:], in1=eb[:], op=ALU.add), reads=[rk, eb], writes=[rk])
                S.op('dve', lambda e: e.tensor_tensor(out=slot_f[:, kk, :], in0=rk[:], in1=obk[:], op=ALU.add), reads=[rk, obk], writes=[slot_f])
            S.op('dve', lambda e: e.tensor_copy(out=slot_i[:], in_=slot_f[:]), reads=[slot_f], writes=[slot_i])
            blk128 = S.sb([128, NOV], F32, 'blk128', ph); ld(blk128, blk128[:], blk128d[:, 0:NOV])
            kcoff = S.sb([128, 16], F32, 'kcoff', ph); ld(kcoff, kcoff[:], kcoffd[:, :])
            pidx = S.sb([128, 1], F32, 'pidx', ph); ld(pidx, pidx[:], pidxd[:, :])
            cmp = S.sb([128, NOV, 64], BF16, 'cmp', ph)
            S.op('dve', lambda e: e.tensor_tensor(out=cmp[:], in0=pend[:, :].unsqueeze(1).to_broadcast([128, NOV, 64]),
                                                  in1=blk128[:, :].unsqueeze(2).to_broadcast([128, NOV, 64]), op=ALU.is_le), reads=[pend, blk128], writes=[cmp])
            bef = S.sb([128, NOV], F32, 'bef', ph); gb = S.sb([128, NOV], F32, 'gb', ph)
            S.op('dve', lambda e: e.tensor_reduce(out=bef[:], in_=cmp[:], axis=AX.X, op=ALU.add), reads=[cmp], writes=[bef])
            skipo = S.sb([128, NOV], F32, 'skipo', ph)
            S.op('dve', lambda e: e.tensor_scalar(out=skipo[:], in0=bef[:], scalar1=64.0, scalar2=float(2 ** 27), op0=ALU.is_ge, op1=ALU.mult), reads=[bef], writes=[skipo])
            S.op('dve', lambda e: e.tensor_scalar(out=bef[:], in0=bef[:], scalar1=63.0, scalar2=None, op0=ALU.min), reads=[bef], writes=[bef])
            idxf = S.sb([128, NOV, 16], F32, 'idxf', ph)
            for mult, nk, dst in ((2048.0, 16, idxg_i), (1024.0, 8, idxd_i)):
                S.op('dve', lambda e: e.tensor_scalar(out=gb[:], in0=bef[:], scalar1=mult, scalar2=pidx[:, 0:1], op0=ALU.mult, op1=ALU.add), reads=[bef, pidx], writes=[gb])
                S.op('dve', lambda e: e.tensor_tensor(out=gb[:], in0=gb[:], in1=skipo[:], op=ALU.add), reads=[gb, skipo], writes=[gb])
                S.op('dve', lambda e: e.tensor_tensor(out=idxf[:, :, 0:nk], in0=gb[:, :].unsqueeze(2).to_broadcast([128, NOV, nk]),
                                                      in1=kcoff[:, 0:nk].unsqueeze(1).to_broadcast([128, NOV, nk]), op=ALU.add), reads=[gb, kcoff], writes=[idxf])
                S.op('dve', lambda e: e.tensor_copy(out=dst[:], in_=idxf[:, :, 0:nk]), reads=[idxf], writes=[dst])
            ri0 = S.sb([128, NBLK, 16], I32, 'ri0', ph)
            S.op('dve', lambda e: e.memset(ri0[:], 0), writes=[ri0])
            S.op('dve', lambda e: e.memset(ri0[:, :, 0:1], 1024), writes=[ri0])
            st(rinfo, rinfo[:, :].rearrange("(b p) c -> p b c", p=128), ri0, ri0[:])
            tokid = S.sb([128, NOWN], I32, 'tokid', ph); ld(tokid, tokid[:], tokidd[:, :])
            ris = [S.sb([128, 16], I32, 'ri%d' % i, ph) for i in range(4)]
            for r_ in ris:
                S.op('dve', lambda e: e.memset(r_[:], 0), writes=[r_])
            it = 0
            for n in range(NOWN):
                for kk in range(2):
                    r_ = ris[it % 4]
                    S.op('dve', lambda e: e.tensor_copy(out=r_[:, 0:1], in_=tokid[:, n:n + 1]), reads=[tokid], writes=[r_])
                    S.op('dve', lambda e: e.tensor_copy(out=r_[:, 1:2].bitcast(F32), in_=wts[:, kk, n:n + 1]), reads=[wts], writes=[r_])
                    S.dma('pool', lambda e: e.indirect_dma_start(out=rinfo[:, :], out_offset=bass.IndirectOffsetOnAxis(ap=slot_i[:, kk, n:n + 1], axis=0),
                                                                 in_=r_[:], in_offset=None), reads=[r_, slot_i], writes=[rinfo])
                    it += 1
            S.barrier()
        if stop_after == 'moe_route':
            o1 = dout("d_slot", [128, 2, NOWN], I32); st(T(o1, 'o'), o1[:, :, :], slot_i, slot_i[:], is_out=True)
            o2 = dout("d_idxg", [128, NOV, 16], I32); st(T(o2, 'o'), o2[:, :, :], idxg_i, idxg_i[:], is_out=True)
            o3 = dout("d_rinfo", [NBLK * 128, 16], I32)
            t = S.sb([128, NBLK, 16], I32, 'dbgr')
            ld(t, t[:], rinfo[:, :].rearrange("(b p) c -> p b c", p=128), reads=[rinfo])
            st(T(o3, 'o'), o3[:, :].rearrange("(b p) c -> p b c", p=128), t, t[:], is_out=True)
            S.finish()
            return nc, list(dbgo)
        with ExitStack() as ph:
            Wg_t = [S.sb([128, 1024], BF16, 'Wg_t%d' % i, ph) for i in range(16)]
            Wu_t = [S.sb([128, 1024], BF16, 'Wu_t%d' % i, ph) for i in range(16)]
            Wd_t = [S.sb([128, D], BF16, 'Wd_t%d' % i, ph) for i in range(8)]
            rts = [S.sb([128, 16], I32, 'rt%d' % i, ph) for i in range(2)]
            xgs = [S.sb([128, D], BF16, 'xg%d' % i, ph) for i in range(2)]
            xgT = S.sb([128, KC, 128], BF16, 'xgT', ph)
            sg = S.sb([128, 1024], F32, 'sg', ph)
            actb = S.sb([128, 1024], BF16, 'actb', ph)
            actT = S.sb([128, 8, 128], BF16, 'actT', ph)
            ybs = [S.sb([128, D], F32, 'yb%d' % i, ph) for i in range(2)]
            pT1 = S.ps([128, 512], F32, 'pT1', ph); pT1v = pT1[:, :].bitcast(BF16)
            pT2 = S.ps([128, 512], F32, 'pT2', ph); pT2v = pT2[:, :].bitcast(BF16)
            pgu = [S.ps([128, 512], F32, 'pgu%d' % i, ph) for i in range(4)]
            pdn = [S.ps([128, 512], F32, 'pdn%d' % i, ph) for i in range(2)]
            NB_RUN = int(os.environ.get('NB_RUN', NBLK))
            bc_g = nc.gpsimd.to_reg(64 * D - 1); bc_d = nc.gpsimd.to_reg(64 * 1024 - 1)
            for b in range(NB_RUN):
                rt = rts[b % 2]; xg = xgs[b % 2]; yb = ybs[b % 2]
                ld(rt, rt[:], rinfo[b * 128:(b + 1) * 128, :], reads=[rinfo])
                S.dma('pool', lambda e: e.indirect_dma_start(out=xg[:], out_offset=None, in_=H2[:, :],
                                                             in_offset=bass.IndirectOffsetOnAxis(ap=rt[:, 0:1], axis=0)), reads=[rt, H2], writes=[xg])
                if b < 64:
                    for kc in range(16):
                        r0 = b * 2048 + kc * 128
                        S.dma('pool', lambda e: e.dma_start(out=Wg_t[kc][:], in_=w_eg[r0:r0 + 128, :]), writes=[Wg_t[kc]])
                        S.dma('pool', lambda e: e.dma_start(out=Wu_t[kc][:], in_=w_eu[r0:r0 + 128, :]), writes=[Wu_t[kc]])
                    for kc in range(8):
                        r0 = b * 1024 + kc * 128
                        S.dma('pool', lambda e: e.dma_start(out=Wd_t[kc][:], in_=w_ed[r0:r0 + 128, :]), writes=[Wd_t[kc]])
                else:
                    ob_ = b - 64
                    for kc in range(16):
                        S.dma('pool', lambda e: e.indirect_dma_start(out=Wg_t[kc][:], out_offset=None, in_=w_eg[:, :],
                                                                     in_offset=bass.IndirectOffsetOnAxis(ap=idxg_i[:, ob_, kc:kc + 1], axis=0), bounds_check=bc_g, oob_is_err=False), reads=[idxg_i], writes=[Wg_t[kc]])
                        S.dma('pool', lambda e: e.indirect_dma_start(out=Wu_t[kc][:], out_offset=None, in_=w_eu[:, :],
                                                                     in_offset=bass.IndirectOffsetOnAxis(ap=idxg_i[:, ob_, kc:kc + 1], axis=0), bounds_check=bc_g, oob_is_err=False), reads=[idxg_i], writes=[Wu_t[kc]])
                    for kc in range(8):
                        S.dma('pool', lambda e: e.indirect_dma_start(out=Wd_t[kc][:], out_offset=None, in_=w_ed[:, :],
                                                                     in_offset=bass.IndirectOffsetOnAxis(ap=idxd_i[:, ob_, kc:kc + 1], axis=0), bounds_check=bc_d, oob_is_err=False), reads=[idxd_i], writes=[Wd_t[kc]])
                for half in range(2):
                    for kk in range(8):
                        k = half * 8 + kk
                        transpose_bf(pT1, pT1v[:, kk * 128:(kk + 1) * 128], xg, xg[:, k * 128:(k + 1) * 128])
                    S.op('act', lambda e: e.copy(out=xgT[:, half * 8:(half + 1) * 8, :], in_=pT1v[:, 0:1024].rearrange("p (a b) -> p a b", a=8)), reads=[pT1], writes=[xgT])
                for kc in range(16):
                    for wi, wt in ((0, Wg_t[kc]), (1, Wu_t[kc])):
                        for nb in range(2):
                            pb = pgu[wi * 2 + nb]
                            S.op('pe', lambda e: e.matmul(pb[:], lhsT=xgT[:, kc, :], rhs=wt[:, nb * 512:(nb + 1) * 512], start=(kc == 0), stop=(kc == 15)),
                                 reads=[xgT, wt], writes=[pb])
                for nb in range(2):
                    S.op('act', lambda e: e.activation(out=sg[:, nb * 512:(nb + 1) * 512], in_=pgu[nb][:], func=AF.Silu), reads=[pgu[nb]], writes=[sg])
                    S.op('dve', lambda e: e.tensor_tensor(out=actb[:, nb * 512:(nb + 1) * 512], in0=sg[:, nb * 512:(nb + 1) * 512], in1=pgu[2 + nb][:], op=ALU.mult),
                         reads=[sg, pgu[2 + nb]], writes=[actb])
                for kk in range(8):
                    transpose_bf(pT2, pT2v[:, kk * 128:(kk + 1) * 128], actb, actb[:, kk * 128:(kk + 1) * 128])
                S.op('act', lambda e: e.copy(out=actT[:], in_=pT2v[:, 0:1024].rearrange("p (a b) -> p a b", a=8)), reads=[pT2], writes=[actT])
                for hf in range(2):
                    for kc in range(8):
                        for i in range(2):
                            c0 = hf * 1024 + i * 512
                            S.op('pe', lambda e: e.matmul(pdn[i][:], lhsT=actT[:, kc, :], rhs=Wd_t[kc][:, c0:c0 + 512], start=(kc == 0), stop=(kc == 7)),
                                 reads=[actT, Wd_t[kc]], writes=[pdn[i]])
                    for i in range(2):
                        c0 = hf * 1024 + i * 512
                        S.op('dve', lambda e: e.tensor_scalar(out=yb[:, c0:c0 + 512], in0=pdn[i][:], scalar1=rt[:, 1:2].bitcast(F32), scalar2=None, op0=ALU.mult),
                             reads=[pdn[i], rt], writes=[yb])
                st(Yd, Yd[b * 128:(b + 1) * 128, :], yb, yb[:])
            S.barrier()
        with ExitStack() as ph:
            g2b = S.sb([128, D], F32, 'g2b', ph); ld(g2b, g2b[:], modd[5:6, :].partition_broadcast(128), reads=[modd])
            gfb = S.sb([128, D], F32, 'gfb', ph); ld(gfb, gfb[:], nfg[0:1, :].partition_broadcast(128))
            xm = [S.sb([128, D], F32, 'xm%d' % i, ph) for i in range(2)]
            y1 = [S.sb([128, D], F32, 'y1%d' % i, ph) for i in range(2)]
            y2 = [S.sb([128, D], F32, 'y2%d' % i, ph) for i in range(2)]
            junkb = S.sb([128, D], BF16, 'junkb', ph)
            for n in range(NOWN):
                x_, a_, b_ = xm[n % 2], y1[n % 2], y2[n % 2]
                ld(x_, x_[:], xmid[n * 128:(n + 1) * 128, :], reads=[xmid])
                for kk, dst in ((0, a_), (1, b_)):
                    S.dma('pool', lambda e: e.indirect_dma_start(out=dst[:], out_offset=None, in_=Yd[:, :],
                                                                 in_offset=bass.IndirectOffsetOnAxis(ap=slot_i[:, kk, n:n + 1], axis=0)), reads=[slot_i, Yd], writes=[dst])
                S.op('dve', lambda e: e.tensor_tensor(out=a_[:], in0=a_[:], in1=b_[:], op=ALU.add), reads=[a_, b_], writes=[a_])
                S.op('dve', lambda e: e.tensor_tensor(out=a_[:], in0=a_[:], in1=g2b[:], op=ALU.mult), reads=[a_, g2b], writes=[a_])
                S.op('dve', lambda e: e.tensor_tensor(out=a_[:], in0=a_[:], in1=x_[:], op=ALU.add), reads=[a_, x_], writes=[a_])
                rms_rstd(a_, a_[:], junkb, ss, rstd)
                S.op('dve', lambda e: e.scalar_tensor_tensor(out=a_[:], in0=a_[:], scalar=rstd[:, 0:1], in1=gfb[:], op0=ALU.mult, op1=ALU.mult),
                     reads=[a_, rstd, gfb], writes=[a_])
                st(T(out, 'out'), out[n * 128:(n + 1) * 128, :], a_, a_[:], is_out=True)
        S.finish()
    return nc, list(dbgo)


def _consts():
    c = {}
    c["ident"] = np.eye(128, dtype=np.float32)
    i = np.arange(128)[:, None]; j = np.arange(256)[None, :]
    valid = (j > i) & (j <= i + 128)
    c["mask"] = np.where(valid, 0.0, -1e30).astype(np.float32)
    gam = np.array(GAM, dtype=np.float64)
    lg = np.log(gam)
    e = np.arange(128)[:, None, None]; cc = np.arange(128)[None, None, :]
    diff = cc - e
    c["decT"] = np.where(diff >= 0, np.exp(np.maximum(diff, 0) * lg[None, :, None]), 0.0).astype(np.float32)
    idx = np.arange(128)[:, None].astype(np.float64)
    c["dq"] = np.exp((idx + 1.0) * lg[None, :]).astype(np.float32)
    c["dk"] = (np.exp((127.0 - idx) * lg[None, :]) / 16.0).astype(np.float32)
    invf = (10000.0 ** (-(np.arange(128, dtype=np.float32) / np.float32(128)))).astype(np.float32)
    c["invf"] = np.broadcast_to(invf[None, :], (128, 128)).copy()
    c["Lst"] = (np.arange(128)[:, None] < np.arange(128)[None, :]).astype(np.float32)
    c["tokid"] = (np.arange(NOWN)[None, :] * 128 + np.arange(128)[:, None]).astype(np.int32)
    c["blk128"] = np.broadcast_to((np.arange(NBLK, dtype=np.float32) * 128.0)[None, :], (128, NBLK)).copy()
    c["kcoff"] = np.broadcast_to((np.arange(16, dtype=np.float32) * 128.0)[None, :], (128, 16)).copy()
    c["pidx"] = np.arange(128, dtype=np.float32)[:, None].copy()
    c["e128"] = np.broadcast_to((np.arange(64, dtype=np.float32) * 128.0)[None, :], (128, 64)).copy()
    return c


def prep_inputs(x, c, positions, norm1_gain, norm2_gain, final_norm_gain, w_ada, b_ada, w_in,
                attn_sinks, ret_norm_gain, w_branch_attn, w_branch_ret, w_out,
                w_router_group, b_router_group, w_router_expert, b_router_expert,
                w_expert_gate, w_expert_up, w_expert_down):
    f = lambda a: np.ascontiguousarray(np.asarray(a))
    x = f(x); c = f(c); positions = f(positions)
    shared = dict(
        w_ada=f(w_ada)[0], b_ada=f(b_ada), w_in=f(w_in)[0], attn_sinks=f(attn_sinks), ret_norm_gain=f(ret_norm_gain),
        w_out=f(w_out)[0],
        w_router=np.concatenate([f(w_router_group)[0], f(w_router_expert)[0]], axis=1),
        b_router=np.concatenate([f(b_router_group), f(b_router_expert)], axis=1),
        w_expert_gate=f(w_expert_gate).reshape(64 * D, 1024), w_expert_up=f(w_expert_up).reshape(64 * D, 1024),
        w_expert_down=f(w_expert_down).reshape(64 * 1024, D),
        norm1_gain=f(norm1_gain), norm2_gain=f(norm2_gain), final_norm_gain=f(final_norm_gain).reshape(1, D),
    )
    def tile_cols(w, off):
        sub = w[:, off:off + D].reshape(KC, 128, 16, 128)
        return np.ascontiguousarray(sub.transpose(2, 1, 0, 3)).reshape(16 * 128, KC * 128)
    w_in0 = shared["w_in"]
    shared["wm_t"] = np.concatenate([tile_cols(f(w_branch_attn)[0], 0), tile_cols(f(w_branch_ret)[0], 0),
                                     tile_cols(w_in0, OFF_GA), tile_cols(w_in0, OFF_GTR)], axis=0)
    shared.update(_consts())
    gam = np.array(GAM, dtype=np.float64); lg = np.log(gam)
    maps = []
    for core in range(8):
        b, q = core // 4, core % 4
        m = dict(shared)
        m["xo"] = x[b, q * 1024:(q + 1) * 1024]
        npre = q * 8
        xp = np.zeros((NPRE * 128, D), np.float32)
        pp = np.zeros((NPRE * 128,), np.int32)
        if npre:
            xp[(NPRE - npre) * 128:] = x[b, :q * 1024]
            pp[(NPRE - npre) * 128:] = positions[b, :q * 1024]
        m["xp"] = xp
        m["pos_o"] = np.ascontiguousarray(positions[b, q * 1024:(q + 1) * 1024].reshape(NOWN, 128).T)
        m["pos_p"] = np.ascontiguousarray(pp.reshape(NPRE, 128).T)
        m["cT"] = np.ascontiguousarray(c[b].reshape(KC, 128).T)
        valid = (np.arange(NPRE) >= NPRE - npre).astype(np.float64)
        idx = np.arange(128)[:, None, None].astype(np.float64)
        jj = np.arange(NPRE)[None, :, None].astype(np.float64)
        pk = np.exp((127.0 - idx) * lg[None, None, :] + 128.0 * (NPRE - 1 - jj) * lg[None, None, :]) / 16.0 * valid[None, :, None]
        m["pk"] = pk.astype(np.float32)
        mk = shared["mask"].copy()
        if q == 0:
            mk[:, :128] = -1e30
        m["mask0"] = mk
        maps.append(m)
    return maps


def kernel(**inputs):
    maps = prep_inputs(**inputs)
    nc, _ = build()
    res = run_bass_kernel_spmd(nc, maps, core_ids=list(range(8)))
    outp = np.zeros((2, 4096, D), np.float32)
    for core in range(8):
        b, q = core // 4, core % 4
        outp[b, q * 1024:(q + 1) * 1024] = res.results[core]["out"]
    return outp
```

```python
import numpy as np
import concourse.bass as bass
import concourse.mybir as mybir
from concourse.bass_utils import run_bass_kernel_spmd

F32 = mybir.dt.float32
BF16 = mybir.dt.bfloat16
I32 = mybir.dt.int32
U32 = mybir.dt.uint32
ALU = mybir.AluOpType
AF = mybir.ActivationFunctionType
AX = mybir.AxisListType


class Tr:
    def __init__(self):
        self.last_w = None
        self.readers = {}


class T:
    def __init__(self, h, name, tr=None):
        self.h = h
        self.name = name
        self.tr = tr or Tr()

    @property
    def last_w(self):
        return self.tr.last_w

    @last_w.setter
    def last_w(self, v):
        self.tr.last_w = v

    @property
    def readers(self):
        return self.tr.readers

    @readers.setter
    def readers(self, v):
        self.tr.readers = v

    def v(self, ap):
        return T(ap, self.name, self.tr)

    def __getitem__(self, k):
        return self.h[k]


class Sched:
    ENG = ('pe', 'dve', 'act', 'pool', 'sp')

    def __init__(self, nc, es):
        self.nc = nc
        self.es = es
        self.eng = {'pe': nc.tensor, 'dve': nc.vector, 'act': nc.scalar, 'pool': nc.gpsimd, 'sp': nc.sync}
        self.ops = {e: [] for e in self.ENG}
        self.cnt = {e: 0 for e in self.ENG}
        self.sem = {e: es.enter_context(nc.semaphore('s_' + e)) for e in self.ENG if e != 'sp'}
        self.seen = {e: {} for e in self.ENG}
        self.NP = 8
        self.dsem = {q: [es.enter_context(nc.semaphore('d_%s%d' % (q, i))) for i in range(self.NP)]
                     for q in ('sp', 'pool', 'act')}
        self.dn = {q: 0 for q in ('sp', 'pool', 'act')}
        self.ntile = 0
        self.out_tokens = []

    def sb(self, shape, dt, name=None, es=None):
        self.ntile += 1
        name = (name or 't') + '_%d' % self.ntile
        h = (es or self.es).enter_context(self.nc.sbuf_tensor(name, list(shape), dt))
        return T(h, name)

    def ps(self, shape, dt, name=None, es=None):
        self.ntile += 1
        name = (name or 'p') + '_%d' % self.ntile
        h = (es or self.es).enter_context(self.nc.psum_tensor(name, list(shape), dt))
        return T(h, name)

    def barrier(self):
        toks = [('eng', f, self.cnt[f]) for f in ('pe', 'dve', 'act', 'pool') if self.cnt[f] > 0]
        for q in ('sp', 'pool', 'act'):
            n = self.dn[q]
            for i in range(max(0, n - self.NP), n):
                toks.append(('dma', self.dsem[q][i % self.NP], 16 * (i // self.NP + 1)))
        for e in self.ENG:
            for tok in toks:
                self._wait(e, tok)

    def dram(self, name, shape, dt, kind='Internal'):
        h = self.nc.dram_tensor(name, list(shape), dt, kind=kind)
        return T(h, name)

    def _wait(self, e, tok):
        if tok is None:
            return
        kind, key, val = tok
        if kind == 'eng' and key == e and e == 'pe':
            return
        if kind == 'eng' and key == 'sp':
            return
        sk = (kind, key if kind == 'eng' else id(key))
        if self.seen[e].get(sk, 0) >= val:
            return
        self.seen[e][sk] = val
        sem = self.sem[key] if kind == 'eng' else key
        eng = self.eng[e]
        eng.wait_ge(sem, val)

    def _deps(self, e, reads, writes):
        for t in reads:
            self._wait(e, t.last_w)
        for t in writes:
            self._wait(e, t.last_w)
            for tok in list(t.readers.values()):
                self._wait(e, tok)

    def _update(self, tok, reads, writes):
        for t in reads:
            kind, key, val = tok
            rk = (kind, key if kind == 'eng' else (id(key)))
            t.readers[rk] = tok
        for t in writes:
            t.last_w = tok
            t.readers = {}

    def op(self, e, fn, reads=(), writes=()):
        assert e in ('pe', 'dve', 'act', 'pool')
        self._deps(e, reads, writes)
        self.cnt[e] += 1
        idx = self.cnt[e]
        eng = self.eng[e]
        sem = self.sem[e]
        fn(eng).then_inc(sem, 1)
        self._update(('eng', e, idx), reads, writes)

    def dma(self, q, fn, reads=(), writes=(), is_out=False):
        self._deps(q, reads, writes)
        n = self.dn[q]
        self.dn[q] += 1
        slot = n % self.NP
        sem = self.dsem[q][slot]
        if n >= self.NP:
            self._wait(q, ('dma', sem, 16 * (n // self.NP)))
        val = 16 * (n // self.NP + 1)
        eng = self.eng[q]
        fn(eng).then_inc(sem, 16)
        tok = ('dma', sem, val)
        self._update(tok, reads, writes)
        if is_out:
            self.out_tokens.append(tok)
        return tok

    def finish(self):
        for tok in self.out_tokens:
            self._wait('sp', tok)
        for q in ('sp', 'pool', 'act'):
            n = self.dn[q]
            for i in range(max(0, n - self.NP), n):
                self._wait('sp', ('dma', self.dsem[q][i % self.NP], 16 * (i // self.NP + 1)))


import math
import os
from contextlib import ExitStack

D = 2048
KC = 16
NOWN = 8
NPRE = 24
NBLK = 80
NOV = 16
OFF_QA, OFF_KA, OFF_VA, OFF_QR, OFF_KR, OFF_VR, OFF_GR, OFF_GA, OFF_GTR = 0, 2048, 2304, 2560, 4608, 6656, 8704, 10752, 12800
EPS = 1e-6
TWO_PI = 2.0 * math.pi
C1 = 6.28125
C2 = TWO_PI - C1
GAM = [1.0 - 2.0 ** (-5.0 - h) for h in range(8)]


def build(stop_after=None):
    nc = bass.Bass("TRN2", target_bir_lowering=False)

    def din(name, shape, dt=F32):
        return nc.dram_tensor(name, list(shape), dt, kind="ExternalInput")

    xo = din("xo", [1024, D]); xp = din("xp", [NPRE * 128, D])
    pos_o = din("pos_o", [128, NOWN], I32); pos_p = din("pos_p", [128, NPRE], I32)
    cT = din("cT", [128, KC]); pkd = din("pk", [128, NPRE, 8]); mask0d = din("mask0", [128, 256])
    w_ada = din("w_ada", [D, 6 * D]); b_ada = din("b_ada", [1, 6 * D]); w_in = din("w_in", [D, 14848])
    sinks = din("attn_sinks", [1, 32]); rgain = din("ret_norm_gain", [1, D])
    wm_t = din("wm_t", [4 * 16 * 128, D]); w_o = din("w_out", [D, D])
    w_rt = din("w_router", [D, 68]); b_rt = din("b_router", [1, 68])
    if stop_after is None or stop_after == 'moe_full':
        w_eg = din("w_expert_gate", [64 * D, 1024]); w_eu = din("w_expert_up", [64 * D, 1024]); w_ed = din("w_expert_down", [64 * 1024, D])
    n1g = din("norm1_gain", [1, D]); n2g = din("norm2_gain", [1, D]); nfg = din("final_norm_gain", [1, D])
    identd = din("ident", [128, 128]); maskd = din("mask", [128, 256]); decTd = din("decT", [128, 8, 128])
    dqd = din("dq", [128, 8]); dkd = din("dk", [128, 8]); invfd = din("invf", [128, 128]); Lstd = din("Lst", [128, 128])
    tokidd = din("tokid", [128, NOWN], I32); blk128d = din("blk128", [128, NBLK]); kcoffd = din("kcoff", [128, 16]); pidxd = din("pidx", [128, 1]); e128d = din("e128", [128, 64])
    out = nc.dram_tensor("out", [1024, D], F32, kind="ExternalOutput")
    dbgo = {}

    def dout(name, shape, dt=F32):
        dbgo[name] = nc.dram_tensor(name, list(shape), dt, kind="ExternalOutput")
        return dbgo[name]

    modd = T(nc.dram_tensor("modd", [6, D], F32, kind="Internal"), "modd")
    xmid = T(nc.dram_tensor("xmid", [1024, D], F32, kind="Internal"), "xmid")
    H2 = T(nc.dram_tensor("H2", [1025, D], BF16, kind="Internal"), "H2")
    rinfo = T(nc.dram_tensor("rinfo", [NBLK * 128, 16], I32, kind="Internal"), "rinfo")
    Yd = T(nc.dram_tensor("Yd", [NBLK * 128, D], F32, kind="Internal"), "Yd")

    with ExitStack() as es:
        S = Sched(nc, es)
        qrr = [0]

        def ld(dst_T, dst_ap, src_ap, q='sp', reads=(), extra_w=()):
            return S.dma(q, lambda e: e.dma_start(out=dst_ap, in_=src_ap), reads=list(reads), writes=[dst_T] + list(extra_w))

        def st(dst_T, dst_ap, src_T, src_ap, q='sp', is_out=False):
            return S.dma(q, lambda e: e.dma_start(out=dst_ap, in_=src_ap), reads=[src_T], writes=[dst_T], is_out=is_out)

        idf = S.sb([128, 128], F32, 'idf'); ld(idf, idf[:], identd[:, :])
        idb = S.sb([128, 128], BF16, 'idb')
        S.op('dve', lambda e: e.tensor_copy(out=idb[:], in_=idf[:]), reads=[idf], writes=[idb])

        def transpose_bf(ps_T, ps_ap, src_T, src_ap):
            S.op('pe', lambda e: e.transpose(out=ps_ap, in_=src_ap, identity=idb[:]), reads=[src_T, idb], writes=[ps_T])

        def rms_rstd(x_T, x_ap, junk_T, ss, rstd, n=D):
            S.op('act', lambda e: e.activation(out=junk_T[:], in_=x_ap, func=AF.Square, accum_out=ss[:]), reads=[x_T], writes=[junk_T, ss])
            S.op('act', lambda e: e.activation(out=ss[:], in_=ss[:], func=AF.Sqrt, scale=1.0 / n, bias=EPS), reads=[ss], writes=[ss])
            S.op('dve', lambda e: e.reciprocal(out=rstd[:], in_=ss[:]), reads=[ss], writes=[rstd])

        with ExitStack() as ph:
            cs = S.sb([128, KC], F32, 'cs', ph); ld(cs, cs[:], cT[:, :])
            sc = S.sb([128, KC], F32, 'sc', ph)
            S.op('act', lambda e: e.activation(out=sc[:], in_=cs[:], func=AF.Silu), reads=[cs], writes=[sc])
            scb = S.sb([128, KC, 128], BF16, 'scb', ph)
            S.op('dve', lambda e: e.tensor_copy(out=scb[:], in_=sc[:, :].unsqueeze(2).to_broadcast([128, KC, 128])), reads=[sc], writes=[scb])
            g1 = S.sb([128, D], F32, 'g1', ph); ld(g1, g1[:], n1g[0:1, :].partition_broadcast(128))
            g2 = S.sb([128, D], F32, 'g2', ph); ld(g2, g2[:], n2g[0:1, :].partition_broadcast(128))
            wbuf = [S.sb([128, KC, 512], BF16, 'wada%d' % i, ph) for i in range(3)]
            bbuf = [S.sb([128, 512], F32, 'bada%d' % i, ph) for i in range(2)]
            rbuf = [S.sb([128, 512], F32, 'rada%d' % i, ph) for i in range(2)]
            pms = [S.ps([128, 512], F32, 'pada%d' % i, ph) for i in range(2)]
            it = 0
            for j in range(6):
                for nb in range(4):
                    c0 = j * D + nb * 512
                    wb, bb, rb, pm = wbuf[it % 3], bbuf[it % 2], rbuf[it % 2], pms[it % 2]
                    for kq in range(4):
                        S.dma('pool', lambda e: e.dma_start(out=wb[:, kq * 4:(kq + 1) * 4, :],
                                                            in_=w_ada[kq * 512:(kq + 1) * 512, c0:c0 + 512].rearrange("(k p) n -> p k n", p=128)), writes=[wb])
                    ld(bb, bb[:], b_ada[0:1, c0:c0 + 512].partition_broadcast(128))
                    for k in range(KC):
                        S.op('pe', lambda e: e.matmul(pm[:], lhsT=scb[:, k, :], rhs=wb[:, k, :], start=(k == 0), stop=(k == KC - 1)),
                             reads=[scb, wb], writes=[pm])
                    S.op('dve', lambda e: e.tensor_tensor(out=rb[:], in0=pm[:], in1=bb[:], op=ALU.add), reads=[pm, bb], writes=[rb])
                    if j in (1, 4):
                        gg = g1 if j == 1 else g2
                        S.op('dve', lambda e: e.scalar_tensor_tensor(out=rb[:], in0=rb[:], scalar=1.0, in1=gg[:, nb * 512:(nb + 1) * 512],
                                                                     op0=ALU.add, op1=ALU.mult), reads=[rb, gg], writes=[rb])
                    st(modd, modd[j:j + 1, nb * 512:(nb + 1) * 512], rb, rb[0:1, :])
                    it += 1
            S.barrier()
        if stop_after == 'ada':
            o = dout("d_mod", [6, D])
            t = S.sb([6, D], F32, 'dbg'); ld(t, t[:], modd[:, :], reads=[modd])
            st(T(o, 'o'), o[:, :], t, t[:], is_out=True)
            S.finish()
            return nc, list(dbgo)
        mx = es.enter_context(ExitStack())
        state_f = S.sb([128, 8, 512], F32, 'state_f', mx)
        S.op('dve', lambda e: e.memset(state_f[:], 0.0), writes=[state_f])
        invf = S.sb([128, 128], F32, 'invf', mx); ld(invf, invf[:], invfd[:, :])
        rp_a = S.sb([128, 128], F32, 'rp_a', mx); rp_b = S.sb([128, 128], F32, 'rp_b', mx)
        rp_k = S.sb([128, 128], I32, 'rp_k', mx); rp_f = S.sb([128, 128], F32, 'rp_f', mx)
        ss = S.sb([128, 1], F32, 'ss', mx); rstd = S.sb([128, 1], F32, 'rstd', mx)

        def rope_tables(pos_T, pos_ap, cos_T, cos_ap, sin_T, sin_ap):
            S.op('dve', lambda e: e.tensor_scalar(out=rp_a[:], in0=invf[:], scalar1=pos_ap, scalar2=None, op0=ALU.mult),
                 reads=[invf, pos_T], writes=[rp_a])
            for which in (0, 1):
                dst_T, dst_ap = (sin_T, sin_ap) if which == 0 else (cos_T, cos_ap)
                if which == 1:
                    S.op('dve', lambda e: e.tensor_scalar(out=rp_a[:], in0=rp_a[:], scalar1=math.pi / 2, scalar2=None, op0=ALU.add),
                         reads=[rp_a], writes=[rp_a])
                S.op('dve', lambda e: e.tensor_scalar(out=rp_k[:], in0=rp_a[:], scalar1=1.0 / TWO_PI, scalar2=None, op0=ALU.mult),
                     reads=[rp_a], writes=[rp_k])
                S.op('dve', lambda e: e.tensor_copy(out=rp_f[:], in_=rp_k[:]), reads=[rp_k], writes=[rp_f])
                S.op('dve', lambda e: e.scalar_tensor_tensor(out=rp_b[:], in0=rp_f[:], scalar=-C1, in1=rp_a[:], op0=ALU.mult, op1=ALU.add),
                     reads=[rp_f, rp_a], writes=[rp_b])
                S.op('dve', lambda e: e.scalar_tensor_tensor(out=rp_b[:], in0=rp_f[:], scalar=-C2, in1=rp_b[:], op0=ALU.mult, op1=ALU.add),
                     reads=[rp_f, rp_b], writes=[rp_b])
                S.op('dve', lambda e: e.tensor_scalar(out=rp_b[:], in0=rp_b[:], scalar1=3.1415925, scalar2=-3.1415925, op0=ALU.min, op1=ALU.max),
                     reads=[rp_b], writes=[rp_b])
                S.op('act', lambda e: e.activation(out=dst_ap, in_=rp_b[:], func=AF.Sin), reads=[rp_b], writes=[dst_T])

        def layer_norm_mod(x_T, hb_T, Ab, shb):
            rms_rstd(x_T, x_T[:], hb_T, ss, rstd)
            S.op('dve', lambda e: e.scalar_tensor_tensor(out=x_T[:], in0=x_T[:], scalar=rstd[:, 0:1], in1=Ab[:], op0=ALU.mult, op1=ALU.mult),
                 reads=[x_T, rstd, Ab], writes=[x_T])
            S.op('dve', lambda e: e.tensor_tensor(out=hb_T[:], in0=x_T[:], in1=shb[:], op=ALU.add), reads=[x_T, shb], writes=[hb_T])

        def make_hT(hb_T, dst_T, dst_fn, pTs):
            for half in range(2):
                pT = pTs[half]
                pv_ = pT[:, :].bitcast(BF16)
                for kk in range(8):
                    k = half * 8 + kk
                    transpose_bf(pT, pv_[:, kk * 128:(kk + 1) * 128], hb_T, hb_T[:, k * 128:(k + 1) * 128])
                S.op('act', lambda e: e.copy(out=dst_fn(half), in_=pv_[:, 0:1024].rearrange("p (a b) -> p a b", a=8)),
                     reads=[pT], writes=[dst_T])

        def rotary(src_T, src_ap4, cos_T, cos_ap, sin_T, sin_ap, rot_T, rot_ap4, tA, tB, n):
            cb = cos_ap.unsqueeze(1).to_broadcast([128, n, 128]); sb_ = sin_ap.unsqueeze(1).to_broadcast([128, n, 128])
            t1 = src_ap4[:, :, 0, :]; t2 = src_ap4[:, :, 1, :]
            S.op('dve', lambda e: e.tensor_tensor(out=tA[:, 0:n, :], in0=t1, in1=cb, op=ALU.mult), reads=[src_T, cos_T], writes=[tA])
            S.op('dve', lambda e: e.tensor_tensor(out=tB[:, 0:n, :], in0=t2, in1=sb_, op=ALU.mult), reads=[src_T, sin_T], writes=[tB])
            S.op('dve', lambda e: e.tensor_tensor(out=rot_ap4[:, :, 0, :], in0=tA[:, 0:n, :], in1=tB[:, 0:n, :], op=ALU.subtract),
                 reads=[tA, tB], writes=[rot_T])
            S.op('dve', lambda e: e.tensor_tensor(out=tA[:, 0:n, :], in0=t1, in1=sb_, op=ALU.mult), reads=[src_T, sin_T], writes=[tA])
            S.op('dve', lambda e: e.tensor_tensor(out=tB[:, 0:n, :], in0=t2, in1=cb, op=ALU.mult), reads=[src_T, cos_T], writes=[tB])
            S.op('dve', lambda e: e.tensor_tensor(out=rot_ap4[:, :, 1, :], in0=tA[:, 0:n, :], in1=tB[:, 0:n, :], op=ALU.add),
                 reads=[tA, tB], writes=[rot_T])

        with ExitStack() as ph:
            Wk = [S.sb([128, 4, 2048], BF16, 'Wk%d' % i, ph) for i in range(4)]
            Wv = [S.sb([128, 4, 2048], BF16, 'Wv%d' % i, ph) for i in range(4)]
            for k in range(KC):
                S.dma('pool', lambda e: e.dma_start(out=Wk[k // 4][:, k % 4, :], in_=w_in[k * 128:(k + 1) * 128, OFF_KR:OFF_KR + 2048]), writes=[Wk[k // 4]])
                S.dma('pool', lambda e: e.dma_start(out=Wv[k // 4][:, k % 4, :], in_=w_in[k * 128:(k + 1) * 128, OFF_VR:OFF_VR + 2048]), writes=[Wv[k // 4]])
            A1b = S.sb([128, D], F32, 'A1b', ph); ld(A1b, A1b[:], modd[1:2, :].partition_broadcast(128), reads=[modd])
            sh1b = S.sb([128, D], F32, 'sh1b', ph); ld(sh1b, sh1b[:], modd[0:1, :].partition_broadcast(128), reads=[modd])
            posi = S.sb([128, NPRE], I32, 'posi', ph); ld(posi, posi[:], pos_p[:, :])
            posf = S.sb([128, NPRE], F32, 'posf', ph)
            S.op('dve', lambda e: e.tensor_copy(out=posf[:], in_=posi[:]), reads=[posi], writes=[posf])
            pkt = S.sb([128, NPRE, 8], F32, 'pkt', ph); ld(pkt, pkt[:], pkd[:, :, :])
            xc = [S.sb([128, D], F32, 'xc%d' % i, ph) for i in range(2)]
            hb = S.sb([128, D], BF16, 'hb', ph)
            hT = S.sb([128, KC, 128], BF16, 'hT', ph)
            cosT = S.sb([128, 128], F32, 'cosT', ph); sinT = S.sb([128, 128], F32, 'sinT', ph)
            rot = S.sb([128, 2, 2, 128], F32, 'rot', ph)
            tA = S.sb([128, 2, 128], F32, 'tA', ph); tB = S.sb([128, 2, 128], F32, 'tB', ph)
            ks = S.sb([128, 8, 256], BF16, 'ks', ph)
            vb = S.sb([128, D], BF16, 'vb', ph)
            pTs = [S.ps([128, 512], F32, 'pT%d' % i, ph) for i in range(2)]
            pkb = [S.ps([128, 512], F32, 'pkb%d' % i, ph) for i in range(2)]
            pvb = [S.ps([128, 512], F32, 'pvb%d' % i, ph) for i in range(2)]
            pst = [S.ps([128, 512], F32, 'pst%d' % i, ph) for i in range(2)]
            ld(xc[0], xc[0][:], xp[0:128, :])
            import os
            for j in range(NPRE if not os.environ.get('SKIP_PREFIX') else 0):
                xcur = xc[j % 2]
                if j + 1 < NPRE:
                    ld(xc[(j + 1) % 2], xc[(j + 1) % 2][:], xp[(j + 1) * 128:(j + 2) * 128, :])
                layer_norm_mod(xcur, hb, A1b, sh1b)
                make_hT(hb, hT, lambda half: hT[:, half * 8:(half + 1) * 8, :], pTs)
                rope_tables(posf, posf[:, j:j + 1], cosT, cosT[:], sinT, sinT[:])
                for nb in range(4):
                    pb = pkb[nb % 2]
                    for k in range(KC):
                        S.op('pe', lambda e: e.matmul(pb[:], lhsT=hT[:, k, :], rhs=Wk[k // 4][:, k % 4, nb * 512:(nb + 1) * 512],
                                                      start=(k == 0), stop=(k == KC - 1)), reads=[hT, Wk[k // 4]], writes=[pb])
                    rotary(pb, pb[:, :].rearrange("p (h t f) -> p h t f", h=2, t=2), cosT, cosT[:], sinT, sinT[:],
                           rot, rot[:], tA, tB, 2)
                    S.op('dve', lambda e: e.tensor_tensor(out=ks[:, 2 * nb:2 * nb + 2, :], in0=rot[:].rearrange("p h t f -> p h (t f)"),
                                                          in1=pkt[:, j, 2 * nb:2 * nb + 2].unsqueeze(2).to_broadcast([128, 2, 256]), op=ALU.mult),
                         reads=[rot, pkt], writes=[ks])
                for nb in range(4):
                    pb = pvb[nb % 2]
                    for k in range(KC):
                        S.op('pe', lambda e: e.matmul(pb[:], lhsT=hT[:, k, :], rhs=Wv[k // 4][:, k % 4, nb * 512:(nb + 1) * 512],
                                                      start=(k == 0), stop=(k == KC - 1)), reads=[hT, Wv[k // 4]], writes=[pb])
                    S.op('act', lambda e: e.copy(out=vb[:, nb * 512:(nb + 1) * 512], in_=pb[:]), reads=[pb], writes=[vb])
                for h in range(8):
                    pb = pst[h % 2]
                    for half in range(2):
                        S.op('pe', lambda e: e.matmul(pb[:, half * 256:(half + 1) * 256], lhsT=ks[:, h, half * 128:(half + 1) * 128],
                                                      rhs=vb[:, h * 256:(h + 1) * 256], start=True, stop=True), reads=[ks, vb], writes=[pb])
                    S.op('dve', lambda e: e.tensor_tensor(out=state_f[:, h, :], in0=state_f[:, h, :], in1=pb[:], op=ALU.add),
                         reads=[state_f, pb], writes=[state_f])
            S.barrier()
        if stop_after == 'prefix':
            o = dout("d_state", [128, 8, 512])
            st(T(o, 'o'), o[:, :, :], state_f, state_f[:], is_out=True)
            S.finish()
            return nc, list(dbgo)
        hT_all = S.sb([128, KC, 1152], BF16, 'hT_all', mx)
        retT = S.sb([128, KC, 1024], BF16, 'retT', mx)
        attnT = S.sb([128, KC, 1024], BF16, 'attnT', mx)
        cos_o = S.sb([128, NOWN, 128], F32, 'cos_o', mx); sin_o = S.sb([128, NOWN, 128], F32, 'sin_o', mx)
        with ExitStack() as ph:
            A1b = S.sb([128, D], F32, 'A1b', ph); ld(A1b, A1b[:], modd[1:2, :].partition_broadcast(128), reads=[modd])
            sh1b = S.sb([128, D], F32, 'sh1b', ph); ld(sh1b, sh1b[:], modd[0:1, :].partition_broadcast(128), reads=[modd])
            posi = S.sb([128, NOWN], I32, 'posio', ph); ld(posi, posi[:], pos_o[:, :])
            posf = S.sb([128, NOWN], F32, 'posfo', ph)
            S.op('dve', lambda e: e.tensor_copy(out=posf[:], in_=posi[:]), reads=[posi], writes=[posf])
            xc = [S.sb([128, D], F32, 'xc%d' % i, ph) for i in range(2)]
            hb = [S.sb([128, D], BF16, 'hb%d' % i, ph) for i in range(2)]
            pTs = [S.ps([128, 512], F32, 'pT%d' % i, ph) for i in range(2)]
            for ci in range(9):
                src = xp[(NPRE - 1) * 128:NPRE * 128, :] if ci == 0 else xo[(ci - 1) * 128:ci * 128, :]
                xcur = xc[ci % 2]; hcur = hb[ci % 2]
                ld(xcur, xcur[:], src)
                layer_norm_mod(xcur, hcur, A1b, sh1b)
                make_hT(hcur, hT_all, lambda half: hT_all[:, half * 8:(half + 1) * 8, ci * 128:(ci + 1) * 128], pTs)
            for n in range(NOWN):
                rope_tables(posf, posf[:, n:n + 1], cos_o, cos_o[:, n, :], sin_o, sin_o[:, n, :])
            S.barrier()

        with ExitStack() as ph:
            Wh = [S.sb([128, 4, 1024], BF16, 'Wh%d' % i, ph) for i in range(4)]
            decT = S.sb([128, 8, 128], F32, 'decT', ph); ld(decT, decT[:], decTd[:, :, :])
            dq = S.sb([128, 8], F32, 'dq', ph); ld(dq, dq[:], dqd[:, :])
            dk = S.sb([128, 8], F32, 'dk', ph); ld(dk, dk[:], dkd[:, :])
            rgb = S.sb([128, D], F32, 'rgb', ph); ld(rgb, rgb[:], rgain[0:1, :].partition_broadcast(128))
            state_b = S.sb([128, 8, 512], BF16, 'state_b', ph)
            S.op('act', lambda e: e.copy(out=state_b[:], in_=state_f[:]), reads=[state_f], writes=[state_b])
            rotq = S.sb([128, 2, 2, 128], F32, 'rotq', ph)
            tA = S.sb([128, 2, 128], F32, 'tA', ph); tB = S.sb([128, 2, 128], F32, 'tB', ph)
            qkb = S.sb([128, 3, 256], BF16, 'qkb', ph)
            kd = S.sb([128, 256], BF16, 'kd', ph)
            vbh = S.sb([128, 256], BF16, 'vbh', ph)
            gs = S.sb([128, 256], F32, 'gs', ph)
            qkT = S.sb([128, 6, 128], BF16, 'qkT', ph)
            iT = S.sb([128, 128], BF16, 'iT', ph)
            junk = S.sb([128, 256], F32, 'junk', ph)
            rtmp = S.sb([128, 256], F32, 'rtmp', ph)
            reth = S.sb([128, 256], BF16, 'reth', ph)
            ss2 = S.sb([128, 1], F32, 'ss2', ph); r2 = S.sb([128, 1], F32, 'r2', ph)
            pp = [S.ps([128, 512], F32, 'pp%d' % i, ph) for i in range(4)]
            pTq = S.ps([128, 512], F32, 'pTq', ph); pTqv = pTq[:, :].bitcast(BF16)
            pi_ = S.ps([128, 512], F32, 'pi', ph); po_ = S.ps([128, 512], F32, 'po', ph); pds = S.ps([128, 512], F32, 'pds', ph)
            it = 0
            for h in range(8 if not os.environ.get('SKIP_RET') else 0):
                offs = [OFF_QR + h * 256, OFF_KR + h * 256, OFF_VR + h * 256, OFF_GR + h * 256]
                for sl in range(4):
                    for wi, off in enumerate(offs):
                        S.dma('pool', lambda e: e.dma_start(out=Wh[sl][:, :, wi * 256:(wi + 1) * 256],
                                                            in_=w_in[sl * 512:(sl + 1) * 512, off:off + 256].rearrange("(k p) n -> p k n", p=128)),
                              writes=[Wh[sl]])
                for n in range(NOWN):
                    tok = slice((n + 1) * 128, (n + 2) * 128)
                    pb0, pb1 = pp[2 * (it % 2)], pp[2 * (it % 2) + 1]
                    for blk, pb in ((0, pb0), (1, pb1)):
                        for k in range(KC):
                            S.op('pe', lambda e: e.matmul(pb[:], lhsT=hT_all[:, k, tok], rhs=Wh[k // 4][:, k % 4, blk * 512:(blk + 1) * 512],
                                                          start=(k == 0), stop=(k == KC - 1)), reads=[hT_all, Wh[k // 4]], writes=[pb])
                    rotary(pb0, pb0[:, :].rearrange("p (h t f) -> p h t f", h=2, t=2), cos_o, cos_o[:, n, :], sin_o, sin_o[:, n, :],
                           rotq, rotq[:], tA, tB, 2)
                    rq = rotq[:, 0, :, :].rearrange("p t f -> p (t f)"); rk = rotq[:, 1, :, :].rearrange("p t f -> p (t f)")
                    S.op('act', lambda e: e.copy(out=qkb[:, 0, :], in_=rq), reads=[rotq], writes=[qkb])
                    S.op('dve', lambda e: e.tensor_scalar(out=qkb[:, 1, :], in0=rq, scalar1=dq[:, h:h + 1], scalar2=None, op0=ALU.mult),
                         reads=[rotq, dq], writes=[qkb])
                    S.op('act', lambda e: e.mul(out=qkb[:, 2, :], in_=rk, mul=1.0 / 16.0), reads=[rotq], writes=[qkb])
                    S.op('dve', lambda e: e.tensor_scalar(out=kd[:], in0=rk, scalar1=dk[:, h:h + 1], scalar2=None, op0=ALU.mult),
                         reads=[rotq, dk], writes=[kd])
                    S.op('act', lambda e: e.copy(out=vbh[:], in_=pb1[:, 0:256]), reads=[pb1], writes=[vbh])
                    S.op('act', lambda e: e.activation(out=gs[:], in_=pb1[:, 256:512], func=AF.Silu), reads=[pb1], writes=[gs])
                    for a in range(3):
                        for half in range(2):
                            transpose_bf(pTq, pTqv[:, (a * 2 + half) * 128:(a * 2 + half + 1) * 128], qkb, qkb[:, a, half * 128:(half + 1) * 128])
                    S.op('act', lambda e: e.copy(out=qkT[:], in_=pTqv[:, 0:768].rearrange("p (a b) -> p a b", a=6)), reads=[pTq], writes=[qkT])
                    for half in range(2):
                        S.op('pe', lambda e: e.matmul(pi_[:, 0:128], lhsT=qkT[:, 4 + half, :], rhs=qkT[:, half, :], start=(half == 0), stop=(half == 1)),
                             reads=[qkT], writes=[pi_])
                    S.op('dve', lambda e: e.tensor_tensor(out=iT[:], in0=pi_[:, 0:128], in1=decT[:, h, :], op=ALU.mult), reads=[pi_, decT], writes=[iT])
                    S.op('pe', lambda e: e.matmul(po_[:, 0:256], lhsT=iT[:], rhs=vbh[:], start=True, stop=False), reads=[iT, vbh], writes=[po_])
                    for half in range(2):
                        S.op('pe', lambda e: e.matmul(po_[:, 0:256], lhsT=qkT[:, 2 + half, :], rhs=state_b[:, h, half * 256:(half + 1) * 256],
                                                      start=False, stop=(half == 1)), reads=[qkT, state_b], writes=[po_])
                    rms_rstd(po_, po_[:, 0:256], junk, ss2, r2, n=256)
                    S.op('dve', lambda e: e.scalar_tensor_tensor(out=rtmp[:], in0=po_[:, 0:256], scalar=r2[:, 0:1], in1=rgb[:, h * 256:(h + 1) * 256],
                                                                 op0=ALU.mult, op1=ALU.mult), reads=[po_, r2, rgb], writes=[rtmp])
                    S.op('dve', lambda e: e.tensor_tensor(out=reth[:], in0=rtmp[:], in1=gs[:], op=ALU.mult), reads=[rtmp, gs], writes=[reth])
                    for half in range(2):
                        transpose_bf(pTq, pTqv[:, 768 + half * 128:768 + (half + 1) * 128], reth, reth[:, half * 128:(half + 1) * 128])
                    S.op('act', lambda e: e.copy(out=retT[:, 2 * h:2 * h + 2, n * 128:(n + 1) * 128],
                                                 in_=pTqv[:, 768:1024].rearrange("p (a b) -> p a b", a=2)), reads=[pTq], writes=[retT])
                    for half in range(2):
                        S.op('pe', lambda e: e.matmul(pds[:, half * 256:(half + 1) * 256], lhsT=kd[:, half * 128:(half + 1) * 128], rhs=vbh[:],
                                                      start=True, stop=True), reads=[kd, vbh], writes=[pds])
                    S.op('dve', lambda e: e.scalar_tensor_tensor(out=state_f[:, h, :], in0=state_f[:, h, :], scalar=float(GAM[h] ** 128), in1=pds[:],
                                                                 op0=ALU.mult, op1=ALU.add), reads=[state_f, pds], writes=[state_f])
                    S.op('act', lambda e: e.copy(out=state_b[:, h, :], in_=state_f[:, h, :]), reads=[state_f], writes=[state_b])
                    it += 1
            S.barrier()
        if stop_after == 'ret':
            o = dout("d_retT", [128, KC, 1024], BF16)
            st(T(o, 'o'), o[:, :, :], retT, retT[:], is_out=True)
            S.finish()
            return nc, list(dbgo)
        with ExitStack() as ph:
            Wg = [S.sb([128, 4, 640], BF16, 'Wg%d' % i, ph) for i in range(4)]
            maskt = S.sb([128, 256], F32, 'maskt', ph); ld(maskt, maskt[:], maskd[:, :])
            mask0t = S.sb([128, 256], F32, 'mask0t', ph); ld(mask0t, mask0t[:], mask0d[:, :])
            sinkb = S.sb([128, 32], F32, 'sinkb', ph); ld(sinkb, sinkb[:], sinks[0:1, :].partition_broadcast(128))
            kT_all = S.sb([128, 2, 1152], BF16, 'kT_all', ph)
            v_all = S.sb([128, 9, 64], BF16, 'v_all', ph)
            qT = S.sb([128, 4, 1024], BF16, 'qT', ph)
            qb = S.sb([128, 512], BF16, 'qb', ph); k2 = S.sb([128, 2, 128], BF16, 'k2', ph)
            S.op('dve', lambda e: e.memset(k2[:], 0.0), writes=[k2])
            S_sb = S.sb([128, 8, 256], F32, 'S_sb', ph)
            P_bf = S.sb([128, 8, 256], BF16, 'P_bf', ph)
            PT = S.sb([128, 8, 2, 128], BF16, 'PT', ph)
            mx8 = S.sb([128, 8], F32, 'mx8', ph); negm = S.sb([128, 8], F32, 'negm', ph); rs = S.sb([128, 8], F32, 'rs', ph)
            es8 = S.sb([128, 8], F32, 'es8', ph); rden = S.sb([128, 8], F32, 'rden', ph)
            at_tok = S.sb([128, 8, 64], BF16, 'at_tok', ph)
            pq0 = S.ps([128, 512], F32, 'pq0', ph); pq1 = S.ps([128, 512], F32, 'pq1', ph)
            pTa = S.ps([128, 512], F32, 'pTa', ph); pTav = pTa[:, :].bitcast(BF16)
            psc = [S.ps([128, 512], F32, 'psc%d' % i, ph) for i in range(2)]
            pPT = S.ps([128, 512], F32, 'pPT', ph); pPTv = pPT[:, :].bitcast(BF16)
            ppv = S.ps([128, 512], F32, 'ppv', ph)
            for g in range(4):
                offs = [(OFF_QA + g * 512, 512, 0), (OFF_KA + g * 64, 64, 512), (OFF_VA + g * 64, 64, 576)]
                for sl in range(4):
                    for off, wd, dst in offs:
                        S.dma('pool', lambda e: e.dma_start(out=Wg[sl][:, :, dst:dst + wd],
                                                            in_=w_in[sl * 512:(sl + 1) * 512, off:off + wd].rearrange("(k p) n -> p k n", p=128)),
                              writes=[Wg[sl]])
                for ci in range(9):
                    tok = slice(ci * 128, (ci + 1) * 128)
                    if ci >= 1:
                        for k in range(KC):
                            S.op('pe', lambda e: e.matmul(pq0[:], lhsT=hT_all[:, k, tok], rhs=Wg[k // 4][:, k % 4, 0:512], start=(k == 0), stop=(k == KC - 1)),
                                 reads=[hT_all, Wg[k // 4]], writes=[pq0])
                        S.op('act', lambda e: e.mul(out=qb[:], in_=pq0[:], mul=0.125), reads=[pq0], writes=[qb])
                    for k in range(KC):
                        S.op('pe', lambda e: e.matmul(pq1[:, 0:128], lhsT=hT_all[:, k, tok], rhs=Wg[k // 4][:, k % 4, 512:640], start=(k == 0), stop=(k == KC - 1)),
                             reads=[hT_all, Wg[k // 4]], writes=[pq1])
                    S.op('dve', lambda e: e.tensor_copy(out=k2[:, 0, 0:64], in_=pq1[:, 0:64]), reads=[pq1], writes=[k2])
                    S.op('dve', lambda e: e.tensor_copy(out=k2[:, 1, 64:128], in_=pq1[:, 0:64]), reads=[pq1], writes=[k2])
                    S.op('dve', lambda e: e.tensor_copy(out=v_all[:, ci, :], in_=pq1[:, 64:128]), reads=[pq1], writes=[v_all])
                    if ci >= 1:
                        for i in range(4):
                            transpose_bf(pTa, pTav[:, i * 128:(i + 1) * 128], qb, qb[:, i * 128:(i + 1) * 128])
                    transpose_bf(pTa, pTav[:, 512:640], k2, k2[:, 0, :])
                    transpose_bf(pTa, pTav[:, 640:768], k2, k2[:, 1, :])
                    if ci >= 1:
                        S.op('act', lambda e: e.copy(out=qT[:, :, (ci - 1) * 128:ci * 128], in_=pTav[:, 0:512].rearrange("p (a b) -> p a b", a=4)),
                             reads=[pTa], writes=[qT])
                    S.op('act', lambda e: e.copy(out=kT_all[:, :, tok], in_=pTav[:, 512:768].rearrange("p (a b) -> p a b", a=2)), reads=[pTa], writes=[kT_all])
                import os
                ATS = int(os.environ.get('ATS', '9'))
                for n in range(NOWN if ATS >= 2 else 0):
                    mk = mask0t if n == 0 else maskt
                    for hh in range(2):
                        for j4 in range(4):
                            j = hh * 4 + j4; i = j // 2; r0 = (j % 2) * 64
                            pb = psc[j4 // 2]
                            S.op('pe', lambda e: e.matmul(pb[:, (j4 % 2) * 256:(j4 % 2 + 1) * 256], lhsT=qT[:, i, n * 128:(n + 1) * 128],
                                                          rhs=kT_all[:, j % 2, n * 128:n * 128 + 256], start=True, stop=True),
                                 reads=[qT, kT_all], writes=[pb])
                        for bnk in range(2):
                            S.op('dve', lambda e: e.tensor_tensor(out=S_sb[:, hh * 4 + bnk * 2:hh * 4 + bnk * 2 + 2, :],
                                                                  in0=psc[bnk][:, :].rearrange("p (a b) -> p a b", a=2),
                                                                  in1=mk[:, :].unsqueeze(1).to_broadcast([128, 2, 256]), op=ALU.add),
                                 reads=[psc[bnk], mk], writes=[S_sb])
                    ATQ = int(os.environ.get('ATQ', '9'))
                    if ATQ < 2:
                        continue
                    S.op('dve', lambda e: e.tensor_reduce(out=mx8[:], in_=S_sb[:], axis=AX.X, op=ALU.max), reads=[S_sb], writes=[mx8])
                    S.op('dve', lambda e: e.tensor_tensor(out=mx8[:], in0=mx8[:], in1=sinkb[:, g * 8:(g + 1) * 8], op=ALU.max), reads=[mx8, sinkb], writes=[mx8])
                    S.op('dve', lambda e: e.tensor_scalar(out=negm[:], in0=mx8[:], scalar1=-1.0, scalar2=None, op0=ALU.mult), reads=[mx8], writes=[negm])
                    if ATQ < 3:
                        continue
                    for j in range(8):
                        S.op('act', lambda e: e.activation(out=P_bf[:, j, :], in_=S_sb[:, j, :], func=AF.Exp, bias=negm[:, j:j + 1], scale=1.0,
                                                           accum_out=rs[:, j:j + 1]), reads=[S_sb, negm], writes=[P_bf, rs])
                    if ATQ < 4:
                        continue
                    S.op('dve', lambda e: e.tensor_tensor(out=es8[:], in0=sinkb[:, g * 8:(g + 1) * 8], in1=mx8[:], op=ALU.subtract), reads=[sinkb, mx8], writes=[es8])
                    S.op('act', lambda e: e.activation(out=es8[:], in_=es8[:], func=AF.Exp), reads=[es8], writes=[es8])
                    S.op('dve', lambda e: e.tensor_tensor(out=es8[:], in0=es8[:], in1=rs[:], op=ALU.add), reads=[es8, rs], writes=[es8])
                    S.op('dve', lambda e: e.reciprocal(out=rden[:], in_=es8[:]), reads=[es8], writes=[rden])
                    if ATS < 3:
                        continue
                    for hh in range(2):
                        for j4 in range(4):
                            for t in range(2):
                                transpose_bf(pPT, pPTv[:, (j4 * 2 + t) * 128:(j4 * 2 + t + 1) * 128], P_bf, P_bf[:, hh * 4 + j4, t * 128:(t + 1) * 128])
                        S.op('act', lambda e: e.copy(out=PT[:, hh * 4:(hh + 1) * 4, :, :], in_=pPTv[:, 0:1024].rearrange("p (a t b) -> p a t b", a=4, t=2)),
                             reads=[pPT], writes=[PT])
                    for j in range(8):
                        for t in range(2):
                            S.op('pe', lambda e: e.matmul(ppv[:, j * 64:(j + 1) * 64], lhsT=PT[:, j, t, :], rhs=v_all[:, n + t, :], start=(t == 0), stop=(t == 1)),
                                 reads=[PT, v_all], writes=[ppv])
                    S.op('dve', lambda e: e.tensor_tensor(out=at_tok[:], in0=ppv[:, :].rearrange("p (a b) -> p a b", a=8),
                                                          in1=rden[:, :].unsqueeze(2).to_broadcast([128, 8, 64]), op=ALU.mult), reads=[ppv, rden], writes=[at_tok])
                    for i in range(4):
                        transpose_bf(pTa, pTav[:, i * 128:(i + 1) * 128], at_tok, at_tok[:].rearrange("p a b -> p (a b)")[:, i * 128:(i + 1) * 128])
                    S.op('act', lambda e: e.copy(out=attnT[:, g * 4:(g + 1) * 4, n * 128:(n + 1) * 128], in_=pTav[:, 0:512].rearrange("p (a b) -> p a b", a=4)),
                         reads=[pTa], writes=[attnT])
            S.barrier()
        if stop_after == 'attn':
            o = dout("d_attnT", [128, KC, 1024], BF16)
            st(T(o, 'o'), o[:, :, :], attnT, attnT[:], is_out=True)
            S.finish()
            return nc, list(dbgo)
        with ExitStack() as ph:
            mT = S.sb([128, KC, 512], BF16, 'mT', ph)
            Wm = [S.sb([128, KC, 128], BF16, 'Wm%d' % i, ph) for i in range(4)]
            Wo = [S.sb([128, 4, 512], BF16, 'Wo%d' % i, ph) for i in range(4)]
            g1b = S.sb([128, D], F32, 'g1b', ph); ld(g1b, g1b[:], modd[2:3, :].partition_broadcast(128), reads=[modd])
            sga = S.sb([128, 512], F32, 'sga', ph); sgr = S.sb([128, 512], F32, 'sgr', ph)
            rr = [S.sb([128, 512], F32, 'rr%d' % i, ph) for i in range(2)]
            xs = [S.sb([128, 512], F32, 'xs%d' % i, ph) for i in range(2)]
            pA = S.ps([128, 512], F32, 'pA', ph); pR = S.ps([128, 512], F32, 'pR', ph)
            pGa = S.ps([128, 512], F32, 'pGa', ph); pGr = S.ps([128, 512], F32, 'pGr', ph)
            po2 = [S.ps([128, 512], F32, 'po2%d' % i, ph) for i in range(2)]
            for th in range(2):
                tsl = slice(th * 512, (th + 1) * 512)
                hsl = slice(128 + th * 512, 128 + (th + 1) * 512)
                for cg in range(16):
                    for wi in range(4):
                        r0 = (wi * 16 + cg) * 128
                        S.dma('pool', lambda e: e.dma_start(out=Wm[wi][:].rearrange("p k n -> p (k n)"), in_=wm_t[r0:r0 + 128, :]), writes=[Wm[wi]])
                    for jj in range(1):
                        csl = slice(0, 128)
                        for pb, wt, act_T, asl in ((pA, Wm[0], attnT, tsl), (pR, Wm[1], retT, tsl), (pGa, Wm[2], hT_all, hsl), (pGr, Wm[3], hT_all, hsl)):
                            for k in range(KC):
                                S.op('pe', lambda e: e.matmul(pb[:], lhsT=wt[:, k, csl], rhs=act_T[:, k, asl], start=(k == 0), stop=(k == KC - 1)),
                                     reads=[wt, act_T], writes=[pb])
                        S.op('act', lambda e: e.activation(out=sga[:], in_=pGa[:], func=AF.Sigmoid), reads=[pGa], writes=[sga])
                        S.op('act', lambda e: e.activation(out=sgr[:], in_=pGr[:], func=AF.Sigmoid), reads=[pGr], writes=[sgr])
                        S.op('dve', lambda e: e.tensor_tensor(out=sga[:], in0=sga[:], in1=pA[:], op=ALU.mult), reads=[sga, pA], writes=[sga])
                        S.op('dve', lambda e: e.tensor_tensor(out=sgr[:], in0=sgr[:], in1=pR[:], op=ALU.mult), reads=[sgr, pR], writes=[sgr])
                        S.op('dve', lambda e: e.tensor_tensor(out=mT[:, cg, :], in0=sga[:], in1=sgr[:], op=ALU.add), reads=[sga, sgr], writes=[mT])
                it = 0
                for nb in range(4):
                    for kq in range(4):
                        S.dma('pool', lambda e: e.dma_start(out=Wo[kq][:], in_=w_o[kq * 512:(kq + 1) * 512, nb * 512:(nb + 1) * 512].rearrange("(k p) n -> p k n", p=128)),
                              writes=[Wo[kq]])
                    for c4 in range(4):
                        n = th * 4 + c4
                        pb = po2[it % 2]; r_ = rr[it % 2]; x_ = xs[it % 2]
                        ld(x_, x_[:], xo[n * 128:(n + 1) * 128, nb * 512:(nb + 1) * 512])
                        for k in range(KC):
                            S.op('pe', lambda e: e.matmul(pb[:], lhsT=mT[:, k, c4 * 128:(c4 + 1) * 128], rhs=Wo[k // 4][:, k % 4, :], start=(k == 0), stop=(k == KC - 1)),
                                 reads=[mT, Wo[k // 4]], writes=[pb])
                        S.op('dve', lambda e: e.tensor_tensor(out=r_[:], in0=pb[:], in1=g1b[:, nb * 512:(nb + 1) * 512], op=ALU.mult), reads=[pb, g1b], writes=[r_])
                        S.op('dve', lambda e: e.tensor_tensor(out=r_[:], in0=r_[:], in1=x_[:], op=ALU.add), reads=[r_, x_], writes=[r_])
                        st(xmid, xmid[n * 128:(n + 1) * 128, nb * 512:(nb + 1) * 512], r_, r_[:])
                        it += 1
            S.barrier()
        mx.close()
        S.barrier()
        if stop_after == 'mix':
            o = dout("d_xmid", [1024, D])
            t = S.sb([128, NOWN, D], F32, 'dbgx')
            ld(t, t[:], xmid[:, :].rearrange("(n p) d -> p n d", p=128), reads=[xmid])
            st(T(o, 'o'), o[:, :].rearrange("(n p) d -> p n d", p=128), t, t[:], is_out=True)
            S.finish()
            return nc, list(dbgo)
        moe = es.enter_context(ExitStack())
        slot_i = S.sb([128, 2, NOWN], I32, 'slot_i', moe)
        idxg_i = S.sb([128, NOV, 16], I32, 'idxg_i', moe)
        idxd_i = S.sb([128, NOV, 8], I32, 'idxd_i', moe)
        ss = S.sb([128, 1], F32, 'ss_m', moe); rstd = S.sb([128, 1], F32, 'rstd_m', moe)
        with ExitStack() as ph:
            A2b = S.sb([128, D], F32, 'A2b', ph); ld(A2b, A2b[:], modd[4:5, :].partition_broadcast(128), reads=[modd])
            sh2b = S.sb([128, D], F32, 'sh2b', ph); ld(sh2b, sh2b[:], modd[3:4, :].partition_broadcast(128), reads=[modd])
            Wr_sb = S.sb([128, KC, 68], F32, 'Wr_sb', ph); ld(Wr_sb, Wr_sb[:], w_rt[:, :].rearrange("(k p) n -> p k n", p=128))
            brb = S.sb([128, 68], F32, 'brb', ph); ld(brb, brb[:], b_rt[0:1, :].partition_broadcast(128))
            zt = S.sb([1, D], BF16, 'zt', ph)
            S.op('dve', lambda e: e.memset(zt[:], 0.0), writes=[zt])
            st(H2, H2[1024:1025, :], zt, zt[:])
            xm = [S.sb([128, D], F32, 'xm%d' % i, ph) for i in range(2)]
            h2b = S.sb([128, D], BF16, 'h2b', ph)
            h2T = S.sb([128, KC, 128], F32, 'h2T', ph)
            lg = S.sb([128, 68], F32, 'lg', ph)
            sm = {k: S.sb([128, 1], F32, 'sm_' + k, ph) for k in ('gmax', 'negg', 'gsum', 'gw', 'm1', 'm2', 'd', 'p1')}
            ohg = S.sb([128, 4], F32, 'ohg', ph); gej = S.sb([128, 4], F32, 'gej', ph)
            t416 = S.sb([128, 4, 16], F32, 't416', ph)
            el = S.sb([128, 16], F32, 'el', ph); el2 = S.sb([128, 16], F32, 'el2', ph)
            oh1 = S.sb([128, 16], F32, 'oh1', ph); oh2 = S.sb([128, 16], F32, 'oh2', ph)
            OH = [S.sb([128, NOWN, 64], F32, 'OH%d' % i, ph) for i in range(2)]
            wts = S.sb([128, 2, NOWN], F32, 'wts', ph)
            Cb = S.sb([128, NOWN, 64], BF16, 'Cb', ph)
            pTf = [S.ps([128, 512], F32, 'pTf%d' % i, ph) for i in range(2)]
            plg = S.ps([128, 512], F32, 'plg', ph)
            pPC = S.ps([128, 512], F32, 'pPC', ph); pcnt = S.ps([128, 512], F32, 'pcnt', ph)
            for n in range(NOWN):
                x_ = xm[n % 2]
                ld(x_, x_[:], xmid[n * 128:(n + 1) * 128, :], reads=[xmid])
                rms_rstd(x_, x_[:], h2b, ss, rstd)
                S.op('dve', lambda e: e.scalar_tensor_tensor(out=x_[:], in0=x_[:], scalar=rstd[:, 0:1], in1=A2b[:], op0=ALU.mult, op1=ALU.mult),
                     reads=[x_, rstd, A2b], writes=[x_])
                S.op('dve', lambda e: e.tensor_tensor(out=x_[:], in0=x_[:], in1=sh2b[:], op=ALU.add), reads=[x_, sh2b], writes=[x_])
                S.op('act', lambda e: e.copy(out=h2b[:], in_=x_[:]), reads=[x_], writes=[h2b])
                st(H2, H2[n * 128:(n + 1) * 128, :], h2b, h2b[:])
                for grp in range(4):
                    pb = pTf[grp % 2]
                    for kk in range(4):
                        k = grp * 4 + kk
                        S.op('pe', lambda e: e.matmul(pb[:, kk * 128:(kk + 1) * 128], lhsT=x_[:, k * 128:(k + 1) * 128], rhs=idf[:], start=True, stop=True),
                             reads=[x_, idf], writes=[pb])
                    S.op('act', lambda e: e.copy(out=h2T[:, grp * 4:(grp + 1) * 4, :], in_=pb[:, :].rearrange("p (a b) -> p a b", a=4)), reads=[pb], writes=[h2T])
                for k in range(KC):
                    S.op('pe', lambda e: e.matmul(plg[:, 0:68], lhsT=h2T[:, k, :], rhs=Wr_sb[:, k, :], start=(k == 0), stop=(k == KC - 1)),
                         reads=[h2T, Wr_sb], writes=[plg])
                S.op('dve', lambda e: e.tensor_tensor(out=lg[:], in0=plg[:, 0:68], in1=brb[:], op=ALU.add), reads=[plg, brb], writes=[lg])
                S.op('dve', lambda e: e.tensor_reduce(out=sm['gmax'][:], in_=lg[:, 0:4], axis=AX.X, op=ALU.max), reads=[lg], writes=[sm['gmax']])
                S.op('dve', lambda e: e.tensor_scalar(out=ohg[:], in0=lg[:, 0:4], scalar1=sm['gmax'][:, 0:1], scalar2=None, op0=ALU.is_ge), reads=[lg, sm['gmax']], writes=[ohg])
                S.op('dve', lambda e: e.tensor_scalar(out=sm['negg'][:], in0=sm['gmax'][:], scalar1=-1.0, scalar2=None, op0=ALU.mult), reads=[sm['gmax']], writes=[sm['negg']])
                S.op('act', lambda e: e.activation(out=gej[:], in_=lg[:, 0:4], func=AF.Exp, bias=sm['negg'][:, 0:1], scale=1.0, accum_out=sm['gsum'][:]),
                     reads=[lg, sm['negg']], writes=[gej, sm['gsum']])
                S.op('dve', lambda e: e.reciprocal(out=sm['gw'][:], in_=sm['gsum'][:]), reads=[sm['gsum']], writes=[sm['gw']])
                S.op('dve', lambda e: e.tensor_tensor(out=t416[:], in0=lg[:, 4:68].rearrange("p (g e) -> p g e", g=4),
                                                      in1=ohg[:, :].unsqueeze(2).to_broadcast([128, 4, 16]), op=ALU.mult), reads=[lg, ohg], writes=[t416])
                S.op('dve', lambda e: e.tensor_reduce(out=el[:], in_=t416[:].rearrange("p g e -> p e g"), axis=AX.X, op=ALU.add), reads=[t416], writes=[el])
                S.op('dve', lambda e: e.tensor_reduce(out=sm['m1'][:], in_=el[:], axis=AX.X, op=ALU.max), reads=[el], writes=[sm['m1']])
                S.op('dve', lambda e: e.tensor_scalar(out=oh1[:], in0=el[:], scalar1=sm['m1'][:, 0:1], scalar2=None, op0=ALU.is_ge), reads=[el, sm['m1']], writes=[oh1])
                S.op('dve', lambda e: e.scalar_tensor_tensor(out=el2[:], in0=oh1[:], scalar=-1e30, in1=el[:], op0=ALU.mult, op1=ALU.add), reads=[oh1, el], writes=[el2])
                S.op('dve', lambda e: e.tensor_reduce(out=sm['m2'][:], in_=el2[:], axis=AX.X, op=ALU.max), reads=[el2], writes=[sm['m2']])
                S.op('dve', lambda e: e.tensor_scalar(out=oh2[:], in0=el2[:], scalar1=sm['m2'][:, 0:1], scalar2=None, op0=ALU.is_ge), reads=[el2, sm['m2']], writes=[oh2])
                S.op('dve', lambda e: e.tensor_tensor(out=sm['d'][:], in0=sm['m2'][:], in1=sm['m1'][:], op=ALU.subtract), reads=[sm['m1'], sm['m2']], writes=[sm['d']])
                S.op('act', lambda e: e.activation(out=sm['d'][:], in_=sm['d'][:], func=AF.Exp), reads=[sm['d']], writes=[sm['d']])
                S.op('dve', lambda e: e.tensor_scalar(out=sm['d'][:], in0=sm['d'][:], scalar1=1.0, scalar2=None, op0=ALU.add), reads=[sm['d']], writes=[sm['d']])
                S.op('dve', lambda e: e.reciprocal(out=sm['p1'][:], in_=sm['d'][:]), reads=[sm['d']], writes=[sm['p1']])
                S.op('dve', lambda e: e.tensor_tensor(out=wts[:, 0, n:n + 1], in0=sm['p1'][:], in1=sm['gw'][:], op=ALU.mult), reads=[sm['p1'], sm['gw']], writes=[wts])
                S.op('dve', lambda e: e.tensor_tensor(out=wts[:, 1, n:n + 1], in0=sm['gw'][:], in1=wts[:, 0, n:n + 1], op=ALU.subtract), reads=[sm['gw'], wts], writes=[wts])
                for kk, oh in ((0, oh1), (1, oh2)):
                    S.op('dve', lambda e: e.tensor_tensor(out=OH[kk][:, n, :].rearrange("p (g e) -> p g e", g=4),
                                                          in0=ohg[:, :].unsqueeze(2).to_broadcast([128, 4, 16]),
                                                          in1=oh[:, :].unsqueeze(1).to_broadcast([128, 4, 16]), op=ALU.mult), reads=[ohg, oh], writes=[OH[kk]])
                S.op('dve', lambda e: e.tensor_tensor(out=Cb[:, n, :], in0=OH[0][:, n, :], in1=OH[1][:, n, :], op=ALU.add), reads=[OH[0], OH[1]], writes=[Cb])
            ones_bf = S.sb([128, 128], BF16, 'ones_bf', ph)
            S.op('dve', lambda e: e.memset(ones_bf[:], 1.0), writes=[ones_bf])
            Lf = S.sb([128, 128], F32, 'Lf', ph); ld(Lf, Lf[:], Lstd[:, :])
            Lb = S.sb([128, 128], BF16, 'Lb', ph)
            S.op('dve', lambda e: e.tensor_copy(out=Lb[:], in_=Lf[:]), reads=[Lf], writes=[Lb])
            for n in range(NOWN):
                for m in range(n + 1):
                    S.op('pe', lambda e: e.matmul(pPC[:, n * 64:(n + 1) * 64], lhsT=(Lb[:] if m == n else ones_bf[:]), rhs=Cb[:, m, :], start=(m == 0), stop=(m == n)),
                         reads=[Lb, ones_bf, Cb], writes=[pPC])
            for m in range(NOWN):
                S.op('pe', lambda e: e.matmul(pcnt[:, 0:64], lhsT=ones_bf[:], rhs=Cb[:, m, :], start=(m == 0), stop=(m == NOWN - 1)), reads=[ones_bf, Cb], writes=[pcnt])
            CAP = int(os.environ.get('OVCAP', 128))
            e128 = S.sb([128, 64], F32, 'e128', ph); ld(e128, e128[:], e128d[:, :])
            ocf = S.sb([128, 64], F32, 'ocf', ph); cnti = S.sb([128, 64], I32, 'cnti', ph); padf = S.sb([128, 64], F32, 'padf', ph)
            S.op('dve', lambda e: e.tensor_scalar(out=ocf[:], in0=pcnt[:, 0:64], scalar1=-float(CAP), scalar2=0.0, op0=ALU.add, op1=ALU.max), reads=[pcnt], writes=[ocf])
            S.op('dve', lambda e: e.tensor_scalar(out=ocf[:], in0=ocf[:], scalar1=127.0, scalar2=None, op0=ALU.add), reads=[ocf], writes=[ocf])
            S.op('dve', lambda e: e.tensor_copy(out=cnti[:], in_=ocf[:]), reads=[ocf], writes=[cnti])
            S.op('dve', lambda e: e.tensor_scalar(out=cnti[:], in0=cnti[:], scalar1=7, scalar2=7, op0=ALU.arith_shift_right, op1=ALU.logical_shift_left), reads=[cnti], writes=[cnti])
            S.op('dve', lambda e: e.tensor_copy(out=padf[:], in_=cnti[:]), reads=[cnti], writes=[padf])
            cs = [S.sb([128, 64], F32, 'cs%d' % i, ph) for i in range(2)]
            S.op('dve', lambda e: e.tensor_copy(out=cs[0][:], in_=padf[:]), reads=[padf], writes=[cs[0]])
            cur = 0
            for s_ in (1, 2, 4, 8, 16, 32):
                a, b_ = cs[cur], cs[1 - cur]
                S.op('dve', lambda e: e.tensor_copy(out=b_[:, 0:s_], in_=a[:, 0:s_]), reads=[a], writes=[b_])
                S.op('dve', lambda e: e.tensor_tensor(out=b_[:, s_:64], in0=a[:, s_:64], in1=a[:, 0:64 - s_], op=ALU.add), reads=[a], writes=[b_])
                cur = 1 - cur
            pend = cs[cur]; ob = cs[1 - cur]
            S.op('dve', lambda e: e.tensor_tensor(out=ob[:], in0=pend[:], in1=padf[:], op=ALU.subtract), reads=[pend, padf], writes=[ob])
            S.op('dve', lambda e: e.tensor_scalar(out=ob[:], in0=ob[:], scalar1=float(64 * 128 - CAP), scalar2=None, op0=ALU.add), reads=[ob], writes=[ob])
            slot_f = S.sb([128, 2, NOWN], F32, 'slot_f', ph)
            tmpb = S.sb([128, NOWN, 64], F32, 'tmpb', ph)
            rk = S.sb([128, NOWN], F32, 'rk', ph); eb = S.sb([128, NOWN], F32, 'eb', ph); obk = S.sb([128, NOWN], F32, 'obk', ph); isov = S.sb([128, NOWN], F32, 'isov', ph)
            for kk in range(2):
                S.op('dve', lambda e: e.tensor_tensor(out=tmpb[:], in0=OH[kk][:], in1=pPC[:, :].rearrange("p (n e) -> p n e", n=NOWN), op=ALU.mult), reads=[OH[kk], pPC], writes=[tmpb])
                S.op('dve', lambda e: e.tensor_reduce(out=rk[:], in_=tmpb[:], axis=AX.X, op=ALU.add), reads=[tmpb], writes=[rk])
                S.op('dve', lambda e: e.tensor_tensor(out=tmpb[:], in0=OH[kk][:], in1=e128[:, :].unsqueeze(1).to_broadcast([128, NOWN, 64]), op=ALU.mult), reads=[OH[kk], e128], writes=[tmpb])
                S.op('dve', lambda e: e.tensor_reduce(out=eb[:], in_=tmpb[:], axis=AX.X, op=ALU.add), reads=[tmpb], writes=[eb])
                S.op('dve', lambda e: e.tensor_tensor(out=tmpb[:], in0=OH[kk][:], in1=ob[:, :].unsqueeze(1).to_broadcast([128, NOWN, 64]), op=ALU.mult), reads=[OH[kk], ob], writes=[tmpb])
                S.op('dve', lambda e: e.tensor_reduce(out=obk[:], in_=tmpb[:], axis=AX.X, op=ALU.add), reads=[tmpb], writes=[obk])
                S.op('dve', lambda e: e.tensor_scalar(out=isov[:], in0=rk[:], scalar1=float(CAP), scalar2=None, op0=ALU.is_ge), reads=[rk], writes=[isov])
                S.op('dve', lambda e: e.tensor_tensor(out=obk[:], in0=obk[:], in1=eb[:], op=ALU.subtract), reads=[obk, eb], writes=[obk])
                S.op('dve', lambda e: e.tensor_tensor(out=obk[:], in0=obk[:], in1=isov[:], op=ALU.mult), reads=[obk, isov], writes=[obk])
                S.op('dve', lambda e: e.tensor_tensor(out=rk[:], in0=rk[:], in1=eb[:], op=ALU.add), reads=[rk, eb], writes=[rk])
                S.op('dve', lambda e: e.tensor_tensor(out=slot_f[:, kk, :], in0=rk[:], in1=obk[:], op=ALU.add), reads=[rk, obk], writes=[slot_f])
            S.op('dve', lambda e: e.tensor_copy(out=slot_i[:], in_=slot_f[:]), reads=[slot_f], writes=[slot_i])
            blk128 = S.sb([128, NOV], F32, 'blk128', ph); ld(blk128, blk128[:], blk128d[:, 0:NOV])
            kcoff = S.sb([128, 16], F32, 'kcoff', ph); ld(kcoff, kcoff[:], kcoffd[:, :])
            pidx = S.sb([128, 1], F32, 'pidx', ph); ld(pidx, pidx[:], pidxd[:, :])
            cmp = S.sb([128, NOV, 64], BF16, 'cmp', ph)
            S.op('dve', lambda e: e.tensor_tensor(out=cmp[:], in0=pend[:, :].unsqueeze(1).to_broadcast([128, NOV, 64]),
                                                  in1=blk128[:, :].unsqueeze(2).to_broadcast([128, NOV, 64]), op=ALU.is_le), reads=[pend, blk128], writes=[cmp])
            bef = S.sb([128, NOV], F32, 'bef', ph); gb = S.sb([128, NOV], F32, 'gb', ph)
            S.op('dve', lambda e: e.tensor_reduce(out=bef[:], in_=cmp[:], axis=AX.X, op=ALU.add), reads=[cmp], writes=[bef])
            skipo = S.sb([128, NOV], F32, 'skipo', ph)
            S.op('dve', lambda e: e.tensor_scalar(out=skipo[:], in0=bef[:], scalar1=64.0, scalar2=float(2 ** 27), op0=ALU.is_ge, op1=ALU.mult), reads=[bef], writes=[skipo])
            S.op('dve', lambda e: e.tensor_scalar(out=bef[:], in0=bef[:], scalar1=63.0, scalar2=None, op0=ALU.min), reads=[bef], writes=[bef])
            idxf = S.sb([128, NOV, 16], F32, 'idxf', ph)
            for mult, nk, dst in ((2048.0, 16, idxg_i), (1024.0, 8, idxd_i)):
                S.op('dve', lambda e: e.tensor_scalar(out=gb[:], in0=bef[:], scalar1=mult, scalar2=pidx[:, 0:1], op0=ALU.mult, op1=ALU.add), reads=[bef, pidx], writes=[gb])
                S.op('dve', lambda e: e.tensor_tensor(out=gb[:], in0=gb[:], in1=skipo[:], op=ALU.add), reads=[gb, skipo], writes=[gb])
                S.op('dve', lambda e: e.tensor_tensor(out=idxf[:, :, 0:nk], in0=gb[:, :].unsqueeze(2).to_broadcast([128, NOV, nk]),
                                                      in1=kcoff[:, 0:nk].unsqueeze(1).to_broadcast([128, NOV, nk]), op=ALU.add), reads=[gb, kcoff], writes=[idxf])
                S.op('dve', lambda e: e.tensor_copy(out=dst[:], in_=idxf[:, :, 0:nk]), reads=[idxf], writes=[dst])
            ri0 = S.sb([128, NBLK, 16], I32, 'ri0', ph)
            S.op('dve', lambda e: e.memset(ri0[:], 0), writes=[ri0])
            S.op('dve', lambda e: e.memset(ri0[:, :, 0:1], 1024), writes=[ri0])
            st(rinfo, rinfo[:, :].rearrange("(b p) c -> p b c", p=128), ri0, ri0[:])
            tokid = S.sb([128, NOWN], I32, 'tokid', ph); ld(tokid, tokid[:], tokidd[:, :])
            ris = [S.sb([128, 16], I32, 'ri%d' % i, ph) for i in range(4)]
            for r_ in ris:
                S.op('dve', lambda e: e.memset(r_[:], 0), writes=[r_])
            it = 0
            for n in range(NOWN):
                for kk in range(2):
                    r_ = ris[it % 4]
                    S.op('dve', lambda e: e.tensor_copy(out=r_[:, 0:1], in_=tokid[:, n:n + 1]), reads=[tokid], writes=[r_])
                    S.op('dve', lambda e: e.tensor_copy(out=r_[:, 1:2].bitcast(F32), in_=wts[:, kk, n:n + 1]), reads=[wts], writes=[r_])
                    S.dma('pool', lambda e: e.indirect_dma_start(out=rinfo[:, :], out_offset=bass.IndirectOffsetOnAxis(ap=slot_i[:, kk, n:n + 1], axis=0),
                                                                 in_=r_[:], in_offset=None), reads=[r_, slot_i], writes=[rinfo])
                    it += 1
            S.barrier()
        if stop_after == 'moe_route':
            o1 = dout("d_slot", [128, 2, NOWN], I32); st(T(o1, 'o'), o1[:, :, :], slot_i, slot_i[:], is_out=True)
            o2 = dout("d_idxg", [128, NOV, 16], I32); st(T(o2, 'o'), o2[:, :, :], idxg_i, idxg_i[:], is_out=True)
            o3 = dout("d_rinfo", [NBLK * 128, 16], I32)
            t = S.sb([128, NBLK, 16], I32, 'dbgr')
            ld(t, t[:], rinfo[:, :].rearrange("(b p) c -> p b c", p=128), reads=[rinfo])
            st(T(o3, 'o'), o3[:, :].rearrange("(b p) c -> p b c", p=128), t, t[:], is_out=True)
            S.finish()
            return nc, list(dbgo)
        with ExitStack() as ph:
            Wg_t = [S.sb([128, 1024], BF16, 'Wg_t%d' % i, ph) for i in range(16)]
            Wu_t = [S.sb([128, 1024], BF16, 'Wu_t%d' % i, ph) for i in range(16)]
            Wd_t = [S.sb([128, D], BF16, 'Wd_t%d' % i, ph) for i in range(8)]
            rt_all = S.sb([128, NBLK, 16], I32, 'rt_all', ph)
            ld(rt_all, rt_all[:], rinfo[:, :].rearrange("(b p) c -> p b c", p=128), reads=[rinfo])
            xgs = [S.sb([128, D], BF16, 'xg%d' % i, ph) for i in range(2)]
            xgT = S.sb([128, KC, 128], BF16, 'xgT', ph)
            sg = S.sb([128, 1024], F32, 'sg', ph)
            actb = S.sb([128, 1024], BF16, 'actb', ph)
            actT = S.sb([128, 8, 128], BF16, 'actT', ph)
            ybs = [S.sb([128, D], F32, 'yb%d' % i, ph) for i in range(2)]
            pT1 = S.ps([128, 512], F32, 'pT1', ph); pT1v = pT1[:, :].bitcast(BF16)
            pT2 = S.ps([128, 512], F32, 'pT2', ph); pT2v = pT2[:, :].bitcast(BF16)
            pgu = [S.ps([128, 512], F32, 'pgu%d' % i, ph) for i in range(4)]
            pdn = [S.ps([128, 512], F32, 'pdn%d' % i, ph) for i in range(2)]
            NB_RUN = int(os.environ.get('NB_RUN', NBLK))
            bc_g = nc.gpsimd.to_reg(64 * D - 1); bc_d = nc.gpsimd.to_reg(64 * 1024 - 1)
            for b in range(NB_RUN):
                rt = rt_all; xg = xgs[b % 2]; yb = ybs[b % 2]
                S.dma('pool', lambda e: e.indirect_dma_start(out=xg[:], out_offset=None, in_=H2[:, :],
                                                             in_offset=bass.IndirectOffsetOnAxis(ap=rt_all[:, b, 0:1], axis=0)), reads=[rt_all, H2], writes=[xg])
                if b < 64:
                    for kc in range(16):
                        r0 = b * 2048 + kc * 128
                        S.dma('pool', lambda e: e.dma_start(out=Wg_t[kc][:], in_=w_eg[r0:r0 + 128, :]), writes=[Wg_t[kc]])
                        S.dma('pool', lambda e: e.dma_start(out=Wu_t[kc][:], in_=w_eu[r0:r0 + 128, :]), writes=[Wu_t[kc]])
                    for kc in range(8):
                        r0 = b * 1024 + kc * 128
                        S.dma('pool', lambda e: e.dma_start(out=Wd_t[kc][:], in_=w_ed[r0:r0 + 128, :]), writes=[Wd_t[kc]])
                else:
                    ob_ = b - 64
                    for kc in range(16):
                        S.dma('pool', lambda e: e.indirect_dma_start(out=Wg_t[kc][:], out_offset=None, in_=w_eg[:, :],
                                                                     in_offset=bass.IndirectOffsetOnAxis(ap=idxg_i[:, ob_, kc:kc + 1], axis=0), bounds_check=bc_g, oob_is_err=False), reads=[idxg_i], writes=[Wg_t[kc]])
                        S.dma('pool', lambda e: e.indirect_dma_start(out=Wu_t[kc][:], out_offset=None, in_=w_eu[:, :],
                                                                     in_offset=bass.IndirectOffsetOnAxis(ap=idxg_i[:, ob_, kc:kc + 1], axis=0), bounds_check=bc_g, oob_is_err=False), reads=[idxg_i], writes=[Wu_t[kc]])
                    for kc in range(8):
                        S.dma('pool', lambda e: e.indirect_dma_start(out=Wd_t[kc][:], out_offset=None, in_=w_ed[:, :],
                                                                     in_offset=bass.IndirectOffsetOnAxis(ap=idxd_i[:, ob_, kc:kc + 1], axis=0), bounds_check=bc_d, oob_is_err=False), reads=[idxd_i], writes=[Wd_t[kc]])
                for half in range(2):
                    for kk in range(8):
                        k = half * 8 + kk
                        transpose_bf(pT1, pT1v[:, kk * 128:(kk + 1) * 128], xg, xg[:, k * 128:(k + 1) * 128])
                    S.op('act', lambda e: e.copy(out=xgT[:, half * 8:(half + 1) * 8, :], in_=pT1v[:, 0:1024].rearrange("p (a b) -> p a b", a=8)), reads=[pT1], writes=[xgT])
                for kc in range(16):
                    for wi, wt in ((0, Wg_t[kc]), (1, Wu_t[kc])):
                        for nb in range(2):
                            pb = pgu[wi * 2 + nb]
                            S.op('pe', lambda e: e.matmul(pb[:], lhsT=xgT[:, kc, :], rhs=wt[:, nb * 512:(nb + 1) * 512], start=(kc == 0), stop=(kc == 15)),
                                 reads=[xgT, wt], writes=[pb])
                for nb in range(2):
                    S.op('act', lambda e: e.activation(out=sg[:, nb * 512:(nb + 1) * 512], in_=pgu[nb][:], func=AF.Silu), reads=[pgu[nb]], writes=[sg])
                    S.op('dve', lambda e: e.tensor_tensor(out=actb[:, nb * 512:(nb + 1) * 512], in0=sg[:, nb * 512:(nb + 1) * 512], in1=pgu[2 + nb][:], op=ALU.mult),
                         reads=[sg, pgu[2 + nb]], writes=[actb])
                for kk in range(8):
                    transpose_bf(pT2, pT2v[:, kk * 128:(kk + 1) * 128], actb, actb[:, kk * 128:(kk + 1) * 128])
                S.op('act', lambda e: e.copy(out=actT[:], in_=pT2v[:, 0:1024].rearrange("p (a b) -> p a b", a=8)), reads=[pT2], writes=[actT])
                for hf in range(2):
                    for kc in range(8):
                        for i in range(2):
                            c0 = hf * 1024 + i * 512
                            S.op('pe', lambda e: e.matmul(pdn[i][:], lhsT=actT[:, kc, :], rhs=Wd_t[kc][:, c0:c0 + 512], start=(kc == 0), stop=(kc == 7)),
                                 reads=[actT, Wd_t[kc]], writes=[pdn[i]])
                    for i in range(2):
                        c0 = hf * 1024 + i * 512
                        S.op('dve', lambda e: e.tensor_scalar(out=yb[:, c0:c0 + 512], in0=pdn[i][:], scalar1=rt_all[:, b, 1:2].bitcast(F32), scalar2=None, op0=ALU.mult),
                             reads=[pdn[i], rt_all], writes=[yb])
                st(Yd, Yd[b * 128:(b + 1) * 128, :], yb, yb[:])
            S.barrier()
        with ExitStack() as ph:
            g2b = S.sb([128, D], F32, 'g2b', ph); ld(g2b, g2b[:], modd[5:6, :].partition_broadcast(128), reads=[modd])
            gfb = S.sb([128, D], F32, 'gfb', ph); ld(gfb, gfb[:], nfg[0:1, :].partition_broadcast(128))
            xm = [S.sb([128, D], F32, 'xm%d' % i, ph) for i in range(2)]
            y1 = [S.sb([128, D], F32, 'y1%d' % i, ph) for i in range(2)]
            y2 = [S.sb([128, D], F32, 'y2%d' % i, ph) for i in range(2)]
            junkb = S.sb([128, D], BF16, 'junkb', ph)
            for n in range(NOWN):
                x_, a_, b_ = xm[n % 2], y1[n % 2], y2[n % 2]
                ld(x_, x_[:], xmid[n * 128:(n + 1) * 128, :], reads=[xmid])
                for kk, dst in ((0, a_), (1, b_)):
                    S.dma('pool', lambda e: e.indirect_dma_start(out=dst[:], out_offset=None, in_=Yd[:, :],
                                                                 in_offset=bass.IndirectOffsetOnAxis(ap=slot_i[:, kk, n:n + 1], axis=0)), reads=[slot_i, Yd], writes=[dst])
                S.op('dve', lambda e: e.tensor_tensor(out=a_[:], in0=a_[:], in1=b_[:], op=ALU.add), reads=[a_, b_], writes=[a_])
                S.op('dve', lambda e: e.tensor_tensor(out=a_[:], in0=a_[:], in1=g2b[:], op=ALU.mult), reads=[a_, g2b], writes=[a_])
                S.op('dve', lambda e: e.tensor_tensor(out=a_[:], in0=a_[:], in1=x_[:], op=ALU.add), reads=[a_, x_], writes=[a_])
                rms_rstd(a_, a_[:], junkb, ss, rstd)
                S.op('dve', lambda e: e.scalar_tensor_tensor(out=a_[:], in0=a_[:], scalar=rstd[:, 0:1], in1=gfb[:], op0=ALU.mult, op1=ALU.mult),
                     reads=[a_, rstd, gfb], writes=[a_])
                st(T(out, 'out'), out[n * 128:(n + 1) * 128, :], a_, a_[:], is_out=True)
        S.finish()
    return nc, list(dbgo)


def _consts():
    c = {}
    c["ident"] = np.eye(128, dtype=np.float32)
    i = np.arange(128)[:, None]; j = np.arange(256)[None, :]
    valid = (j > i) & (j <= i + 128)
    c["mask"] = np.where(valid, 0.0, -1e30).astype(np.float32)
    gam = np.array(GAM, dtype=np.float64)
    lg = np.log(gam)
    e = np.arange(128)[:, None, None]; cc = np.arange(128)[None, None, :]
    diff = cc - e
    c["decT"] = np.where(diff >= 0, np.exp(np.maximum(diff, 0) * lg[None, :, None]), 0.0).astype(np.float32)
    idx = np.arange(128)[:, None].astype(np.float64)
    c["dq"] = np.exp((idx + 1.0) * lg[None, :]).astype(np.float32)
    c["dk"] = (np.exp((127.0 - idx) * lg[None, :]) / 16.0).astype(np.float32)
    invf = (10000.0 ** (-(np.arange(128, dtype=np.float32) / np.float32(128)))).astype(np.float32)
    c["invf"] = np.broadcast_to(invf[None, :], (128, 128)).copy()
    c["Lst"] = (np.arange(128)[:, None] < np.arange(128)[None, :]).astype(np.float32)
    c["tokid"] = (np.arange(NOWN)[None, :] * 128 + np.arange(128)[:, None]).astype(np.int32)
    c["blk128"] = np.broadcast_to((np.arange(NBLK, dtype=np.float32) * 128.0)[None, :], (128, NBLK)).copy()
    c["kcoff"] = np.broadcast_to((np.arange(16, dtype=np.float32) * 128.0)[None, :], (128, 16)).copy()
    c["pidx"] = np.arange(128, dtype=np.float32)[:, None].copy()
    c["e128"] = np.broadcast_to((np.arange(64, dtype=np.float32) * 128.0)[None, :], (128, 64)).copy()
    return c


def prep_inputs(x, c, positions, norm1_gain, norm2_gain, final_norm_gain, w_ada, b_ada, w_in,
                attn_sinks, ret_norm_gain, w_branch_attn, w_branch_ret, w_out,
                w_router_group, b_router_group, w_router_expert, b_router_expert,
                w_expert_gate, w_expert_up, w_expert_down):
    f = lambda a: np.ascontiguousarray(np.asarray(a))
    x = f(x); c = f(c); positions = f(positions)
    shared = dict(
        w_ada=f(w_ada)[0], b_ada=f(b_ada), w_in=f(w_in)[0], attn_sinks=f(attn_sinks), ret_norm_gain=f(ret_norm_gain),
        w_out=f(w_out)[0],
        w_router=np.concatenate([f(w_router_group)[0], f(w_router_expert)[0]], axis=1),
        b_router=np.concatenate([f(b_router_group), f(b_router_expert)], axis=1),
        w_expert_gate=f(w_expert_gate).reshape(64 * D, 1024), w_expert_up=f(w_expert_up).reshape(64 * D, 1024),
        w_expert_down=f(w_expert_down).reshape(64 * 1024, D),
        norm1_gain=f(norm1_gain), norm2_gain=f(norm2_gain), final_norm_gain=f(final_norm_gain).reshape(1, D),
    )
    def tile_cols(w, off):
        sub = w[:, off:off + D].reshape(KC, 128, 16, 128)
        return np.ascontiguousarray(sub.transpose(2, 1, 0, 3)).reshape(16 * 128, KC * 128)
    w_in0 = shared["w_in"]
    shared["wm_t"] = np.concatenate([tile_cols(f(w_branch_attn)[0], 0), tile_cols(f(w_branch_ret)[0], 0),
                                     tile_cols(w_in0, OFF_GA), tile_cols(w_in0, OFF_GTR)], axis=0)
    shared.update(_consts())
    gam = np.array(GAM, dtype=np.float64); lg = np.log(gam)
    maps = []
    for core in range(8):
        b, q = core // 4, core % 4
        m = dict(shared)
        m["xo"] = x[b, q * 1024:(q + 1) * 1024]
        npre = q * 8
        xp = np.zeros((NPRE * 128, D), np.float32)
        pp = np.zeros((NPRE * 128,), np.int32)
        if npre:
            xp[(NPRE - npre) * 128:] = x[b, :q * 1024]
            pp[(NPRE - npre) * 128:] = positions[b, :q * 1024]
        m["xp"] = xp
        m["pos_o"] = np.ascontiguousarray(positions[b, q * 1024:(q + 1) * 1024].reshape(NOWN, 128).T)
        m["pos_p"] = np.ascontiguousarray(pp.reshape(NPRE, 128).T)
        m["cT"] = np.ascontiguousarray(c[b].reshape(KC, 128).T)
        valid = (np.arange(NPRE) >= NPRE - npre).astype(np.float64)
        idx = np.arange(128)[:, None, None].astype(np.float64)
        jj = np.arange(NPRE)[None, :, None].astype(np.float64)
        pk = np.exp((127.0 - idx) * lg[None, None, :] + 128.0 * (NPRE - 1 - jj) * lg[None, None, :]) / 16.0 * valid[None, :, None]
        m["pk"] = pk.astype(np.float32)
        mk = shared["mask"].copy()
        if q == 0:
            mk[:, :128] = -1e30
        m["mask0"] = mk
        maps.append(m)
    return maps


def kernel(**inputs):
    maps = prep_inputs(**inputs)
    nc, _ = build()
    res = run_bass_kernel_spmd(nc, maps, core_ids=list(range(8)))
    outp = np.zeros((2, 4096, D), np.float32)
    for core in range(8):
        b, q = core // 4, core % 4
        outp[b, q * 1024:(q + 1) * 1024] = res.results[core]["out"]
    return outp
```

```python
import numpy as np
import concourse.bass as bass
import concourse.mybir as mybir
from concourse.bass_utils import run_bass_kernel_spmd

F32 = mybir.dt.float32
BF16 = mybir.dt.bfloat16
I32 = mybir.dt.int32
U32 = mybir.dt.uint32
ALU = mybir.AluOpType
AF = mybir.ActivationFunctionType
AX = mybir.AxisListType


class Tr:
    def __init__(self):
        self.last_w = None
        self.readers = {}


class T:
    def __init__(self, h, name, tr=None):
        self.h = h
        self.name = name
        self.tr = tr or Tr()

    @property
    def last_w(self):
        return self.tr.last_w

    @last_w.setter
    def last_w(self, v):
        self.tr.last_w = v

    @property
    def readers(self):
        return self.tr.readers

    @readers.setter
    def readers(self, v):
        self.tr.readers = v

    def v(self, ap):
        return T(ap, self.name, self.tr)

    def __getitem__(self, k):
        return self.h[k]


class Sched:
    ENG = ('pe', 'dve', 'act', 'pool', 'sp')

    def __init__(self, nc, es):
        self.nc = nc
        self.es = es
        self.eng = {'pe': nc.tensor, 'dve': nc.vector, 'act': nc.scalar, 'pool': nc.gpsimd, 'sp': nc.sync}
        self.ops = {e: [] for e in self.ENG}
        self.cnt = {e: 0 for e in self.ENG}
        self.sem = {e: es.enter_context(nc.semaphore('s_' + e)) for e in self.ENG if e != 'sp'}
        self.seen = {e: {} for e in self.ENG}
        self.NP = 8
        self.dsem = {q: [es.enter_context(nc.semaphore('d_%s%d' % (q, i))) for i in range(self.NP)]
                     for q in ('sp', 'pool', 'act')}
        self.dn = {q: 0 for q in ('sp', 'pool', 'act')}
        self.ntile = 0
        self.out_tokens = []

    def sb(self, shape, dt, name=None, es=None):
        self.ntile += 1
        name = (name or 't') + '_%d' % self.ntile
        h = (es or self.es).enter_context(self.nc.sbuf_tensor(name, list(shape), dt))
        return T(h, name)

    def ps(self, shape, dt, name=None, es=None):
        self.ntile += 1
        name = (name or 'p') + '_%d' % self.ntile
        h = (es or self.es).enter_context(self.nc.psum_tensor(name, list(shape), dt))
        return T(h, name)

    def barrier(self):
        toks = [('eng', f, self.cnt[f]) for f in ('pe', 'dve', 'act', 'pool') if self.cnt[f] > 0]
        for q in ('sp', 'pool', 'act'):
            n = self.dn[q]
            for i in range(max(0, n - self.NP), n):
                toks.append(('dma', self.dsem[q][i % self.NP], 16 * (i // self.NP + 1)))
        for e in self.ENG:
            for tok in toks:
                self._wait(e, tok)

    def dram(self, name, shape, dt, kind='Internal'):
        h = self.nc.dram_tensor(name, list(shape), dt, kind=kind)
        return T(h, name)

    def _wait(self, e, tok):
        if tok is None:
            return
        kind, key, val = tok
        if kind == 'eng' and key == e and e == 'pe':
            return
        if kind == 'eng' and key == 'sp':
            return
        sk = (kind, key if kind == 'eng' else id(key))
        if self.seen[e].get(sk, 0) >= val:
            return
        self.seen[e][sk] = val
        sem = self.sem[key] if kind == 'eng' else key
        eng = self.eng[e]
        eng.wait_ge(sem, val)

    def _deps(self, e, reads, writes):
        for t in reads:
            self._wait(e, t.last_w)
        for t in writes:
            self._wait(e, t.last_w)
            for tok in list(t.readers.values()):
                self._wait(e, tok)

    def _update(self, tok, reads, writes):
        for t in reads:
            kind, key, val = tok
            rk = (kind, key if kind == 'eng' else (id(key)))
            t.readers[rk] = tok
        for t in writes:
            t.last_w = tok
            t.readers = {}

    def op(self, e, fn, reads=(), writes=()):
        assert e in ('pe', 'dve', 'act', 'pool')
        self._deps(e, reads, writes)
        self.cnt[e] += 1
        idx = self.cnt[e]
        eng = self.eng[e]
        sem = self.sem[e]
        fn(eng).then_inc(sem, 1)
        self._update(('eng', e, idx), reads, writes)

    def dma(self, q, fn, reads=(), writes=(), is_out=False):
        self._deps(q, reads, writes)
        n = self.dn[q]
        self.dn[q] += 1
        slot = n % self.NP
        sem = self.dsem[q][slot]
        if n >= self.NP:
            self._wait(q, ('dma', sem, 16 * (n // self.NP)))
        val = 16 * (n // self.NP + 1)
        eng = self.eng[q]
        fn(eng).then_inc(sem, 16)
        tok = ('dma', sem, val)
        self._update(tok, reads, writes)
        if is_out:
            self.out_tokens.append(tok)
        return tok

    def finish(self):
        for tok in self.out_tokens:
            self._wait('sp', tok)
        for q in ('sp', 'pool', 'act'):
            n = self.dn[q]
            for i in range(max(0, n - self.NP), n):
                self._wait('sp', ('dma', self.dsem[q][i % self.NP], 16 * (i // self.NP + 1)))


import math
from contextlib import ExitStack

D = 2048
KC = 16
NOWN = 8
NPRE = 24
NBLK = 80
NOV = 16
OFF_QA, OFF_KA, OFF_VA, OFF_QR, OFF_KR, OFF_VR, OFF_GR, OFF_GA, OFF_GTR = 0, 2048, 2304, 2560, 4608, 6656, 8704, 10752, 12800
EPS = 1e-6
TWO_PI = 2.0 * math.pi
C1 = 6.28125
C2 = TWO_PI - C1
GAM = [1.0 - 2.0 ** (-5.0 - h) for h in range(8)]


def build(stop_after=None):
    nc = bass.Bass("TRN2", target_bir_lowering=False)

    def din(name, shape, dt=F32):
        return nc.dram_tensor(name, list(shape), dt, kind="ExternalInput")

    xo = din("xo", [1024, D]); xp = din("xp", [NPRE * 128, D])
    pos_o = din("pos_o", [128, NOWN], I32); pos_p = din("pos_p", [128, NPRE], I32)
    cT = din("cT", [128, KC]); pkd = din("pk", [128, NPRE, 8]); mask0d = din("mask0", [128, 256])
    w_ada = din("w_ada", [D, 6 * D]); b_ada = din("b_ada", [1, 6 * D]); w_in = din("w_in", [D, 14848])
    sinks = din("attn_sinks", [1, 32]); rgain = din("ret_norm_gain", [1, D])
    wm_t = din("wm_t", [4 * 16 * 128, D]); w_o = din("w_out", [D, D])
    w_rt = din("w_router", [D, 68]); b_rt = din("b_router", [1, 68])
    if stop_after is None or stop_after == 'moe_full':
        w_eg = din("w_expert_gate", [64 * D, 1024]); w_eu = din("w_expert_up", [64 * D, 1024]); w_ed = din("w_expert_down", [64 * 1024, D])
    n1g = din("norm1_gain", [1, D]); n2g = din("norm2_gain", [1, D]); nfg = din("final_norm_gain", [1, D])
    identd = din("ident", [128, 128]); maskd = din("mask", [128, 256]); decTd = din("decT", [128, 8, 128])
    dqd = din("dq", [128, 8]); dkd = din("dk", [128, 8]); invfd = din("invf", [128, 128]); Lstd = din("Lst", [128, 128])
    tokidd = din("tokid", [128, NOWN], I32); blk128d = din("blk128", [128, NBLK]); kcoffd = din("kcoff", [128, 16]); pidxd = din("pidx", [128, 1]); e128d = din("e128", [128, 64])
    out = nc.dram_tensor("out", [1024, D], F32, kind="ExternalOutput")
    dbgo = {}

    def dout(name, shape, dt=F32):
        dbgo[name] = nc.dram_tensor(name, list(shape), dt, kind="ExternalOutput")
        return dbgo[name]

    modd = T(nc.dram_tensor("modd", [6, D], F32, kind="Internal"), "modd")
    xmid = T(nc.dram_tensor("xmid", [1024, D], F32, kind="Internal"), "xmid")
    H2 = T(nc.dram_tensor("H2", [1025, D], BF16, kind="Internal"), "H2")
    rinfo = T(nc.dram_tensor("rinfo", [NBLK * 128, 16], I32, kind="Internal"), "rinfo")
    Yd = T(nc.dram_tensor("Yd", [NBLK * 128, D], F32, kind="Internal"), "Yd")

    with ExitStack() as es:
        S = Sched(nc, es)
        qrr = [0]

        def ld(dst_T, dst_ap, src_ap, q='sp', reads=(), extra_w=()):
            return S.dma(q, lambda e: e.dma_start(out=dst_ap, in_=src_ap), reads=list(reads), writes=[dst_T] + list(extra_w))

        def st(dst_T, dst_ap, src_T, src_ap, q='sp', is_out=False):
            return S.dma(q, lambda e: e.dma_start(out=dst_ap, in_=src_ap), reads=[src_T], writes=[dst_T], is_out=is_out)

        idf = S.sb([128, 128], F32, 'idf'); ld(idf, idf[:], identd[:, :])
        idb = S.sb([128, 128], BF16, 'idb')
        S.op('dve', lambda e: e.tensor_copy(out=idb[:], in_=idf[:]), reads=[idf], writes=[idb])

        def transpose_bf(ps_T, ps_ap, src_T, src_ap):
            S.op('pe', lambda e: e.transpose(out=ps_ap, in_=src_ap, identity=idb[:]), reads=[src_T, idb], writes=[ps_T])

        def rms_rstd(x_T, x_ap, junk_T, ss, rstd, n=D):
            S.op('act', lambda e: e.activation(out=junk_T[:], in_=x_ap, func=AF.Square, accum_out=ss[:]), reads=[x_T], writes=[junk_T, ss])
            S.op('act', lambda e: e.activation(out=ss[:], in_=ss[:], func=AF.Sqrt, scale=1.0 / n, bias=EPS), reads=[ss], writes=[ss])
            S.op('dve', lambda e: e.reciprocal(out=rstd[:], in_=ss[:]), reads=[ss], writes=[rstd])

        with ExitStack() as ph:
            cs = S.sb([128, KC], F32, 'cs', ph); ld(cs, cs[:], cT[:, :])
            sc = S.sb([128, KC], F32, 'sc', ph)
            S.op('act', lambda e: e.activation(out=sc[:], in_=cs[:], func=AF.Silu), reads=[cs], writes=[sc])
            scb = S.sb([128, KC, 128], BF16, 'scb', ph)
            S.op('dve', lambda e: e.tensor_copy(out=scb[:], in_=sc[:, :].unsqueeze(2).to_broadcast([128, KC, 128])), reads=[sc], writes=[scb])
            g1 = S.sb([128, D], F32, 'g1', ph); ld(g1, g1[:], n1g[0:1, :].partition_broadcast(128))
            g2 = S.sb([128, D], F32, 'g2', ph); ld(g2, g2[:], n2g[0:1, :].partition_broadcast(128))
            wbuf = [S.sb([128, KC, 512], BF16, 'wada%d' % i, ph) for i in range(3)]
            bbuf = [S.sb([128, 512], F32, 'bada%d' % i, ph) for i in range(2)]
            rbuf = [S.sb([128, 512], F32, 'rada%d' % i, ph) for i in range(2)]
            pms = [S.ps([128, 512], F32, 'pada%d' % i, ph) for i in range(2)]
            it = 0
            for j in range(6):
                for nb in range(4):
                    c0 = j * D + nb * 512
                    wb, bb, rb, pm = wbuf[it % 3], bbuf[it % 2], rbuf[it % 2], pms[it % 2]
                    for kq in range(4):
                        S.dma('pool', lambda e: e.dma_start(out=wb[:, kq * 4:(kq + 1) * 4, :],
                                                            in_=w_ada[kq * 512:(kq + 1) * 512, c0:c0 + 512].rearrange("(k p) n -> p k n", p=128)), writes=[wb])
                    ld(bb, bb[:], b_ada[0:1, c0:c0 + 512].partition_broadcast(128))
                    for k in range(KC):
                        S.op('pe', lambda e: e.matmul(pm[:], lhsT=scb[:, k, :], rhs=wb[:, k, :], start=(k == 0), stop=(k == KC - 1)),
                             reads=[scb, wb], writes=[pm])
                    S.op('dve', lambda e: e.tensor_tensor(out=rb[:], in0=pm[:], in1=bb[:], op=ALU.add), reads=[pm, bb], writes=[rb])
                    if j in (1, 4):
                        gg = g1 if j == 1 else g2
                        S.op('dve', lambda e: e.scalar_tensor_tensor(out=rb[:], in0=rb[:], scalar=1.0, in1=gg[:, nb * 512:(nb + 1) * 512],
                                                                     op0=ALU.add, op1=ALU.mult), reads=[rb, gg], writes=[rb])
                    st(modd, modd[j:j + 1, nb * 512:(nb + 1) * 512], rb, rb[0:1, :])
                    it += 1
            S.barrier()
        if stop_after == 'ada':
            o = dout("d_mod", [6, D])
            t = S.sb([6, D], F32, 'dbg'); ld(t, t[:], modd[:, :], reads=[modd])
            st(T(o, 'o'), o[:, :], t, t[:], is_out=True)
            S.finish()
            return nc, list(dbgo)
        mx = es.enter_context(ExitStack())
        state_f = S.sb([128, 8, 512], F32, 'state_f', mx)
        S.op('dve', lambda e: e.memset(state_f[:], 0.0), writes=[state_f])
        invf = S.sb([128, 128], F32, 'invf', mx); ld(invf, invf[:], invfd[:, :])
        rp_a = S.sb([128, 128], F32, 'rp_a', mx); rp_b = S.sb([128, 128], F32, 'rp_b', mx)
        rp_k = S.sb([128, 128], I32, 'rp_k', mx); rp_f = S.sb([128, 128], F32, 'rp_f', mx)
        ss = S.sb([128, 1], F32, 'ss', mx); rstd = S.sb([128, 1], F32, 'rstd', mx)

        def rope_tables(pos_T, pos_ap, cos_T, cos_ap, sin_T, sin_ap):
            S.op('dve', lambda e: e.tensor_scalar(out=rp_a[:], in0=invf[:], scalar1=pos_ap, scalar2=None, op0=ALU.mult),
                 reads=[invf, pos_T], writes=[rp_a])
            for which in (0, 1):
                dst_T, dst_ap = (sin_T, sin_ap) if which == 0 else (cos_T, cos_ap)
                if which == 1:
                    S.op('dve', lambda e: e.tensor_scalar(out=rp_a[:], in0=rp_a[:], scalar1=math.pi / 2, scalar2=None, op0=ALU.add),
                         reads=[rp_a], writes=[rp_a])
                S.op('dve', lambda e: e.tensor_scalar(out=rp_k[:], in0=rp_a[:], scalar1=1.0 / TWO_PI, scalar2=None, op0=ALU.mult),
                     reads=[rp_a], writes=[rp_k])
                S.op('dve', lambda e: e.tensor_copy(out=rp_f[:], in_=rp_k[:]), reads=[rp_k], writes=[rp_f])
                S.op('dve', lambda e: e.scalar_tensor_tensor(out=rp_b[:], in0=rp_f[:], scalar=-C1, in1=rp_a[:], op0=ALU.mult, op1=ALU.add),
                     reads=[rp_f, rp_a], writes=[rp_b])
                S.op('dve', lambda e: e.scalar_tensor_tensor(out=rp_b[:], in0=rp_f[:], scalar=-C2, in1=rp_b[:], op0=ALU.mult, op1=ALU.add),
                     reads=[rp_f, rp_b], writes=[rp_b])
                S.op('dve', lambda e: e.tensor_scalar(out=rp_b[:], in0=rp_b[:], scalar1=3.1415925, scalar2=-3.1415925, op0=ALU.min, op1=ALU.max),
                     reads=[rp_b], writes=[rp_b])
                S.op('act', lambda e: e.activation(out=dst_ap, in_=rp_b[:], func=AF.Sin), reads=[rp_b], writes=[dst_T])

        def layer_norm_mod(x_T, hb_T, Ab, shb):
            rms_rstd(x_T, x_T[:], hb_T, ss, rstd)
            S.op('dve', lambda e: e.scalar_tensor_tensor(out=x_T[:], in0=x_T[:], scalar=rstd[:, 0:1], in1=Ab[:], op0=ALU.mult, op1=ALU.mult),
                 reads=[x_T, rstd, Ab], writes=[x_T])
            S.op('dve', lambda e: e.tensor_tensor(out=hb_T[:], in0=x_T[:], in1=shb[:], op=ALU.add), reads=[x_T, shb], writes=[hb_T])

        def make_hT(hb_T, dst_T, dst_fn, pTs):
            for half in range(2):
                pT = pTs[half]
                pv_ = pT[:, :].bitcast(BF16)
                for kk in range(8):
                    k = half * 8 + kk
                    transpose_bf(pT, pv_[:, kk * 128:(kk + 1) * 128], hb_T, hb_T[:, k * 128:(k + 1) * 128])
                S.op('act', lambda e: e.copy(out=dst_fn(half), in_=pv_[:, 0:1024].rearrange("p (a b) -> p a b", a=8)),
                     reads=[pT], writes=[dst_T])

        def rotary(src_T, src_ap4, cos_T, cos_ap, sin_T, sin_ap, rot_T, rot_ap4, tA, tB, n):
            cb = cos_ap.unsqueeze(1).to_broadcast([128, n, 128]); sb_ = sin_ap.unsqueeze(1).to_broadcast([128, n, 128])
            t1 = src_ap4[:, :, 0, :]; t2 = src_ap4[:, :, 1, :]
            S.op('dve', lambda e: e.tensor_tensor(out=tA[:, 0:n, :], in0=t1, in1=cb, op=ALU.mult), reads=[src_T, cos_T], writes=[tA])
            S.op('dve', lambda e: e.tensor_tensor(out=tB[:, 0:n, :], in0=t2, in1=sb_, op=ALU.mult), reads=[src_T, sin_T], writes=[tB])
            S.op('dve', lambda e: e.tensor_tensor(out=rot_ap4[:, :, 0, :], in0=tA[:, 0:n, :], in1=tB[:, 0:n, :], op=ALU.subtract),
                 reads=[tA, tB], writes=[rot_T])
            S.op('dve', lambda e: e.tensor_tensor(out=tA[:, 0:n, :], in0=t1, in1=sb_, op=ALU.mult), reads=[src_T, sin_T], writes=[tA])
            S.op('dve', lambda e: e.tensor_tensor(out=tB[:, 0:n, :], in0=t2, in1=cb, op=ALU.mult), reads=[src_T, cos_T], writes=[tB])
            S.op('dve', lambda e: e.tensor_tensor(out=rot_ap4[:, :, 1, :], in0=tA[:, 0:n, :], in1=tB[:, 0:n, :], op=ALU.add),
                 reads=[tA, tB], writes=[rot_T])

        with ExitStack() as ph:
            Wk = [S.sb([128, 4, 2048], BF16, 'Wk%d' % i, ph) for i in range(4)]
            Wv = [S.sb([128, 4, 2048], BF16, 'Wv%d' % i, ph) for i in range(4)]
            for k in range(KC):
                S.dma('pool', lambda e: e.dma_start(out=Wk[k // 4][:, k % 4, :], in_=w_in[k * 128:(k + 1) * 128, OFF_KR:OFF_KR + 2048]), writes=[Wk[k // 4]])
                S.dma('pool', lambda e: e.dma_start(out=Wv[k // 4][:, k % 4, :], in_=w_in[k * 128:(k + 1) * 128, OFF_VR:OFF_VR + 2048]), writes=[Wv[k // 4]])
            A1b = S.sb([128, D], F32, 'A1b', ph); ld(A1b, A1b[:], modd[1:2, :].partition_broadcast(128), reads=[modd])
            sh1b = S.sb([128, D], F32, 'sh1b', ph); ld(sh1b, sh1b[:], modd[0:1, :].partition_broadcast(128), reads=[modd])
            posi = S.sb([128, NPRE], I32, 'posi', ph); ld(posi, posi[:], pos_p[:, :])
            posf = S.sb([128, NPRE], F32, 'posf', ph)
            S.op('dve', lambda e: e.tensor_copy(out=posf[:], in_=posi[:]), reads=[posi], writes=[posf])
            pkt = S.sb([128, NPRE, 8], F32, 'pkt', ph); ld(pkt, pkt[:], pkd[:, :, :])
            xc = [S.sb([128, D], F32, 'xc%d' % i, ph) for i in range(2)]
            hb = S.sb([128, D], BF16, 'hb', ph)
            hT = S.sb([128, KC, 128], BF16, 'hT', ph)
            cosT = S.sb([128, 128], F32, 'cosT', ph); sinT = S.sb([128, 128], F32, 'sinT', ph)
            rot = S.sb([128, 2, 2, 128], F32, 'rot', ph)
            tA = S.sb([128, 2, 128], F32, 'tA', ph); tB = S.sb([128, 2, 128], F32, 'tB', ph)
            ks = S.sb([128, 8, 256], BF16, 'ks', ph)
            vb = S.sb([128, D], BF16, 'vb', ph)
            pTs = [S.ps([128, 512], F32, 'pT%d' % i, ph) for i in range(2)]
            pkb = [S.ps([128, 512], F32, 'pkb%d' % i, ph) for i in range(2)]
            pvb = [S.ps([128, 512], F32, 'pvb%d' % i, ph) for i in range(2)]
            pst = [S.ps([128, 512], F32, 'pst%d' % i, ph) for i in range(2)]
            ld(xc[0], xc[0][:], xp[0:128, :])
            for j in range(NPRE):
                xcur = xc[j % 2]
                if j + 1 < NPRE:
                    ld(xc[(j + 1) % 2], xc[(j + 1) % 2][:], xp[(j + 1) * 128:(j + 2) * 128, :])
                layer_norm_mod(xcur, hb, A1b, sh1b)
                make_hT(hb, hT, lambda half: hT[:, half * 8:(half + 1) * 8, :], pTs)
                rope_tables(posf, posf[:, j:j + 1], cosT, cosT[:], sinT, sinT[:])
                for nb in range(4):
                    pb = pkb[nb % 2]
                    for k in range(KC):
                        S.op('pe', lambda e: e.matmul(pb[:], lhsT=hT[:, k, :], rhs=Wk[k // 4][:, k % 4, nb * 512:(nb + 1) * 512],
                                                      start=(k == 0), stop=(k == KC - 1)), reads=[hT, Wk[k // 4]], writes=[pb])
                    rotary(pb, pb[:, :].rearrange("p (h t f) -> p h t f", h=2, t=2), cosT, cosT[:], sinT, sinT[:],
                           rot, rot[:], tA, tB, 2)
                    S.op('dve', lambda e: e.tensor_tensor(out=ks[:, 2 * nb:2 * nb + 2, :], in0=rot[:].rearrange("p h t f -> p h (t f)"),
                                                          in1=pkt[:, j, 2 * nb:2 * nb + 2].unsqueeze(2).to_broadcast([128, 2, 256]), op=ALU.mult),
                         reads=[rot, pkt], writes=[ks])
                for nb in range(4):
                    pb = pvb[nb % 2]
                    for k in range(KC):
                        S.op('pe', lambda e: e.matmul(pb[:], lhsT=hT[:, k, :], rhs=Wv[k // 4][:, k % 4, nb * 512:(nb + 1) * 512],
                                                      start=(k == 0), stop=(k == KC - 1)), reads=[hT, Wv[k // 4]], writes=[pb])
                    S.op('act', lambda e: e.copy(out=vb[:, nb * 512:(nb + 1) * 512], in_=pb[:]), reads=[pb], writes=[vb])
                for h in range(8):
                    pb = pst[h % 2]
                    for half in range(2):
                        S.op('pe', lambda e: e.matmul(pb[:, half * 256:(half + 1) * 256], lhsT=ks[:, h, half * 128:(half + 1) * 128],
                                                      rhs=vb[:, h * 256:(h + 1) * 256], start=True, stop=True), reads=[ks, vb], writes=[pb])
                    S.op('dve', lambda e: e.tensor_tensor(out=state_f[:, h, :], in0=state_f[:, h, :], in1=pb[:], op=ALU.add),
                         reads=[state_f, pb], writes=[state_f])
            S.barrier()
        if stop_after == 'prefix':
            o = dout("d_state", [128, 8, 512])
            st(T(o, 'o'), o[:, :, :], state_f, state_f[:], is_out=True)
            S.finish()
            return nc, list(dbgo)
        hT_all = S.sb([128, KC, 1152], BF16, 'hT_all', mx)
        retT = S.sb([128, KC, 1024], BF16, 'retT', mx)
        attnT = S.sb([128, KC, 1024], BF16, 'attnT', mx)
        cos_o = S.sb([128, NOWN, 128], F32, 'cos_o', mx); sin_o = S.sb([128, NOWN, 128], F32, 'sin_o', mx)
        with ExitStack() as ph:
            A1b = S.sb([128, D], F32, 'A1b', ph); ld(A1b, A1b[:], modd[1:2, :].partition_broadcast(128), reads=[modd])
            sh1b = S.sb([128, D], F32, 'sh1b', ph); ld(sh1b, sh1b[:], modd[0:1, :].partition_broadcast(128), reads=[modd])
            posi = S.sb([128, NOWN], I32, 'posio', ph); ld(posi, posi[:], pos_o[:, :])
            posf = S.sb([128, NOWN], F32, 'posfo', ph)
            S.op('dve', lambda e: e.tensor_copy(out=posf[:], in_=posi[:]), reads=[posi], writes=[posf])
            xc = [S.sb([128, D], F32, 'xc%d' % i, ph) for i in range(2)]
            hb = [S.sb([128, D], BF16, 'hb%d' % i, ph) for i in range(2)]
            pTs = [S.ps([128, 512], F32, 'pT%d' % i, ph) for i in range(2)]
            for ci in range(9):
                src = xp[(NPRE - 1) * 128:NPRE * 128, :] if ci == 0 else xo[(ci - 1) * 128:ci * 128, :]
                xcur = xc[ci % 2]; hcur = hb[ci % 2]
                ld(xcur, xcur[:], src)
                layer_norm_mod(xcur, hcur, A1b, sh1b)
                make_hT(hcur, hT_all, lambda half: hT_all[:, half * 8:(half + 1) * 8, ci * 128:(ci + 1) * 128], pTs)
            for n in range(NOWN):
                rope_tables(posf, posf[:, n:n + 1], cos_o, cos_o[:, n, :], sin_o, sin_o[:, n, :])
            S.barrier()

        with ExitStack() as ph:
            Wh = [S.sb([128, 4, 1024], BF16, 'Wh%d' % i, ph) for i in range(4)]
            decT = S.sb([128, 8, 128], F32, 'decT', ph); ld(decT, decT[:], decTd[:, :, :])
            dq = S.sb([128, 8], F32, 'dq', ph); ld(dq, dq[:], dqd[:, :])
            dk = S.sb([128, 8], F32, 'dk', ph); ld(dk, dk[:], dkd[:, :])
            rgb = S.sb([128, D], F32, 'rgb', ph); ld(rgb, rgb[:], rgain[0:1, :].partition_broadcast(128))
            state_b = S.sb([128, 8, 512], BF16, 'state_b', ph)
            S.op('act', lambda e: e.copy(out=state_b[:], in_=state_f[:]), reads=[state_f], writes=[state_b])
            rotq = S.sb([128, 2, 2, 128], F32, 'rotq', ph)
            tA = S.sb([128, 2, 128], F32, 'tA', ph); tB = S.sb([128, 2, 128], F32, 'tB', ph)
            qkb = S.sb([128, 3, 256], BF16, 'qkb', ph)
            kd = S.sb([128, 256], BF16, 'kd', ph)
            vbh = S.sb([128, 256], BF16, 'vbh', ph)
            gs = S.sb([128, 256], F32, 'gs', ph)
            qkT = S.sb([128, 6, 128], BF16, 'qkT', ph)
            iT = S.sb([128, 128], BF16, 'iT', ph)
            junk = S.sb([128, 256], F32, 'junk', ph)
            rtmp = S.sb([128, 256], F32, 'rtmp', ph)
            reth = S.sb([128, 256], BF16, 'reth', ph)
            ss2 = S.sb([128, 1], F32, 'ss2', ph); r2 = S.sb([128, 1], F32, 'r2', ph)
            pp = [S.ps([128, 512], F32, 'pp%d' % i, ph) for i in range(4)]
            pTq = S.ps([128, 512], F32, 'pTq', ph); pTqv = pTq[:, :].bitcast(BF16)
            pi_ = S.ps([128, 512], F32, 'pi', ph); po_ = S.ps([128, 512], F32, 'po', ph); pds = S.ps([128, 512], F32, 'pds', ph)
            it = 0
            for h in range(8):
                offs = [OFF_QR + h * 256, OFF_KR + h * 256, OFF_VR + h * 256, OFF_GR + h * 256]
                for sl in range(4):
                    for wi, off in enumerate(offs):
                        S.dma('pool', lambda e: e.dma_start(out=Wh[sl][:, :, wi * 256:(wi + 1) * 256],
                                                            in_=w_in[sl * 512:(sl + 1) * 512, off:off + 256].rearrange("(k p) n -> p k n", p=128)),
                              writes=[Wh[sl]])
                for n in range(NOWN):
                    tok = slice((n + 1) * 128, (n + 2) * 128)
                    pb0, pb1 = pp[2 * (it % 2)], pp[2 * (it % 2) + 1]
                    for blk, pb in ((0, pb0), (1, pb1)):
                        for k in range(KC):
                            S.op('pe', lambda e: e.matmul(pb[:], lhsT=hT_all[:, k, tok], rhs=Wh[k // 4][:, k % 4, blk * 512:(blk + 1) * 512],
                                                          start=(k == 0), stop=(k == KC - 1)), reads=[hT_all, Wh[k // 4]], writes=[pb])
                    rotary(pb0, pb0[:, :].rearrange("p (h t f) -> p h t f", h=2, t=2), cos_o, cos_o[:, n, :], sin_o, sin_o[:, n, :],
                           rotq, rotq[:], tA, tB, 2)
                    rq = rotq[:, 0, :, :].rearrange("p t f -> p (t f)"); rk = rotq[:, 1, :, :].rearrange("p t f -> p (t f)")
                    S.op('act', lambda e: e.copy(out=qkb[:, 0, :], in_=rq), reads=[rotq], writes=[qkb])
                    S.op('dve', lambda e: e.tensor_scalar(out=qkb[:, 1, :], in0=rq, scalar1=dq[:, h:h + 1], scalar2=None, op0=ALU.mult),
                         reads=[rotq, dq], writes=[qkb])
                    S.op('act', lambda e: e.mul(out=qkb[:, 2, :], in_=rk, mul=1.0 / 16.0), reads=[rotq], writes=[qkb])
                    S.op('dve', lambda e: e.tensor_scalar(out=kd[:], in0=rk, scalar1=dk[:, h:h + 1], scalar2=None, op0=ALU.mult),
                         reads=[rotq, dk], writes=[kd])
                    S.op('act', lambda e: e.copy(out=vbh[:], in_=pb1[:, 0:256]), reads=[pb1], writes=[vbh])
                    S.op('act', lambda e: e.activation(out=gs[:], in_=pb1[:, 256:512], func=AF.Silu), reads=[pb1], writes=[gs])
                    for a in range(3):
                        for half in range(2):
                            transpose_bf(pTq, pTqv[:, (a * 2 + half) * 128:(a * 2 + half + 1) * 128], qkb, qkb[:, a, half * 128:(half + 1) * 128])
                    S.op('act', lambda e: e.copy(out=qkT[:], in_=pTqv[:, 0:768].rearrange("p (a b) -> p a b", a=6)), reads=[pTq], writes=[qkT])
                    for half in range(2):
                        S.op('pe', lambda e: e.matmul(pi_[:, 0:128], lhsT=qkT[:, 4 + half, :], rhs=qkT[:, half, :], start=(half == 0), stop=(half == 1)),
                             reads=[qkT], writes=[pi_])
                    S.op('dve', lambda e: e.tensor_tensor(out=iT[:], in0=pi_[:, 0:128], in1=decT[:, h, :], op=ALU.mult), reads=[pi_, decT], writes=[iT])
                    S.op('pe', lambda e: e.matmul(po_[:, 0:256], lhsT=iT[:], rhs=vbh[:], start=True, stop=False), reads=[iT, vbh], writes=[po_])
                    for half in range(2):
                        S.op('pe', lambda e: e.matmul(po_[:, 0:256], lhsT=qkT[:, 2 + half, :], rhs=state_b[:, h, half * 256:(half + 1) * 256],
                                                      start=False, stop=(half == 1)), reads=[qkT, state_b], writes=[po_])
                    rms_rstd(po_, po_[:, 0:256], junk, ss2, r2, n=256)
                    S.op('dve', lambda e: e.scalar_tensor_tensor(out=rtmp[:], in0=po_[:, 0:256], scalar=r2[:, 0:1], in1=rgb[:, h * 256:(h + 1) * 256],
                                                                 op0=ALU.mult, op1=ALU.mult), reads=[po_, r2, rgb], writes=[rtmp])
                    S.op('dve', lambda e: e.tensor_tensor(out=reth[:], in0=rtmp[:], in1=gs[:], op=ALU.mult), reads=[rtmp, gs], writes=[reth])
                    for half in range(2):
                        transpose_bf(pTq, pTqv[:, 768 + half * 128:768 + (half + 1) * 128], reth, reth[:, half * 128:(half + 1) * 128])
                    S.op('act', lambda e: e.copy(out=retT[:, 2 * h:2 * h + 2, n * 128:(n + 1) * 128],
                                                 in_=pTqv[:, 768:1024].rearrange("p (a b) -> p a b", a=2)), reads=[pTq], writes=[retT])
                    for half in range(2):
                        S.op('pe', lambda e: e.matmul(pds[:, half * 256:(half + 1) * 256], lhsT=kd[:, half * 128:(half + 1) * 128], rhs=vbh[:],
                                                      start=True, stop=True), reads=[kd, vbh], writes=[pds])
                    S.op('dve', lambda e: e.scalar_tensor_tensor(out=state_f[:, h, :], in0=state_f[:, h, :], scalar=float(GAM[h] ** 128), in1=pds[:],
                                                                 op0=ALU.mult, op1=ALU.add), reads=[state_f, pds], writes=[state_f])
                    S.op('act', lambda e: e.copy(out=state_b[:, h, :], in_=state_f[:, h, :]), reads=[state_f], writes=[state_b])
                    it += 1
            S.barrier()
        if stop_after == 'ret':
            o = dout("d_retT", [128, KC, 1024], BF16)
            st(T(o, 'o'), o[:, :, :], retT, retT[:], is_out=True)
            S.finish()
            return nc, list(dbgo)
        with ExitStack() as ph:
            Wg = [S.sb([128, 4, 640], BF16, 'Wg%d' % i, ph) for i in range(4)]
            maskt = S.sb([128, 256], F32, 'maskt', ph); ld(maskt, maskt[:], maskd[:, :])
            mask0t = S.sb([128, 256], F32, 'mask0t', ph); ld(mask0t, mask0t[:], mask0d[:, :])
            sinkb = S.sb([128, 32], F32, 'sinkb', ph); ld(sinkb, sinkb[:], sinks[0:1, :].partition_broadcast(128))
            kT_all = S.sb([128, 2, 1152], BF16, 'kT_all', ph)
            v_all = S.sb([128, 9, 64], BF16, 'v_all', ph)
            qT = S.sb([128, 4, 1024], BF16, 'qT', ph)
            qb = S.sb([128, 512], BF16, 'qb', ph); k2 = S.sb([128, 2, 128], BF16, 'k2', ph)
            S.op('dve', lambda e: e.memset(k2[:], 0.0), writes=[k2])
            S_sb = S.sb([128, 8, 256], F32, 'S_sb', ph)
            P_bf = S.sb([128, 8, 256], BF16, 'P_bf', ph)
            PT = S.sb([128, 8, 2, 128], BF16, 'PT', ph)
            mx8 = S.sb([128, 8], F32, 'mx8', ph); negm = S.sb([128, 8], F32, 'negm', ph); rs = S.sb([128, 8], F32, 'rs', ph)
            es8 = S.sb([128, 8], F32, 'es8', ph); rden = S.sb([128, 8], F32, 'rden', ph)
            at_tok = S.sb([128, 8, 64], BF16, 'at_tok', ph)
            pq0 = S.ps([128, 512], F32, 'pq0', ph); pq1 = S.ps([128, 512], F32, 'pq1', ph)
            pTa = S.ps([128, 512], F32, 'pTa', ph); pTav = pTa[:, :].bitcast(BF16)
            psc = [S.ps([128, 512], F32, 'psc%d' % i, ph) for i in range(2)]
            pPT = S.ps([128, 512], F32, 'pPT', ph); pPTv = pPT[:, :].bitcast(BF16)
            ppv = S.ps([128, 512], F32, 'ppv', ph)
            for g in range(4):
                offs = [(OFF_QA + g * 512, 512, 0), (OFF_KA + g * 64, 64, 512), (OFF_VA + g * 64, 64, 576)]
                for sl in range(4):
                    for off, wd, dst in offs:
                        S.dma('pool', lambda e: e.dma_start(out=Wg[sl][:, :, dst:dst + wd],
                                                            in_=w_in[sl * 512:(sl + 1) * 512, off:off + wd].rearrange("(k p) n -> p k n", p=128)),
                              writes=[Wg[sl]])
                for ci in range(9):
                    tok = slice(ci * 128, (ci + 1) * 128)
                    if ci >= 1:
                        for k in range(KC):
                            S.op('pe', lambda e: e.matmul(pq0[:], lhsT=hT_all[:, k, tok], rhs=Wg[k // 4][:, k % 4, 0:512], start=(k == 0), stop=(k == KC - 1)),
                                 reads=[hT_all, Wg[k // 4]], writes=[pq0])
                        S.op('act', lambda e: e.mul(out=qb[:], in_=pq0[:], mul=0.125), reads=[pq0], writes=[qb])
                    for k in range(KC):
                        S.op('pe', lambda e: e.matmul(pq1[:, 0:128], lhsT=hT_all[:, k, tok], rhs=Wg[k // 4][:, k % 4, 512:640], start=(k == 0), stop=(k == KC - 1)),
                             reads=[hT_all, Wg[k // 4]], writes=[pq1])
                    S.op('dve', lambda e: e.tensor_copy(out=k2[:, 0, 0:64], in_=pq1[:, 0:64]), reads=[pq1], writes=[k2])
                    S.op('dve', lambda e: e.tensor_copy(out=k2[:, 1, 64:128], in_=pq1[:, 0:64]), reads=[pq1], writes=[k2])
                    S.op('dve', lambda e: e.tensor_copy(out=v_all[:, ci, :], in_=pq1[:, 64:128]), reads=[pq1], writes=[v_all])
                    if ci >= 1:
                        for i in range(4):
                            transpose_bf(pTa, pTav[:, i * 128:(i + 1) * 128], qb, qb[:, i * 128:(i + 1) * 128])
                    transpose_bf(pTa, pTav[:, 512:640], k2, k2[:, 0, :])
                    transpose_bf(pTa, pTav[:, 640:768], k2, k2[:, 1, :])
                    if ci >= 1:
                        S.op('act', lambda e: e.copy(out=qT[:, :, (ci - 1) * 128:ci * 128], in_=pTav[:, 0:512].rearrange("p (a b) -> p a b", a=4)),
                             reads=[pTa], writes=[qT])
                    S.op('act', lambda e: e.copy(out=kT_all[:, :, tok], in_=pTav[:, 512:768].rearrange("p (a b) -> p a b", a=2)), reads=[pTa], writes=[kT_all])
                for n in range(NOWN):
                    mk = mask0t if n == 0 else maskt
                    for hh in range(2):
                        for j4 in range(4):
                            j = hh * 4 + j4; i = j // 2; r0 = (j % 2) * 64
                            pb = psc[j4 // 2]
                            S.op('pe', lambda e: e.matmul(pb[:, (j4 % 2) * 256:(j4 % 2 + 1) * 256], lhsT=qT[:, i, n * 128:(n + 1) * 128],
                                                          rhs=kT_all[:, j % 2, n * 128:n * 128 + 256], start=True, stop=True),
                                 reads=[qT, kT_all], writes=[pb])
                        for bnk in range(2):
                            S.op('dve', lambda e: e.tensor_tensor(out=S_sb[:, hh * 4 + bnk * 2:hh * 4 + bnk * 2 + 2, :],
                                                                  in0=psc[bnk][:, :].rearrange("p (a b) -> p a b", a=2),
                                                                  in1=mk[:, :].unsqueeze(1).to_broadcast([128, 2, 256]), op=ALU.add),
                                 reads=[psc[bnk], mk], writes=[S_sb])
                    S.op('dve', lambda e: e.tensor_reduce(out=mx8[:], in_=S_sb[:], axis=AX.X, op=ALU.max), reads=[S_sb], writes=[mx8])
                    S.op('dve', lambda e: e.tensor_tensor(out=mx8[:], in0=mx8[:], in1=sinkb[:, g * 8:(g + 1) * 8], op=ALU.max), reads=[mx8, sinkb], writes=[mx8])
                    S.op('dve', lambda e: e.tensor_scalar(out=negm[:], in0=mx8[:], scalar1=-1.0, scalar2=None, op0=ALU.mult), reads=[mx8], writes=[negm])
                    for j in range(8):
                        S.op('act', lambda e: e.activation(out=P_bf[:, j, :], in_=S_sb[:, j, :], func=AF.Exp, bias=negm[:, j:j + 1], scale=1.0,
                                                           accum_out=rs[:, j:j + 1]), reads=[S_sb, negm], writes=[P_bf, rs])
                    S.op('dve', lambda e: e.tensor_tensor(out=es8[:], in0=sinkb[:, g * 8:(g + 1) * 8], in1=mx8[:], op=ALU.subtract), reads=[sinkb, mx8], writes=[es8])
                    S.op('act', lambda e: e.activation(out=es8[:], in_=es8[:], func=AF.Exp), reads=[es8], writes=[es8])
                    S.op('dve', lambda e: e.tensor_tensor(out=es8[:], in0=es8[:], in1=rs[:], op=ALU.add), reads=[es8, rs], writes=[es8])
                    S.op('dve', lambda e: e.reciprocal(out=rden[:], in_=es8[:]), reads=[es8], writes=[rden])
                    for hh in range(2):
                        for j4 in range(4):
                            for t in range(2):
                                transpose_bf(pPT, pPTv[:, (j4 * 2 + t) * 128:(j4 * 2 + t + 1) * 128], P_bf, P_bf[:, hh * 4 + j4, t * 128:(t + 1) * 128])
                        S.op('act', lambda e: e.copy(out=PT[:, hh * 4:(hh + 1) * 4, :, :], in_=pPTv[:, 0:1024].rearrange("p (a t b) -> p a t b", a=4, t=2)),
                             reads=[pPT], writes=[PT])
                    for j in range(8):
                        for t in range(2):
                            S.op('pe', lambda e: e.matmul(ppv[:, j * 64:(j + 1) * 64], lhsT=PT[:, j, t, :], rhs=v_all[:, n + t, :], start=(t == 0), stop=(t == 1)),
                                 reads=[PT, v_all], writes=[ppv])
                    S.op('dve', lambda e: e.tensor_tensor(out=at_tok[:], in0=ppv[:, :].rearrange("p (a b) -> p a b", a=8),
                                                          in1=rden[:, :].unsqueeze(2).to_broadcast([128, 8, 64]), op=ALU.mult), reads=[ppv, rden], writes=[at_tok])
                    for i in range(4):
                        transpose_bf(pTa, pTav[:, i * 128:(i + 1) * 128], at_tok, at_tok[:].rearrange("p a b -> p (a b)")[:, i * 128:(i + 1) * 128])
                    S.op('act', lambda e: e.copy(out=attnT[:, g * 4:(g + 1) * 4, n * 128:(n + 1) * 128], in_=pTav[:, 0:512].rearrange("p (a b) -> p a b", a=4)),
                         reads=[pTa], writes=[attnT])
            S.barrier()
        if stop_after == 'attn':
            o = dout("d_attnT", [128, KC, 1024], BF16)
            st(T(o, 'o'), o[:, :, :], attnT, attnT[:], is_out=True)
            S.finish()
            return nc, list(dbgo)
        with ExitStack() as ph:
            mT = S.sb([128, KC, 512], BF16, 'mT', ph)
            Wm = [S.sb([128, KC, 128], BF16, 'Wm%d' % i, ph) for i in range(4)]
            Wo = [S.sb([128, 4, 512], BF16, 'Wo%d' % i, ph) for i in range(4)]
            g1b = S.sb([128, D], F32, 'g1b', ph); ld(g1b, g1b[:], modd[2:3, :].partition_broadcast(128), reads=[modd])
            sga = S.sb([128, 512], F32, 'sga', ph); sgr = S.sb([128, 512], F32, 'sgr', ph)
            rr = [S.sb([128, 512], F32, 'rr%d' % i, ph) for i in range(2)]
            xs = [S.sb([128, 512], F32, 'xs%d' % i, ph) for i in range(2)]
            pA = S.ps([128, 512], F32, 'pA', ph); pR = S.ps([128, 512], F32, 'pR', ph)
            pGa = S.ps([128, 512], F32, 'pGa', ph); pGr = S.ps([128, 512], F32, 'pGr', ph)
            po2 = [S.ps([128, 512], F32, 'po2%d' % i, ph) for i in range(2)]
            for th in range(2):
                tsl = slice(th * 512, (th + 1) * 512)
                hsl = slice(128 + th * 512, 128 + (th + 1) * 512)
                for cg in range(16):
                    for wi in range(4):
                        r0 = (wi * 16 + cg) * 128
                        S.dma('pool', lambda e: e.dma_start(out=Wm[wi][:].rearrange("p k n -> p (k n)"), in_=wm_t[r0:r0 + 128, :]), writes=[Wm[wi]])
                    for jj in range(1):
                        csl = slice(0, 128)
                        for pb, wt, act_T, asl in ((pA, Wm[0], attnT, tsl), (pR, Wm[1], retT, tsl), (pGa, Wm[2], hT_all, hsl), (pGr, Wm[3], hT_all, hsl)):
                            for k in range(KC):
                                S.op('pe', lambda e: e.matmul(pb[:], lhsT=wt[:, k, csl], rhs=act_T[:, k, asl], start=(k == 0), stop=(k == KC - 1)),
                                     reads=[wt, act_T], writes=[pb])
                        S.op('act', lambda e: e.activation(out=sga[:], in_=pGa[:], func=AF.Sigmoid), reads=[pGa], writes=[sga])
                        S.op('act', lambda e: e.activation(out=sgr[:], in_=pGr[:], func=AF.Sigmoid), reads=[pGr], writes=[sgr])
                        S.op('dve', lambda e: e.tensor_tensor(out=sga[:], in0=sga[:], in1=pA[:], op=ALU.mult), reads=[sga, pA], writes=[sga])
                        S.op('dve', lambda e: e.tensor_tensor(out=sgr[:], in0=sgr[:], in1=pR[:], op=ALU.mult), reads=[sgr, pR], writes=[sgr])
                        S.op('dve', lambda e: e.tensor_tensor(out=mT[:, cg, :], in0=sga[:], in1=sgr[:], op=ALU.add), reads=[sga, sgr], writes=[mT])
                it = 0
                for nb in range(4):
                    for kq in range(4):
                        S.dma('pool', lambda e: e.dma_start(out=Wo[kq][:], in_=w_o[kq * 512:(kq + 1) * 512, nb * 512:(nb + 1) * 512].rearrange("(k p) n -> p k n", p=128)),
                              writes=[Wo[kq]])
                    for c4 in range(4):
                        n = th * 4 + c4
                        pb = po2[it % 2]; r_ = rr[it % 2]; x_ = xs[it % 2]
                        ld(x_, x_[:], xo[n * 128:(n + 1) * 128, nb * 512:(nb + 1) * 512])
                        for k in range(KC):
                            S.op('pe', lambda e: e.matmul(pb[:], lhsT=mT[:, k, c4 * 128:(c4 + 1) * 128], rhs=Wo[k // 4][:, k % 4, :], start=(k == 0), stop=(k == KC - 1)),
                                 reads=[mT, Wo[k // 4]], writes=[pb])
                        S.op('dve', lambda e: e.tensor_tensor(out=r_[:], in0=pb[:], in1=g1b[:, nb * 512:(nb + 1) * 512], op=ALU.mult), reads=[pb, g1b], writes=[r_])
                        S.op('dve', lambda e: e.tensor_tensor(out=r_[:], in0=r_[:], in1=x_[:], op=ALU.add), reads=[r_, x_], writes=[r_])
                        st(xmid, xmid[n * 128:(n + 1) * 128, nb * 512:(nb + 1) * 512], r_, r_[:])
                        it += 1
            S.barrier()
        mx.close()
        S.barrier()
        if stop_after == 'mix':
            o = dout("d_xmid", [1024, D])
            t = S.sb([128, NOWN, D], F32, 'dbgx')
            ld(t, t[:], xmid[:, :].rearrange("(n p) d -> p n d", p=128), reads=[xmid])
            st(T(o, 'o'), o[:, :].rearrange("(n p) d -> p n d", p=128), t, t[:], is_out=True)
            S.finish()
            return nc, list(dbgo)
        moe = es.enter_context(ExitStack())
        slot_i = S.sb([128, 2, NOWN], I32, 'slot_i', moe)
        idxg_i = S.sb([128, NOV, 16], I32, 'idxg_i', moe)
        idxd_i = S.sb([128, NOV, 8], I32, 'idxd_i', moe)
        ss = S.sb([128, 1], F32, 'ss_m', moe); rstd = S.sb([128, 1], F32, 'rstd_m', moe)
        with ExitStack() as ph:
            A2b = S.sb([128, D], F32, 'A2b', ph); ld(A2b, A2b[:], modd[4:5, :].partition_broadcast(128), reads=[modd])
            sh2b = S.sb([128, D], F32, 'sh2b', ph); ld(sh2b, sh2b[:], modd[3:4, :].partition_broadcast(128), reads=[modd])
            Wr_sb = S.sb([128, KC, 68], F32, 'Wr_sb', ph); ld(Wr_sb, Wr_sb[:], w_rt[:, :].rearrange("(k p) n -> p k n", p=128))
            brb = S.sb([128, 68], F32, 'brb', ph); ld(brb, brb[:], b_rt[0:1, :].partition_broadcast(128))
            zt = S.sb([1, D], BF16, 'zt', ph)
            S.op('dve', lambda e: e.memset(zt[:], 0.0), writes=[zt])
            st(H2, H2[1024:1025, :], zt, zt[:])
            xm = [S.sb([128, D], F32, 'xm%d' % i, ph) for i in range(2)]
            h2b = S.sb([128, D], BF16, 'h2b', ph)
            h2T = S.sb([128, KC, 128], F32, 'h2T', ph)
            lg = S.sb([128, 68], F32, 'lg', ph)
            sm = {k: S.sb([128, 1], F32, 'sm_' + k, ph) for k in ('gmax', 'negg', 'gsum', 'gw', 'm1', 'm2', 'd', 'p1')}
            ohg = S.sb([128, 4], F32, 'ohg', ph); gej = S.sb([128, 4], F32, 'gej', ph)
            t416 = S.sb([128, 4, 16], F32, 't416', ph)
            el = S.sb([128, 16], F32, 'el', ph); el2 = S.sb([128, 16], F32, 'el2', ph)
            oh1 = S.sb([128, 16], F32, 'oh1', ph); oh2 = S.sb([128, 16], F32, 'oh2', ph)
            OH = [S.sb([128, NOWN, 64], F32, 'OH%d' % i, ph) for i in range(2)]
            wts = S.sb([128, 2, NOWN], F32, 'wts', ph)
            Cb = S.sb([128, NOWN, 64], BF16, 'Cb', ph)
            pTf = [S.ps([128, 512], F32, 'pTf%d' % i, ph) for i in range(2)]
            plg = S.ps([128, 512], F32, 'plg', ph)
            pPC = S.ps([128, 512], F32, 'pPC', ph); pcnt = S.ps([128, 512], F32, 'pcnt', ph)
            for n in range(NOWN):
                x_ = xm[n % 2]
                ld(x_, x_[:], xmid[n * 128:(n + 1) * 128, :], reads=[xmid])
                rms_rstd(x_, x_[:], h2b, ss, rstd)
                S.op('dve', lambda e: e.scalar_tensor_tensor(out=x_[:], in0=x_[:], scalar=rstd[:, 0:1], in1=A2b[:], op0=ALU.mult, op1=ALU.mult),
                     reads=[x_, rstd, A2b], writes=[x_])
                S.op('dve', lambda e: e.tensor_tensor(out=x_[:], in0=x_[:], in1=sh2b[:], op=ALU.add), reads=[x_, sh2b], writes=[x_])
                S.op('act', lambda e: e.copy(out=h2b[:], in_=x_[:]), reads=[x_], writes=[h2b])
                st(H2, H2[n * 128:(n + 1) * 128, :], h2b, h2b[:])
                for grp in range(4):
                    pb = pTf[grp % 2]
                    for kk in range(4):
                        k = grp * 4 + kk
                        S.op('pe', lambda e: e.matmul(pb[:, kk * 128:(kk + 1) * 128], lhsT=x_[:, k * 128:(k + 1) * 128], rhs=idf[:], start=True, stop=True),
                             reads=[x_, idf], writes=[pb])
                    S.op('act', lambda e: e.copy(out=h2T[:, grp * 4:(grp + 1) * 4, :], in_=pb[:, :].rearrange("p (a b) -> p a b", a=4)), reads=[pb], writes=[h2T])
                for k in range(KC):
                    S.op('pe', lambda e: e.matmul(plg[:, 0:68], lhsT=h2T[:, k, :], rhs=Wr_sb[:, k, :], start=(k == 0), stop=(k == KC - 1)),
                         reads=[h2T, Wr_sb], writes=[plg])
                S.op('dve', lambda e: e.tensor_tensor(out=lg[:], in0=plg[:, 0:68], in1=brb[:], op=ALU.add), reads=[plg, brb], writes=[lg])
                S.op('dve', lambda e: e.tensor_reduce(out=sm['gmax'][:], in_=lg[:, 0:4], axis=AX.X, op=ALU.max), reads=[lg], writes=[sm['gmax']])
                S.op('dve', lambda e: e.tensor_scalar(out=ohg[:], in0=lg[:, 0:4], scalar1=sm['gmax'][:, 0:1], scalar2=None, op0=ALU.is_ge), reads=[lg, sm['gmax']], writes=[ohg])
                S.op('dve', lambda e: e.tensor_scalar(out=sm['negg'][:], in0=sm['gmax'][:], scalar1=-1.0, scalar2=None, op0=ALU.mult), reads=[sm['gmax']], writes=[sm['negg']])
                S.op('act', lambda e: e.activation(out=gej[:], in_=lg[:, 0:4], func=AF.Exp, bias=sm['negg'][:, 0:1], scale=1.0, accum_out=sm['gsum'][:]),
                     reads=[lg, sm['negg']], writes=[gej, sm['gsum']])
                S.op('dve', lambda e: e.reciprocal(out=sm['gw'][:], in_=sm['gsum'][:]), reads=[sm['gsum']], writes=[sm['gw']])
                S.op('dve', lambda e: e.tensor_tensor(out=t416[:], in0=lg[:, 4:68].rearrange("p (g e) -> p g e", g=4),
                                                      in1=ohg[:, :].unsqueeze(2).to_broadcast([128, 4, 16]), op=ALU.mult), reads=[lg, ohg], writes=[t416])
                S.op('dve', lambda e: e.tensor_reduce(out=el[:], in_=t416[:].rearrange("p g e -> p e g"), axis=AX.X, op=ALU.add), reads=[t416], writes=[el])
                S.op('dve', lambda e: e.tensor_reduce(out=sm['m1'][:], in_=el[:], axis=AX.X, op=ALU.max), reads=[el], writes=[sm['m1']])
                S.op('dve', lambda e: e.tensor_scalar(out=oh1[:], in0=el[:], scalar1=sm['m1'][:, 0:1], scalar2=None, op0=ALU.is_ge), reads=[el, sm['m1']], writes=[oh1])
                S.op('dve', lambda e: e.scalar_tensor_tensor(out=el2[:], in0=oh1[:], scalar=-1e30, in1=el[:], op0=ALU.mult, op1=ALU.add), reads=[oh1, el], writes=[el2])
                S.op('dve', lambda e: e.tensor_reduce(out=sm['m2'][:], in_=el2[:], axis=AX.X, op=ALU.max), reads=[el2], writes=[sm['m2']])
                S.op('dve', lambda e: e.tensor_scalar(out=oh2[:], in0=el2[:], scalar1=sm['m2'][:, 0:1], scalar2=None, op0=ALU.is_ge), reads=[el2, sm['m2']], writes=[oh2])
                S.op('dve', lambda e: e.tensor_tensor(out=sm['d'][:], in0=sm['m2'][:], in1=sm['m1'][:], op=ALU.subtract), reads=[sm['m1'], sm['m2']], writes=[sm['d']])
                S.op('act', lambda e: e.activation(out=sm['d'][:], in_=sm['d'][:], func=AF.Exp), reads=[sm['d']], writes=[sm['d']])
                S.op('dve', lambda e: e.tensor_scalar(out=sm['d'][:], in0=sm['d'][:], scalar1=1.0, scalar2=None, op0=ALU.add), reads=[sm['d']], writes=[sm['d']])
                S.op('dve', lambda e: e.reciprocal(out=sm['p1'][:], in_=sm['d'][:]), reads=[sm['d']], writes=[sm['p1']])
                S.op('dve', lambda e: e.tensor_tensor(out=wts[:, 0, n:n + 1], in0=sm['p1'][:], in1=sm['gw'][:], op=ALU.mult), reads=[sm['p1'], sm['gw']], writes=[wts])
                S.op('dve', lambda e: e.tensor_tensor(out=wts[:, 1, n:n + 1], in0=sm['gw'][:], in1=wts[:, 0, n:n + 1], op=ALU.subtract), reads=[sm['gw'], wts], writes=[wts])
                for kk, oh in ((0, oh1), (1, oh2)):
                    S.op('dve', lambda e: e.tensor_tensor(out=OH[kk][:, n, :].rearrange("p (g e) -> p g e", g=4),
                                                          in0=ohg[:, :].unsqueeze(2).to_broadcast([128, 4, 16]),
                                                          in1=oh[:, :].unsqueeze(1).to_broadcast([128, 4, 16]), op=ALU.mult), reads=[ohg, oh], writes=[OH[kk]])
                S.op('dve', lambda e: e.tensor_tensor(out=Cb[:, n, :], in0=OH[0][:, n, :], in1=OH[1][:, n, :], op=ALU.add), reads=[OH[0], OH[1]], writes=[Cb])
            ones_bf = S.sb([128, 128], BF16, 'ones_bf', ph)
            S.op('dve', lambda e: e.memset(ones_bf[:], 1.0), writes=[ones_bf])
            Lf = S.sb([128, 128], F32, 'Lf', ph); ld(Lf, Lf[:], Lstd[:, :])
            Lb = S.sb([128, 128], BF16, 'Lb', ph)
            S.op('dve', lambda e: e.tensor_copy(out=Lb[:], in_=Lf[:]), reads=[Lf], writes=[Lb])
            for n in range(NOWN):
                for m in range(n + 1):
                    S.op('pe', lambda e: e.matmul(pPC[:, n * 64:(n + 1) * 64], lhsT=(Lb[:] if m == n else ones_bf[:]), rhs=Cb[:, m, :], start=(m == 0), stop=(m == n)),
                         reads=[Lb, ones_bf, Cb], writes=[pPC])
            for m in range(NOWN):
                S.op('pe', lambda e: e.matmul(pcnt[:, 0:64], lhsT=ones_bf[:], rhs=Cb[:, m, :], start=(m == 0), stop=(m == NOWN - 1)), reads=[ones_bf, Cb], writes=[pcnt])
            CAP = 128
            e128 = S.sb([128, 64], F32, 'e128', ph); ld(e128, e128[:], e128d[:, :])
            ocf = S.sb([128, 64], F32, 'ocf', ph); cnti = S.sb([128, 64], I32, 'cnti', ph); padf = S.sb([128, 64], F32, 'padf', ph)
            S.op('dve', lambda e: e.tensor_scalar(out=ocf[:], in0=pcnt[:, 0:64], scalar1=-float(CAP), scalar2=0.0, op0=ALU.add, op1=ALU.max), reads=[pcnt], writes=[ocf])
            S.op('dve', lambda e: e.tensor_scalar(out=ocf[:], in0=ocf[:], scalar1=127.0, scalar2=None, op0=ALU.add), reads=[ocf], writes=[ocf])
            S.op('dve', lambda e: e.tensor_copy(out=cnti[:], in_=ocf[:]), reads=[ocf], writes=[cnti])
            S.op('dve', lambda e: e.tensor_scalar(out=cnti[:], in0=cnti[:], scalar1=7, scalar2=7, op0=ALU.arith_shift_right, op1=ALU.logical_shift_left), reads=[cnti], writes=[cnti])
            S.op('dve', lambda e: e.tensor_copy(out=padf[:], in_=cnti[:]), reads=[cnti], writes=[padf])
            cs = [S.sb([128, 64], F32, 'cs%d' % i, ph) for i in range(2)]
            S.op('dve', lambda e: e.tensor_copy(out=cs[0][:], in_=padf[:]), reads=[padf], writes=[cs[0]])
            cur = 0
            for s_ in (1, 2, 4, 8, 16, 32):
                a, b_ = cs[cur], cs[1 - cur]
                S.op('dve', lambda e: e.tensor_copy(out=b_[:, 0:s_], in_=a[:, 0:s_]), reads=[a], writes=[b_])
                S.op('dve', lambda e: e.tensor_tensor(out=b_[:, s_:64], in0=a[:, s_:64], in1=a[:, 0:64 - s_], op=ALU.add), reads=[a], writes=[b_])
                cur = 1 - cur
            pend = cs[cur]; ob = cs[1 - cur]
            S.op('dve', lambda e: e.tensor_tensor(out=ob[:], in0=pend[:], in1=padf[:], op=ALU.subtract), reads=[pend, padf], writes=[ob])
            S.op('dve', lambda e: e.tensor_scalar(out=ob[:], in0=ob[:], scalar1=float(64 * 128 - CAP), scalar2=None, op0=ALU.add), reads=[ob], writes=[ob])
            slot_f = S.sb([128, 2, NOWN], F32, 'slot_f', ph)
            tmpb = S.sb([128, NOWN, 64], F32, 'tmpb', ph)
            rk = S.sb([128, NOWN], F32, 'rk', ph); eb = S.sb([128, NOWN], F32, 'eb', ph); obk = S.sb([128, NOWN], F32, 'obk', ph); isov = S.sb([128, NOWN], F32, 'isov', ph)
            for kk in range(2):
                S.op('dve', lambda e: e.tensor_tensor(out=tmpb[:], in0=OH[kk][:], in1=pPC[:, :].rearrange("p (n e) -> p n e", n=NOWN), op=ALU.mult), reads=[OH[kk], pPC], writes=[tmpb])
                S.op('dve', lambda e: e.tensor_reduce(out=rk[:], in_=tmpb[:], axis=AX.X, op=ALU.add), reads=[tmpb], writes=[rk])
                S.op('dve', lambda e: e.tensor_tensor(out=tmpb[:], in0=OH[kk][:], in1=e128[:, :].unsqueeze(1).to_broadcast([128, NOWN, 64]), op=ALU.mult), reads=[OH[kk], e128], writes=[tmpb])
                S.op('dve', lambda e: e.tensor_reduce(out=eb[:], in_=tmpb[:], axis=AX.X, op=ALU.add), reads=[tmpb], writes=[eb])
                S.op('dve', lambda e: e.tensor_tensor(out=tmpb[:], in0=OH[kk][:], in1=ob[:, :].unsqueeze(1).to_broadcast([128, NOWN, 64]), op=ALU.mult), reads=[OH[kk], ob], writes=[tmpb])
                S.op('dve', lambda e: e.tensor_reduce(out=obk[:], in_=tmpb[:], axis=AX.X, op=ALU.add), reads=[tmpb], writes=[obk])
                S.op('dve', lambda e: e.tensor_scalar(out=isov[:], in0=rk[:], scalar1=float(CAP), scalar2=None, op0=ALU.is_ge), reads=[rk], writes=[isov])
                S.op('dve', lambda e: e.tensor_tensor(out=obk[:], in0=obk[:], in1=eb[:], op=ALU.subtract), reads=[obk, eb], writes=[obk])
                S.op('dve', lambda e: e.tensor_tensor(out=obk[:], in0=obk[:], in1=isov[:], op=ALU.mult), reads=[obk, isov], writes=[obk])
                S.op('dve', lambda e: e.tensor_tensor(out=rk[:], in0=rk[:], in1=eb[:], op=ALU.add), reads=[rk, eb], writes=[rk])
                S.op('dve', lambda e: e.tensor_tensor(out=slot_f[:, kk, :], in0=rk[:], in1=obk[:], op=ALU.add), reads=[rk, obk], writes=[slot_f])
            S.op('dve', lambda e: e.tensor_copy(out=slot_i[:], in_=slot_f[:]), reads=[slot_f], writes=[slot_i])
            blk128 = S.sb([128, NOV], F32, 'blk128', ph); ld(blk128, blk128[:], blk128d[:, 0:NOV])
            kcoff = S.sb([128, 16], F32, 'kcoff', ph); ld(kcoff, kcoff[:], kcoffd[:, :])
            pidx = S.sb([128, 1], F32, 'pidx', ph); ld(pidx, pidx[:], pidxd[:, :])
            cmp = S.sb([128, NOV, 64], BF16, 'cmp', ph)
            S.op('dve', lambda e: e.tensor_tensor(out=cmp[:], in0=pend[:, :].unsqueeze(1).to_broadcast([128, NOV, 64]),
                                                  in1=blk128[:, :].unsqueeze(2).to_broadcast([128, NOV, 64]), op=ALU.is_le), reads=[pend, blk128], writes=[cmp])
            bef = S.sb([128, NOV], F32, 'bef', ph); gb = S.sb([128, NOV], F32, 'gb', ph)
            S.op('dve', lambda e: e.tensor_reduce(out=bef[:], in_=cmp[:], axis=AX.X, op=ALU.add), reads=[cmp], writes=[bef])
            skipo = S.sb([128, NOV], F32, 'skipo', ph)
            S.op('dve', lambda e: e.tensor_scalar(out=skipo[:], in0=bef[:], scalar1=64.0, scalar2=float(2 ** 27), op0=ALU.is_ge, op1=ALU.mult), reads=[bef], writes=[skipo])
            S.op('dve', lambda e: e.tensor_scalar(out=bef[:], in0=bef[:], scalar1=63.0, scalar2=None, op0=ALU.min), reads=[bef], writes=[bef])
            idxf = S.sb([128, NOV, 16], F32, 'idxf', ph)
            for mult, nk, dst in ((2048.0, 16, idxg_i), (1024.0, 8, idxd_i)):
                S.op('dve', lambda e: e.tensor_scalar(out=gb[:], in0=bef[:], scalar1=mult, scalar2=pidx[:, 0:1], op0=ALU.mult, op1=ALU.add), reads=[bef, pidx], writes=[gb])
                S.op('dve', lambda e: e.tensor_tensor(out=gb[:], in0=gb[:], in1=skipo[:], op=ALU.add), reads=[gb, skipo], writes=[gb])
                S.op('dve', lambda e: e.tensor_tensor(out=idxf[:, :, 0:nk], in0=gb[:, :].unsqueeze(2).to_broadcast([128, NOV, nk]),
                                                      in1=kcoff[:, 0:nk].unsqueeze(1).to_broadcast([128, NOV, nk]), op=ALU.add), reads=[gb, kcoff], writes=[idxf])
                S.op('dve', lambda e: e.tensor_copy(out=dst[:], in_=idxf[:, :, 0:nk]), reads=[idxf], writes=[dst])
            ri0 = S.sb([128, NBLK, 16], I32, 'ri0', ph)
            S.op('dve', lambda e: e.memset(ri0[:], 0), writes=[ri0])
            S.op('dve', lambda e: e.memset(ri0[:, :, 0:1], 1024), writes=[ri0])
            st(rinfo, rinfo[:, :].rearrange("(b p) c -> p b c", p=128), ri0, ri0[:])
            tokid = S.sb([128, NOWN], I32, 'tokid', ph); ld(tokid, tokid[:], tokidd[:, :])
            ris = [S.sb([128, 16], I32, 'ri%d' % i, ph) for i in range(4)]
            for r_ in ris:
                S.op('dve', lambda e: e.memset(r_[:], 0), writes=[r_])
            it = 0
            for n in range(NOWN):
                for kk in range(2):
                    r_ = ris[it % 4]
                    S.op('dve', lambda e: e.tensor_copy(out=r_[:, 0:1], in_=tokid[:, n:n + 1]), reads=[tokid], writes=[r_])
                    S.op('dve', lambda e: e.tensor_copy(out=r_[:, 1:2].bitcast(F32), in_=wts[:, kk, n:n + 1]), reads=[wts], writes=[r_])
                    S.dma('pool', lambda e: e.indirect_dma_start(out=rinfo[:, :], out_offset=bass.IndirectOffsetOnAxis(ap=slot_i[:, kk, n:n + 1], axis=0),
                                                                 in_=r_[:], in_offset=None), reads=[r_, slot_i], writes=[rinfo])
                    it += 1
            S.barrier()
        if stop_after == 'moe_route':
            o1 = dout("d_slot", [128, 2, NOWN], I32); st(T(o1, 'o'), o1[:, :, :], slot_i, slot_i[:], is_out=True)
            o2 = dout("d_idxg", [128, NOV, 16], I32); st(T(o2, 'o'), o2[:, :, :], idxg_i, idxg_i[:], is_out=True)
            o3 = dout("d_rinfo", [NBLK * 128, 16], I32)
            t = S.sb([128, NBLK, 16], I32, 'dbgr')
            ld(t, t[:], rinfo[:, :].rearrange("(b p) c -> p b c", p=128), reads=[rinfo])
            st(T(o3, 'o'), o3[:, :].rearrange("(b p) c -> p b c", p=128), t, t[:], is_out=True)
            S.finish()
            return nc, list(dbgo)
        with ExitStack() as ph:
            Wg_t = [S.sb([128, 1024], BF16, 'Wg_t%d' % i, ph) for i in range(16)]
            Wu_t = [S.sb([128, 1024], BF16, 'Wu_t%d' % i, ph) for i in range(16)]
            Wd_t = [S.sb([128, D], BF16, 'Wd_t%d' % i, ph) for i in range(8)]
            rt_all = S.sb([128, NBLK, 16], I32, 'rt_all', ph)
            ld(rt_all, rt_all[:], rinfo[:, :].rearrange("(b p) c -> p b c", p=128), reads=[rinfo])
            xgs = [S.sb([128, D], BF16, 'xg%d' % i, ph) for i in range(2)]
            xgT = S.sb([128, KC, 128], BF16, 'xgT', ph)
            sg = S.sb([128, 1024], F32, 'sg', ph)
            actb = S.sb([128, 1024], BF16, 'actb', ph)
            actT = S.sb([128, 8, 128], BF16, 'actT', ph)
            ybs = [S.sb([128, D], F32, 'yb%d' % i, ph) for i in range(2)]
            pT1 = S.ps([128, 512], F32, 'pT1', ph); pT1v = pT1[:, :].bitcast(BF16)
            pT2 = S.ps([128, 512], F32, 'pT2', ph); pT2v = pT2[:, :].bitcast(BF16)
            pgu = [S.ps([128, 512], F32, 'pgu%d' % i, ph) for i in range(4)]
            pdn = [S.ps([128, 512], F32, 'pdn%d' % i, ph) for i in range(2)]
            bc_g = nc.gpsimd.to_reg(64 * D - 1); bc_d = nc.gpsimd.to_reg(64 * 1024 - 1)
            for b in range(NBLK):
                rt = rt_all; xg = xgs[b % 2]; yb = ybs[b % 2]
                S.dma('pool', lambda e: e.indirect_dma_start(out=xg[:], out_offset=None, in_=H2[:, :],
                                                             in_offset=bass.IndirectOffsetOnAxis(ap=rt_all[:, b, 0:1], axis=0)), reads=[rt_all, H2], writes=[xg])
                if b < 64:
                    for kc in range(16):
                        r0 = b * 2048 + kc * 128
                        S.dma('pool', lambda e: e.dma_start(out=Wg_t[kc][:], in_=w_eg[r0:r0 + 128, :]), writes=[Wg_t[kc]])
                        S.dma('pool', lambda e: e.dma_start(out=Wu_t[kc][:], in_=w_eu[r0:r0 + 128, :]), writes=[Wu_t[kc]])
                    for kc in range(8):
                        r0 = b * 1024 + kc * 128
                        S.dma('pool', lambda e: e.dma_start(out=Wd_t[kc][:], in_=w_ed[r0:r0 + 128, :]), writes=[Wd_t[kc]])
                else:
                    ob_ = b - 64
                    for kc in range(16):
                        S.dma('pool', lambda e: e.indirect_dma_start(out=Wg_t[kc][:], out_offset=None, in_=w_eg[:, :],
                                                                     in_offset=bass.IndirectOffsetOnAxis(ap=idxg_i[:, ob_, kc:kc + 1], axis=0), bounds_check=bc_g, oob_is_err=False), reads=[idxg_i], writes=[Wg_t[kc]])
                        S.dma('pool', lambda e: e.indirect_dma_start(out=Wu_t[kc][:], out_offset=None, in_=w_eu[:, :],
                                                                     in_offset=bass.IndirectOffsetOnAxis(ap=idxg_i[:, ob_, kc:kc + 1], axis=0), bounds_check=bc_g, oob_is_err=False), reads=[idxg_i], writes=[Wu_t[kc]])
                    for kc in range(8):
                        S.dma('pool', lambda e: e.indirect_dma_start(out=Wd_t[kc][:], out_offset=None, in_=w_ed[:, :],
                                                                     in_offset=bass.IndirectOffsetOnAxis(ap=idxd_i[:, ob_, kc:kc + 1], axis=0), bounds_check=bc_d, oob_is_err=False), reads=[idxd_i], writes=[Wd_t[kc]])
                for half in range(2):
                    for kk in range(8):
                        k = half * 8 + kk
                        transpose_bf(pT1, pT1v[:, kk * 128:(kk + 1) * 128], xg, xg[:, k * 128:(k + 1) * 128])
                    S.op('act', lambda e: e.copy(out=xgT[:, half * 8:(half + 1) * 8, :], in_=pT1v[:, 0:1024].rearrange("p (a b) -> p a b", a=8)), reads=[pT1], writes=[xgT])
                for kc in range(16):
                    for wi, wt in ((0, Wg_t[kc]), (1, Wu_t[kc])):
                        for nb in range(2):
                            pb = pgu[wi * 2 + nb]
                            S.op('pe', lambda e: e.matmul(pb[:], lhsT=xgT[:, kc, :], rhs=wt[:, nb * 512:(nb + 1) * 512], start=(kc == 0), stop=(kc == 15)),
                                 reads=[xgT, wt], writes=[pb])
                for nb in range(2):
                    S.op('act', lambda e: e.activation(out=sg[:, nb * 512:(nb + 1) * 512], in_=pgu[nb][:], func=AF.Silu), reads=[pgu[nb]], writes=[sg])
                    S.op('dve', lambda e: e.tensor_tensor(out=actb[:, nb * 512:(nb + 1) * 512], in0=sg[:, nb * 512:(nb + 1) * 512], in1=pgu[2 + nb][:], op=ALU.mult),
                         reads=[sg, pgu[2 + nb]], writes=[actb])
                for kk in range(8):
                    transpose_bf(pT2, pT2v[:, kk * 128:(kk + 1) * 128], actb, actb[:, kk * 128:(kk + 1) * 128])
                S.op('act', lambda e: e.copy(out=actT[:], in_=pT2v[:, 0:1024].rearrange("p (a b) -> p a b", a=8)), reads=[pT2], writes=[actT])
                for hf in range(2):
                    for kc in range(8):
                        for i in range(2):
                            c0 = hf * 1024 + i * 512
                            S.op('pe', lambda e: e.matmul(pdn[i][:], lhsT=actT[:, kc, :], rhs=Wd_t[kc][:, c0:c0 + 512], start=(kc == 0), stop=(kc == 7)),
                                 reads=[actT, Wd_t[kc]], writes=[pdn[i]])
                    for i in range(2):
                        c0 = hf * 1024 + i * 512
                        S.op('dve', lambda e: e.tensor_scalar(out=yb[:, c0:c0 + 512], in0=pdn[i][:], scalar1=rt_all[:, b, 1:2].bitcast(F32), scalar2=None, op0=ALU.mult),
                             reads=[pdn[i], rt_all], writes=[yb])
                st(Yd, Yd[b * 128:(b + 1) * 128, :], yb, yb[:])
            S.barrier()
        with ExitStack() as ph:
            g2b = S.sb([128, D], F32, 'g2b', ph); ld(g2b, g2b[:], modd[5:6, :].partition_broadcast(128), reads=[modd])
            gfb = S.sb([128, D], F32, 'gfb', ph); ld(gfb, gfb[:], nfg[0:1, :].partition_broadcast(128))
            xm = [S.sb([128, D], F32, 'xm%d' % i, ph) for i in range(2)]
            y1 = [S.sb([128, D], F32, 'y1%d' % i, ph) for i in range(2)]
            y2 = [S.sb([128, D], F32, 'y2%d' % i, ph) for i in range(2)]
            junkb = S.sb([128, D], BF16, 'junkb', ph)
            for n in range(NOWN):
                x_, a_, b_ = xm[n % 2], y1[n % 2], y2[n % 2]
                ld(x_, x_[:], xmid[n * 128:(n + 1) * 128, :], reads=[xmid])
                for kk, dst in ((0, a_), (1, b_)):
                    S.dma('pool', lambda e: e.indirect_dma_start(out=dst[:], out_offset=None, in_=Yd[:, :],
                                                                 in_offset=bass.IndirectOffsetOnAxis(ap=slot_i[:, kk, n:n + 1], axis=0)), reads=[slot_i, Yd], writes=[dst])
                S.op('dve', lambda e: e.tensor_tensor(out=a_[:], in0=a_[:], in1=b_[:], op=ALU.add), reads=[a_, b_], writes=[a_])
                S.op('dve', lambda e: e.tensor_tensor(out=a_[:], in0=a_[:], in1=g2b[:], op=ALU.mult), reads=[a_, g2b], writes=[a_])
                S.op('dve', lambda e: e.tensor_tensor(out=a_[:], in0=a_[:], in1=x_[:], op=ALU.add), reads=[a_, x_], writes=[a_])
                rms_rstd(a_, a_[:], junkb, ss, rstd)
                S.op('dve', lambda e: e.scalar_tensor_tensor(out=a_[:], in0=a_[:], scalar=rstd[:, 0:1], in1=gfb[:], op0=ALU.mult, op1=ALU.mult),
                     reads=[a_, rstd, gfb], writes=[a_])
                st(T(out, 'out'), out[n * 128:(n + 1) * 128, :], a_, a_[:], is_out=True)
        S.finish()
    return nc, list(dbgo)


def _consts():
    c = {}
    c["ident"] = np.eye(128, dtype=np.float32)
    i = np.arange(128)[:, None]; j = np.arange(256)[None, :]
    valid = (j > i) & (j <= i + 128)
    c["mask"] = np.where(valid, 0.0, -1e30).astype(np.float32)
    gam = np.array(GAM, dtype=np.float64)
    lg = np.log(gam)
    e = np.arange(128)[:, None, None]; cc = np.arange(128)[None, None, :]
    diff = cc - e
    c["decT"] = np.where(diff >= 0, np.exp(np.maximum(diff, 0) * lg[None, :, None]), 0.0).astype(np.float32)
    idx = np.arange(128)[:, None].astype(np.float64)
    c["dq"] = np.exp((idx + 1.0) * lg[None, :]).astype(np.float32)
    c["dk"] = (np.exp((127.0 - idx) * lg[None, :]) / 16.0).astype(np.float32)
    invf = (10000.0 ** (-(np.arange(128, dtype=np.float32) / np.float32(128)))).astype(np.float32)
    c["invf"] = np.broadcast_to(invf[None, :], (128, 128)).copy()
    c["Lst"] = (np.arange(128)[:, None] < np.arange(128)[None, :]).astype(np.float32)
    c["tokid"] = (np.arange(NOWN)[None, :] * 128 + np.arange(128)[:, None]).astype(np.int32)
    c["blk128"] = np.broadcast_to((np.arange(NBLK, dtype=np.float32) * 128.0)[None, :], (128, NBLK)).copy()
    c["kcoff"] = np.broadcast_to((np.arange(16, dtype=np.float32) * 128.0)[None, :], (128, 16)).copy()
    c["pidx"] = np.arange(128, dtype=np.float32)[:, None].copy()
    c["e128"] = np.broadcast_to((np.arange(64, dtype=np.float32) * 128.0)[None, :], (128, 64)).copy()
    return c


def prep_inputs(x, c, positions, norm1_gain, norm2_gain, final_norm_gain, w_ada, b_ada, w_in,
                attn_sinks, ret_norm_gain, w_branch_attn, w_branch_ret, w_out,
                w_router_group, b_router_group, w_router_expert, b_router_expert,
                w_expert_gate, w_expert_up, w_expert_down):
    f = lambda a: np.ascontiguousarray(np.asarray(a))
    x = f(x); c = f(c); positions = f(positions)
    shared = dict(
        w_ada=f(w_ada)[0], b_ada=f(b_ada), w_in=f(w_in)[0], attn_sinks=f(attn_sinks), ret_norm_gain=f(ret_norm_gain),
        w_out=f(w_out)[0],
        w_router=np.concatenate([f(w_router_group)[0], f(w_router_expert)[0]], axis=1),
        b_router=np.concatenate([f(b_router_group), f(b_router_expert)], axis=1),
        w_expert_gate=f(w_expert_gate).reshape(64 * D, 1024), w_expert_up=f(w_expert_up).reshape(64 * D, 1024),
        w_expert_down=f(w_expert_down).reshape(64 * 1024, D),
        norm1_gain=f(norm1_gain), norm2_gain=f(norm2_gain), final_norm_gain=f(final_norm_gain).reshape(1, D),
    )
    def tile_cols(w, off):
        sub = w[:, off:off + D].reshape(KC, 128, 16, 128)
        return np.ascontiguousarray(sub.transpose(2, 1, 0, 3)).reshape(16 * 128, KC * 128)
    w_in0 = shared["w_in"]
    shared["wm_t"] = np.concatenate([tile_cols(f(w_branch_attn)[0], 0), tile_cols(f(w_branch_ret)[0], 0),
                                     tile_cols(w_in0, OFF_GA), tile_cols(w_in0, OFF_GTR)], axis=0)
    shared.update(_consts())
    gam = np.array(GAM, dtype=np.float64); lg = np.log(gam)
    maps = []
    for core in range(8):
        b, q = core // 4, core % 4
        m = dict(shared)
        m["xo"] = x[b, q * 1024:(q + 1) * 1024]
        npre = q * 8
        xp = np.zeros((NPRE * 128, D), np.float32)
        pp = np.zeros((NPRE * 128,), np.int32)
        if npre:
            xp[(NPRE - npre) * 128:] = x[b, :q * 1024]
            pp[(NPRE - npre) * 128:] = positions[b, :q * 1024]
        m["xp"] = xp
        m["pos_o"] = np.ascontiguousarray(positions[b, q * 1024:(q + 1) * 1024].reshape(NOWN, 128).T)
        m["pos_p"] = np.ascontiguousarray(pp.reshape(NPRE, 128).T)
        m["cT"] = np.ascontiguousarray(c[b].reshape(KC, 128).T)
        valid = (np.arange(NPRE) >= NPRE - npre).astype(np.float64)
        idx = np.arange(128)[:, None, None].astype(np.float64)
        jj = np.arange(NPRE)[None, :, None].astype(np.float64)
        pk = np.exp((127.0 - idx) * lg[None, None, :] + 128.0 * (NPRE - 1 - jj) * lg[None, None, :]) / 16.0 * valid[None, :, None]
        m["pk"] = pk.astype(np.float32)
        mk = shared["mask"].copy()
        if q == 0:
            mk[:, :128] = -1e30
        m["mask0"] = mk
        maps.append(m)
    return maps


def kernel(**inputs):
    maps = prep_inputs(**inputs)
    nc, _ = build()
    res = run_bass_kernel_spmd(nc, maps, core_ids=list(range(8)))
    outp = np.zeros((2, 4096, D), np.float32)
    for core in range(8):
        b, q = core // 4, core % 4
        outp[b, q * 1024:(q + 1) * 1024] = res.results[core]["out"]
    return outp
```

```python
import numpy as np
import concourse.bass as bass
import concourse.mybir as mybir
from concourse.bass_utils import run_bass_kernel_spmd

F32 = mybir.dt.float32
BF16 = mybir.dt.bfloat16
I32 = mybir.dt.int32
U32 = mybir.dt.uint32
ALU = mybir.AluOpType
AF = mybir.ActivationFunctionType
AX = mybir.AxisListType


class Tr:
    def __init__(self):
        self.last_w = None
        self.readers = {}


class T:
    def __init__(self, h, name, tr=None):
        self.h = h
        self.name = name
        self.tr = tr or Tr()

    @property
    def last_w(self):
        return self.tr.last_w

    @last_w.setter
    def last_w(self, v):
        self.tr.last_w = v

    @property
    def readers(self):
        return self.tr.readers

    @readers.setter
    def readers(self, v):
        self.tr.readers = v

    def v(self, ap):
        return T(ap, self.name, self.tr)

    def __getitem__(self, k):
        return self.h[k]


class Sched:
    ENG = ('pe', 'dve', 'act', 'pool', 'sp')

    def __init__(self, nc, es):
        self.nc = nc
        self.es = es
        self.eng = {'pe': nc.tensor, 'dve': nc.vector, 'act': nc.scalar, 'pool': nc.gpsimd, 'sp': nc.sync}
        self.ops = {e: [] for e in self.ENG}
        self.cnt = {e: 0 for e in self.ENG}
        self.sem = {e: es.enter_context(nc.semaphore('s_' + e)) for e in self.ENG if e != 'sp'}
        self.seen = {e: {} for e in self.ENG}
        self.NP = 8
        self.dsem = {q: [es.enter_context(nc.semaphore('d_%s%d' % (q, i))) for i in range(self.NP)]
                     for q in ('sp', 'pool', 'act')}
        self.dn = {q: 0 for q in ('sp', 'pool', 'act')}
        self.ntile = 0
        self.out_tokens = []

    def sb(self, shape, dt, name=None, es=None):
        self.ntile += 1
        name = (name or 't') + '_%d' % self.ntile
        h = (es or self.es).enter_context(self.nc.sbuf_tensor(name, list(shape), dt))
        return T(h, name)

    def ps(self, shape, dt, name=None, es=None):
        self.ntile += 1
        name = (name or 'p') + '_%d' % self.ntile
        h = (es or self.es).enter_context(self.nc.psum_tensor(name, list(shape), dt))
        return T(h, name)

    def barrier(self):
        toks = [('eng', f, self.cnt[f]) for f in ('pe', 'dve', 'act', 'pool') if self.cnt[f] > 0]
        for q in ('sp', 'pool', 'act'):
            n = self.dn[q]
            for i in range(max(0, n - self.NP), n):
                toks.append(('dma', self.dsem[q][i % self.NP], 16 * (i // self.NP + 1)))
        for e in self.ENG:
            for tok in toks:
                self._wait(e, tok)

    def dram(self, name, shape, dt, kind='Internal'):
        h = self.nc.dram_tensor(name, list(shape), dt, kind=kind)
        return T(h, name)

    def _wait(self, e, tok):
        if tok is None:
            return
        kind, key, val = tok
        if kind == 'eng' and key == e and e == 'pe':
            return
        if kind == 'eng' and key == 'sp':
            return
        sk = (kind, key if kind == 'eng' else id(key))
        if self.seen[e].get(sk, 0) >= val:
            return
        self.seen[e][sk] = val
        sem = self.sem[key] if kind == 'eng' else key
        eng = self.eng[e]
        eng.wait_ge(sem, val)

    def _deps(self, e, reads, writes):
        for t in reads:
            self._wait(e, t.last_w)
        for t in writes:
            self._wait(e, t.last_w)
            for tok in list(t.readers.values()):
                self._wait(e, tok)

    def _update(self, tok, reads, writes):
        for t in reads:
            kind, key, val = tok
            rk = (kind, key if kind == 'eng' else (id(key)))
            t.readers[rk] = tok
        for t in writes:
            t.last_w = tok
            t.readers = {}

    def op(self, e, fn, reads=(), writes=()):
        assert e in ('pe', 'dve', 'act', 'pool')
        self._deps(e, reads, writes)
        self.cnt[e] += 1
        idx = self.cnt[e]
        eng = self.eng[e]
        sem = self.sem[e]
        fn(eng).then_inc(sem, 1)
        self._update(('eng', e, idx), reads, writes)

    def dma(self, q, fn, reads=(), writes=(), is_out=False):
        self._deps(q, reads, writes)
        n = self.dn[q]
        self.dn[q] += 1
        slot = n % self.NP
        sem = self.dsem[q][slot]
        if n >= self.NP:
            self._wait(q, ('dma', sem, 16 * (n // self.NP)))
        val = 16 * (n // self.NP + 1)
        eng = self.eng[q]
        fn(eng).then_inc(sem, 16)
        tok = ('dma', sem, val)
        self._update(tok, reads, writes)
        if is_out:
            self.out_tokens.append(tok)
        return tok

    def finish(self):
        for tok in self.out_tokens:
            self._wait('sp', tok)
        for q in ('sp', 'pool', 'act'):
            n = self.dn[q]
            for i in range(max(0, n - self.NP), n):
                self._wait('sp', ('dma', self.dsem[q][i % self.NP], 16 * (i // self.NP + 1)))


import math
from contextlib import ExitStack

D = 2048
KC = 16
NOWN = 8
NPRE = 24
NBLK = 80
NOV = 16
OFF_QA, OFF_KA, OFF_VA, OFF_QR, OFF_KR, OFF_VR, OFF_GR, OFF_GA, OFF_GTR = 0, 2048, 2304, 2560, 4608, 6656, 8704, 10752, 12800
EPS = 1e-6
TWO_PI = 2.0 * math.pi
C1 = 6.28125
C2 = TWO_PI - C1
GAM = [1.0 - 2.0 ** (-5.0 - h) for h in range(8)]


def build(stop_after=None):
    nc = bass.Bass("TRN2", target_bir_lowering=False)

    def din(name, shape, dt=F32):
        return nc.dram_tensor(name, list(shape), dt, kind="ExternalInput")

    xo = din("xo", [1024, D]); xp = din("xp", [NPRE * 128, D])
    pos_o = din("pos_o", [128, NOWN], I32); pos_p = din("pos_p", [128, NPRE], I32)
    cT = din("cT", [128, KC]); pkd = din("pk", [128, NPRE, 8]); mask0d = din("mask0", [128, 256])
    w_ada = din("w_ada", [D, 6 * D]); b_ada = din("b_ada", [1, 6 * D]); w_in = din("w_in", [D, 14848])
    sinks = din("attn_sinks", [1, 32]); rgain = din("ret_norm_gain", [1, D])
    wm_t = din("wm_t", [4 * 16 * 128, D]); w_o = din("w_out", [D, D])
    w_rt = din("w_router", [D, 68]); b_rt = din("b_router", [1, 68])
    if stop_after is None or stop_after == 'moe_full':
        w_eg = din("w_expert_gate", [64 * D, 1024]); w_eu = din("w_expert_up", [64 * D, 1024]); w_ed = din("w_expert_down", [64 * 1024, D])
    n1g = din("norm1_gain", [1, D]); n2g = din("norm2_gain", [1, D]); nfg = din("final_norm_gain", [1, D])
    identd = din("ident", [128, 128]); maskd = din("mask", [128, 256]); decTd = din("decT", [128, 8, 128])
    dqd = din("dq", [128, 8]); dkd = din("dk", [128, 8]); invfd = din("invf", [128, 128]); Lstd = din("Lst", [128, 128])
    tokidd = din("tokid", [128, NOWN], I32); blk128d = din("blk128", [128, NBLK]); kcoffd = din("kcoff", [128, 16]); pidxd = din("pidx", [128, 1]); e128d = din("e128", [128, 64])
    out = nc.dram_tensor("out", [1024, D], F32, kind="ExternalOutput")
    dbgo = {}

    def dout(name, shape, dt=F32):
        dbgo[name] = nc.dram_tensor(name, list(shape), dt, kind="ExternalOutput")
        return dbgo[name]

    modd = T(nc.dram_tensor("modd", [6, D], F32, kind="Internal"), "modd")
    xmid = T(nc.dram_tensor("xmid", [1024, D], F32, kind="Internal"), "xmid")
    H2 = T(nc.dram_tensor("H2", [1025, D], BF16, kind="Internal"), "H2")
    rinfo = T(nc.dram_tensor("rinfo", [NBLK * 128, 16], I32, kind="Internal"), "rinfo")
    Yd = T(nc.dram_tensor("Yd", [NBLK * 128, D], F32, kind="Internal"), "Yd")

    with ExitStack() as es:
        S = Sched(nc, es)
        qrr = [0]

        def ld(dst_T, dst_ap, src_ap, q='sp', reads=(), extra_w=()):
            return S.dma(q, lambda e: e.dma_start(out=dst_ap, in_=src_ap), reads=list(reads), writes=[dst_T] + list(extra_w))

        def st(dst_T, dst_ap, src_T, src_ap, q='sp', is_out=False):
            return S.dma(q, lambda e: e.dma_start(out=dst_ap, in_=src_ap), reads=[src_T], writes=[dst_T], is_out=is_out)

        idf = S.sb([128, 128], F32, 'idf'); ld(idf, idf[:], identd[:, :])
        idb = S.sb([128, 128], BF16, 'idb')
        S.op('dve', lambda e: e.tensor_copy(out=idb[:], in_=idf[:]), reads=[idf], writes=[idb])

        def transpose_bf(ps_T, ps_ap, src_T, src_ap):
            S.op('pe', lambda e: e.transpose(out=ps_ap, in_=src_ap, identity=idb[:]), reads=[src_T, idb], writes=[ps_T])

        def rms_rstd(x_T, x_ap, junk_T, ss, rstd, n=D):
            S.op('act', lambda e: e.activation(out=junk_T[:], in_=x_ap, func=AF.Square, accum_out=ss[:]), reads=[x_T], writes=[junk_T, ss])
            S.op('act', lambda e: e.activation(out=ss[:], in_=ss[:], func=AF.Sqrt, scale=1.0 / n, bias=EPS), reads=[ss], writes=[ss])
            S.op('dve', lambda e: e.reciprocal(out=rstd[:], in_=ss[:]), reads=[ss], writes=[rstd])

        with ExitStack() as ph:
            cs = S.sb([128, KC], F32, 'cs', ph); ld(cs, cs[:], cT[:, :])
            sc = S.sb([128, KC], F32, 'sc', ph)
            S.op('act', lambda e: e.activation(out=sc[:], in_=cs[:], func=AF.Silu), reads=[cs], writes=[sc])
            scb = S.sb([128, KC, 128], BF16, 'scb', ph)
            S.op('dve', lambda e: e.tensor_copy(out=scb[:], in_=sc[:, :].unsqueeze(2).to_broadcast([128, KC, 128])), reads=[sc], writes=[scb])
            g1 = S.sb([128, D], F32, 'g1', ph); ld(g1, g1[:], n1g[0:1, :].partition_broadcast(128))
            g2 = S.sb([128, D], F32, 'g2', ph); ld(g2, g2[:], n2g[0:1, :].partition_broadcast(128))
            wbuf = [S.sb([128, KC, 512], BF16, 'wada%d' % i, ph) for i in range(3)]
            bbuf = [S.sb([128, 512], F32, 'bada%d' % i, ph) for i in range(2)]
            rbuf = [S.sb([128, 512], F32, 'rada%d' % i, ph) for i in range(2)]
            pms = [S.ps([128, 512], F32, 'pada%d' % i, ph) for i in range(2)]
            it = 0
            for j in range(6):
                for nb in range(4):
                    c0 = j * D + nb * 512
                    wb, bb, rb, pm = wbuf[it % 3], bbuf[it % 2], rbuf[it % 2], pms[it % 2]
                    for kq in range(4):
                        S.dma('pool', lambda e: e.dma_start(out=wb[:, kq * 4:(kq + 1) * 4, :],
                                                            in_=w_ada[kq * 512:(kq + 1) * 512, c0:c0 + 512].rearrange("(k p) n -> p k n", p=128)), writes=[wb])
                    ld(bb, bb[:], b_ada[0:1, c0:c0 + 512].partition_broadcast(128))
                    for k in range(KC):
                        S.op('pe', lambda e: e.matmul(pm[:], lhsT=scb[:, k, :], rhs=wb[:, k, :], start=(k == 0), stop=(k == KC - 1)),
                             reads=[scb, wb], writes=[pm])
                    S.op('dve', lambda e: e.tensor_tensor(out=rb[:], in0=pm[:], in1=bb[:], op=ALU.add), reads=[pm, bb], writes=[rb])
                    if j in (1, 4):
                        gg = g1 if j == 1 else g2
                        S.op('dve', lambda e: e.scalar_tensor_tensor(out=rb[:], in0=rb[:], scalar=1.0, in1=gg[:, nb * 512:(nb + 1) * 512],
                                                                     op0=ALU.add, op1=ALU.mult), reads=[rb, gg], writes=[rb])
                    st(modd, modd[j:j + 1, nb * 512:(nb + 1) * 512], rb, rb[0:1, :])
                    it += 1
            S.barrier()
        if stop_after == 'ada':
            o = dout("d_mod", [6, D])
            t = S.sb([6, D], F32, 'dbg'); ld(t, t[:], modd[:, :], reads=[modd])
            st(T(o, 'o'), o[:, :], t, t[:], is_out=True)
            S.finish()
            return nc, list(dbgo)
        mx = es.enter_context(ExitStack())
        state_f = S.sb([128, 8, 512], F32, 'state_f', mx)
        S.op('dve', lambda e: e.memset(state_f[:], 0.0), writes=[state_f])
        invf = S.sb([128, 128], F32, 'invf', mx); ld(invf, invf[:], invfd[:, :])
        rp_a = S.sb([128, 128], F32, 'rp_a', mx); rp_b = S.sb([128, 128], F32, 'rp_b', mx)
        rp_k = S.sb([128, 128], I32, 'rp_k', mx); rp_f = S.sb([128, 128], F32, 'rp_f', mx)
        ss = S.sb([128, 1], F32, 'ss', mx); rstd = S.sb([128, 1], F32, 'rstd', mx)

        def rope_tables(pos_T, pos_ap, cos_T, cos_ap, sin_T, sin_ap):
            S.op('dve', lambda e: e.tensor_scalar(out=rp_a[:], in0=invf[:], scalar1=pos_ap, scalar2=None, op0=ALU.mult),
                 reads=[invf, pos_T], writes=[rp_a])
            for which in (0, 1):
                dst_T, dst_ap = (sin_T, sin_ap) if which == 0 else (cos_T, cos_ap)
                if which == 1:
                    S.op('dve', lambda e: e.tensor_scalar(out=rp_a[:], in0=rp_a[:], scalar1=math.pi / 2, scalar2=None, op0=ALU.add),
                         reads=[rp_a], writes=[rp_a])
                S.op('dve', lambda e: e.tensor_scalar(out=rp_k[:], in0=rp_a[:], scalar1=1.0 / TWO_PI, scalar2=None, op0=ALU.mult),
                     reads=[rp_a], writes=[rp_k])
                S.op('dve', lambda e: e.tensor_copy(out=rp_f[:], in_=rp_k[:]), reads=[rp_k], writes=[rp_f])
                S.op('dve', lambda e: e.scalar_tensor_tensor(out=rp_b[:], in0=rp_f[:], scalar=-C1, in1=rp_a[:], op0=ALU.mult, op1=ALU.add),
                     reads=[rp_f, rp_a], writes=[rp_b])
                S.op('dve', lambda e: e.scalar_tensor_tensor(out=rp_b[:], in0=rp_f[:], scalar=-C2, in1=rp_b[:], op0=ALU.mult, op1=ALU.add),
                     reads=[rp_f, rp_b], writes=[rp_b])
                S.op('dve', lambda e: e.tensor_scalar(out=rp_b[:], in0=rp_b[:], scalar1=3.1415925, scalar2=-3.1415925, op0=ALU.min, op1=ALU.max),
                     reads=[rp_b], writes=[rp_b])
                S.op('act', lambda e: e.activation(out=dst_ap, in_=rp_b[:], func=AF.Sin), reads=[rp_b], writes=[dst_T])

        def layer_norm_mod(x_T, hb_T, Ab, shb):
            rms_rstd(x_T, x_T[:], hb_T, ss, rstd)
            S.op('dve', lambda e: e.scalar_tensor_tensor(out=x_T[:], in0=x_T[:], scalar=rstd[:, 0:1], in1=Ab[:], op0=ALU.mult, op1=ALU.mult),
                 reads=[x_T, rstd, Ab], writes=[x_T])
            S.op('dve', lambda e: e.tensor_tensor(out=hb_T[:], in0=x_T[:], in1=shb[:], op=ALU.add), reads=[x_T, shb], writes=[hb_T])

        def make_hT(hb_T, dst_T, dst_fn, pTs):
            for half in range(2):
                pT = pTs[half]
                pv_ = pT[:, :].bitcast(BF16)
                for kk in range(8):
                    k = half * 8 + kk
                    transpose_bf(pT, pv_[:, kk * 128:(kk + 1) * 128], hb_T, hb_T[:, k * 128:(k + 1) * 128])
                S.op('act', lambda e: e.copy(out=dst_fn(half), in_=pv_[:, 0:1024].rearrange("p (a b) -> p a b", a=8)),
                     reads=[pT], writes=[dst_T])

        def rotary(src_T, src_ap4, cos_T, cos_ap, sin_T, sin_ap, rot_T, rot_ap4, tA, tB, n):
            cb = cos_ap.unsqueeze(1).to_broadcast([128, n, 128]); sb_ = sin_ap.unsqueeze(1).to_broadcast([128, n, 128])
            t1 = src_ap4[:, :, 0, :]; t2 = src_ap4[:, :, 1, :]
            S.op('dve', lambda e: e.tensor_tensor(out=tA[:, 0:n, :], in0=t1, in1=cb, op=ALU.mult), reads=[src_T, cos_T], writes=[tA])
            S.op('dve', lambda e: e.tensor_tensor(out=tB[:, 0:n, :], in0=t2, in1=sb_, op=ALU.mult), reads=[src_T, sin_T], writes=[tB])
            S.op('dve', lambda e: e.tensor_tensor(out=rot_ap4[:, :, 0, :], in0=tA[:, 0:n, :], in1=tB[:, 0:n, :], op=ALU.subtract),
                 reads=[tA, tB], writes=[rot_T])
            S.op('dve', lambda e: e.tensor_tensor(out=tA[:, 0:n, :], in0=t1, in1=sb_, op=ALU.mult), reads=[src_T, sin_T], writes=[tA])
            S.op('dve', lambda e: e.tensor_tensor(out=tB[:, 0:n, :], in0=t2, in1=cb, op=ALU.mult), reads=[src_T, cos_T], writes=[tB])
            S.op('dve', lambda e: e.tensor_tensor(out=rot_ap4[:, :, 1, :], in0=tA[:, 0:n, :], in1=tB[:, 0:n, :], op=ALU.add),
                 reads=[tA, tB], writes=[rot_T])

        with ExitStack() as ph:
            Wk = [S.sb([128, 4, 2048], BF16, 'Wk%d' % i, ph) for i in range(4)]
            Wv = [S.sb([128, 4, 2048], BF16, 'Wv%d' % i, ph) for i in range(4)]
            for k in range(KC):
                S.dma('pool', lambda e: e.dma_start(out=Wk[k // 4][:, k % 4, :], in_=w_in[k * 128:(k + 1) * 128, OFF_KR:OFF_KR + 2048]), writes=[Wk[k // 4]])
                S.dma('pool', lambda e: e.dma_start(out=Wv[k // 4][:, k % 4, :], in_=w_in[k * 128:(k + 1) * 128, OFF_VR:OFF_VR + 2048]), writes=[Wv[k // 4]])
            A1b = S.sb([128, D], F32, 'A1b', ph); ld(A1b, A1b[:], modd[1:2, :].partition_broadcast(128), reads=[modd])
            sh1b = S.sb([128, D], F32, 'sh1b', ph); ld(sh1b, sh1b[:], modd[0:1, :].partition_broadcast(128), reads=[modd])
            posi = S.sb([128, NPRE], I32, 'posi', ph); ld(posi, posi[:], pos_p[:, :])
            posf = S.sb([128, NPRE], F32, 'posf', ph)
            S.op('dve', lambda e: e.tensor_copy(out=posf[:], in_=posi[:]), reads=[posi], writes=[posf])
            pkt = S.sb([128, NPRE, 8], F32, 'pkt', ph); ld(pkt, pkt[:], pkd[:, :, :])
            xc = [S.sb([128, D], F32, 'xc%d' % i, ph) for i in range(2)]
            hb = S.sb([128, D], BF16, 'hb', ph)
            hT = S.sb([128, KC, 128], BF16, 'hT', ph)
            cosT = S.sb([128, 128], F32, 'cosT', ph); sinT = S.sb([128, 128], F32, 'sinT', ph)
            rot = S.sb([128, 2, 2, 128], F32, 'rot', ph)
            tA = S.sb([128, 2, 128], F32, 'tA', ph); tB = S.sb([128, 2, 128], F32, 'tB', ph)
            ks = S.sb([128, 8, 256], BF16, 'ks', ph)
            vb = S.sb([128, D], BF16, 'vb', ph)
            pTs = [S.ps([128, 512], F32, 'pT%d' % i, ph) for i in range(2)]
            pkb = [S.ps([128, 512], F32, 'pkb%d' % i, ph) for i in range(2)]
            pvb = [S.ps([128, 512], F32, 'pvb%d' % i, ph) for i in range(2)]
            pst = [S.ps([128, 512], F32, 'pst%d' % i, ph) for i in range(2)]
            ld(xc[0], xc[0][:], xp[0:128, :])
            for j in range(NPRE):
                xcur = xc[j % 2]
                if j + 1 < NPRE:
                    ld(xc[(j + 1) % 2], xc[(j + 1) % 2][:], xp[(j + 1) * 128:(j + 2) * 128, :])
                layer_norm_mod(xcur, hb, A1b, sh1b)
                make_hT(hb, hT, lambda half: hT[:, half * 8:(half + 1) * 8, :], pTs)
                rope_tables(posf, posf[:, j:j + 1], cosT, cosT[:], sinT, sinT[:])
                for nb in range(4):
                    pb = pkb[nb % 2]
                    for k in range(KC):
                        S.op('pe', lambda e: e.matmul(pb[:], lhsT=hT[:, k, :], rhs=Wk[k // 4][:, k % 4, nb * 512:(nb + 1) * 512],
                                                      start=(k == 0), stop=(k == KC - 1)), reads=[hT, Wk[k // 4]], writes=[pb])
                    rotary(pb, pb[:, :].rearrange("p (h t f) -> p h t f", h=2, t=2), cosT, cosT[:], sinT, sinT[:],
                           rot, rot[:], tA, tB, 2)
                    S.op('dve', lambda e: e.tensor_tensor(out=ks[:, 2 * nb:2 * nb + 2, :], in0=rot[:].rearrange("p h t f -> p h (t f)"),
                                                          in1=pkt[:, j, 2 * nb:2 * nb + 2].unsqueeze(2).to_broadcast([128, 2, 256]), op=ALU.mult),
                         reads=[rot, pkt], writes=[ks])
                for nb in range(4):
                    pb = pvb[nb % 2]
                    for k in range(KC):
                        S.op('pe', lambda e: e.matmul(pb[:], lhsT=hT[:, k, :], rhs=Wv[k // 4][:, k % 4, nb * 512:(nb + 1) * 512],
                                                      start=(k == 0), stop=(k == KC - 1)), reads=[hT, Wv[k // 4]], writes=[pb])
                    S.op('act', lambda e: e.copy(out=vb[:, nb * 512:(nb + 1) * 512], in_=pb[:]), reads=[pb], writes=[vb])
                for h in range(8):
                    pb = pst[h % 2]
                    for half in range(2):
                        S.op('pe', lambda e: e.matmul(pb[:, half * 256:(half + 1) * 256], lhsT=ks[:, h, half * 128:(half + 1) * 128],
                                                      rhs=vb[:, h * 256:(h + 1) * 256], start=True, stop=True), reads=[ks, vb], writes=[pb])
                    S.op('dve', lambda e: e.tensor_tensor(out=state_f[:, h, :], in0=state_f[:, h, :], in1=pb[:], op=ALU.add),
                         reads=[state_f, pb], writes=[state_f])
            S.barrier()
        if stop_after == 'prefix':
            o = dout("d_state", [128, 8, 512])
            st(T(o, 'o'), o[:, :, :], state_f, state_f[:], is_out=True)
            S.finish()
            return nc, list(dbgo)
        hT_all = S.sb([128, KC, 1152], BF16, 'hT_all', mx)
        retT = S.sb([128, KC, 1024], BF16, 'retT', mx)
        attnT = S.sb([128, KC, 1024], BF16, 'attnT', mx)
        cos_o = S.sb([128, NOWN, 128], F32, 'cos_o', mx); sin_o = S.sb([128, NOWN, 128], F32, 'sin_o', mx)
        with ExitStack() as ph:
            A1b = S.sb([128, D], F32, 'A1b', ph); ld(A1b, A1b[:], modd[1:2, :].partition_broadcast(128), reads=[modd])
            sh1b = S.sb([128, D], F32, 'sh1b', ph); ld(sh1b, sh1b[:], modd[0:1, :].partition_broadcast(128), reads=[modd])
            posi = S.sb([128, NOWN], I32, 'posio', ph); ld(posi, posi[:], pos_o[:, :])
            posf = S.sb([128, NOWN], F32, 'posfo', ph)
            S.op('dve', lambda e: e.tensor_copy(out=posf[:], in_=posi[:]), reads=[posi], writes=[posf])
            xc = [S.sb([128, D], F32, 'xc%d' % i, ph) for i in range(2)]
            hb = [S.sb([128, D], BF16, 'hb%d' % i, ph) for i in range(2)]
            pTs = [S.ps([128, 512], F32, 'pT%d' % i, ph) for i in range(2)]
            for ci in range(9):
                src = xp[(NPRE - 1) * 128:NPRE * 128, :] if ci == 0 else xo[(ci - 1) * 128:ci * 128, :]
                xcur = xc[ci % 2]; hcur = hb[ci % 2]
                ld(xcur, xcur[:], src)
                layer_norm_mod(xcur, hcur, A1b, sh1b)
                make_hT(hcur, hT_all, lambda half: hT_all[:, half * 8:(half + 1) * 8, ci * 128:(ci + 1) * 128], pTs)
            for n in range(NOWN):
                rope_tables(posf, posf[:, n:n + 1], cos_o, cos_o[:, n, :], sin_o, sin_o[:, n, :])
            S.barrier()

        with ExitStack() as ph:
            Wh = [S.sb([128, 4, 1024], BF16, 'Wh%d' % i, ph) for i in range(4)]
            decT = S.sb([128, 8, 128], F32, 'decT', ph); ld(decT, decT[:], decTd[:, :, :])
            dq = S.sb([128, 8], F32, 'dq', ph); ld(dq, dq[:], dqd[:, :])
            dk = S.sb([128, 8], F32, 'dk', ph); ld(dk, dk[:], dkd[:, :])
            rgb = S.sb([128, D], F32, 'rgb', ph); ld(rgb, rgb[:], rgain[0:1, :].partition_broadcast(128))
            state_b = S.sb([128, 8, 512], BF16, 'state_b', ph)
            S.op('act', lambda e: e.copy(out=state_b[:], in_=state_f[:]), reads=[state_f], writes=[state_b])
            rotq = S.sb([128, 2, 2, 128], F32, 'rotq', ph)
            tA = S.sb([128, 2, 128], F32, 'tA', ph); tB = S.sb([128, 2, 128], F32, 'tB', ph)
            qkb = S.sb([128, 3, 256], BF16, 'qkb', ph)
            kd = S.sb([128, 256], BF16, 'kd', ph)
            vbh = S.sb([128, 256], BF16, 'vbh', ph)
            gs = S.sb([128, 256], F32, 'gs', ph)
            qkT = S.sb([128, 6, 128], BF16, 'qkT', ph)
            iT = S.sb([128, 128], BF16, 'iT', ph)
            junk = S.sb([128, 256], F32, 'junk', ph)
            rtmp = S.sb([128, 256], F32, 'rtmp', ph)
            reth = S.sb([128, 256], BF16, 'reth', ph)
            ss2 = S.sb([128, 1], F32, 'ss2', ph); r2 = S.sb([128, 1], F32, 'r2', ph)
            pp = [S.ps([128, 512], F32, 'pp%d' % i, ph) for i in range(4)]
            pTq = S.ps([128, 512], F32, 'pTq', ph); pTqv = pTq[:, :].bitcast(BF16)
            pi_ = S.ps([128, 512], F32, 'pi', ph); po_ = S.ps([128, 512], F32, 'po', ph); pds = S.ps([128, 512], F32, 'pds', ph)
            it = 0
            for h in range(8):
                offs = [OFF_QR + h * 256, OFF_KR + h * 256, OFF_VR + h * 256, OFF_GR + h * 256]
                for sl in range(4):
                    for wi, off in enumerate(offs):
                        S.dma('pool', lambda e: e.dma_start(out=Wh[sl][:, :, wi * 256:(wi + 1) * 256],
                                                            in_=w_in[sl * 512:(sl + 1) * 512, off:off + 256].rearrange("(k p) n -> p k n", p=128)),
                              writes=[Wh[sl]])
                for n in range(NOWN):
                    tok = slice((n + 1) * 128, (n + 2) * 128)
                    pb0, pb1 = pp[2 * (it % 2)], pp[2 * (it % 2) + 1]
                    for blk, pb in ((0, pb0), (1, pb1)):
                        for k in range(KC):
                            S.op('pe', lambda e: e.matmul(pb[:], lhsT=hT_all[:, k, tok], rhs=Wh[k // 4][:, k % 4, blk * 512:(blk + 1) * 512],
                                                          start=(k == 0), stop=(k == KC - 1)), reads=[hT_all, Wh[k // 4]], writes=[pb])
                    rotary(pb0, pb0[:, :].rearrange("p (h t f) -> p h t f", h=2, t=2), cos_o, cos_o[:, n, :], sin_o, sin_o[:, n, :],
                           rotq, rotq[:], tA, tB, 2)
                    rq = rotq[:, 0, :, :].rearrange("p t f -> p (t f)"); rk = rotq[:, 1, :, :].rearrange("p t f -> p (t f)")
                    S.op('act', lambda e: e.copy(out=qkb[:, 0, :], in_=rq), reads=[rotq], writes=[qkb])
                    S.op('dve', lambda e: e.tensor_scalar(out=qkb[:, 1, :], in0=rq, scalar1=dq[:, h:h + 1], scalar2=None, op0=ALU.mult),
                         reads=[rotq, dq], writes=[qkb])
                    S.op('act', lambda e: e.mul(out=qkb[:, 2, :], in_=rk, mul=1.0 / 16.0), reads=[rotq], writes=[qkb])
                    S.op('dve', lambda e: e.tensor_scalar(out=kd[:], in0=rk, scalar1=dk[:, h:h + 1], scalar2=None, op0=ALU.mult),
                         reads=[rotq, dk], writes=[kd])
                    S.op('act', lambda e: e.copy(out=vbh[:], in_=pb1[:, 0:256]), reads=[pb1], writes=[vbh])
                    S.op('act', lambda e: e.activation(out=gs[:], in_=pb1[:, 256:512], func=AF.Silu), reads=[pb1], writes=[gs])
                    for a in range(3):
                        for half in range(2):
                            transpose_bf(pTq, pTqv[:, (a * 2 + half) * 128:(a * 2 + half + 1) * 128], qkb, qkb[:, a, half * 128:(half + 1) * 128])
                    S.op('act', lambda e: e.copy(out=qkT[:], in_=pTqv[:, 0:768].rearrange("p (a b) -> p a b", a=6)), reads=[pTq], writes=[qkT])
                    for half in range(2):
                        S.op('pe', lambda e: e.matmul(pi_[:, 0:128], lhsT=qkT[:, 4 + half, :], rhs=qkT[:, half, :], start=(half == 0), stop=(half == 1)),
                             reads=[qkT], writes=[pi_])
                    S.op('dve', lambda e: e.tensor_tensor(out=iT[:], in0=pi_[:, 0:128], in1=decT[:, h, :], op=ALU.mult), reads=[pi_, decT], writes=[iT])
                    S.op('pe', lambda e: e.matmul(po_[:, 0:256], lhsT=iT[:], rhs=vbh[:], start=True, stop=False), reads=[iT, vbh], writes=[po_])
                    for half in range(2):
                        S.op('pe', lambda e: e.matmul(po_[:, 0:256], lhsT=qkT[:, 2 + half, :], rhs=state_b[:, h, half * 256:(half + 1) * 256],
                                                      start=False, stop=(half == 1)), reads=[qkT, state_b], writes=[po_])
                    rms_rstd(po_, po_[:, 0:256], junk, ss2, r2, n=256)
                    S.op('dve', lambda e: e.scalar_tensor_tensor(out=rtmp[:], in0=po_[:, 0:256], scalar=r2[:, 0:1], in1=rgb[:, h * 256:(h + 1) * 256],
                                                                 op0=ALU.mult, op1=ALU.mult), reads=[po_, r2, rgb], writes=[rtmp])
                    S.op('dve', lambda e: e.tensor_tensor(out=reth[:], in0=rtmp[:], in1=gs[:], op=ALU.mult), reads=[rtmp, gs], writes=[reth])
                    for half in range(2):
                        transpose_bf(pTq, pTqv[:, 768 + half * 128:768 + (half + 1) * 128], reth, reth[:, half * 128:(half + 1) * 128])
                    S.op('act', lambda e: e.copy(out=retT[:, 2 * h:2 * h + 2, n * 128:(n + 1) * 128],
                                                 in_=pTqv[:, 768:1024].rearrange("p (a b) -> p a b", a=2)), reads=[pTq], writes=[retT])
                    for half in range(2):
                        S.op('pe', lambda e: e.matmul(pds[:, half * 256:(half + 1) * 256], lhsT=kd[:, half * 128:(half + 1) * 128], rhs=vbh[:],
                                                      start=True, stop=True), reads=[kd, vbh], writes=[pds])
                    S.op('dve', lambda e: e.scalar_tensor_tensor(out=state_f[:, h, :], in0=state_f[:, h, :], scalar=float(GAM[h] ** 128), in1=pds[:],
                                                                 op0=ALU.mult, op1=ALU.add), reads=[state_f, pds], writes=[state_f])
                    S.op('act', lambda e: e.copy(out=state_b[:, h, :], in_=state_f[:, h, :]), reads=[state_f], writes=[state_b])
                    it += 1
            S.barrier()
        if stop_after == 'ret':
            o = dout("d_retT", [128, KC, 1024], BF16)
            st(T(o, 'o'), o[:, :, :], retT, retT[:], is_out=True)
            S.finish()
            return nc, list(dbgo)
        with ExitStack() as ph:
            Wg = [S.sb([128, 4, 640], BF16, 'Wg%d' % i, ph) for i in range(4)]
            maskt = S.sb([128, 256], F32, 'maskt', ph); ld(maskt, maskt[:], maskd[:, :])
            mask0t = S.sb([128, 256], F32, 'mask0t', ph); ld(mask0t, mask0t[:], mask0d[:, :])
            sinkb = S.sb([128, 32], F32, 'sinkb', ph); ld(sinkb, sinkb[:], sinks[0:1, :].partition_broadcast(128))
            kT_all = S.sb([128, 2, 1152], BF16, 'kT_all', ph)
            v_all = S.sb([128, 9, 64], BF16, 'v_all', ph)
            qT = S.sb([128, 4, 1024], BF16, 'qT', ph)
            qb = S.sb([128, 512], BF16, 'qb', ph); k2 = S.sb([128, 2, 128], BF16, 'k2', ph)
            S.op('dve', lambda e: e.memset(k2[:], 0.0), writes=[k2])
            S_sb = S.sb([128, 8, 256], F32, 'S_sb', ph)
            P_bf = S.sb([128, 8, 256], BF16, 'P_bf', ph)
            PT = S.sb([128, 8, 2, 128], BF16, 'PT', ph)
            mx8 = S.sb([128, 8], F32, 'mx8', ph); negm = S.sb([128, 8], F32, 'negm', ph); rs = S.sb([128, 8], F32, 'rs', ph)
            es8 = S.sb([128, 8], F32, 'es8', ph); rden = S.sb([128, 8], F32, 'rden', ph)
            at_tok = S.sb([128, 8, 64], BF16, 'at_tok', ph)
            pq0 = S.ps([128, 512], F32, 'pq0', ph); pq1 = S.ps([128, 512], F32, 'pq1', ph)
            pTa = S.ps([128, 512], F32, 'pTa', ph); pTav = pTa[:, :].bitcast(BF16)
            psc = [S.ps([128, 512], F32, 'psc%d' % i, ph) for i in range(2)]
            pPT = S.ps([128, 512], F32, 'pPT', ph); pPTv = pPT[:, :].bitcast(BF16)
            ppv = S.ps([128, 512], F32, 'ppv', ph)
            for g in range(4):
                offs = [(OFF_QA + g * 512, 512, 0), (OFF_KA + g * 64, 64, 512), (OFF_VA + g * 64, 64, 576)]
                for sl in range(4):
                    for off, wd, dst in offs:
                        S.dma('pool', lambda e: e.dma_start(out=Wg[sl][:, :, dst:dst + wd],
                                                            in_=w_in[sl * 512:(sl + 1) * 512, off:off + wd].rearrange("(k p) n -> p k n", p=128)),
                              writes=[Wg[sl]])
                for ci in range(9):
                    tok = slice(ci * 128, (ci + 1) * 128)
                    if ci >= 1:
                        for k in range(KC):
                            S.op('pe', lambda e: e.matmul(pq0[:], lhsT=hT_all[:, k, tok], rhs=Wg[k // 4][:, k % 4, 0:512], start=(k == 0), stop=(k == KC - 1)),
                                 reads=[hT_all, Wg[k // 4]], writes=[pq0])
                        S.op('act', lambda e: e.mul(out=qb[:], in_=pq0[:], mul=0.125), reads=[pq0], writes=[qb])
                    for k in range(KC):
                        S.op('pe', lambda e: e.matmul(pq1[:, 0:128], lhsT=hT_all[:, k, tok], rhs=Wg[k // 4][:, k % 4, 512:640], start=(k == 0), stop=(k == KC - 1)),
                             reads=[hT_all, Wg[k // 4]], writes=[pq1])
                    S.op('dve', lambda e: e.tensor_copy(out=k2[:, 0, 0:64], in_=pq1[:, 0:64]), reads=[pq1], writes=[k2])
                    S.op('dve', lambda e: e.tensor_copy(out=k2[:, 1, 64:128], in_=pq1[:, 0:64]), reads=[pq1], writes=[k2])
                    S.op('dve', lambda e: e.tensor_copy(out=v_all[:, ci, :], in_=pq1[:, 64:128]), reads=[pq1], writes=[v_all])
                    if ci >= 1:
                        for i in range(4):
                            transpose_bf(pTa, pTav[:, i * 128:(i + 1) * 128], qb, qb[:, i * 128:(i + 1) * 128])
                    transpose_bf(pTa, pTav[:, 512:640], k2, k2[:, 0, :])
                    transpose_bf(pTa, pTav[:, 640:768], k2, k2[:, 1, :])
                    if ci >= 1:
                        S.op('act', lambda e: e.copy(out=qT[:, :, (ci - 1) * 128:ci * 128], in_=pTav[:, 0:512].rearrange("p (a b) -> p a b", a=4)),
                             reads=[pTa], writes=[qT])
                    S.op('act', lambda e: e.copy(out=kT_all[:, :, tok], in_=pTav[:, 512:768].rearrange("p (a b) -> p a b", a=2)), reads=[pTa], writes=[kT_all])
                for n in range(NOWN):
                    mk = mask0t if n == 0 else maskt
                    for hh in range(2):
                        for j4 in range(4):
                            j = hh * 4 + j4; i = j // 2; r0 = (j % 2) * 64
                            pb = psc[j4 // 2]
                            S.op('pe', lambda e: e.matmul(pb[:, (j4 % 2) * 256:(j4 % 2 + 1) * 256], lhsT=qT[:, i, n * 128:(n + 1) * 128],
                                                          rhs=kT_all[:, j % 2, n * 128:n * 128 + 256], start=True, stop=True),
                                 reads=[qT, kT_all], writes=[pb])
                        for bnk in range(2):
                            S.op('dve', lambda e: e.tensor_tensor(out=S_sb[:, hh * 4 + bnk * 2:hh * 4 + bnk * 2 + 2, :],
                                                                  in0=psc[bnk][:, :].rearrange("p (a b) -> p a b", a=2),
                                                                  in1=mk[:, :].unsqueeze(1).to_broadcast([128, 2, 256]), op=ALU.add),
                                 reads=[psc[bnk], mk], writes=[S_sb])
                    S.op('dve', lambda e: e.tensor_reduce(out=mx8[:], in_=S_sb[:], axis=AX.X, op=ALU.max), reads=[S_sb], writes=[mx8])
                    S.op('dve', lambda e: e.tensor_tensor(out=mx8[:], in0=mx8[:], in1=sinkb[:, g * 8:(g + 1) * 8], op=ALU.max), reads=[mx8, sinkb], writes=[mx8])
                    S.op('dve', lambda e: e.tensor_scalar(out=negm[:], in0=mx8[:], scalar1=-1.0, scalar2=None, op0=ALU.mult), reads=[mx8], writes=[negm])
                    for j in range(8):
                        S.op('act', lambda e: e.activation(out=P_bf[:, j, :], in_=S_sb[:, j, :], func=AF.Exp, bias=negm[:, j:j + 1], scale=1.0,
                                                           accum_out=rs[:, j:j + 1]), reads=[S_sb, negm], writes=[P_bf, rs])
                    S.op('dve', lambda e: e.tensor_tensor(out=es8[:], in0=sinkb[:, g * 8:(g + 1) * 8], in1=mx8[:], op=ALU.subtract), reads=[sinkb, mx8], writes=[es8])
                    S.op('act', lambda e: e.activation(out=es8[:], in_=es8[:], func=AF.Exp), reads=[es8], writes=[es8])
                    S.op('dve', lambda e: e.tensor_tensor(out=es8[:], in0=es8[:], in1=rs[:], op=ALU.add), reads=[es8, rs], writes=[es8])
                    S.op('dve', lambda e: e.reciprocal(out=rden[:], in_=es8[:]), reads=[es8], writes=[rden])
                    for hh in range(2):
                        for j4 in range(4):
                            for t in range(2):
                                transpose_bf(pPT, pPTv[:, (j4 * 2 + t) * 128:(j4 * 2 + t + 1) * 128], P_bf, P_bf[:, hh * 4 + j4, t * 128:(t + 1) * 128])
                        S.op('act', lambda e: e.copy(out=PT[:, hh * 4:(hh + 1) * 4, :, :], in_=pPTv[:, 0:1024].rearrange("p (a t b) -> p a t b", a=4, t=2)),
                             reads=[pPT], writes=[PT])
                    for j in range(8):
                        for t in range(2):
                            S.op('pe', lambda e: e.matmul(ppv[:, j * 64:(j + 1) * 64], lhsT=PT[:, j, t, :], rhs=v_all[:, n + t, :], start=(t == 0), stop=(t == 1)),
                                 reads=[PT, v_all], writes=[ppv])
                    S.op('dve', lambda e: e.tensor_tensor(out=at_tok[:], in0=ppv[:, :].rearrange("p (a b) -> p a b", a=8),
                                                          in1=rden[:, :].unsqueeze(2).to_broadcast([128, 8, 64]), op=ALU.mult), reads=[ppv, rden], writes=[at_tok])
                    for i in range(4):
                        transpose_bf(pTa, pTav[:, i * 128:(i + 1) * 128], at_tok, at_tok[:].rearrange("p a b -> p (a b)")[:, i * 128:(i + 1) * 128])
                    S.op('act', lambda e: e.copy(out=attnT[:, g * 4:(g + 1) * 4, n * 128:(n + 1) * 128], in_=pTav[:, 0:512].rearrange("p (a b) -> p a b", a=4)),
                         reads=[pTa], writes=[attnT])
            S.barrier()
        if stop_after == 'attn':
            o = dout("d_attnT", [128, KC, 1024], BF16)
            st(T(o, 'o'), o[:, :, :], attnT, attnT[:], is_out=True)
            S.finish()
            return nc, list(dbgo)
        with ExitStack() as ph:
            mT = S.sb([128, KC, 512], BF16, 'mT', ph)
            Wm = [S.sb([128, KC, 128], BF16, 'Wm%d' % i, ph) for i in range(4)]
            Wo = [S.sb([128, 4, 512], BF16, 'Wo%d' % i, ph) for i in range(4)]
            g1b = S.sb([128, D], F32, 'g1b', ph); ld(g1b, g1b[:], modd[2:3, :].partition_broadcast(128), reads=[modd])
            sga = S.sb([128, 512], F32, 'sga', ph); sgr = S.sb([128, 512], F32, 'sgr', ph)
            rr = [S.sb([128, 512], F32, 'rr%d' % i, ph) for i in range(2)]
            xs = [S.sb([128, 512], F32, 'xs%d' % i, ph) for i in range(2)]
            pA = S.ps([128, 512], F32, 'pA', ph); pR = S.ps([128, 512], F32, 'pR', ph)
            pGa = S.ps([128, 512], F32, 'pGa', ph); pGr = S.ps([128, 512], F32, 'pGr', ph)
            po2 = [S.ps([128, 512], F32, 'po2%d' % i, ph) for i in range(2)]
            for th in range(2):
                tsl = slice(th * 512, (th + 1) * 512)
                hsl = slice(128 + th * 512, 128 + (th + 1) * 512)
                for cg in range(16):
                    for wi in range(4):
                        r0 = (wi * 16 + cg) * 128
                        S.dma('pool', lambda e: e.dma_start(out=Wm[wi][:].rearrange("p k n -> p (k n)"), in_=wm_t[r0:r0 + 128, :]), writes=[Wm[wi]])
                    for jj in range(1):
                        csl = slice(0, 128)
                        for pb, wt, act_T, asl in ((pA, Wm[0], attnT, tsl), (pR, Wm[1], retT, tsl), (pGa, Wm[2], hT_all, hsl), (pGr, Wm[3], hT_all, hsl)):
                            for k in range(KC):
                                S.op('pe', lambda e: e.matmul(pb[:], lhsT=wt[:, k, csl], rhs=act_T[:, k, asl], start=(k == 0), stop=(k == KC - 1)),
                                     reads=[wt, act_T], writes=[pb])
                        S.op('act', lambda e: e.activation(out=sga[:], in_=pGa[:], func=AF.Sigmoid), reads=[pGa], writes=[sga])
                        S.op('act', lambda e: e.activation(out=sgr[:], in_=pGr[:], func=AF.Sigmoid), reads=[pGr], writes=[sgr])
                        S.op('dve', lambda e: e.tensor_tensor(out=sga[:], in0=sga[:], in1=pA[:], op=ALU.mult), reads=[sga, pA], writes=[sga])
                        S.op('dve', lambda e: e.tensor_tensor(out=sgr[:], in0=sgr[:], in1=pR[:], op=ALU.mult), reads=[sgr, pR], writes=[sgr])
                        S.op('dve', lambda e: e.tensor_tensor(out=mT[:, cg, :], in0=sga[:], in1=sgr[:], op=ALU.add), reads=[sga, sgr], writes=[mT])
                it = 0
                for nb in range(4):
                    for kq in range(4):
                        S.dma('pool', lambda e: e.dma_start(out=Wo[kq][:], in_=w_o[kq * 512:(kq + 1) * 512, nb * 512:(nb + 1) * 512].rearrange("(k p) n -> p k n", p=128)),
                              writes=[Wo[kq]])
                    for c4 in range(4):
                        n = th * 4 + c4
                        pb = po2[it % 2]; r_ = rr[it % 2]; x_ = xs[it % 2]
                        ld(x_, x_[:], xo[n * 128:(n + 1) * 128, nb * 512:(nb + 1) * 512])
                        for k in range(KC):
                            S.op('pe', lambda e: e.matmul(pb[:], lhsT=mT[:, k, c4 * 128:(c4 + 1) * 128], rhs=Wo[k // 4][:, k % 4, :], start=(k == 0), stop=(k == KC - 1)),
                                 reads=[mT, Wo[k // 4]], writes=[pb])
                        S.op('dve', lambda e: e.tensor_tensor(out=r_[:], in0=pb[:], in1=g1b[:, nb * 512:(nb + 1) * 512], op=ALU.mult), reads=[pb, g1b], writes=[r_])
                        S.op('dve', lambda e: e.tensor_tensor(out=r_[:], in0=r_[:], in1=x_[:], op=ALU.add), reads=[r_, x_], writes=[r_])
                        st(xmid, xmid[n * 128:(n + 1) * 128, nb * 512:(nb + 1) * 512], r_, r_[:])
                        it += 1
            S.barrier()
        mx.close()
        S.barrier()
        if stop_after == 'mix':
            o = dout("d_xmid", [1024, D])
            t = S.sb([128, NOWN, D], F32, 'dbgx')
            ld(t, t[:], xmid[:, :].rearrange("(n p) d -> p n d", p=128), reads=[xmid])
            st(T(o, 'o'), o[:, :].rearrange("(n p) d -> p n d", p=128), t, t[:], is_out=True)
            S.finish()
            return nc, list(dbgo)
        moe = es.enter_context(ExitStack())
        slot_i = S.sb([128, 2, NOWN], I32, 'slot_i', moe)
        idxg_i = S.sb([128, NOV, 16], I32, 'idxg_i', moe)
        idxd_i = S.sb([128, NOV, 8], I32, 'idxd_i', moe)
        ss = S.sb([128, 1], F32, 'ss_m', moe); rstd = S.sb([128, 1], F32, 'rstd_m', moe)
        Wg_t = [S.sb([128, 1024], BF16, 'Wg_t%d' % i, moe) for i in range(16)]
        Wu_t = [S.sb([128, 1024], BF16, 'Wu_t%d' % i, moe) for i in range(16)]
        Wd_t = [S.sb([128, D], BF16, 'Wd_t%d' % i, moe) for i in range(8)]

        def load_static_weights(b):
            for kc in range(16):
                r0 = b * 2048 + kc * 128
                S.dma('pool', lambda e: e.dma_start(out=Wg_t[kc][:], in_=w_eg[r0:r0 + 128, :]), writes=[Wg_t[kc]])
                S.dma('pool', lambda e: e.dma_start(out=Wu_t[kc][:], in_=w_eu[r0:r0 + 128, :]), writes=[Wu_t[kc]])
            for kc in range(8):
                r0 = b * 1024 + kc * 128
                S.dma('pool', lambda e: e.dma_start(out=Wd_t[kc][:], in_=w_ed[r0:r0 + 128, :]), writes=[Wd_t[kc]])

        if stop_after is None:
            load_static_weights(0)
        with ExitStack() as ph:
            A2b = S.sb([128, D], F32, 'A2b', ph); ld(A2b, A2b[:], modd[4:5, :].partition_broadcast(128), reads=[modd])
            sh2b = S.sb([128, D], F32, 'sh2b', ph); ld(sh2b, sh2b[:], modd[3:4, :].partition_broadcast(128), reads=[modd])
            Wr_sb = S.sb([128, KC, 68], F32, 'Wr_sb', ph); ld(Wr_sb, Wr_sb[:], w_rt[:, :].rearrange("(k p) n -> p k n", p=128))
            brb = S.sb([128, 68], F32, 'brb', ph); ld(brb, brb[:], b_rt[0:1, :].partition_broadcast(128))
            zt = S.sb([1, D], BF16, 'zt', ph)
            S.op('dve', lambda e: e.memset(zt[:], 0.0), writes=[zt])
            st(H2, H2[1024:1025, :], zt, zt[:])
            xm = [S.sb([128, D], F32, 'xm%d' % i, ph) for i in range(2)]
            h2b = S.sb([128, D], BF16, 'h2b', ph)
            h2T = S.sb([128, KC, 128], F32, 'h2T', ph)
            lg = S.sb([128, 68], F32, 'lg', ph)
            sm = {k: S.sb([128, 1], F32, 'sm_' + k, ph) for k in ('gmax', 'negg', 'gsum', 'gw', 'm1', 'm2', 'd', 'p1')}
            ohg = S.sb([128, 4], F32, 'ohg', ph); gej = S.sb([128, 4], F32, 'gej', ph)
            t416 = S.sb([128, 4, 16], F32, 't416', ph)
            el = S.sb([128, 16], F32, 'el', ph); el2 = S.sb([128, 16], F32, 'el2', ph)
            oh1 = S.sb([128, 16], F32, 'oh1', ph); oh2 = S.sb([128, 16], F32, 'oh2', ph)
            OH = [S.sb([128, NOWN, 64], F32, 'OH%d' % i, ph) for i in range(2)]
            wts = S.sb([128, 2, NOWN], F32, 'wts', ph)
            Cb = S.sb([128, NOWN, 64], BF16, 'Cb', ph)
            pTf = [S.ps([128, 512], F32, 'pTf%d' % i, ph) for i in range(2)]
            plg = S.ps([128, 512], F32, 'plg', ph)
            pPC = S.ps([128, 512], F32, 'pPC', ph); pcnt = S.ps([128, 512], F32, 'pcnt', ph)
            for n in range(NOWN):
                x_ = xm[n % 2]
                ld(x_, x_[:], xmid[n * 128:(n + 1) * 128, :], reads=[xmid])
                rms_rstd(x_, x_[:], h2b, ss, rstd)
                S.op('dve', lambda e: e.scalar_tensor_tensor(out=x_[:], in0=x_[:], scalar=rstd[:, 0:1], in1=A2b[:], op0=ALU.mult, op1=ALU.mult),
                     reads=[x_, rstd, A2b], writes=[x_])
                S.op('dve', lambda e: e.tensor_tensor(out=x_[:], in0=x_[:], in1=sh2b[:], op=ALU.add), reads=[x_, sh2b], writes=[x_])
                S.op('act', lambda e: e.copy(out=h2b[:], in_=x_[:]), reads=[x_], writes=[h2b])
                st(H2, H2[n * 128:(n + 1) * 128, :], h2b, h2b[:])
                for grp in range(4):
                    pb = pTf[grp % 2]
                    for kk in range(4):
                        k = grp * 4 + kk
                        S.op('pe', lambda e: e.matmul(pb[:, kk * 128:(kk + 1) * 128], lhsT=x_[:, k * 128:(k + 1) * 128], rhs=idf[:], start=True, stop=True),
                             reads=[x_, idf], writes=[pb])
                    S.op('act', lambda e: e.copy(out=h2T[:, grp * 4:(grp + 1) * 4, :], in_=pb[:, :].rearrange("p (a b) -> p a b", a=4)), reads=[pb], writes=[h2T])
                for k in range(KC):
                    S.op('pe', lambda e: e.matmul(plg[:, 0:68], lhsT=h2T[:, k, :], rhs=Wr_sb[:, k, :], start=(k == 0), stop=(k == KC - 1)),
                         reads=[h2T, Wr_sb], writes=[plg])
                S.op('dve', lambda e: e.tensor_tensor(out=lg[:], in0=plg[:, 0:68], in1=brb[:], op=ALU.add), reads=[plg, brb], writes=[lg])
                S.op('dve', lambda e: e.tensor_reduce(out=sm['gmax'][:], in_=lg[:, 0:4], axis=AX.X, op=ALU.max), reads=[lg], writes=[sm['gmax']])
                S.op('dve', lambda e: e.tensor_scalar(out=ohg[:], in0=lg[:, 0:4], scalar1=sm['gmax'][:, 0:1], scalar2=None, op0=ALU.is_ge), reads=[lg, sm['gmax']], writes=[ohg])
                S.op('dve', lambda e: e.tensor_scalar(out=sm['negg'][:], in0=sm['gmax'][:], scalar1=-1.0, scalar2=None, op0=ALU.mult), reads=[sm['gmax']], writes=[sm['negg']])
                S.op('act', lambda e: e.activation(out=gej[:], in_=lg[:, 0:4], func=AF.Exp, bias=sm['negg'][:, 0:1], scale=1.0, accum_out=sm['gsum'][:]),
                     reads=[lg, sm['negg']], writes=[gej, sm['gsum']])
                S.op('dve', lambda e: e.reciprocal(out=sm['gw'][:], in_=sm['gsum'][:]), reads=[sm['gsum']], writes=[sm['gw']])
                S.op('dve', lambda e: e.tensor_tensor(out=t416[:], in0=lg[:, 4:68].rearrange("p (g e) -> p g e", g=4),
                                                      in1=ohg[:, :].unsqueeze(2).to_broadcast([128, 4, 16]), op=ALU.mult), reads=[lg, ohg], writes=[t416])
                S.op('dve', lambda e: e.tensor_reduce(out=el[:], in_=t416[:].rearrange("p g e -> p e g"), axis=AX.X, op=ALU.add), reads=[t416], writes=[el])
                S.op('dve', lambda e: e.tensor_reduce(out=sm['m1'][:], in_=el[:], axis=AX.X, op=ALU.max), reads=[el], writes=[sm['m1']])
                S.op('dve', lambda e: e.tensor_scalar(out=oh1[:], in0=el[:], scalar1=sm['m1'][:, 0:1], scalar2=None, op0=ALU.is_ge), reads=[el, sm['m1']], writes=[oh1])
                S.op('dve', lambda e: e.scalar_tensor_tensor(out=el2[:], in0=oh1[:], scalar=-1e30, in1=el[:], op0=ALU.mult, op1=ALU.add), reads=[oh1, el], writes=[el2])
                S.op('dve', lambda e: e.tensor_reduce(out=sm['m2'][:], in_=el2[:], axis=AX.X, op=ALU.max), reads=[el2], writes=[sm['m2']])
                S.op('dve', lambda e: e.tensor_scalar(out=oh2[:], in0=el2[:], scalar1=sm['m2'][:, 0:1], scalar2=None, op0=ALU.is_ge), reads=[el2, sm['m2']], writes=[oh2])
                S.op('dve', lambda e: e.tensor_tensor(out=sm['d'][:], in0=sm['m2'][:], in1=sm['m1'][:], op=ALU.subtract), reads=[sm['m1'], sm['m2']], writes=[sm['d']])
                S.op('act', lambda e: e.activation(out=sm['d'][:], in_=sm['d'][:], func=AF.Exp), reads=[sm['d']], writes=[sm['d']])
                S.op('dve', lambda e: e.tensor_scalar(out=sm['d'][:], in0=sm['d'][:], scalar1=1.0, scalar2=None, op0=ALU.add), reads=[sm['d']], writes=[sm['d']])
                S.op('dve', lambda e: e.reciprocal(out=sm['p1'][:], in_=sm['d'][:]), reads=[sm['d']], writes=[sm['p1']])
                S.op('dve', lambda e: e.tensor_tensor(out=wts[:, 0, n:n + 1], in0=sm['p1'][:], in1=sm['gw'][:], op=ALU.mult), reads=[sm['p1'], sm['gw']], writes=[wts])
                S.op('dve', lambda e: e.tensor_tensor(out=wts[:, 1, n:n + 1], in0=sm['gw'][:], in1=wts[:, 0, n:n + 1], op=ALU.subtract), reads=[sm['gw'], wts], writes=[wts])
                for kk, oh in ((0, oh1), (1, oh2)):
                    S.op('dve', lambda e: e.tensor_tensor(out=OH[kk][:, n, :].rearrange("p (g e) -> p g e", g=4),
                                                          in0=ohg[:, :].unsqueeze(2).to_broadcast([128, 4, 16]),
                                                          in1=oh[:, :].unsqueeze(1).to_broadcast([128, 4, 16]), op=ALU.mult), reads=[ohg, oh], writes=[OH[kk]])
                S.op('dve', lambda e: e.tensor_tensor(out=Cb[:, n, :], in0=OH[0][:, n, :], in1=OH[1][:, n, :], op=ALU.add), reads=[OH[0], OH[1]], writes=[Cb])
            ones_bf = S.sb([128, 128], BF16, 'ones_bf', ph)
            S.op('dve', lambda e: e.memset(ones_bf[:], 1.0), writes=[ones_bf])
            Lf = S.sb([128, 128], F32, 'Lf', ph); ld(Lf, Lf[:], Lstd[:, :])
            Lb = S.sb([128, 128], BF16, 'Lb', ph)
            S.op('dve', lambda e: e.tensor_copy(out=Lb[:], in_=Lf[:]), reads=[Lf], writes=[Lb])
            for n in range(NOWN):
                for m in range(n + 1):
                    S.op('pe', lambda e: e.matmul(pPC[:, n * 64:(n + 1) * 64], lhsT=(Lb[:] if m == n else ones_bf[:]), rhs=Cb[:, m, :], start=(m == 0), stop=(m == n)),
                         reads=[Lb, ones_bf, Cb], writes=[pPC])
            for m in range(NOWN):
                S.op('pe', lambda e: e.matmul(pcnt[:, 0:64], lhsT=ones_bf[:], rhs=Cb[:, m, :], start=(m == 0), stop=(m == NOWN - 1)), reads=[ones_bf, Cb], writes=[pcnt])
            CAP = 128
            e128 = S.sb([128, 64], F32, 'e128', ph); ld(e128, e128[:], e128d[:, :])
            ocf = S.sb([128, 64], F32, 'ocf', ph); cnti = S.sb([128, 64], I32, 'cnti', ph); padf = S.sb([128, 64], F32, 'padf', ph)
            S.op('dve', lambda e: e.tensor_scalar(out=ocf[:], in0=pcnt[:, 0:64], scalar1=-float(CAP), scalar2=0.0, op0=ALU.add, op1=ALU.max), reads=[pcnt], writes=[ocf])
            S.op('dve', lambda e: e.tensor_scalar(out=ocf[:], in0=ocf[:], scalar1=127.0, scalar2=None, op0=ALU.add), reads=[ocf], writes=[ocf])
            S.op('dve', lambda e: e.tensor_copy(out=cnti[:], in_=ocf[:]), reads=[ocf], writes=[cnti])
            S.op('dve', lambda e: e.tensor_scalar(out=cnti[:], in0=cnti[:], scalar1=7, scalar2=7, op0=ALU.arith_shift_right, op1=ALU.logical_shift_left), reads=[cnti], writes=[cnti])
            S.op('dve', lambda e: e.tensor_copy(out=padf[:], in_=cnti[:]), reads=[cnti], writes=[padf])
            cs = [S.sb([128, 64], F32, 'cs%d' % i, ph) for i in range(2)]
            S.op('dve', lambda e: e.tensor_copy(out=cs[0][:], in_=padf[:]), reads=[padf], writes=[cs[0]])
            cur = 0
            for s_ in (1, 2, 4, 8, 16, 32):
                a, b_ = cs[cur], cs[1 - cur]
                S.op('dve', lambda e: e.tensor_copy(out=b_[:, 0:s_], in_=a[:, 0:s_]), reads=[a], writes=[b_])
                S.op('dve', lambda e: e.tensor_tensor(out=b_[:, s_:64], in0=a[:, s_:64], in1=a[:, 0:64 - s_], op=ALU.add), reads=[a], writes=[b_])
                cur = 1 - cur
            pend = cs[cur]; ob = cs[1 - cur]
            S.op('dve', lambda e: e.tensor_tensor(out=ob[:], in0=pend[:], in1=padf[:], op=ALU.subtract), reads=[pend, padf], writes=[ob])
            S.op('dve', lambda e: e.tensor_scalar(out=ob[:], in0=ob[:], scalar1=float(64 * 128 - CAP), scalar2=None, op0=ALU.add), reads=[ob], writes=[ob])
            slot_f = S.sb([128, 2, NOWN], F32, 'slot_f', ph)
            tmpb = S.sb([128, NOWN, 64], F32, 'tmpb', ph)
            rk = S.sb([128, NOWN], F32, 'rk', ph); eb = S.sb([128, NOWN], F32, 'eb', ph); obk = S.sb([128, NOWN], F32, 'obk', ph); isov = S.sb([128, NOWN], F32, 'isov', ph)
            for kk in range(2):
                S.op('dve', lambda e: e.tensor_tensor(out=tmpb[:], in0=OH[kk][:], in1=pPC[:, :].rearrange("p (n e) -> p n e", n=NOWN), op=ALU.mult), reads=[OH[kk], pPC], writes=[tmpb])
                S.op('dve', lambda e: e.tensor_reduce(out=rk[:], in_=tmpb[:], axis=AX.X, op=ALU.add), reads=[tmpb], writes=[rk])
                S.op('dve', lambda e: e.tensor_tensor(out=tmpb[:], in0=OH[kk][:], in1=e128[:, :].unsqueeze(1).to_broadcast([128, NOWN, 64]), op=ALU.mult), reads=[OH[kk], e128], writes=[tmpb])
                S.op('dve', lambda e: e.tensor_reduce(out=eb[:], in_=tmpb[:], axis=AX.X, op=ALU.add), reads=[tmpb], writes=[eb])
                S.op('dve', lambda e: e.tensor_tensor(out=tmpb[:], in0=OH[kk][:], in1=ob[:, :].unsqueeze(1).to_broadcast([128, NOWN, 64]), op=ALU.mult), reads=[OH[kk], ob], writes=[tmpb])
                S.op('dve', lambda e: e.tensor_reduce(out=obk[:], in_=tmpb[:], axis=AX.X, op=ALU.add), reads=[tmpb], writes=[obk])
                S.op('dve', lambda e: e.tensor_scalar(out=isov[:], in0=rk[:], scalar1=float(CAP), scalar2=None, op0=ALU.is_ge), reads=[rk], writes=[isov])
                S.op('dve', lambda e: e.tensor_tensor(out=obk[:], in0=obk[:], in1=eb[:], op=ALU.subtract), reads=[obk, eb], writes=[obk])
                S.op('dve', lambda e: e.tensor_tensor(out=obk[:], in0=obk[:], in1=isov[:], op=ALU.mult), reads=[obk, isov], writes=[obk])
                S.op('dve', lambda e: e.tensor_tensor(out=rk[:], in0=rk[:], in1=eb[:], op=ALU.add), reads=[rk, eb], writes=[rk])
                S.op('dve', lambda e: e.tensor_tensor(out=slot_f[:, kk, :], in0=rk[:], in1=obk[:], op=ALU.add), reads=[rk, obk], writes=[slot_f])
            S.op('dve', lambda e: e.tensor_copy(out=slot_i[:], in_=slot_f[:]), reads=[slot_f], writes=[slot_i])
            blk128 = S.sb([128, NOV], F32, 'blk128', ph); ld(blk128, blk128[:], blk128d[:, 0:NOV])
            kcoff = S.sb([128, 16], F32, 'kcoff', ph); ld(kcoff, kcoff[:], kcoffd[:, :])
            pidx = S.sb([128, 1], F32, 'pidx', ph); ld(pidx, pidx[:], pidxd[:, :])
            cmp = S.sb([128, NOV, 64], BF16, 'cmp', ph)
            S.op('dve', lambda e: e.tensor_tensor(out=cmp[:], in0=pend[:, :].unsqueeze(1).to_broadcast([128, NOV, 64]),
                                                  in1=blk128[:, :].unsqueeze(2).to_broadcast([128, NOV, 64]), op=ALU.is_le), reads=[pend, blk128], writes=[cmp])
            bef = S.sb([128, NOV], F32, 'bef', ph); gb = S.sb([128, NOV], F32, 'gb', ph)
            S.op('dve', lambda e: e.tensor_reduce(out=bef[:], in_=cmp[:], axis=AX.X, op=ALU.add), reads=[cmp], writes=[bef])
            skipo = S.sb([128, NOV], F32, 'skipo', ph)
            S.op('dve', lambda e: e.tensor_scalar(out=skipo[:], in0=bef[:], scalar1=64.0, scalar2=float(2 ** 27), op0=ALU.is_ge, op1=ALU.mult), reads=[bef], writes=[skipo])
            S.op('dve', lambda e: e.tensor_scalar(out=bef[:], in0=bef[:], scalar1=63.0, scalar2=None, op0=ALU.min), reads=[bef], writes=[bef])
            idxf = S.sb([128, NOV, 16], F32, 'idxf', ph)
            for mult, nk, dst in ((2048.0, 16, idxg_i), (1024.0, 8, idxd_i)):
                S.op('dve', lambda e: e.tensor_scalar(out=gb[:], in0=bef[:], scalar1=mult, scalar2=pidx[:, 0:1], op0=ALU.mult, op1=ALU.add), reads=[bef, pidx], writes=[gb])
                S.op('dve', lambda e: e.tensor_tensor(out=gb[:], in0=gb[:], in1=skipo[:], op=ALU.add), reads=[gb, skipo], writes=[gb])
                S.op('dve', lambda e: e.tensor_tensor(out=idxf[:, :, 0:nk], in0=gb[:, :].unsqueeze(2).to_broadcast([128, NOV, nk]),
                                                      in1=kcoff[:, 0:nk].unsqueeze(1).to_broadcast([128, NOV, nk]), op=ALU.add), reads=[gb, kcoff], writes=[idxf])
                S.op('dve', lambda e: e.tensor_copy(out=dst[:], in_=idxf[:, :, 0:nk]), reads=[idxf], writes=[dst])
            ri0 = S.sb([128, NBLK, 16], I32, 'ri0', ph)
            S.op('dve', lambda e: e.memset(ri0[:], 0), writes=[ri0])
            S.op('dve', lambda e: e.memset(ri0[:, :, 0:1], 1024), writes=[ri0])
            st(rinfo, rinfo[:, :].rearrange("(b p) c -> p b c", p=128), ri0, ri0[:])
            tokid = S.sb([128, NOWN], I32, 'tokid', ph); ld(tokid, tokid[:], tokidd[:, :])
            ris = [S.sb([128, 16], I32, 'ri%d' % i, ph) for i in range(4)]
            for r_ in ris:
                S.op('dve', lambda e: e.memset(r_[:], 0), writes=[r_])
            it = 0
            for n in range(NOWN):
                for kk in range(2):
                    r_ = ris[it % 4]
                    S.op('dve', lambda e: e.tensor_copy(out=r_[:, 0:1], in_=tokid[:, n:n + 1]), reads=[tokid], writes=[r_])
                    S.op('dve', lambda e: e.tensor_copy(out=r_[:, 1:2].bitcast(F32), in_=wts[:, kk, n:n + 1]), reads=[wts], writes=[r_])
                    S.dma('pool', lambda e: e.indirect_dma_start(out=rinfo[:, :], out_offset=bass.IndirectOffsetOnAxis(ap=slot_i[:, kk, n:n + 1], axis=0),
                                                                 in_=r_[:], in_offset=None), reads=[r_, slot_i], writes=[rinfo])
                    it += 1
            S.barrier()
        if stop_after == 'moe_route':
            o1 = dout("d_slot", [128, 2, NOWN], I32); st(T(o1, 'o'), o1[:, :, :], slot_i, slot_i[:], is_out=True)
            o2 = dout("d_idxg", [128, NOV, 16], I32); st(T(o2, 'o'), o2[:, :, :], idxg_i, idxg_i[:], is_out=True)
            o3 = dout("d_rinfo", [NBLK * 128, 16], I32)
            t = S.sb([128, NBLK, 16], I32, 'dbgr')
            ld(t, t[:], rinfo[:, :].rearrange("(b p) c -> p b c", p=128), reads=[rinfo])
            st(T(o3, 'o'), o3[:, :].rearrange("(b p) c -> p b c", p=128), t, t[:], is_out=True)
            S.finish()
            return nc, list(dbgo)
        with ExitStack() as ph:
            rt_all = S.sb([128, NBLK, 16], I32, 'rt_all', ph)
            ld(rt_all, rt_all[:], rinfo[:, :].rearrange("(b p) c -> p b c", p=128), reads=[rinfo])
            xgs = [S.sb([128, D], BF16, 'xg%d' % i, ph) for i in range(2)]
            xgT = S.sb([128, KC, 128], BF16, 'xgT', ph)
            sg = S.sb([128, 1024], F32, 'sg', ph)
            actb = S.sb([128, 1024], BF16, 'actb', ph)
            actT = S.sb([128, 8, 128], BF16, 'actT', ph)
            ybs = [S.sb([128, D], F32, 'yb%d' % i, ph) for i in range(2)]
            pT1 = S.ps([128, 512], F32, 'pT1', ph); pT1v = pT1[:, :].bitcast(BF16)
            pT2 = S.ps([128, 512], F32, 'pT2', ph); pT2v = pT2[:, :].bitcast(BF16)
            pgu = [S.ps([128, 512], F32, 'pgu%d' % i, ph) for i in range(4)]
            pdn = [S.ps([128, 512], F32, 'pdn%d' % i, ph) for i in range(2)]
            bc_g = nc.gpsimd.to_reg(64 * D - 1); bc_d = nc.gpsimd.to_reg(64 * 1024 - 1)
            for b in range(NBLK):
                rt = rt_all; xg = xgs[b % 2]; yb = ybs[b % 2]
                S.dma('pool', lambda e: e.indirect_dma_start(out=xg[:], out_offset=None, in_=H2[:, :],
                                                             in_offset=bass.IndirectOffsetOnAxis(ap=rt_all[:, b, 0:1], axis=0)), reads=[rt_all, H2], writes=[xg])
                if b < 64:
                    if b > 0:
                        load_static_weights(b)
                else:
                    ob_ = b - 64
                    for kc in range(16):
                        S.dma('pool', lambda e: e.indirect_dma_start(out=Wg_t[kc][:], out_offset=None, in_=w_eg[:, :],
                                                                     in_offset=bass.IndirectOffsetOnAxis(ap=idxg_i[:, ob_, kc:kc + 1], axis=0), bounds_check=bc_g, oob_is_err=False), reads=[idxg_i], writes=[Wg_t[kc]])
                        S.dma('pool', lambda e: e.indirect_dma_start(out=Wu_t[kc][:], out_offset=None, in_=w_eu[:, :],
                                                                     in_offset=bass.IndirectOffsetOnAxis(ap=idxg_i[:, ob_, kc:kc + 1], axis=0), bounds_check=bc_g, oob_is_err=False), reads=[idxg_i], writes=[Wu_t[kc]])
                    for kc in range(8):
                        S.dma('pool', lambda e: e.indirect_dma_start(out=Wd_t[kc][:], out_offset=None, in_=w_ed[:, :],
                                                                     in_offset=bass.IndirectOffsetOnAxis(ap=idxd_i[:, ob_, kc:kc + 1], axis=0), bounds_check=bc_d, oob_is_err=False), reads=[idxd_i], writes=[Wd_t[kc]])
                for half in range(2):
                    for kk in range(8):
                        k = half * 8 + kk
                        transpose_bf(pT1, pT1v[:, kk * 128:(kk + 1) * 128], xg, xg[:, k * 128:(k + 1) * 128])
                    S.op('act', lambda e: e.copy(out=xgT[:, half * 8:(half + 1) * 8, :], in_=pT1v[:, 0:1024].rearrange("p (a b) -> p a b", a=8)), reads=[pT1], writes=[xgT])
                for kc in range(16):
                    for wi, wt in ((0, Wg_t[kc]), (1, Wu_t[kc])):
                        for nb in range(2):
                            pb = pgu[wi * 2 + nb]
                            S.op('pe', lambda e: e.matmul(pb[:], lhsT=xgT[:, kc, :], rhs=wt[:, nb * 512:(nb + 1) * 512], start=(kc == 0), stop=(kc == 15)),
                                 reads=[xgT, wt], writes=[pb])
                for nb in range(2):
                    S.op('act', lambda e: e.activation(out=sg[:, nb * 512:(nb + 1) * 512], in_=pgu[nb][:], func=AF.Silu), reads=[pgu[nb]], writes=[sg])
                    S.op('dve', lambda e: e.tensor_tensor(out=actb[:, nb * 512:(nb + 1) * 512], in0=sg[:, nb * 512:(nb + 1) * 512], in1=pgu[2 + nb][:], op=ALU.mult),
                         reads=[sg, pgu[2 + nb]], writes=[actb])
                for kk in range(8):
                    transpose_bf(pT2, pT2v[:, kk * 128:(kk + 1) * 128], actb, actb[:, kk * 128:(kk + 1) * 128])
                S.op('act', lambda e: e.copy(out=actT[:], in_=pT2v[:, 0:1024].rearrange("p (a b) -> p a b", a=8)), reads=[pT2], writes=[actT])
                for hf in range(2):
                    for kc in range(8):
                        for i in range(2):
                            c0 = hf * 1024 + i * 512
                            S.op('pe', lambda e: e.matmul(pdn[i][:], lhsT=actT[:, kc, :], rhs=Wd_t[kc][:, c0:c0 + 512], start=(kc == 0), stop=(kc == 7)),
                                 reads=[actT, Wd_t[kc]], writes=[pdn[i]])
                    for i in range(2):
                        c0 = hf * 1024 + i * 512
                        S.op('dve', lambda e: e.tensor_scalar(out=yb[:, c0:c0 + 512], in0=pdn[i][:], scalar1=rt_all[:, b, 1:2].bitcast(F32), scalar2=None, op0=ALU.mult),
                             reads=[pdn[i], rt_all], writes=[yb])
                st(Yd, Yd[b * 128:(b + 1) * 128, :], yb, yb[:])
            S.barrier()
        with ExitStack() as ph:
            g2b = S.sb([128, D], F32, 'g2b', ph); ld(g2b, g2b[:], modd[5:6, :].partition_broadcast(128), reads=[modd])
            gfb = S.sb([128, D], F32, 'gfb', ph); ld(gfb, gfb[:], nfg[0:1, :].partition_broadcast(128))
            xm = [S.sb([128, D], F32, 'xm%d' % i, ph) for i in range(2)]
            y1 = [S.sb([128, D], F32, 'y1%d' % i, ph) for i in range(2)]
            y2 = [S.sb([128, D], F32, 'y2%d' % i, ph) for i in range(2)]
            junkb = S.sb([128, D], BF16, 'junkb', ph)
            for n in range(NOWN):
                x_, a_, b_ = xm[n % 2], y1[n % 2], y2[n % 2]
                ld(x_, x_[:], xmid[n * 128:(n + 1) * 128, :], reads=[xmid])
                for kk, dst in ((0, a_), (1, b_)):
                    S.dma('pool', lambda e: e.indirect_dma_start(out=dst[:], out_offset=None, in_=Yd[:, :],
                                                                 in_offset=bass.IndirectOffsetOnAxis(ap=slot_i[:, kk, n:n + 1], axis=0)), reads=[slot_i, Yd], writes=[dst])
                S.op('dve', lambda e: e.tensor_tensor(out=a_[:], in0=a_[:], in1=b_[:], op=ALU.add), reads=[a_, b_], writes=[a_])
                S.op('dve', lambda e: e.tensor_tensor(out=a_[:], in0=a_[:], in1=g2b[:], op=ALU.mult), reads=[a_, g2b], writes=[a_])
                S.op('dve', lambda e: e.tensor_tensor(out=a_[:], in0=a_[:], in1=x_[:], op=ALU.add), reads=[a_, x_], writes=[a_])
                rms_rstd(a_, a_[:], junkb, ss, rstd)
                S.op('dve', lambda e: e.scalar_tensor_tensor(out=a_[:], in0=a_[:], scalar=rstd[:, 0:1], in1=gfb[:], op0=ALU.mult, op1=ALU.mult),
                     reads=[a_, rstd, gfb], writes=[a_])
                st(T(out, 'out'), out[n * 128:(n + 1) * 128, :], a_, a_[:], is_out=True)
        S.finish()
    return nc, list(dbgo)


def _consts():
    c = {}
    c["ident"] = np.eye(128, dtype=np.float32)
    i = np.arange(128)[:, None]; j = np.arange(256)[None, :]
    valid = (j > i) & (j <= i + 128)
    c["mask"] = np.where(valid, 0.0, -1e30).astype(np.float32)
    gam = np.array(GAM, dtype=np.float64)
    lg = np.log(gam)
    e = np.arange(128)[:, None, None]; cc = np.arange(128)[None, None, :]
    diff = cc - e
    c["decT"] = np.where(diff >= 0, np.exp(np.maximum(diff, 0) * lg[None, :, None]), 0.0).astype(np.float32)
    idx = np.arange(128)[:, None].astype(np.float64)
    c["dq"] = np.exp((idx + 1.0) * lg[None, :]).astype(np.float32)
    c["dk"] = (np.exp((127.0 - idx) * lg[None, :]) / 16.0).astype(np.float32)
    invf = (10000.0 ** (-(np.arange(128, dtype=np.float32) / np.float32(128)))).astype(np.float32)
    c["invf"] = np.broadcast_to(invf[None, :], (128, 128)).copy()
    c["Lst"] = (np.arange(128)[:, None] < np.arange(128)[None, :]).astype(np.float32)
    c["tokid"] = (np.arange(NOWN)[None, :] * 128 + np.arange(128)[:, None]).astype(np.int32)
    c["blk128"] = np.broadcast_to((np.arange(NBLK, dtype=np.float32) * 128.0)[None, :], (128, NBLK)).copy()
    c["kcoff"] = np.broadcast_to((np.arange(16, dtype=np.float32) * 128.0)[None, :], (128, 16)).copy()
    c["pidx"] = np.arange(128, dtype=np.float32)[:, None].copy()
    c["e128"] = np.broadcast_to((np.arange(64, dtype=np.float32) * 128.0)[None, :], (128, 64)).copy()
    return c


def prep_inputs(x, c, positions, norm1_gain, norm2_gain, final_norm_gain, w_ada, b_ada, w_in,
                attn_sinks, ret_norm_gain, w_branch_attn, w_branch_ret, w_out,
                w_router_group, b_router_group, w_router_expert, b_router_expert,
                w_expert_gate, w_expert_up, w_expert_down):
    f = lambda a: np.ascontiguousarray(np.asarray(a))
    x = f(x); c = f(c); positions = f(positions)
    shared = dict(
        w_ada=f(w_ada)[0], b_ada=f(b_ada), w_in=f(w_in)[0], attn_sinks=f(attn_sinks), ret_norm_gain=f(ret_norm_gain),
        w_out=f(w_out)[0],
        w_router=np.concatenate([f(w_router_group)[0], f(w_router_expert)[0]], axis=1),
        b_router=np.concatenate([f(b_router_group), f(b_router_expert)], axis=1),
        w_expert_gate=f(w_expert_gate).reshape(64 * D, 1024), w_expert_up=f(w_expert_up).reshape(64 * D, 1024),
        w_expert_down=f(w_expert_down).reshape(64 * 1024, D),
        norm1_gain=f(norm1_gain), norm2_gain=f(norm2_gain), final_norm_gain=f(final_norm_gain).reshape(1, D),
    )
    def tile_cols(w, off):
        sub = w[:, off:off + D].reshape(KC, 128, 16, 128)
        return np.ascontiguousarray(sub.transpose(2, 1, 0, 3)).reshape(16 * 128, KC * 128)
    w_in0 = shared["w_in"]
    shared["wm_t"] = np.concatenate([tile_cols(f(w_branch_attn)[0], 0), tile_cols(f(w_branch_ret)[0], 0),
                                     tile_cols(w_in0, OFF_GA), tile_cols(w_in0, OFF_GTR)], axis=0)
    shared.update(_consts())
    gam = np.array(GAM, dtype=np.float64); lg = np.log(gam)
    maps = []
    for core in range(8):
        b, q = core // 4, core % 4
        m = dict(shared)
        m["xo"] = x[b, q * 1024:(q + 1) * 1024]
        npre = q * 8
        xp = np.zeros((NPRE * 128, D), np.float32)
        pp = np.zeros((NPRE * 128,), np.int32)
        if npre:
            xp[(NPRE - npre) * 128:] = x[b, :q * 1024]
            pp[(NPRE - npre) * 128:] = positions[b, :q * 1024]
        m["xp"] = xp
        m["pos_o"] = np.ascontiguousarray(positions[b, q * 1024:(q + 1) * 1024].reshape(NOWN, 128).T)
        m["pos_p"] = np.ascontiguousarray(pp.reshape(NPRE, 128).T)
        m["cT"] = np.ascontiguousarray(c[b].reshape(KC, 128).T)
        valid = (np.arange(NPRE) >= NPRE - npre).astype(np.float64)
        idx = np.arange(128)[:, None, None].astype(np.float64)
        jj = np.arange(NPRE)[None, :, None].astype(np.float64)
        pk = np.exp((127.0 - idx) * lg[None, None, :] + 128.0 * (NPRE - 1 - jj) * lg[None, None, :]) / 16.0 * valid[None, :, None]
        m["pk"] = pk.astype(np.float32)
        mk = shared["mask"].copy()
        if q == 0:
            mk[:, :128] = -1e30
        m["mask0"] = mk
        maps.append(m)
    return maps


def kernel(**inputs):
    maps = prep_inputs(**inputs)
    nc, _ = build()
    res = run_bass_kernel_spmd(nc, maps, core_ids=list(range(8)))
    outp = np.zeros((2, 4096, D), np.float32)
    for core in range(8):
        b, q = core // 4, core % 4
        outp[b, q * 1024:(q + 1) * 1024] = res.results[core]["out"]
    return outp
```
